# Optimizing a Trainium2 kernel written in Bass

```python
import jax, jax.numpy as jnp
from jax import lax
import numpy as np

D_MODEL = 1024
BATCH = 8
SEQ = 2048
DEPTH = 4

HEAD_DIM = 64
N_DSWA_HEADS = 8
N_RET_HEADS = 8
DSWA_WIDTH = N_DSWA_HEADS * HEAD_DIM
RET_WIDTH = N_RET_HEADS * HEAD_DIM
HYB_WIDTHS = (DSWA_WIDTH, DSWA_WIDTH, DSWA_WIDTH, RET_WIDTH, RET_WIDTH, RET_WIDTH, RET_WIDTH)
HYB_IN_WIDTH = sum(HYB_WIDTHS)
HYB_SPLITS = tuple(int(s) for s in np.cumsum(HYB_WIDTHS)[:-1])
HYB_MIX_WIDTH = DSWA_WIDTH + RET_WIDTH
DSWA_BRANCHES = ((128, 1), (512, 4), (2048, 16))
DSWA_BLOCK = 128
ROPE_THETA = 500000.0
ROPE_DIMS = HEAD_DIM // 4
RET_CHUNK = 128
RET_ROPE_THETA = 10000.0
GMLP_WIDTH = D_MODEL
GMLP_CHUNK = 128
GMLP_GROUPS = 8
GMLP_GROUP_DIM = GMLP_WIDTH // GMLP_GROUPS
D_FF = 2816
N_EVEN = (DEPTH + 1) // 2
N_ODD = DEPTH // 2
EPS = 1e-6
NEG_INF = -1e30

kernel_name = 'hybrid_dilated_retention_gmlp_macaron'


def _rmsnorm(x, g):
    xf = x.astype(jnp.float32)
    y = xf * lax.rsqrt(jnp.mean(xf * xf, axis=-1, keepdims=True) + EPS)
    return (y * g.astype(jnp.float32)).astype(x.dtype)


def _layernorm(x, g, b):
    xf = x.astype(jnp.float32)
    mu = jnp.mean(xf, axis=-1, keepdims=True)
    var = jnp.mean(jnp.square(xf - mu), axis=-1, keepdims=True)
    y = (xf - mu) * lax.rsqrt(var + EPS)
    return (y * g.astype(jnp.float32) + b.astype(jnp.float32)).astype(x.dtype)


def _rope(x, rot_dims, theta):
    t = x.shape[2]
    half = rot_dims // 2
    inv = theta ** (-(jnp.arange(half, dtype=jnp.float32) * 2.0 / rot_dims))
    ang = jnp.arange(t, dtype=jnp.float32)[:, None] * inv[None, :]
    cos, sin = jnp.cos(ang), jnp.sin(ang)
    xr = x[..., :rot_dims].astype(jnp.float32)
    x1, x2 = xr[..., :half], xr[..., half:]
    rot = jnp.concatenate([x1 * cos - x2 * sin, x2 * cos + x1 * sin], axis=-1).astype(x.dtype)
    return jnp.concatenate([rot, x[..., rot_dims:]], axis=-1)


def _swiglu(x, w_gate, w_up, w_down):
    return (jax.nn.silu(x @ w_gate) * (x @ w_up)) @ w_down


def _heads(z, n):
    b, t, _ = z.shape
    return z.reshape(b, t, n, HEAD_DIM).transpose(0, 2, 1, 3)


def _merge(z):
    b, h, t, e = z.shape
    return z.transpose(0, 2, 1, 3).reshape(b, t, h * e)


def _dilated_branch(q, k, v, window, dilation):
    b, h, t, e = q.shape
    length = t // dilation
    band = window // dilation
    n_blk = -(-length // DSWA_BLOCK)
    pad = n_blk * DSWA_BLOCK - length
    n_prev = -(-band // DSWA_BLOCK)
    n_keys = (n_prev + 1) * DSWA_BLOCK

    def to_blocks(z):
        z = z.reshape(b, h, length, dilation, e).transpose(0, 1, 3, 2, 4)
        z = jnp.pad(z, ((0, 0), (0, 0), (0, 0), (0, pad), (0, 0)))
        return z.reshape(b, h, dilation, n_blk, DSWA_BLOCK, e)

    def with_prev(z):
        parts = [jnp.pad(z, ((0, 0), (0, 0), (0, 0), (p, 0), (0, 0), (0, 0)))[:, :, :, :n_blk]
                 for p in range(n_prev, 0, -1)]
        return jnp.concatenate(parts + [z], axis=4)

    qb = to_blocks(q)
    kb = with_prev(to_blocks(k))
    vb = with_prev(to_blocks(v))
    s = jnp.einsum('bhrnqe,bhrnke->bhrnqk', qb, kb).astype(jnp.float32)
    qi = jnp.arange(DSWA_BLOCK)[:, None]
    kj = jnp.arange(n_keys)[None, :]
    dist = n_prev * DSWA_BLOCK + qi - kj
    in_band = (dist >= 0) & (dist <= band)
    key_pos = (jnp.arange(n_blk)[:, None] - n_prev) * DSWA_BLOCK + jnp.arange(n_keys)[None, :]
    mask = in_band[None] & (key_pos >= 0)[:, None, :]
    s = jnp.where(mask, s, NEG_INF)
    lse = jax.nn.logsumexp(s, axis=-1)
    p = jnp.exp(s - lse[..., None]).astype(v.dtype)
    o = jnp.einsum('bhrnqk,bhrnke->bhrnqe', p, vb)
    o = o.reshape(b, h, dilation, n_blk * DSWA_BLOCK, e)[:, :, :, :length]
    o = o.transpose(0, 1, 3, 2, 4).reshape(b, h, t, e)
    lse = lse.reshape(b, h, dilation, n_blk * DSWA_BLOCK)[..., :length]
    lse = lse.transpose(0, 1, 3, 2).reshape(b, h, t)
    return o, lse


def _dilated_attention(q, k, v):
    outs, lses = [], []
    for window, dilation in DSWA_BRANCHES:
        o, l = _dilated_branch(q, k, v, window, dilation)
        outs.append(o)
        lses.append(l)
    wts = jax.nn.softmax(jnp.stack(lses, axis=0), axis=0)
    o = jnp.sum(wts[..., None] * jnp.stack(outs, axis=0).astype(jnp.float32), axis=0)
    return o.astype(q.dtype)


def _retention(q, k, v):
    b, h, t, e = q.shape
    c = RET_CHUNK
    nc = t // c
    log_g = jnp.log(1.0 - jnp.exp2(-5.0 - jnp.arange(h, dtype=jnp.float32)))
    idx = jnp.arange(c, dtype=jnp.float32)
    diff = idx[:, None] - idx[None, :]
    decay = jnp.where(diff >= 0, jnp.exp(log_g[:, None, None] * jnp.maximum(diff, 0.0)), 0.0)
    qc = q.reshape(b, h, nc, c, e)
    kc = k.reshape(b, h, nc, c, e)
    vc = v.reshape(b, h, nc, c, e)
    scores = jnp.einsum('bhncd,bhnmd->bhncm', qc, kc) * decay[None, :, None]
    inner = jnp.einsum('bhncm,bhnme->bhnce', scores.astype(v.dtype), vc)
    zeta = jnp.exp(log_g[:, None] * (c - 1.0 - idx)[None, :])
    kv = jnp.einsum('bhnmd,bhnme->bhnde', kc * zeta[None, :, None, :, None].astype(k.dtype), vc)
    chunk_decay = jnp.exp(log_g * c).astype(kv.dtype)[None, :, None, None]

    def step(state, kv_n):
        return state * chunk_decay + kv_n, state

    _, states = lax.scan(step, jnp.zeros_like(kv[:, :, 0]), jnp.moveaxis(kv, 2, 0))
    states = jnp.moveaxis(states, 0, 2)
    xi = jnp.exp(log_g[:, None] * (idx + 1.0)[None, :])
    cross = jnp.einsum('bhncd,bhnde->bhnce', qc, states) * xi[None, :, None, :, None].astype(q.dtype)
    out = (inner + cross).reshape(b, h, t, e).astype(jnp.float32)
    out = out * lax.rsqrt(jnp.mean(out * out, axis=-1, keepdims=True) + EPS)
    return out.astype(q.dtype)


def _hybrid_mixer(hx, w_in, w_out):
    proj = hx @ w_in
    qa, ka, va, qr, kr, vr, gr = jnp.split(proj, HYB_SPLITS, axis=-1)
    qa = _rope(_heads(qa, N_DSWA_HEADS), ROPE_DIMS, ROPE_THETA) * (HEAD_DIM ** -0.5)
    ka = _rope(_heads(ka, N_DSWA_HEADS), ROPE_DIMS, ROPE_THETA)
    a = _merge(_dilated_attention(qa, ka, _heads(va, N_DSWA_HEADS)))
    qr = _rope(_heads(qr, N_RET_HEADS), HEAD_DIM, RET_ROPE_THETA)
    kr = _rope(_heads(kr, N_RET_HEADS), HEAD_DIM, RET_ROPE_THETA) * (HEAD_DIM ** -0.5)
    r = jax.nn.silu(gr) * _merge(_retention(qr, kr, _heads(vr, N_RET_HEADS)))
    return jnp.concatenate([a, r], axis=-1) @ w_out


def _gmlp_mixer(hx, w_in, ln_g, ln_b, w_s, b_s, w_out):
    z = jax.nn.gelu(hx @ w_in, approximate=False)
    u, v = jnp.split(z, 2, axis=-1)
    v = _layernorm(v, ln_g, ln_b)
    b, t, _ = v.shape
    nc = t // GMLP_CHUNK
    v = v.reshape(b, nc, GMLP_CHUNK, GMLP_GROUPS, GMLP_GROUP_DIM)
    causal = jnp.tril(jnp.ones((GMLP_CHUNK, GMLP_CHUNK), dtype=bool))
    w = jnp.where(causal[None], w_s, 0)
    s = jnp.einsum('gij,bnjge->bnige', w, v) + b_s.T[None, None, :, :, None]
    return (u * s.reshape(b, t, GMLP_WIDTH)) @ w_out


def setup_inputs(seed: int = 0) -> dict:
    key = jax.random.key(seed)
    ks = jax.random.split(key, 16)
    f32 = jnp.float32
    nrm = lambda k, shape, scale: jax.random.normal(k, shape, f32) * scale
    return {
        'x': nrm(ks[0], (BATCH, SEQ, D_MODEL), 1.0),
        'norm_g': 1.0 + nrm(ks[1], (DEPTH, 6, D_MODEL), 0.05),
        'ffn_w_gate': nrm(ks[2], (DEPTH, 2, D_MODEL, D_FF), D_MODEL ** -0.5),
        'ffn_w_up': nrm(ks[3], (DEPTH, 2, D_MODEL, D_FF), D_MODEL ** -0.5),
        'ffn_w_down': nrm(ks[4], (DEPTH, 2, D_FF, D_MODEL), D_FF ** -0.5),
        'hyb_w_in': nrm(ks[5], (N_EVEN, D_MODEL, HYB_IN_WIDTH), D_MODEL ** -0.5),
        'hyb_w_out': nrm(ks[6], (N_EVEN, HYB_MIX_WIDTH, D_MODEL), HYB_MIX_WIDTH ** -0.5),
        'gmlp_w_in': nrm(ks[7], (N_ODD, D_MODEL, 2 * GMLP_WIDTH), D_MODEL ** -0.5),
        'gmlp_ln_g': 1.0 + nrm(ks[8], (N_ODD, GMLP_WIDTH), 0.05),
        'gmlp_ln_b': nrm(ks[9], (N_ODD, GMLP_WIDTH), 0.02),
        'gmlp_w_s': nrm(ks[10], (N_ODD, GMLP_GROUPS, GMLP_CHUNK, GMLP_CHUNK), GMLP_CHUNK ** -0.5),
        'gmlp_b_s': 1.0 + nrm(ks[11], (N_ODD, GMLP_GROUPS, GMLP_CHUNK), 0.02),
        'gmlp_w_out': nrm(ks[12], (N_ODD, GMLP_WIDTH, D_MODEL), GMLP_WIDTH ** -0.5),
    }


def reference(x, norm_g, ffn_w_gate, ffn_w_up, ffn_w_down, hyb_w_in, hyb_w_out,
              gmlp_w_in, gmlp_ln_g, gmlp_ln_b, gmlp_w_s, gmlp_b_s, gmlp_w_out):
    for layer in range(DEPTH):
        g = norm_g[layer]
        f = _swiglu(_rmsnorm(x, g[0]), ffn_w_gate[layer, 0], ffn_w_up[layer, 0], ffn_w_down[layer, 0])
        x = x + 0.5 * _rmsnorm(f, g[1])
        hx = _rmsnorm(x, g[2])
        j = layer // 2
        if layer % 2 == 0:
            m = _hybrid_mixer(hx, hyb_w_in[j], hyb_w_out[j])
        else:
            m = _gmlp_mixer(hx, gmlp_w_in[j], gmlp_ln_g[j], gmlp_ln_b[j],
                            gmlp_w_s[j], gmlp_b_s[j], gmlp_w_out[j])
        x = x + _rmsnorm(m, g[3])
        f = _swiglu(_rmsnorm(x, g[4]), ffn_w_gate[layer, 1], ffn_w_up[layer, 1], ffn_w_down[layer, 1])
        x = x + 0.5 * _rmsnorm(f, g[5])
    return x
```

```python
import math
from contextlib import ExitStack
import numpy as np
import concourse.bass as bass
import concourse.mybir as mybir
from concourse.bass_utils import run_bass_kernel_spmd

F32 = mybir.dt.float32
BF16 = mybir.dt.bfloat16
U8 = mybir.dt.uint8
AF = mybir.ActivationFunctionType
ALU = mybir.AluOpType

T = 2048
D = 1024
FF = 2816
NJ = 22
EPS = 1e-6
DEPTH = 4
NEXT = 4 * 640 + 4 * 768
DEBUG = {}


class Sched:
    ENGS = ("pe", "act", "dve", "pool", "sp")

    def __init__(self, nc):
        self.nc = nc
        self.ops = []
        self.pools = {}
        self.phase = 0

    def barrier(self, fn):
        self.ops.append(dict(eng="dve", fn=fn, r=(), w=(("PH",),), dma=None, late=None, ndma=1))
        self.phase += 1

    def op(self, eng, fn, r=(), w=(), dma=None, ndma=1):
        o = dict(eng=eng, fn=fn, r=tuple(r) + (("PH",),), w=tuple(w), dma=dma, late=None, ndma=ndma)
        self.ops.append(o)
        return o

    def consume(self, pool, nslots, eng, load_fn_of_slot, extra_r=(), ndma=1, lookahead=None):
        p = self.pools.setdefault((pool, self.phase), dict(n=0, nslots=nslots, marks=[]))
        i = p["n"]
        p["n"] += 1
        slot = i % nslots
        key = (pool, slot)
        mark = dict(eng=None, fn=None, r=(), w=(), dma=None, late=[])
        self.ops.append(mark)
        p["marks"].append(mark)
        dop = dict(eng=eng, fn=(lambda e, s=slot: load_fn_of_slot(e, s)), r=tuple(extra_r) + (("PH",),),
                   w=(key,), dma=("dma",) + key, late=None, ndma=ndma)
        la = (nslots - 1) if lookahead is None else lookahead
        tgt = p["marks"][max(0, i - la)]
        tgt["late"].append(dop)
        return slot, key

    def finalize(self):
        out = []
        for o in self.ops:
            if o["late"] is not None:
                out.extend(o["late"])
            else:
                out.append(o)
        self.ops = ops = out
        last_w = {}
        readers = {}
        for i, o in enumerate(ops):
            deps = {}
            for k in o["r"]:
                if k in last_w:
                    deps[last_w[k]] = True
            for k in o["w"]:
                if k in last_w:
                    deps.setdefault(last_w[k], False)
                for rd in readers.get(k, ()):
                    deps.setdefault(rd, False)
            deps.pop(i, None)
            o["deps"] = deps
            for k in o["r"]:
                readers.setdefault(k, []).append(i)
            for k in o["w"]:
                last_w[k] = i
                readers[k] = []
        for o in ops:
            o["signal"] = False
            o["need"] = []
        for o in ops:
            for d, raw in sorted(o["deps"].items()):
                p = ops[d]
                if p["dma"] is not None:
                    o["need"].append(d)
                elif p["eng"] == o["eng"]:
                    if o["eng"] == "pe":
                        continue
                    if raw or o["dma"] is not None:
                        p["signal"] = True
                        o["need"].append(d)
                else:
                    p["signal"] = True
                    o["need"].append(d)
        cnt = {e: 0 for e in self.ENGS}
        for o in ops:
            if o["dma"] is None and o["signal"]:
                cnt[o["eng"]] += 1
                o["sval"] = cnt[o["eng"]]
        self.stats = dict(cnt)
        self.dma_keys = sorted({o["dma"] for o in ops if o["dma"] is not None}, key=str)

    def emit(self):
        nc = self.nc
        ops = self.ops
        with ExitStack() as es:
            psem = {e: es.enter_context(nc.semaphore("p_" + e)) for e in self.ENGS}
            dsem = {k: es.enter_context(nc.semaphore("d_" + "_".join(str(x) for x in k[1:])))
                    for k in self.dma_keys}
            dtot = {k: 0 for k in self.dma_keys}
            for o in ops:
                if o["dma"] is not None:
                    dtot[o["dma"]] += 16 * o["ndma"]
                    o["dval"] = dtot[o["dma"]]
            block = es.enter_context(nc.Block())

            def run(ename, eng):
                waited = {}
                for o in ops:
                    if o["eng"] != ename:
                        continue
                    grp = {}
                    for d in o["need"]:
                        p = ops[d]
                        if p["dma"] is not None:
                            key, val, sem = ("d", p["dma"]), p["dval"], dsem[p["dma"]]
                        else:
                            key, val, sem = ("p", p["eng"]), p["sval"], psem[p["eng"]]
                        if key not in grp or grp[key][0] < val:
                            grp[key] = (val, sem)
                    for key, (val, sem) in grp.items():
                        if waited.get(key, 0) >= val:
                            continue
                        waited[key] = val
                        eng.wait_ge(sem, val)
                    if o["fn"] is None:
                        continue
                    ins = o["fn"](eng)
                    if o["dma"] is not None:
                        lst = ins if isinstance(ins, (list, tuple)) else [ins]
                        assert len(lst) == o["ndma"]
                        for x in lst:
                            x.then_inc(dsem[o["dma"]], 16)
                    elif o["signal"]:
                        ins.then_inc(psem[ename], 1)

            block.tensor(lambda e: run("pe", e))
            block.scalar(lambda e: run("act", e))
            block.vector(lambda e: run("dve", e))
            block.gpsimd(lambda e: run("pool", e))
            block.sync(lambda e: run("sp", e))


def _rope_tables():
    t = np.arange(T, dtype=np.float32)
    ca = np.ones((128, T), np.float32)
    sa = np.zeros((128, T), np.float32)
    half = 8
    inv = (np.float32(500000.0) ** (-(np.arange(half, dtype=np.float32) * 2.0 / 16))).astype(np.float32)
    ang = t[None, :] * inv[:, None]
    for hh in range(2):
        b = hh * 64
        ca[b:b + half] = np.cos(ang)
        ca[b + half:b + 2 * half] = np.cos(ang)
        sa[b:b + half] = -np.sin(ang)
        sa[b + half:b + 2 * half] = np.sin(ang)
    cr = np.zeros((128, T), np.float32)
    sr = np.zeros((128, T), np.float32)
    half = 32
    inv = (np.float32(10000.0) ** (-(np.arange(half, dtype=np.float32) * 2.0 / 64))).astype(np.float32)
    ang = t[None, :] * inv[:, None]
    for hh in range(2):
        b = hh * 64
        cr[b:b + half] = np.cos(ang)
        cr[b + half:b + 64] = np.cos(ang)
        sr[b:b + half] = -np.sin(ang)
        sr[b + half:b + 64] = np.sin(ang)
    return ca, sa, cr, sr


def _consts():
    idx = np.arange(128)
    k = idx[:, None]
    q = idx[None, :]
    cur = (k <= q).astype(np.float32)
    prev = (k >= q).astype(np.float32)
    m_n = np.concatenate([cur, prev], axis=1)
    m_f = np.concatenate([cur, np.zeros_like(prev)], axis=1)
    m_fnnn = np.concatenate([m_f, m_n, m_n, m_n], axis=1)
    ones = np.ones((128, 128), np.float32)
    blk = np.zeros((128, 128), np.float32)
    blk[:64, :64] = 1
    blk[64:, 64:] = 1
    ident = np.eye(128, dtype=np.float32)
    cb = np.concatenate([ones, blk, ident], axis=1)
    hb = np.concatenate([m_n, m_fnnn], axis=1)
    h = np.arange(8, dtype=np.float64)
    log_g = np.log(1.0 - np.exp2(-5.0 - h))
    diff = (q - k).astype(np.float64)
    dm = np.zeros((128, 8, 128), np.float64)
    for hh in range(8):
        dm[:, hh, :] = np.where(diff >= 0, np.exp(log_g[hh] * np.maximum(diff, 0)), 0.0) / 8.0
    xi = np.zeros((128, 4, 128), np.float64)
    zt = np.zeros((128, 4, 128), np.float64)
    cd = np.zeros((128, 8), np.float64)
    for p in range(128):
        for pr in range(4):
            hh = pr * 2 + p // 64
            xi[p, pr, :] = np.exp(log_g[hh] * (idx + 1.0))
            cd[p, pr] = np.exp(log_g[hh] * 128.0)
    for col in range(128):
        for pr in range(4):
            hh = pr * 2 + col // 64
            zt[:, pr, col] = np.exp(log_g[hh] * (127.0 - idx)) / 8.0
    hf = np.concatenate([dm.reshape(128, -1), xi.reshape(128, -1), zt.reshape(128, -1), cd],
                        axis=1).astype(np.float32)
    return cb.astype(np.float32), hb.astype(np.float32), hf


def _hyb_cols():
    cols = []
    def perm(base, half, rot):
        out = []
        for hh in range(2):
            for j in range(64):
                if j < half:
                    jj = j + half
                elif j < 2 * half:
                    jj = j - half
                else:
                    jj = j
                out.append(base + hh * 64 + jj)
        return out
    for p in range(4):
        qa = 0 + p * 128
        ka = 512 + p * 128
        va = 1024 + p * 128
        cols += list(range(qa, qa + 128)) + perm(qa, 8, 16)
        cols += list(range(ka, ka + 128)) + perm(ka, 8, 16)
        cols += list(range(va, va + 128))
    for p in range(4):
        qr = 1536 + p * 128
        kr = 2048 + p * 128
        vr = 2560 + p * 128
        gr = 3072 + p * 128
        cols += list(range(qr, qr + 128)) + perm(qr, 32, 64)
        cols += list(range(kr, kr + 128)) + perm(kr, 32, 64)
        cols += list(range(gr, gr + 128))
        cols += list(range(vr, vr + 128))
    assert len(cols) == NEXT
    return np.array(cols, dtype=np.int64)


def ssl(st, cnt, d=1):
    return slice(st, st + (cnt - 1) * d + 1, d)


def build_program(layers, stop_after=None):
    nc = bass.Bass("TRN2", target_bir_lowering=False)

    def din(name, shape):
        return nc.dram_tensor(name, list(shape), F32, kind="ExternalInput").ap()

    xT = din("xT", [D, T])
    yT = nc.dram_tensor("yT", [D, T], F32, kind="ExternalOutput").ap()
    g_all = din("g_all", [128, 192])
    wg_d = din("ffn_w_gate", [DEPTH, 2, D, FF])
    wu_d = din("ffn_w_up", [DEPTH, 2, D, FF])
    wd_d = din("ffn_w_down", [DEPTH, 2, FF, D])
    hin_d = din("hyb_w_in_ext", [2, D, NEXT])
    hout_d = din("hyb_w_out", [2, D, D])
    gin_d = din("gmlp_w_in", [2, D, 2 * D])
    gout_d = din("gmlp_w_out", [2, D, D])
    glng_d = din("gmlp_ln_g", [2, D])
    glnb_d = din("gmlp_ln_b", [2, D])
    gws_d = din("gmlp_w_sT", [2, 128, 8 * 128])
    gbs_d = din("gmlp_b_s", [2, 8 * 128])
    ropeA_c = din("ropeA_c", [128, T])
    ropeA_s = din("ropeA_s", [128, T])
    ropeR_c = din("ropeR_c", [128, T])
    ropeR_s = din("ropeR_s", [128, T])
    cb_d = din("cb", [128, 384])
    hb_d = din("hb", [128, 1280])
    hf_d = din("hf", [128, 2056])

    es = ExitStack()
    X = es.enter_context(nc.sbuf_tensor("X", [128, 8, T], F32))
    G = es.enter_context(nc.sbuf_tensor("G", [128, 192], F32))
    CB = es.enter_context(nc.sbuf_tensor("CB", [128, 384], BF16))
    MASKC = es.enter_context(nc.sbuf_tensor("MASKC", [128, 128], BF16))
    EPSB = es.enter_context(nc.sbuf_tensor("EPSB", [128, 4], F32))
    SCRB = 140 * 1024
    SCR = es.enter_context(nc.sbuf_tensor("SCR", [128, SCRB], U8))
    PSALL = es.enter_context(nc.psum_tensor("PSALL", [128, 4096], F32))
    ONES = CB[:, 0:128]
    BLK = CB[:, 128:256]
    IDENT = CB[:, 256:384]

    def PS(b, n=1):
        return PSALL[:, b * 512:(b + n) * 512]

    class Carver:
        def __init__(self, off=0):
            self.off = off

        def take(self, dtype, shape):
            n = 1
            for s_ in shape[1:]:
                n *= s_
            nb = n * (4 if dtype == F32 else 2)
            nb = (nb + 31) // 32 * 32
            ap = SCR[:, self.off:self.off + nb].bitcast(dtype)[:, 0:n]
            if len(shape) == 3:
                ap = ap.rearrange("p (a b) -> p a b", a=shape[1])
            elif len(shape) == 4:
                ap = ap.rearrange("p (a b c) -> p a b c", a=shape[1], b=shape[2])
            self.off += nb
            assert self.off <= SCRB, (self.off, SCRB)
            return ap

    S = Sched(nc)

    def barrier():
        S.barrier(lambda e: e.memset(EPSB[:, 2:3], 0.0))

    S.op("sp", lambda e: e.dma_start(out=G[:], in_=g_all), w=[("G",)], dma=("dma", "G"))
    S.op("pool", lambda e: e.dma_start(out=CB[:], in_=cb_d), w=[("CB",)], dma=("dma", "CB"))
    S.op("pool", lambda e: e.dma_start(out=MASKC[:], in_=hb_d[:, 0:128]), w=[("MASKC",)], dma=("dma", "MASKC"))
    S.op("dve", lambda e: e.memset(EPSB[:, 0:1], EPS), w=[("EPSB0",)])
    S.op("dve", lambda e: e.memset(EPSB[:, 1:2], math.log(0.5)), r=[("EPSB0",)], w=[("EPSB",)])
    xv = xT.rearrange("(c p) t -> p c t", p=128)
    yv = yT.rearrange("(c p) t -> p c t", p=128)
    for g in range(4):
        S.op("sp", lambda e, g=g: e.dma_start(out=X[:, :, g * 512:(g + 1) * 512],
                                              in_=xv[:, :, g * 512:(g + 1) * 512]),
             w=[("X", g)], dma=("dma", "X", g))

    def rstd_from_sq(sq_ap, nk, rs_ap, inv_n, ln_half, keys_r, key_rs, lhs=None):
        lhs = ONES if lhs is None else lhs

        def mm(e):
            ins = None
            for k in range(nk):
                src = sq_ap[:, k, :] if nk > 1 else sq_ap
                ins = e.matmul(PS(6), lhsT=lhs, rhs=src, start=(k == 0), stop=(k == nk - 1))
            return ins
        S.op("pe", mm, r=list(keys_r) + [("CB",)], w=[("ps", 6)])
        S.op("act", lambda e: e.activation(out=rs_ap, in_=PS(6), func=AF.Ln, scale=inv_n, bias=EPSB[:, 0:1]),
             r=[("ps", 6), ("EPSB",)], w=[key_rs])
        if ln_half:
            S.op("act", lambda e: e.activation(out=rs_ap, in_=rs_ap, func=AF.Exp, scale=-0.5, bias=EPSB[:, 1:2]),
                 r=[key_rs, ("EPSB",)], w=[key_rs])
        else:
            S.op("act", lambda e: e.activation(out=rs_ap, in_=rs_ap, func=AF.Exp, scale=-0.5),
                 r=[key_rs], w=[key_rs])

    def prenorm(gi, g, dst_fn, dst_key, SQ, RS):
        tok = slice(g * 512, (g + 1) * 512)
        S.op("act", lambda e: e.activation(out=SQ, in_=X[:, :, tok], func=AF.Square),
             r=[("X", g)], w=[("SQ",)])
        rstd_from_sq(SQ, 8, RS, 1.0 / D, False, [("SQ",)], ("RS",))
        for k in range(8):
            S.op("dve", lambda e, k=k: e.scalar_tensor_tensor(
                out=dst_fn(k), in0=X[:, k, tok], scalar=G[:, gi * 8 + k:gi * 8 + k + 1], in1=RS,
                op0=ALU.mult, op1=ALU.mult),
                r=[("X", g), ("RS",), ("G",)], w=[dst_key(k)])

    def proj_postnorm(src_fn, src_keys, nk, w_view, gi, g, half_factor, FB, SQ, RS, WDS):
        tok = slice(g * 512, (g + 1) * 512)
        for c in range(8):
            slot, key = S.consume("WD", 2, "pool",
                                  lambda e, s, c=c: e.dma_start(out=WDS[:, s, 0:nk, :],
                                                                in_=w_view[:, :, c * 128:(c + 1) * 128]))
            b = 4 + c % 2

            def mm(e, slot=slot, b=b):
                ins = None
                for j in range(nk):
                    ins = e.matmul(PS(b), lhsT=WDS[:, slot, j, :], rhs=src_fn(j), start=(j == 0), stop=(j == nk - 1))
                return ins
            S.op("pe", mm, r=[key] + list(src_keys), w=[("ps", b)])
            S.op("act", lambda e, c=c, b=b: e.activation(out=FB[:, c, :], in_=PS(b), func=AF.Copy),
                 r=[("ps", b)], w=[("F", c)])
        S.op("act", lambda e: e.activation(out=SQ, in_=FB, func=AF.Square),
             r=[("F", c) for c in range(8)], w=[("SQ",)])
        rstd_from_sq(SQ, 8, RS, 1.0 / D, half_factor, [("SQ",)], ("RS",))
        for c in range(8):
            S.op("dve", lambda e, c=c: e.scalar_tensor_tensor(
                out=FB[:, c, :], in0=FB[:, c, :], scalar=G[:, gi * 8 + c:gi * 8 + c + 1], in1=RS,
                op0=ALU.mult, op1=ALU.mult), r=[("F", c), ("RS",), ("G",)], w=[("F", c)])
        S.op("pool", lambda e: e.tensor_tensor(out=X[:, :, tok], in0=X[:, :, tok], in1=FB, op=ALU.add),
             r=[("F", c) for c in range(8)] + [("X", g)], w=[("X", g)])

    def ffn(l, i):
        cv = Carver()
        H = cv.take(BF16, [128, 8, 1024])
        ACTB = cv.take(BF16, [128, NJ, 1024])
        FB = cv.take(F32, [128, 8, 512])
        SQ = cv.take(BF16, [128, 8, 512])
        RS = cv.take(F32, [128, 512])
        SG = cv.take(F32, [128, 2, 512])
        WGU = cv.take(BF16, [128, 3, 16, 128])
        WDS = cv.take(BF16, [128, 2, NJ, 128])
        wgv = wg_d[l, i].rearrange("(k p) n -> p k n", p=128)
        wuv = wu_d[l, i].rearrange("(k p) n -> p k n", p=128)
        wdv = wd_d[l, i].rearrange("(j p) n -> p j n", p=128)
        gpre = (l * 6 + (0 if i == 0 else 4))
        gpost = gpre + 1
        for hf in range(2):
            for tt in range(2):
                g = hf * 2 + tt
                prenorm(gpre, g, lambda k, tt=tt: H[:, k, tt * 512:(tt + 1) * 512],
                        lambda k, tt=tt: ("H", tt), SQ, RS)
            for j in range(NJ):
                def ld(e, s, j=j):
                    a = e.dma_start(out=WGU[:, s, 0:8, :], in_=wgv[:, :, j * 128:(j + 1) * 128])
                    b = e.dma_start(out=WGU[:, s, 8:16, :], in_=wuv[:, :, j * 128:(j + 1) * 128])
                    return [a, b]
                slot, key = S.consume("WGU", 3, "pool", ld, ndma=2)
                for tt in range(2):
                    bg, bu = tt, 2 + tt

                    def mm(e, slot=slot, tt=tt, bg=bg, bu=bu):
                        ins = None
                        for k in range(8):
                            ins = e.matmul(PS(bg), lhsT=WGU[:, slot, k, :], rhs=H[:, k, tt * 512:(tt + 1) * 512],
                                           start=(k == 0), stop=(k == 7))
                        for k in range(8):
                            ins = e.matmul(PS(bu), lhsT=WGU[:, slot, 8 + k, :], rhs=H[:, k, tt * 512:(tt + 1) * 512],
                                           start=(k == 0), stop=(k == 7))
                        return ins
                    S.op("pe", mm, r=[key, ("H", tt)], w=[("ps", bg), ("ps", bu)])
                    S.op("act", lambda e, tt=tt, bg=bg: e.activation(out=SG[:, tt, :], in_=PS(bg), func=AF.Silu),
                         r=[("ps", bg)], w=[("SG", tt)])
                    S.op("dve", lambda e, bu=bu, j=j, tt=tt: e.tensor_tensor(
                        out=ACTB[:, j, tt * 512:(tt + 1) * 512], in0=SG[:, tt, :], in1=PS(bu), op=ALU.mult),
                        r=[("SG", tt), ("ps", bu)], w=[("A", j, tt)])
            for tt in range(2):
                g = hf * 2 + tt
                proj_postnorm(lambda j, tt=tt: ACTB[:, j, tt * 512:(tt + 1) * 512],
                              [("A", j, tt) for j in range(NJ)], NJ, wdv, gpost, g, True, FB, SQ, RS, WDS)

    def gmlp(l):
        jl = l // 2
        cv = Carver()
        H = cv.take(BF16, [128, 8, 512])
        UT = cv.take(BF16, [128, 8, 512])
        YT = cv.take(BF16, [128, 8, 512])
        VG = cv.take(F32, [128, 4, 1024])
        VN = cv.take(BF16, [128, 4, 1024])
        LNG = cv.take(F32, [128, 1024])
        LNB = cv.take(F32, [128, 1024])
        WST = cv.take(BF16, [128, 8, 128])
        BS = cv.take(F32, [128, 8, 128])
        WIN = cv.take(BF16, [128, 3, 8, 128])
        WV = cv.take(BF16, [128, 2, 8, 512])
        FB = cv.take(F32, [128, 8, 512])
        SQ = cv.take(BF16, [128, 8, 512])
        RS = cv.take(F32, [128, 512])
        Y1 = cv.take(F32, [128, 2, 512])
        ST = cv.take(F32, [128, 4, 16])
        MV = cv.take(F32, [128, 4, 2])
        RSD = cv.take(F32, [128, 4])
        WDS = cv.take(BF16, [128, 2, 8, 128])
        winv = gin_d[jl].rearrange("(k p) n -> p k n", p=128)
        woutv = gout_d[jl].rearrange("(j p) n -> p j n", p=128)
        gi = l * 6 + 2
        S.op("sp", lambda e: e.dma_start(out=LNG, in_=glng_d[jl:jl + 1, :].to_broadcast([128, D])),
             w=[("LNG",)], dma=("dma", "LNG"))
        S.op("sp", lambda e: e.dma_start(out=LNB, in_=glnb_d[jl:jl + 1, :].to_broadcast([128, D])),
             w=[("LNB",)], dma=("dma", "LNB"))
        S.op("sp", lambda e: e.dma_start(out=BS.rearrange("p a b -> p (a b)"),
                                         in_=gbs_d[jl:jl + 1, :].to_broadcast([128, D])),
             w=[("BS",)], dma=("dma", "BS"))
        S.op("pool", lambda e: e.dma_start(out=WST.rearrange("p a b -> p (a b)"), in_=gws_d[jl]),
             w=[("WST0",)], dma=("dma", "WST"))
        S.op("dve", lambda e: e.tensor_tensor(out=WST, in0=WST, in1=MASKC[:, None, :].to_broadcast([128, 8, 128]),
                                              op=ALU.mult),
             r=[("WST0",), ("MASKC",)], w=[("WST",)])
        GS = DEBUG.get("gstage", 9)
        for g in range(4):
            prenorm(gi, g, lambda k: H[:, k, :], lambda k: ("H",), SQ, RS)
            for ct in range(8 if GS >= 1 else 0):
                slot, key = S.consume("GWIN", 3, "pool",
                                      lambda e, s, ct=ct: e.dma_start(out=WIN[:, s], in_=winv[:, :, ct * 128:(ct + 1) * 128]))
                b = ct % 2

                def mm(e, slot=slot, b=b):
                    ins = None
                    for k in range(8):
                        ins = e.matmul(PS(b), lhsT=WIN[:, slot, k, :], rhs=H[:, k, :], start=(k == 0), stop=(k == 7))
                    return ins
                S.op("pe", mm, r=[key, ("H",)], w=[("ps", b)])
                S.op("act", lambda e, ct=ct, b=b: e.activation(out=UT[:, ct, :], in_=PS(b), func=AF.Gelu),
                     r=[("ps", b)], w=[("UT", ct)])
            for hv in range(2 if GS >= 2 else 0):
                slot, key = S.consume("GWV", 2, "pool",
                                      lambda e, s, hv=hv: e.dma_start(out=WV[:, s],
                                                                       in_=winv[:, :, D + hv * 512:D + (hv + 1) * 512]))
                for m in range(4):
                    b = 2 + (hv * 4 + m) % 2

                    def mm(e, slot=slot, b=b, m=m):
                        ins = None
                        for k in range(8):
                            ins = e.matmul(PS(b), lhsT=H[:, k, m * 128:(m + 1) * 128], rhs=WV[:, slot, k, :],
                                           start=(k == 0), stop=(k == 7))
                        return ins
                    S.op("pe", mm, r=[key, ("H",)], w=[("ps", b)])
                    S.op("act", lambda e, b=b, m=m, hv=hv: e.activation(
                        out=VG[:, m, hv * 512:(hv + 1) * 512], in_=PS(b), func=AF.Gelu),
                        r=[("ps", b)], w=[("VG", m, hv)])
                    S.op("dve", lambda e, m=m, hv=hv: e.bn_stats(out=ST[:, m, hv * 6:(hv + 1) * 6],
                                                                  in_=VG[:, m, hv * 512:(hv + 1) * 512]),
                         r=[("VG", m, hv)], w=[("STAT", m, hv)])
            if GS < 3:
                continue
            for m in range(4):
                S.op("dve", lambda e, m=m: e.bn_aggr(out=MV[:, m, :], in_=ST[:, m, 0:12]),
                     r=[("STAT", m, 0), ("STAT", m, 1)], w=[("MV", m)])
            S.op("act", lambda e: e.activation(out=RSD, in_=MV[:, :, 1], func=AF.Ln, bias=EPSB[:, 0:1]),
                 r=[("MV", m) for m in range(4)] + [("EPSB",)], w=[("RSD",)])
            S.op("act", lambda e: e.activation(out=RSD, in_=RSD, func=AF.Exp, scale=-0.5),
                 r=[("RSD",)], w=[("RSD",)])
            for m in range(4):
                vk = [("VG", m, 0), ("VG", m, 1)]
                S.op("dve", lambda e, m=m: e.tensor_scalar(out=VG[:, m, :], in0=VG[:, m, :], scalar1=MV[:, m, 0:1],
                                                           scalar2=RSD[:, m:m + 1], op0=ALU.subtract, op1=ALU.mult),
                     r=vk + [("MV", m), ("RSD",)], w=vk)
                S.op("dve", lambda e, m=m: e.tensor_tensor(out=VG[:, m, :], in0=VG[:, m, :], in1=LNG, op=ALU.mult),
                     r=vk + [("LNG",)], w=vk)
                S.op("pool", lambda e, m=m: e.tensor_tensor(out=VN[:, m, :], in0=VG[:, m, :], in1=LNB, op=ALU.add),
                     r=vk + [("LNB",)], w=[("VN", m)])
            if GS < 4:
                continue
            for gg in range(8):
                b = 4 + gg % 2

                def mm(e, gg=gg, b=b):
                    ins = None
                    for m in range(4):
                        ins = e.matmul(PS(b)[:, m * 128:(m + 1) * 128], lhsT=VN[:, m, gg * 128:(gg + 1) * 128],
                                       rhs=WST[:, gg, :], start=True, stop=True)
                    return ins
                S.op("pe", mm, r=[("VN", m) for m in range(4)] + [("WST",)], w=[("ps", b)])
                u = gg % 2
                S.op("dve", lambda e, gg=gg, b=b, u=u: e.tensor_tensor(
                    out=Y1[:, u, :].rearrange("p (m i) -> p m i", m=4),
                    in0=PS(b).rearrange("p (m i) -> p m i", m=4),
                    in1=BS[:, gg:gg + 1, :].to_broadcast([128, 4, 128]), op=ALU.add),
                    r=[("ps", b), ("BS",)], w=[("Y1", u)])
                S.op("pool", lambda e, gg=gg, u=u: e.tensor_tensor(out=YT[:, gg, :], in0=Y1[:, u, :], in1=UT[:, gg, :],
                                                                   op=ALU.mult),
                     r=[("Y1", u), ("UT", gg)], w=[("YT", gg)])
            if GS < 5:
                continue
            proj_postnorm(lambda j: YT[:, j, :], [("YT", j) for j in range(8)], 8, woutv, gi + 1, g, False,
                          FB, SQ, RS, WDS)

    def hybrid(l):
        jl = l // 2
        cv = Carver()
        HT = cv.take(BF16, [128, 8, T])
        MIX = cv.take(BF16, [128, 8, T])
        WIN = cv.take(BF16, [128, 6, 8, 128])
        ROPE = cv.take(F32, [128, 2, 2, 512])
        TMP = cv.take(F32, [128, 2, 512])
        RS = cv.take(F32, [128, 512])
        pair_off = cv.off
        winv = hin_d[jl].rearrange("(k p) n -> p k n", p=128)
        woutv = hout_d[jl].rearrange("(j p) n -> p j n", p=128)
        gi = l * 6 + 2
        SQ0 = Carver(pair_off).take(BF16, [128, 8, 512])
        for g in range(4):
            prenorm(gi, g, lambda k, g=g: HT[:, k, g * 512:(g + 1) * 512], lambda k, g=g: ("HT", g), SQ0, RS)
        barrier()
        HTK = [("HT", g) for g in range(4)]

        def win_block(col0):
            return S.consume("HWIN", 6, "pool",
                             lambda e, s, col0=col0: e.dma_start(out=WIN[:, s], in_=winv[:, :, col0:col0 + 128]),
                             lookahead=2)

        def rope_proj(col0, rc, rs_, dsts, fam):
            blks = [win_block(col0 + i * 128) for i in range(4)]
            for g in range(4):
                tok = slice(g * 512, (g + 1) * 512)

                def ldrope(e, s, g=g):
                    a = e.dma_start(out=ROPE[:, s, 0, :], in_=rc[:, g * 512:(g + 1) * 512])
                    b = e.dma_start(out=ROPE[:, s, 1, :], in_=rs_[:, g * 512:(g + 1) * 512])
                    return [a, b]
                rslot, rkey = S.consume("ROPE", 2, "sp", ldrope, ndma=2)
                for qi in range(2):
                    u = (g * 2 + qi) % 2
                    b0, b1 = 2 * u, 2 * u + 1
                    (s0, k0), (s1, k1) = blks[2 * qi], blks[2 * qi + 1]

                    def mm(e, s0=s0, s1=s1, b0=b0, b1=b1, tok=tok):
                        ins = None
                        for k in range(8):
                            ins = e.matmul(PS(b0), lhsT=WIN[:, s0, k, :], rhs=HT[:, k, tok], start=(k == 0), stop=(k == 7))
                        for k in range(8):
                            ins = e.matmul(PS(b1), lhsT=WIN[:, s1, k, :], rhs=HT[:, k, tok], start=(k == 0), stop=(k == 7))
                        return ins
                    S.op("pe", mm, r=[k0, k1, ("HT", g)], w=[("ps", b0), ("ps", b1)])
                    S.op("dve", lambda e, b0=b0, rslot=rslot: e.tensor_tensor(
                        out=TMP[:, 0, :], in0=PS(b0), in1=ROPE[:, rslot, 0, :], op=ALU.mult),
                        r=[("ps", b0), rkey], w=[("TMP", 0)])
                    S.op("dve", lambda e, b1=b1, rslot=rslot: e.tensor_tensor(
                        out=TMP[:, 1, :], in0=PS(b1), in1=ROPE[:, rslot, 1, :], op=ALU.mult),
                        r=[("ps", b1), rkey], w=[("TMP", 1)])
                    dst = dsts[qi]
                    S.op("pool", lambda e, dst=dst, tok=tok: e.tensor_tensor(
                        out=dst[:, tok], in0=TMP[:, 0, :], in1=TMP[:, 1, :], op=ALU.add),
                        r=[("TMP", 0), ("TMP", 1)], w=[("QK", fam, qi, g)])

        def dswa_pair(p):
            cvp = Carver(pair_off)
            HB = cvp.take(BF16, [128, 1280])
            QT = cvp.take(BF16, [128, T])
            KT = cvp.take(BF16, [128, T])
            VA = cvp.take(BF16, [128, 3, 16, 192])
            ACC = cvp.take(F32, [128, 2, T])
            E = cvp.take(BF16, [128, 2, 1024])
            M_N = HB[:, 0:256]
            M_F = HB[:, 256:1280]
            RC = TMP
            col0 = p * 640
            if p == 0:
                S.op("pool", lambda e: e.dma_start(out=HB, in_=hb_d), w=[("HB",)], dma=("dma", "HB"))
                S.op("pool", lambda e: e.memset(VA[:, :, :, 64:128], 1.0), w=[("VA1",)])
            rope_proj(col0, ropeA_c, ropeA_s, [QT, KT], "a")
            vs, vk = win_block(col0 + 512)
            for bi, d in enumerate((1, 4, 16)):
                tpc = 16 // d
                for i4 in range(4):
                    b = 4 + (bi * 4 + i4) % 2

                    def mm(e, b=b, d=d, i4=i4, tpc=tpc):
                        ins = None
                        for ii in range(4):
                            i = i4 * 4 + ii
                            r, blk = i // tpc, i % tpc
                            st = blk * 128 * d + r
                            for k in range(8):
                                ins = e.matmul(PS(b)[:, ii * 128:(ii + 1) * 128],
                                               lhsT=HT[:, k, ssl(st, 128, d)], rhs=WIN[:, vs, k, :],
                                               start=(k == 0), stop=(k == 7))
                        return ins
                    S.op("pe", mm, r=[vk] + HTK, w=[("ps", b)])
                    S.op("act", lambda e, b=b, bi=bi, i4=i4: e.activation(
                        out=VA[:, bi, i4 * 4:(i4 + 1) * 4, :].rearrange("p i (s c) -> p i s c", s=3)[:, :, 0:3:2, :],
                        in_=PS(b).rearrange("p (i s c) -> p i s c", i=4, s=2), func=AF.Copy),
                        r=[("ps", b), ("VA1",)], w=[("VA", bi, i4)])
            QKK = [("QK", "a", qi, g) for qi in range(2) for g in range(4)]
            unit = 0
            for hh in range(2):
                hs = slice(hh * 64, hh * 64 + 64)
                vcols = slice(hh * 64, hh * 64 + 128)
                for bi, d in enumerate((1, 4, 16)):
                    tpc = 16 // d
                    for i4 in range(4):
                        su = unit % 2
                        unit += 1
                        sb0 = 2 * su
                        ob = 4 + su
                        blocks = []
                        for ii in range(4):
                            i = i4 * 4 + ii
                            r, n = i // tpc, i % tpc
                            blocks.append((i, n, n * 128 * d + r, (n - 1) * 128 * d + r))
                        noprev = (d == 16)
                        W = 128 if noprev else 256

                        def mm_s(e, blocks=blocks, sb0=sb0, d=d, hs=hs, noprev=noprev, W=W):
                            ins = None
                            for ii, (i, n, st, pst) in enumerate(blocks):
                                qsl = ssl(st, 128, d)
                                ins = e.matmul(PS(sb0, 2)[:, ii * W:ii * W + 128], lhsT=KT[hs, qsl], rhs=QT[hs, qsl],
                                               start=True, stop=True)
                                if not noprev:
                                    ksl = qsl if n == 0 else ssl(pst, 128, d)
                                    ins = e.matmul(PS(sb0, 2)[:, ii * W + 128:ii * W + 256], lhsT=KT[hs, ksl],
                                                   rhs=QT[hs, qsl], start=True, stop=True)
                            return ins
                        S.op("pe", mm_s, r=QKK, w=[("ps", sb0), ("ps", sb0 + 1)])
                        S.op("act", lambda e, sb0=sb0, su=su, W=W: e.activation(
                            out=E[:, su, 0:4 * W], in_=PS(sb0, 2)[:, 0:4 * W], func=AF.Exp, scale=0.125),
                            r=[("ps", sb0), ("ps", sb0 + 1)], w=[("E", su)])
                        if noprev:
                            msk = M_N[:, None, 0:128].to_broadcast([128, 4, 128])
                            ev = E[:, su, 0:512].rearrange("p (a b) -> p a b", a=4)
                        elif blocks[0][1] == 0:
                            msk = M_F
                            ev = E[:, su, :]
                        else:
                            msk = M_N[:, None, :].to_broadcast([128, 4, 256])
                            ev = E[:, su, :].rearrange("p (a b) -> p a b", a=4)
                        S.op("dve", lambda e, ev=ev, msk=msk: e.tensor_tensor(out=ev, in0=ev, in1=msk, op=ALU.mult),
                             r=[("E", su), ("HB",)], w=[("E", su)])

                        def mm_o(e, blocks=blocks, ob=ob, su=su, bi=bi, vcols=vcols, noprev=noprev, W=W):
                            ins = None
                            for ii, (i, n, st, pst) in enumerate(blocks):
                                hasprev = (not noprev) and n > 0
                                ins = e.matmul(PS(ob)[:, ii * 128:(ii + 1) * 128], lhsT=VA[:, bi, i, vcols],
                                               rhs=E[:, su, ii * W:ii * W + 128], start=True, stop=not hasprev)
                                if hasprev:
                                    ins = e.matmul(PS(ob)[:, ii * 128:(ii + 1) * 128], lhsT=VA[:, bi, i - 1, vcols],
                                                   rhs=E[:, su, ii * W + 128:ii * W + 256], start=False, stop=True)
                            return ins
                        S.op("pe", mm_o, r=[("E", su)] + [("VA", bi, x) for x in range(4)], w=[("ps", ob)])
                        if d == 16:
                            r0 = blocks[0][0]
                            dst = ACC[:, hh, :].rearrange("p (m r) -> p r m", r=16)[:, r0:r0 + 4, :]
                            src = PS(ob).rearrange("p (a m) -> p a m", a=4)
                        else:
                            dst = ACC[:, hh, ssl(blocks[0][2], 512, d)]
                            src = PS(ob)
                        if bi == 0:
                            S.op("dve", lambda e, dst=dst, src=src: e.tensor_copy(out=dst, in_=src),
                                 r=[("ps", ob)], w=[("ACC", hh)])
                        else:
                            S.op("dve", lambda e, dst=dst, src=src: e.tensor_tensor(out=dst, in0=dst, in1=src, op=ALU.add),
                                 r=[("ps", ob), ("ACC", hh)], w=[("ACC", hh)])
                orow = slice(hh * 64, hh * 64 + 64)
                drow = slice(64 - hh * 64, 128 - hh * 64)
                for g in range(4):
                    tok = slice(g * 512, (g + 1) * 512)
                    u = g % 2
                    S.op("dve", lambda e, hh=hh, tok=tok, u=u, orow=orow, drow=drow: e.reciprocal(
                        out=RC[orow, u, :], in_=ACC[drow, hh, tok]), r=[("ACC", hh)], w=[("TMP", u)])
                    S.op("dve", lambda e, hh=hh, tok=tok, u=u, orow=orow: e.tensor_tensor(
                        out=MIX[orow, p, tok], in0=ACC[orow, hh, tok], in1=RC[orow, u, :], op=ALU.mult),
                        r=[("TMP", u), ("ACC", hh)], w=[("MIX", p, hh, g)])

        def ret_pair(p):
            cvp = Carver(pair_off)
            HF = cvp.take(F32, [128, 2056])
            QT = cvp.take(BF16, [128, T])
            KT = cvp.take(BF16, [128, T])
            QX = cvp.take(BF16, [128, T])
            GT = cvp.take(BF16, [128, T])
            VT = cvp.take(BF16, [128, 16, 128])
            KZ = cvp.take(BF16, [128, 16, 128])
            R32 = cvp.take(F32, [128, T])
            ST32 = cvp.take(F32, [128, 64])
            STB = cvp.take(BF16, [128, 2, 64])
            SD = cvp.take(BF16, [128, 2, 256])
            SQH = cvp.take(BF16, [128, 512])
            DM = HF[:, 0:1024].rearrange("p (h c) -> p h c", h=8)
            XI = HF[:, 1024:1536].rearrange("p (a c) -> p a c", a=4)
            ZT = HF[:, 1536:2048].rearrange("p (a c) -> p a c", a=4)
            CD = HF[:, 2048:2056]
            col0 = 4 * 640 + p * 768
            if p == 0:
                S.op("sp", lambda e: e.dma_start(out=HF, in_=hf_d), w=[("HF",)], dma=("dma", "HF"))
            rope_proj(col0, ropeR_c, ropeR_s, [QT, KT], "r")
            for g in range(4):
                tok = slice(g * 512, (g + 1) * 512)
                S.op("pool", lambda e, tok=tok: e.tensor_tensor(
                    out=QX[:, tok].rearrange("p (a c) -> p a c", a=4), in0=QT[:, tok].rearrange("p (a c) -> p a c", a=4),
                    in1=XI[:, p:p + 1, :].to_broadcast([128, 4, 128]), op=ALU.mult),
                    r=[("QK", "r", 0, g), ("HF",)], w=[("QX", g)])
            gs, gk = win_block(col0 + 512)
            for g in range(4):
                tok = slice(g * 512, (g + 1) * 512)
                b = g % 2

                def mm(e, b=b, tok=tok):
                    ins = None
                    for k in range(8):
                        ins = e.matmul(PS(b), lhsT=WIN[:, gs, k, :], rhs=HT[:, k, tok], start=(k == 0), stop=(k == 7))
                    return ins
                S.op("pe", mm, r=[gk, ("HT", g)], w=[("ps", b)])
                S.op("act", lambda e, b=b, tok=tok: e.activation(out=GT[:, tok], in_=PS(b), func=AF.Silu),
                     r=[("ps", b)], w=[("GT", g)])
            vs, vk = win_block(col0 + 640)
            for i4 in range(4):
                b = 2 + i4 % 2

                def mm(e, b=b, i4=i4):
                    ins = None
                    for ii in range(4):
                        i = i4 * 4 + ii
                        for k in range(8):
                            ins = e.matmul(PS(b)[:, ii * 128:(ii + 1) * 128], lhsT=HT[:, k, i * 128:(i + 1) * 128],
                                           rhs=WIN[:, vs, k, :], start=(k == 0), stop=(k == 7))
                    return ins
                S.op("pe", mm, r=[vk] + HTK, w=[("ps", b)])
                S.op("act", lambda e, b=b, i4=i4: e.activation(
                    out=VT[:, i4 * 4:(i4 + 1) * 4, :], in_=PS(b).rearrange("p (i c) -> p i c", i=4), func=AF.Copy),
                    r=[("ps", b)], w=[("VT", i4)])
            PSB7 = PS(7).bitcast(BF16)
            for i4 in range(DEBUG.get("ntr", 4)):
                def tr(e, i4=i4):
                    ins = None
                    for ii in range(4):
                        i = i4 * 4 + ii
                        ins = e.transpose(PSB7[:, ii * 128:(ii + 1) * 128], KT[:, i * 128:(i + 1) * 128], IDENT)
                    return ins
                S.op("pe", tr, r=[("QK", "r", 1, i4), ("CB",)], w=[("ps", 7)])
                S.op("dve", lambda e, i4=i4: e.tensor_tensor(
                    out=KZ[:, i4 * 4:(i4 + 1) * 4, :], in0=PSB7[:, 0:512].rearrange("p (i c) -> p i c", i=4),
                    in1=ZT[:, p:p + 1, :].to_broadcast([128, 4, 128]), op=ALU.mult),
                    r=[("ps", 7), ("HF",)], w=[("KZ", i4)])
            S.op("dve", lambda e: e.memset(ST32, 0.0), w=[("ST32",)])
            for n in range(DEBUG.get("nrec", 16)):
                g = n // 4
                ck = slice(n * 128, (n + 1) * 128)
                sb = n % 2
                b0 = 2 * sb
                kvb = 6 + n % 2

                def mm_s(e, ck=ck, b0=b0):
                    e.matmul(PS(b0)[:, 0:128], lhsT=KT[0:64, ck], rhs=QT[0:64, ck], start=True, stop=True)
                    return e.matmul(PS(b0 + 1)[:, 0:128], lhsT=KT[64:128, ck], rhs=QT[64:128, ck], start=True, stop=True)
                S.op("pe", mm_s, r=[("QK", "r", 0, g), ("QK", "r", 1, g)], w=[("ps", b0), ("ps", b0 + 1)])
                S.op("dve", lambda e, sb=sb, b0=b0: e.tensor_tensor(
                    out=SD[:, sb, :].rearrange("p (h c) -> p h c", h=2),
                    in0=PS(b0, 2).rearrange("p (h c) -> p h c", h=2)[:, :, 0:128], in1=DM[:, 2 * p:2 * p + 2, :],
                    op=ALU.mult),
                    r=[("ps", b0), ("ps", b0 + 1), ("HF",)], w=[("SD", sb)])

                def mm_kv(e, n=n, kvb=kvb):
                    e.matmul(PS(kvb)[0:64, 0:64], lhsT=KZ[:, n, 0:64], rhs=VT[:, n, 0:64], start=True, stop=True)
                    return e.matmul(PS(kvb)[64:128, 0:64], lhsT=KZ[:, n, 64:128], rhs=VT[:, n, 64:128],
                                    start=True, stop=True)
                if DEBUG.get("kv", 1):
                    S.op("pe", mm_kv, r=[("KZ", n // 4), ("VT", n // 4)], w=[("ps", kvb)])

                def mm_o(e, n=n, sb=sb, ck=ck):
                    oc = slice((n % 4) * 128, (n % 4 + 1) * 128)
                    st = n % 2
                    ins = None
                    for hh in range(2):
                        hs = slice(hh * 64, hh * 64 + 64)
                        ins = e.matmul(PS(4 + hh)[hs, oc], lhsT=VT[:, n, hs], rhs=SD[:, sb, hh * 128:(hh + 1) * 128],
                                       start=True, stop=(n == 0))
                        if n > 0:
                            ins = e.matmul(PS(4 + hh)[hs, oc], lhsT=STB[hs, st, :], rhs=QX[hs, ck], start=False, stop=True)
                    return ins
                rk = [("SD", sb), ("VT", n // 4), ("QX", g)] + ([("STB", n % 2)] if n > 0 else [])
                if DEBUG.get("mo", 1):
                    S.op("pe", mm_o, r=rk, w=[("ps", 4), ("ps", 5)])
                if n < 15 and DEBUG.get("su", 1):
                    S.op("dve", lambda e, kvb=kvb: e.scalar_tensor_tensor(
                        out=ST32, in0=ST32, scalar=CD[:, p:p + 1], in1=PS(kvb)[:, 0:64], op0=ALU.mult, op1=ALU.add),
                        r=[("ps", kvb), ("ST32",), ("HF",)], w=[("ST32",)])
                    S.op("act", lambda e, n=n: e.activation(out=STB[:, (n + 1) % 2, :], in_=ST32, func=AF.Copy),
                         r=[("ST32",)], w=[("STB", (n + 1) % 2)])
                if n % 4 == 3:
                    tok = slice(g * 512, (g + 1) * 512)
                    S.op("act", lambda e, tok=tok: e.activation(out=R32[0:64, tok], in_=PS(4)[0:64, :], func=AF.Copy),
                         r=[("ps", 4)], w=[("R32a", g)])
                    S.op("act", lambda e, tok=tok: e.activation(out=R32[64:128, tok], in_=PS(5)[64:128, :], func=AF.Copy),
                         r=[("ps", 5)], w=[("R32", g)])
            for g in range(4):
                tok = slice(g * 512, (g + 1) * 512)
                S.op("act", lambda e, tok=tok: e.activation(out=SQH, in_=R32[:, tok], func=AF.Square),
                     r=[("R32", g), ("R32a", g)], w=[("SQH",)])
                rstd_from_sq(SQH, 1, RS, 1.0 / 64, False, [("SQH",)], ("RS",), lhs=BLK)
                S.op("dve", lambda e, tok=tok: e.tensor_tensor(out=TMP[:, 0, :], in0=R32[:, tok], in1=RS, op=ALU.mult),
                     r=[("R32", g), ("R32a", g), ("RS",)], w=[("TMP", 0)])
                S.op("pool", lambda e, tok=tok: e.tensor_tensor(out=MIX[:, 4 + p, tok], in0=TMP[:, 0, :], in1=GT[:, tok],
                                                                op=ALU.mult),
                     r=[("TMP", 0), ("GT", g)], w=[("MIX", 4 + p, 0, g), ("MIX", 4 + p, 1, g)])

        for p in range(DEBUG.get("ndswa", 4)):
            dswa_pair(p)
        barrier()
        for p in range(DEBUG.get("nret", 4)):
            ret_pair(p)
        barrier()
        if DEBUG.get("dump_mix"):
            for g in range(4):
                S.op("act", lambda e, g=g: e.activation(out=X[:, :, g * 512:(g + 1) * 512], in_=MIX[:, :, g * 512:(g + 1) * 512],
                                                        func=AF.Copy),
                     r=[("MIX", j, hh, g) for j in range(8) for hh in range(2)] + [("X", g)], w=[("X", g)])
            return
        cvo = Carver(pair_off)
        FB = cvo.take(F32, [128, 8, 512])
        SQ = cvo.take(BF16, [128, 8, 512])
        WDS = cvo.take(BF16, [128, 2, 8, 128])
        for g in range(4):
            tok = slice(g * 512, (g + 1) * 512)
            proj_postnorm(lambda j, tok=tok: MIX[:, j, tok],
                          [("MIX", j, hh, g) for j in range(8) for hh in range(2)], 8, woutv, gi + 1, g, False,
                          FB, SQ, RS, WDS)

    count = 0
    done = False
    for l in layers:
        for ph in range(3):
            if ph == 0:
                ffn(l, 0)
            elif ph == 1:
                if DEBUG.get("skipmix"):
                    pass
                elif l % 2 == 0:
                    hybrid(l)
                else:
                    gmlp(l)
            else:
                ffn(l, 1)
            barrier()
            count += 1
            if stop_after is not None and count >= stop_after:
                done = True
                break
        if done:
            break

    for g in range(4):
        S.op("sp", lambda e, g=g: e.dma_start(out=yv[:, :, g * 512:(g + 1) * 512], in_=X[:, :, g * 512:(g + 1) * 512]),
             r=[("X", g)], w=[("Y", g)], dma=("dma", "Y", g))
    S.op("sp", None, r=[("Y", g) for g in range(4)])
    S.finalize()
    S.emit()
    es.close()
    return nc, S


_CACHE = {}


def _host_consts():
    if "c" not in _CACHE:
        ca, sa, cr, sr = _rope_tables()
        cb, hb, hf = _consts()
        _CACHE["c"] = dict(ropeA_c=ca, ropeA_s=sa, ropeR_c=cr, ropeR_s=sr, cb=cb, hb=hb, hf=hf)
        _CACHE["cols"] = _hyb_cols()
    return _CACHE["c"], _CACHE["cols"]


def prepare_inputs(inputs):
    consts, cols = _host_consts()
    f = lambda a: np.ascontiguousarray(np.asarray(a, dtype=np.float32))
    ng = f(inputs["norm_g"])
    g_all = np.ascontiguousarray(ng.reshape(4, 6, 8, 128).transpose(3, 0, 1, 2).reshape(128, 192))
    shared = dict(
        g_all=g_all,
        ffn_w_gate=f(inputs["ffn_w_gate"]), ffn_w_up=f(inputs["ffn_w_up"]), ffn_w_down=f(inputs["ffn_w_down"]),
        hyb_w_in_ext=np.ascontiguousarray(f(inputs["hyb_w_in"])[:, :, cols]),
        hyb_w_out=f(inputs["hyb_w_out"]),
        gmlp_w_in=f(inputs["gmlp_w_in"]), gmlp_w_out=f(inputs["gmlp_w_out"]),
        gmlp_ln_g=f(inputs["gmlp_ln_g"]), gmlp_ln_b=f(inputs["gmlp_ln_b"]),
        gmlp_w_sT=np.ascontiguousarray(f(inputs["gmlp_w_s"]).transpose(0, 3, 1, 2).reshape(2, 128, 1024)),
        gmlp_b_s=np.ascontiguousarray(f(inputs["gmlp_b_s"]).reshape(2, 1024)),
        **consts,
    )
    return shared


def kernel(**inputs):
    x = np.asarray(inputs["x"], dtype=np.float32)
    shared = prepare_inputs(inputs)
    if "nc" not in _CACHE:
        _CACHE["nc"] = build_program([0, 1, 2, 3])[0]
    nc = _CACHE["nc"]
    in_maps = []
    for b in range(8):
        m = dict(shared)
        m["xT"] = np.ascontiguousarray(x[b].T)
        in_maps.append(m)
    res = run_bass_kernel_spmd(nc, in_maps, core_ids=list(range(8)))
    out = np.stack([np.ascontiguousarray(res.results[b]["yT"].T) for b in range(8)], axis=0)
    return out.astype(np.float32)
```

```python
import math
from contextlib import ExitStack
import numpy as np
import concourse.bass as bass
import concourse.mybir as mybir
from concourse.bass_utils import run_bass_kernel_spmd

F32 = mybir.dt.float32
BF16 = mybir.dt.bfloat16
U8 = mybir.dt.uint8
AF = mybir.ActivationFunctionType
ALU = mybir.AluOpType

T = 2048
D = 1024
FF = 2816
NJ = 22
EPS = 1e-6
DEPTH = 4
NEXT = 4 * 640 + 4 * 768
DEBUG = {}


class Sched:
    ENGS = ("pe", "act", "dve", "pool", "sp")

    def __init__(self, nc):
        self.nc = nc
        self.ops = []
        self.pools = {}
        self.phase = 0

    def barrier(self, fn):
        self.ops.append(dict(eng="dve", fn=fn, r=(), w=(("PH",),), dma=None, late=None, ndma=1))
        self.phase += 1

    def op(self, eng, fn, r=(), w=(), dma=None, ndma=1):
        o = dict(eng=eng, fn=fn, r=tuple(r) + (("PH",),), w=tuple(w), dma=dma, late=None, ndma=ndma)
        self.ops.append(o)
        return o

    def consume(self, pool, nslots, eng, load_fn_of_slot, extra_r=(), ndma=1, lookahead=None):
        p = self.pools.setdefault((pool, self.phase), dict(n=0, nslots=nslots, marks=[]))
        i = p["n"]
        p["n"] += 1
        slot = i % nslots
        key = (pool, slot)
        mark = dict(eng=None, fn=None, r=(), w=(), dma=None, late=[])
        self.ops.append(mark)
        p["marks"].append(mark)
        dop = dict(eng=eng, fn=(lambda e, s=slot: load_fn_of_slot(e, s)), r=tuple(extra_r) + (("PH",),),
                   w=(key,), dma=("dma",) + key, late=None, ndma=ndma)
        la = (nslots - 1) if lookahead is None else lookahead
        tgt = p["marks"][max(0, i - la)]
        tgt["late"].append(dop)
        return slot, key

    def finalize(self):
        out = []
        for o in self.ops:
            if o["late"] is not None:
                out.extend(o["late"])
            else:
                out.append(o)
        self.ops = ops = out
        last_w = {}
        readers = {}
        for i, o in enumerate(ops):
            deps = {}
            for k in o["r"]:
                if k in last_w:
                    deps[last_w[k]] = True
            for k in o["w"]:
                if k in last_w:
                    deps.setdefault(last_w[k], False)
                for rd in readers.get(k, ()):
                    deps.setdefault(rd, False)
            deps.pop(i, None)
            o["deps"] = deps
            for k in o["r"]:
                readers.setdefault(k, []).append(i)
            for k in o["w"]:
                last_w[k] = i
                readers[k] = []
        for o in ops:
            o["signal"] = False
            o["need"] = []
        for o in ops:
            for d, raw in sorted(o["deps"].items()):
                p = ops[d]
                if p["dma"] is not None:
                    o["need"].append(d)
                elif p["eng"] == o["eng"]:
                    if o["eng"] == "pe":
                        continue
                    if raw or o["dma"] is not None:
                        p["signal"] = True
                        o["need"].append(d)
                else:
                    p["signal"] = True
                    o["need"].append(d)
        cnt = {e: 0 for e in self.ENGS}
        for o in ops:
            if o["dma"] is None and o["signal"]:
                cnt[o["eng"]] += 1
                o["sval"] = cnt[o["eng"]]
        self.stats = dict(cnt)
        self.dma_keys = sorted({o["dma"] for o in ops if o["dma"] is not None}, key=str)

    def emit(self):
        nc = self.nc
        ops = self.ops
        with ExitStack() as es:
            psem = {e: es.enter_context(nc.semaphore("p_" + e)) for e in self.ENGS}
            dsem = {k: es.enter_context(nc.semaphore("d_" + "_".join(str(x) for x in k[1:])))
                    for k in self.dma_keys}
            dtot = {k: 0 for k in self.dma_keys}
            for o in ops:
                if o["dma"] is not None:
                    dtot[o["dma"]] += 16 * o["ndma"]
                    o["dval"] = dtot[o["dma"]]
            block = es.enter_context(nc.Block())

            def run(ename, eng):
                waited = {}
                for o in ops:
                    if o["eng"] != ename:
                        continue
                    grp = {}
                    for d in o["need"]:
                        p = ops[d]
                        if p["dma"] is not None:
                            key, val, sem = ("d", p["dma"]), p["dval"], dsem[p["dma"]]
                        else:
                            key, val, sem = ("p", p["eng"]), p["sval"], psem[p["eng"]]
                        if key not in grp or grp[key][0] < val:
                            grp[key] = (val, sem)
                    for key, (val, sem) in grp.items():
                        if waited.get(key, 0) >= val:
                            continue
                        waited[key] = val
                        eng.wait_ge(sem, val)
                    if o["fn"] is None:
                        continue
                    ins = o["fn"](eng)
                    if o["dma"] is not None:
                        lst = ins if isinstance(ins, (list, tuple)) else [ins]
                        assert len(lst) == o["ndma"]
                        for x in lst:
                            x.then_inc(dsem[o["dma"]], 16)
                    elif o["signal"]:
                        ins.then_inc(psem[ename], 1)

            block.tensor(lambda e: run("pe", e))
            block.scalar(lambda e: run("act", e))
            block.vector(lambda e: run("dve", e))
            block.gpsimd(lambda e: run("pool", e))
            block.sync(lambda e: run("sp", e))


def _rope_tables():
    t = np.arange(T, dtype=np.float32)
    ca = np.ones((128, T), np.float32)
    sa = np.zeros((128, T), np.float32)
    half = 8
    inv = (np.float32(500000.0) ** (-(np.arange(half, dtype=np.float32) * 2.0 / 16))).astype(np.float32)
    ang = t[None, :] * inv[:, None]
    for hh in range(2):
        b = hh * 64
        ca[b:b + half] = np.cos(ang)
        ca[b + half:b + 2 * half] = np.cos(ang)
        sa[b:b + half] = -np.sin(ang)
        sa[b + half:b + 2 * half] = np.sin(ang)
    cr = np.zeros((128, T), np.float32)
    sr = np.zeros((128, T), np.float32)
    half = 32
    inv = (np.float32(10000.0) ** (-(np.arange(half, dtype=np.float32) * 2.0 / 64))).astype(np.float32)
    ang = t[None, :] * inv[:, None]
    for hh in range(2):
        b = hh * 64
        cr[b:b + half] = np.cos(ang)
        cr[b + half:b + 64] = np.cos(ang)
        sr[b:b + half] = -np.sin(ang)
        sr[b + half:b + 64] = np.sin(ang)
    return ca, sa, cr, sr


def _consts():
    idx = np.arange(128)
    k = idx[:, None]
    q = idx[None, :]
    cur = (k <= q).astype(np.float32)
    prev = (k >= q).astype(np.float32)
    m_n = np.concatenate([cur, prev], axis=1)
    m_f = np.concatenate([cur, np.zeros_like(prev)], axis=1)
    m_fnnn = np.concatenate([m_f, m_n, m_n, m_n], axis=1)
    ones = np.ones((128, 128), np.float32)
    blk = np.zeros((128, 128), np.float32)
    blk[:64, :64] = 1
    blk[64:, 64:] = 1
    ident = np.eye(128, dtype=np.float32)
    cb = np.concatenate([ones, blk, ident], axis=1)
    hb = np.concatenate([m_n, m_fnnn], axis=1)
    h = np.arange(8, dtype=np.float64)
    log_g = np.log(1.0 - np.exp2(-5.0 - h))
    diff = (q - k).astype(np.float64)
    dm = np.zeros((128, 8, 128), np.float64)
    for hh in range(8):
        dm[:, hh, :] = np.where(diff >= 0, np.exp(log_g[hh] * np.maximum(diff, 0)), 0.0) / 8.0
    xi = np.zeros((128, 4, 128), np.float64)
    zt = np.zeros((128, 4, 128), np.float64)
    cd = np.zeros((128, 8), np.float64)
    for p in range(128):
        for pr in range(4):
            hh = pr * 2 + p // 64
            xi[p, pr, :] = np.exp(log_g[hh] * (idx + 1.0))
            cd[p, pr] = np.exp(log_g[hh] * 128.0)
    for col in range(128):
        for pr in range(4):
            hh = pr * 2 + col // 64
            zt[:, pr, col] = np.exp(log_g[hh] * (127.0 - idx)) / 8.0
    hf = np.concatenate([dm.reshape(128, -1), xi.reshape(128, -1), zt.reshape(128, -1), cd],
                        axis=1).astype(np.float32)
    return cb.astype(np.float32), hb.astype(np.float32), hf


def _hyb_cols():
    cols = []
    def perm(base, half, rot):
        out = []
        for hh in range(2):
            for j in range(64):
                if j < half:
                    jj = j + half
                elif j < 2 * half:
                    jj = j - half
                else:
                    jj = j
                out.append(base + hh * 64 + jj)
        return out
    for p in range(4):
        qa = 0 + p * 128
        ka = 512 + p * 128
        va = 1024 + p * 128
        cols += list(range(qa, qa + 128)) + perm(qa, 8, 16)
        cols += list(range(ka, ka + 128)) + perm(ka, 8, 16)
        cols += list(range(va, va + 128))
    for p in range(4):
        qr = 1536 + p * 128
        kr = 2048 + p * 128
        vr = 2560 + p * 128
        gr = 3072 + p * 128
        cols += list(range(qr, qr + 128)) + perm(qr, 32, 64)
        cols += list(range(kr, kr + 128)) + perm(kr, 32, 64)
        cols += list(range(gr, gr + 128))
        cols += list(range(vr, vr + 128))
    assert len(cols) == NEXT
    return np.array(cols, dtype=np.int64)


def ssl(st, cnt, d=1):
    return slice(st, st + (cnt - 1) * d + 1, d)


def build_program(layers, stop_after=None):
    nc = bass.Bass("TRN2", target_bir_lowering=False)

    def din(name, shape):
        return nc.dram_tensor(name, list(shape), F32, kind="ExternalInput").ap()

    xT = din("xT", [D, T])
    yT = nc.dram_tensor("yT", [D, T], F32, kind="ExternalOutput").ap()
    g_all = din("g_all", [128, 192])
    wg_d = din("ffn_w_gate", [DEPTH, 2, D, FF])
    wu_d = din("ffn_w_up", [DEPTH, 2, D, FF])
    wd_d = din("ffn_w_down", [DEPTH, 2, FF, D])
    hin_d = din("hyb_w_in_ext", [2, D, NEXT])
    hout_d = din("hyb_w_out", [2, D, D])
    gin_d = din("gmlp_w_in", [2, D, 2 * D])
    gout_d = din("gmlp_w_out", [2, D, D])
    glng_d = din("gmlp_ln_g", [2, D])
    glnb_d = din("gmlp_ln_b", [2, D])
    gws_d = din("gmlp_w_sT", [2, 128, 8 * 128])
    gbs_d = din("gmlp_b_s", [2, 8 * 128])
    ropeA_c = din("ropeA_c", [128, T])
    ropeA_s = din("ropeA_s", [128, T])
    ropeR_c = din("ropeR_c", [128, T])
    ropeR_s = din("ropeR_s", [128, T])
    cb_d = din("cb", [128, 384])
    hb_d = din("hb", [128, 1280])
    hf_d = din("hf", [128, 2056])

    es = ExitStack()
    X = es.enter_context(nc.sbuf_tensor("X", [128, 8, T], F32))
    G = es.enter_context(nc.sbuf_tensor("G", [128, 192], F32))
    CB = es.enter_context(nc.sbuf_tensor("CB", [128, 384], BF16))
    MASKC = es.enter_context(nc.sbuf_tensor("MASKC", [128, 128], BF16))
    EPSB = es.enter_context(nc.sbuf_tensor("EPSB", [128, 4], F32))
    SCRB = 142 * 1024
    SCR = es.enter_context(nc.sbuf_tensor("SCR", [128, SCRB], U8))
    PSALL = es.enter_context(nc.psum_tensor("PSALL", [128, 4096], F32))
    ONES = CB[:, 0:128]
    BLK = CB[:, 128:256]
    IDENT = CB[:, 256:384]

    def PS(b, n=1):
        return PSALL[:, b * 512:(b + n) * 512]

    class Carver:
        def __init__(self, off=0):
            self.off = off

        def take(self, dtype, shape):
            n = 1
            for s_ in shape[1:]:
                n *= s_
            nb = n * (4 if dtype == F32 else 2)
            nb = (nb + 31) // 32 * 32
            ap = SCR[:, self.off:self.off + nb].bitcast(dtype)[:, 0:n]
            if len(shape) == 3:
                ap = ap.rearrange("p (a b) -> p a b", a=shape[1])
            elif len(shape) == 4:
                ap = ap.rearrange("p (a b c) -> p a b c", a=shape[1], b=shape[2])
            self.off += nb
            assert self.off <= SCRB, (self.off, SCRB)
            return ap

    S = Sched(nc)

    def barrier():
        S.barrier(lambda e: e.memset(EPSB[:, 2:3], 0.0))

    S.op("sp", lambda e: e.dma_start(out=G[:], in_=g_all), w=[("G",)], dma=("dma", "G"))
    S.op("pool", lambda e: e.dma_start(out=CB[:], in_=cb_d), w=[("CB",)], dma=("dma", "CB"))
    S.op("pool", lambda e: e.dma_start(out=MASKC[:], in_=hb_d[:, 0:128]), w=[("MASKC",)], dma=("dma", "MASKC"))
    S.op("dve", lambda e: e.memset(EPSB[:, 0:1], EPS), w=[("EPSB0",)])
    S.op("dve", lambda e: e.memset(EPSB[:, 1:2], math.log(0.5)), r=[("EPSB0",)], w=[("EPSB",)])
    xv = xT.rearrange("(c p) t -> p c t", p=128)
    yv = yT.rearrange("(c p) t -> p c t", p=128)
    for g in range(4):
        S.op("sp", lambda e, g=g: e.dma_start(out=X[:, :, g * 512:(g + 1) * 512],
                                              in_=xv[:, :, g * 512:(g + 1) * 512]),
             w=[("X", g)], dma=("dma", "X", g))

    def rstd_from_sq(sq_ap, nk, rs_ap, inv_n, ln_half, keys_r, key_rs, lhs=None, bank=6):
        lhs = ONES if lhs is None else lhs

        def mm(e):
            ins = None
            for k in range(nk):
                src = sq_ap[:, k, :] if nk > 1 else sq_ap
                ins = e.matmul(PS(bank), lhsT=lhs, rhs=src, start=(k == 0), stop=(k == nk - 1))
            return ins
        S.op("pe", mm, r=list(keys_r) + [("CB",)], w=[("ps", bank)])
        S.op("act", lambda e: e.activation(out=rs_ap, in_=PS(bank), func=AF.Ln, scale=inv_n, bias=EPSB[:, 0:1]),
             r=[("ps", bank), ("EPSB",)], w=[key_rs])
        if ln_half:
            S.op("act", lambda e: e.activation(out=rs_ap, in_=rs_ap, func=AF.Exp, scale=-0.5, bias=EPSB[:, 1:2]),
                 r=[key_rs, ("EPSB",)], w=[key_rs])
        else:
            S.op("act", lambda e: e.activation(out=rs_ap, in_=rs_ap, func=AF.Exp, scale=-0.5),
                 r=[key_rs], w=[key_rs])

    def prenorm(gi, g, dst_fn, dst_key, SQ, RS, stage="ab", ksq=("SQ",), krs=("RS",), bank=6):
        tok = slice(g * 512, (g + 1) * 512)
        if "a" in stage:
            S.op("act", lambda e: e.activation(out=SQ, in_=X[:, :, tok], func=AF.Square),
                 r=[("X", g)], w=[ksq])
        if "b" not in stage:
            return
        rstd_from_sq(SQ, 8, RS, 1.0 / D, False, [ksq], krs, bank=bank)
        for k in range(8):
            S.op("dve", lambda e, k=k: e.scalar_tensor_tensor(
                out=dst_fn(k), in0=X[:, k, tok], scalar=G[:, gi * 8 + k:gi * 8 + k + 1], in1=RS,
                op0=ALU.mult, op1=ALU.mult),
                r=[("X", g), krs, ("G",)], w=[dst_key(k)])

    def proj_postnorm(src_fn, src_keys, nk, w_view, gi, g, half_factor, FB, SQ, RS, WDS):
        tok = slice(g * 512, (g + 1) * 512)
        for c in range(8):
            slot, key = S.consume("WD", 2, "pool",
                                  lambda e, s, c=c: e.dma_start(out=WDS[:, s, 0:nk, :],
                                                                in_=w_view[:, :, c * 128:(c + 1) * 128]))
            b = 4 + c % 2

            def mm(e, slot=slot, b=b):
                ins = None
                for j in range(nk):
                    ins = e.matmul(PS(b), lhsT=WDS[:, slot, j, :], rhs=src_fn(j), start=(j == 0), stop=(j == nk - 1))
                return ins
            S.op("pe", mm, r=[key] + list(src_keys), w=[("ps", b)])
            S.op("act", lambda e, c=c, b=b: e.activation(out=FB[:, c, :], in_=PS(b), func=AF.Copy),
                 r=[("ps", b)], w=[("F", c)])
        S.op("act", lambda e: e.activation(out=SQ, in_=FB, func=AF.Square),
             r=[("F", c) for c in range(8)], w=[("SQ",)])
        rstd_from_sq(SQ, 8, RS, 1.0 / D, half_factor, [("SQ",)], ("RS",))
        for c in range(8):
            S.op("dve", lambda e, c=c: e.scalar_tensor_tensor(
                out=FB[:, c, :], in0=FB[:, c, :], scalar=G[:, gi * 8 + c:gi * 8 + c + 1], in1=RS,
                op0=ALU.mult, op1=ALU.mult), r=[("F", c), ("RS",), ("G",)], w=[("F", c)])
        S.op("pool", lambda e: e.tensor_tensor(out=X[:, :, tok], in0=X[:, :, tok], in1=FB, op=ALU.add),
             r=[("F", c) for c in range(8)] + [("X", g)], w=[("X", g)])

    def ffn_seq(items):
        cv = Carver()
        H = cv.take(BF16, [128, 8, 1024])
        ACTB = cv.take(BF16, [128, NJ, 1024])
        FB = cv.take(F32, [128, 8, 1024])
        SQ = cv.take(BF16, [128, 8, 512])
        RS = cv.take(F32, [128, 512])
        SQ2 = cv.take(BF16, [128, 8, 512])
        RS2 = cv.take(F32, [128, 512])
        SG = cv.take(F32, [128, 2, 512])
        WGU = cv.take(BF16, [128, 3, 16, 128])
        WDS = cv.take(BF16, [128, 2, NJ, 128])
        halves = [(l, i, hf) for (l, i) in items for hf in range(2)]

        def gidx(l, i):
            return l * 6 + (0 if i == 0 else 4)

        def pre(hv, stage, tts):
            l, i, hf = hv
            for tt in tts:
                prenorm(gidx(l, i), hf * 2 + tt, lambda k, tt=tt: H[:, k, tt * 512:(tt + 1) * 512],
                        lambda k, tt=tt: ("H", tt), SQ2, RS2, stage=stage, ksq=("SQ2",), krs=("RS2",), bank=7)

        def phase1(hv):
            l, i, hf = hv
            wgv = wg_d[l, i].rearrange("(k p) n -> p k n", p=128)
            wuv = wu_d[l, i].rearrange("(k p) n -> p k n", p=128)
            for j in range(NJ):
                def ld(e, s, j=j):
                    a_ = e.dma_start(out=WGU[:, s, 0:8, :], in_=wgv[:, :, j * 128:(j + 1) * 128])
                    b_ = e.dma_start(out=WGU[:, s, 8:16, :], in_=wuv[:, :, j * 128:(j + 1) * 128])
                    return [a_, b_]
                slot, key = S.consume("WGU", 3, "pool", ld, ndma=2)
                for tt in range(2):
                    bg, bu = tt, 2 + tt

                    def mm(e, slot=slot, tt=tt, bg=bg, bu=bu):
                        ins = None
                        for k in range(8):
                            ins = e.matmul(PS(bg), lhsT=WGU[:, slot, k, :], rhs=H[:, k, tt * 512:(tt + 1) * 512],
                                           start=(k == 0), stop=(k == 7))
                        for k in range(8):
                            ins = e.matmul(PS(bu), lhsT=WGU[:, slot, 8 + k, :], rhs=H[:, k, tt * 512:(tt + 1) * 512],
                                           start=(k == 0), stop=(k == 7))
                        return ins
                    S.op("pe", mm, r=[key, ("H", tt)], w=[("ps", bg), ("ps", bu)])
                    S.op("act", lambda e, tt=tt, bg=bg: e.activation(out=SG[:, tt, :], in_=PS(bg), func=AF.Silu),
                         r=[("ps", bg)], w=[("SG", tt)])
                    S.op("dve", lambda e, bu=bu, j=j, tt=tt: e.tensor_tensor(
                        out=ACTB[:, j, tt * 512:(tt + 1) * 512], in0=SG[:, tt, :], in1=PS(bu), op=ALU.mult),
                        r=[("SG", tt), ("ps", bu)], w=[("A", j, tt)])

        def phase2(hv, hook):
            l, i, hf = hv
            wdv = wd_d[l, i].rearrange("(j p) n -> p j n", p=128)
            gi = gidx(l, i) + 1
            for c in range(8):
                slot, key = S.consume("WD", 2, "pool",
                                      lambda e, s, c=c: e.dma_start(out=WDS[:, s, :, :],
                                                                    in_=wdv[:, :, c * 128:(c + 1) * 128]))
                for tt in range(2):
                    b = 4 + tt

                    def mm(e, slot=slot, b=b, tt=tt):
                        ins = None
                        for j in range(NJ):
                            ins = e.matmul(PS(b), lhsT=WDS[:, slot, j, :], rhs=ACTB[:, j, tt * 512:(tt + 1) * 512],
                                           start=(j == 0), stop=(j == NJ - 1))
                        return ins
                    S.op("pe", mm, r=[key] + [("A", j, tt) for j in range(NJ)], w=[("ps", b)])
                    S.op("act", lambda e, c=c, b=b, tt=tt: e.activation(out=FB[:, c, tt * 512:(tt + 1) * 512], in_=PS(b),
                                                                         func=AF.Copy),
                         r=[("ps", b)], w=[("F", c, tt)])
                if c == 0 and hook is not None:
                    hook()
            for tt in range(2):
                g = hf * 2 + tt
                tok = slice(g * 512, (g + 1) * 512)
                fsl = slice(tt * 512, (tt + 1) * 512)
                fk = [("F", c, tt) for c in range(8)]
                S.op("act", lambda e, fsl=fsl: e.activation(out=SQ, in_=FB[:, :, fsl], func=AF.Square),
                     r=fk, w=[("SQ",)])
                rstd_from_sq(SQ, 8, RS, 1.0 / D, True, [("SQ",)], ("RS",))
                for c in range(8):
                    S.op("dve", lambda e, c=c, fsl=fsl: e.scalar_tensor_tensor(
                        out=FB[:, c, fsl], in0=FB[:, c, fsl], scalar=G[:, gi * 8 + c:gi * 8 + c + 1], in1=RS,
                        op0=ALU.mult, op1=ALU.mult), r=[("F", c, tt), ("RS",), ("G",)], w=[("F", c, tt)])
                S.op("pool", lambda e, tok=tok, fsl=fsl: e.tensor_tensor(out=X[:, :, tok], in0=X[:, :, tok], in1=FB[:, :, fsl],
                                                                         op=ALU.add),
                     r=fk + [("X", g)], w=[("X", g)])

        pre(halves[0], "ab", (0, 1))
        for idx, hv in enumerate(halves):
            phase1(hv)
            nxt = halves[idx + 1] if idx + 1 < len(halves) else None
            if nxt is not None:
                pre(nxt, "a", (0,))

                def hook(nxt=nxt):
                    pre(nxt, "b", (0,))
                    pre(nxt, "ab", (1,))
                phase2(hv, hook)
            else:
                phase2(hv, None)

    def gmlp(l):
        jl = l // 2
        cv = Carver()
        H = cv.take(BF16, [128, 8, 512])
        UT = cv.take(BF16, [128, 8, 512])
        YT = cv.take(BF16, [128, 8, 512])
        VG = cv.take(F32, [128, 4, 1024])
        VN = cv.take(BF16, [128, 4, 1024])
        LNG = cv.take(F32, [128, 1024])
        LNB = cv.take(F32, [128, 1024])
        WST = cv.take(BF16, [128, 8, 128])
        BS = cv.take(F32, [128, 8, 128])
        WIN = cv.take(BF16, [128, 3, 8, 128])
        WV = cv.take(BF16, [128, 2, 8, 512])
        FB = cv.take(F32, [128, 8, 512])
        SQ = cv.take(BF16, [128, 8, 512])
        RS = cv.take(F32, [128, 512])
        Y1 = cv.take(F32, [128, 2, 512])
        ST = cv.take(F32, [128, 4, 16])
        MV = cv.take(F32, [128, 4, 2])
        RSD = cv.take(F32, [128, 4])
        WDS = cv.take(BF16, [128, 2, 8, 128])
        winv = gin_d[jl].rearrange("(k p) n -> p k n", p=128)
        woutv = gout_d[jl].rearrange("(j p) n -> p j n", p=128)
        gi = l * 6 + 2
        S.op("sp", lambda e: e.dma_start(out=LNG, in_=glng_d[jl:jl + 1, :].to_broadcast([128, D])),
             w=[("LNG",)], dma=("dma", "LNG"))
        S.op("sp", lambda e: e.dma_start(out=LNB, in_=glnb_d[jl:jl + 1, :].to_broadcast([128, D])),
             w=[("LNB",)], dma=("dma", "LNB"))
        S.op("sp", lambda e: e.dma_start(out=BS.rearrange("p a b -> p (a b)"),
                                         in_=gbs_d[jl:jl + 1, :].to_broadcast([128, D])),
             w=[("BS",)], dma=("dma", "BS"))
        S.op("pool", lambda e: e.dma_start(out=WST.rearrange("p a b -> p (a b)"), in_=gws_d[jl]),
             w=[("WST0",)], dma=("dma", "WST"))
        S.op("dve", lambda e: e.tensor_tensor(out=WST, in0=WST, in1=MASKC[:, None, :].to_broadcast([128, 8, 128]),
                                              op=ALU.mult),
             r=[("WST0",), ("MASKC",)], w=[("WST",)])
        GS = DEBUG.get("gstage", 9)
        for g in range(4):
            prenorm(gi, g, lambda k: H[:, k, :], lambda k: ("H",), SQ, RS)
            for ct in range(8 if GS >= 1 else 0):
                slot, key = S.consume("GWIN", 3, "pool",
                                      lambda e, s, ct=ct: e.dma_start(out=WIN[:, s], in_=winv[:, :, ct * 128:(ct + 1) * 128]))
                b = ct % 2

                def mm(e, slot=slot, b=b):
                    ins = None
                    for k in range(8):
                        ins = e.matmul(PS(b), lhsT=WIN[:, slot, k, :], rhs=H[:, k, :], start=(k == 0), stop=(k == 7))
                    return ins
                S.op("pe", mm, r=[key, ("H",)], w=[("ps", b)])
                S.op("act", lambda e, ct=ct, b=b: e.activation(out=UT[:, ct, :], in_=PS(b), func=AF.Gelu),
                     r=[("ps", b)], w=[("UT", ct)])
            for hv in range(2 if GS >= 2 else 0):
                slot, key = S.consume("GWV", 2, "pool",
                                      lambda e, s, hv=hv: e.dma_start(out=WV[:, s],
                                                                       in_=winv[:, :, D + hv * 512:D + (hv + 1) * 512]))
                for m in range(4):
                    b = 2 + (hv * 4 + m) % 2

                    def mm(e, slot=slot, b=b, m=m):
                        ins = None
                        for k in range(8):
                            ins = e.matmul(PS(b), lhsT=H[:, k, m * 128:(m + 1) * 128], rhs=WV[:, slot, k, :],
                                           start=(k == 0), stop=(k == 7))
                        return ins
                    S.op("pe", mm, r=[key, ("H",)], w=[("ps", b)])
                    S.op("act", lambda e, b=b, m=m, hv=hv: e.activation(
                        out=VG[:, m, hv * 512:(hv + 1) * 512], in_=PS(b), func=AF.Gelu),
                        r=[("ps", b)], w=[("VG", m, hv)])
                    S.op("dve", lambda e, m=m, hv=hv: e.bn_stats(out=ST[:, m, hv * 6:(hv + 1) * 6],
                                                                  in_=VG[:, m, hv * 512:(hv + 1) * 512]),
                         r=[("VG", m, hv)], w=[("STAT", m, hv)])
            if GS < 3:
                continue
            for m in range(4):
                S.op("dve", lambda e, m=m: e.bn_aggr(out=MV[:, m, :], in_=ST[:, m, 0:12]),
                     r=[("STAT", m, 0), ("STAT", m, 1)], w=[("MV", m)])
            S.op("act", lambda e: e.activation(out=RSD, in_=MV[:, :, 1], func=AF.Ln, bias=EPSB[:, 0:1]),
                 r=[("MV", m) for m in range(4)] + [("EPSB",)], w=[("RSD",)])
            S.op("act", lambda e: e.activation(out=RSD, in_=RSD, func=AF.Exp, scale=-0.5),
                 r=[("RSD",)], w=[("RSD",)])
            for m in range(4):
                vk = [("VG", m, 0), ("VG", m, 1)]
                S.op("dve", lambda e, m=m: e.tensor_scalar(out=VG[:, m, :], in0=VG[:, m, :], scalar1=MV[:, m, 0:1],
                                                           scalar2=RSD[:, m:m + 1], op0=ALU.subtract, op1=ALU.mult),
                     r=vk + [("MV", m), ("RSD",)], w=vk)
                S.op("dve", lambda e, m=m: e.tensor_tensor(out=VG[:, m, :], in0=VG[:, m, :], in1=LNG, op=ALU.mult),
                     r=vk + [("LNG",)], w=vk)
                S.op("pool", lambda e, m=m: e.tensor_tensor(out=VN[:, m, :], in0=VG[:, m, :], in1=LNB, op=ALU.add),
                     r=vk + [("LNB",)], w=[("VN", m)])
            if GS < 4:
                continue
            for gg in range(8):
                b = 4 + gg % 2

                def mm(e, gg=gg, b=b):
                    ins = None
                    for m in range(4):
                        ins = e.matmul(PS(b)[:, m * 128:(m + 1) * 128], lhsT=VN[:, m, gg * 128:(gg + 1) * 128],
                                       rhs=WST[:, gg, :], start=True, stop=True)
                    return ins
                S.op("pe", mm, r=[("VN", m) for m in range(4)] + [("WST",)], w=[("ps", b)])
                u = gg % 2
                S.op("dve", lambda e, gg=gg, b=b, u=u: e.tensor_tensor(
                    out=Y1[:, u, :].rearrange("p (m i) -> p m i", m=4),
                    in0=PS(b).rearrange("p (m i) -> p m i", m=4),
                    in1=BS[:, gg:gg + 1, :].to_broadcast([128, 4, 128]), op=ALU.add),
                    r=[("ps", b), ("BS",)], w=[("Y1", u)])
                S.op("pool", lambda e, gg=gg, u=u: e.tensor_tensor(out=YT[:, gg, :], in0=Y1[:, u, :], in1=UT[:, gg, :],
                                                                   op=ALU.mult),
                     r=[("Y1", u), ("UT", gg)], w=[("YT", gg)])
            if GS < 5:
                continue
            proj_postnorm(lambda j: YT[:, j, :], [("YT", j) for j in range(8)], 8, woutv, gi + 1, g, False,
                          FB, SQ, RS, WDS)

    def hybrid(l):
        jl = l // 2
        cv = Carver()
        HT = cv.take(BF16, [128, 8, T])
        MIX = cv.take(BF16, [128, 8, T])
        WIN = cv.take(BF16, [128, 6, 8, 128])
        ROPE = cv.take(F32, [128, 2, 2, 512])
        TMP = cv.take(F32, [128, 2, 512])
        RS = cv.take(F32, [128, 512])
        pair_off = cv.off
        winv = hin_d[jl].rearrange("(k p) n -> p k n", p=128)
        woutv = hout_d[jl].rearrange("(j p) n -> p j n", p=128)
        gi = l * 6 + 2
        SQ0 = Carver(pair_off).take(BF16, [128, 8, 512])
        for g in range(4):
            prenorm(gi, g, lambda k, g=g: HT[:, k, g * 512:(g + 1) * 512], lambda k, g=g: ("HT", g), SQ0, RS)
        barrier()
        HTK = [("HT", g) for g in range(4)]

        def win_block(col0):
            return S.consume("HWIN", 6, "pool",
                             lambda e, s, col0=col0: e.dma_start(out=WIN[:, s], in_=winv[:, :, col0:col0 + 128]),
                             lookahead=2)

        def rope_proj(col0, rc, rs_, dsts, fam):
            blks = [win_block(col0 + i * 128) for i in range(4)]
            for g in range(4):
                tok = slice(g * 512, (g + 1) * 512)

                def ldrope(e, s, g=g):
                    a = e.dma_start(out=ROPE[:, s, 0, :], in_=rc[:, g * 512:(g + 1) * 512])
                    b = e.dma_start(out=ROPE[:, s, 1, :], in_=rs_[:, g * 512:(g + 1) * 512])
                    return [a, b]
                rslot, rkey = S.consume("ROPE", 2, "sp", ldrope, ndma=2)
                for qi in range(2):
                    u = (g * 2 + qi) % 2
                    b0, b1 = 2 * u, 2 * u + 1
                    (s0, k0), (s1, k1) = blks[2 * qi], blks[2 * qi + 1]

                    def mm(e, s0=s0, s1=s1, b0=b0, b1=b1, tok=tok):
                        ins = None
                        for k in range(8):
                            ins = e.matmul(PS(b0), lhsT=WIN[:, s0, k, :], rhs=HT[:, k, tok], start=(k == 0), stop=(k == 7))
                        for k in range(8):
                            ins = e.matmul(PS(b1), lhsT=WIN[:, s1, k, :], rhs=HT[:, k, tok], start=(k == 0), stop=(k == 7))
                        return ins
                    S.op("pe", mm, r=[k0, k1, ("HT", g)], w=[("ps", b0), ("ps", b1)])
                    S.op("dve", lambda e, b0=b0, rslot=rslot: e.tensor_tensor(
                        out=TMP[:, 0, :], in0=PS(b0), in1=ROPE[:, rslot, 0, :], op=ALU.mult),
                        r=[("ps", b0), rkey], w=[("TMP", 0)])
                    S.op("dve", lambda e, b1=b1, rslot=rslot: e.tensor_tensor(
                        out=TMP[:, 1, :], in0=PS(b1), in1=ROPE[:, rslot, 1, :], op=ALU.mult),
                        r=[("ps", b1), rkey], w=[("TMP", 1)])
                    dst = dsts[qi]
                    S.op("pool", lambda e, dst=dst, tok=tok: e.tensor_tensor(
                        out=dst[:, tok], in0=TMP[:, 0, :], in1=TMP[:, 1, :], op=ALU.add),
                        r=[("TMP", 0), ("TMP", 1)], w=[("QK", fam, qi, g)])

        def dswa_pair(p):
            cvp = Carver(pair_off)
            HB = cvp.take(BF16, [128, 1280])
            QT = cvp.take(BF16, [128, T])
            KT = cvp.take(BF16, [128, T])
            VA = cvp.take(BF16, [128, 3, 16, 192])
            ACC = cvp.take(F32, [128, 2, T])
            E = cvp.take(BF16, [128, 2, 1024])
            M_N = HB[:, 0:256]
            M_F = HB[:, 256:1280]
            RC = TMP
            col0 = p * 640
            if p == 0:
                S.op("pool", lambda e: e.dma_start(out=HB, in_=hb_d), w=[("HB",)], dma=("dma", "HB"))
                S.op("pool", lambda e: e.memset(VA[:, :, :, 64:128], 1.0), w=[("VA1",)])
            rope_proj(col0, ropeA_c, ropeA_s, [QT, KT], "a")
            vs, vk = win_block(col0 + 512)
            for bi, d in enumerate((1, 4, 16)):
                tpc = 16 // d
                for i4 in range(4):
                    b = 4 + (bi * 4 + i4) % 2

                    def mm(e, b=b, d=d, i4=i4, tpc=tpc):
                        ins = None
                        for ii in range(4):
                            i = i4 * 4 + ii
                            r, blk = i // tpc, i % tpc
                            st = blk * 128 * d + r
                            for k in range(8):
                                ins = e.matmul(PS(b)[:, ii * 128:(ii + 1) * 128],
                                               lhsT=HT[:, k, ssl(st, 128, d)], rhs=WIN[:, vs, k, :],
                                               start=(k == 0), stop=(k == 7))
                        return ins
                    S.op("pe", mm, r=[vk] + HTK, w=[("ps", b)])
                    S.op("act", lambda e, b=b, bi=bi, i4=i4: e.activation(
                        out=VA[:, bi, i4 * 4:(i4 + 1) * 4, :].rearrange("p i (s c) -> p i s c", s=3)[:, :, 0:3:2, :],
                        in_=PS(b).rearrange("p (i s c) -> p i s c", i=4, s=2), func=AF.Copy),
                        r=[("ps", b), ("VA1",)], w=[("VA", bi, i4)])
            QKK = [("QK", "a", qi, g) for qi in range(2) for g in range(4)]
            unit = 0
            for hh in range(2):
                hs = slice(hh * 64, hh * 64 + 64)
                vcols = slice(hh * 64, hh * 64 + 128)
                for bi, d in enumerate((1, 4, 16)):
                    tpc = 16 // d
                    for i4 in range(4):
                        su = unit % 2
                        unit += 1
                        sb0 = 2 * su
                        ob = 4 + su
                        blocks = []
                        for ii in range(4):
                            i = i4 * 4 + ii
                            r, n = i // tpc, i % tpc
                            blocks.append((i, n, n * 128 * d + r, (n - 1) * 128 * d + r))
                        noprev = (d == 16)
                        W = 128 if noprev else 256

                        def mm_s(e, blocks=blocks, sb0=sb0, d=d, hs=hs, noprev=noprev, W=W):
                            ins = None
                            for ii, (i, n, st, pst) in enumerate(blocks):
                                qsl = ssl(st, 128, d)
                                ins = e.matmul(PS(sb0, 2)[:, ii * W:ii * W + 128], lhsT=KT[hs, qsl], rhs=QT[hs, qsl],
                                               start=True, stop=True)
                                if not noprev:
                                    ksl = qsl if n == 0 else ssl(pst, 128, d)
                                    ins = e.matmul(PS(sb0, 2)[:, ii * W + 128:ii * W + 256], lhsT=KT[hs, ksl],
                                                   rhs=QT[hs, qsl], start=True, stop=True)
                            return ins
                        S.op("pe", mm_s, r=QKK, w=[("ps", sb0), ("ps", sb0 + 1)])
                        S.op("act", lambda e, sb0=sb0, su=su, W=W: e.activation(
                            out=E[:, su, 0:4 * W], in_=PS(sb0, 2)[:, 0:4 * W], func=AF.Exp, scale=0.125),
                            r=[("ps", sb0), ("ps", sb0 + 1)], w=[("E", su)])
                        if noprev:
                            msk = M_N[:, None, 0:128].to_broadcast([128, 4, 128])
                            ev = E[:, su, 0:512].rearrange("p (a b) -> p a b", a=4)
                        elif blocks[0][1] == 0:
                            msk = M_F
                            ev = E[:, su, :]
                        else:
                            msk = M_N[:, None, :].to_broadcast([128, 4, 256])
                            ev = E[:, su, :].rearrange("p (a b) -> p a b", a=4)
                        S.op("dve", lambda e, ev=ev, msk=msk: e.tensor_tensor(out=ev, in0=ev, in1=msk, op=ALU.mult),
                             r=[("E", su), ("HB",)], w=[("E", su)])

                        def mm_o(e, blocks=blocks, ob=ob, su=su, bi=bi, vcols=vcols, noprev=noprev, W=W):
                            ins = None
                            for ii, (i, n, st, pst) in enumerate(blocks):
                                hasprev = (not noprev) and n > 0
                                ins = e.matmul(PS(ob)[:, ii * 128:(ii + 1) * 128], lhsT=VA[:, bi, i, vcols],
                                               rhs=E[:, su, ii * W:ii * W + 128], start=True, stop=not hasprev)
                                if hasprev:
                                    ins = e.matmul(PS(ob)[:, ii * 128:(ii + 1) * 128], lhsT=VA[:, bi, i - 1, vcols],
                                                   rhs=E[:, su, ii * W + 128:ii * W + 256], start=False, stop=True)
                            return ins
                        S.op("pe", mm_o, r=[("E", su)] + [("VA", bi, x) for x in range(4)], w=[("ps", ob)])
                        if d == 16:
                            r0 = blocks[0][0]
                            dst = ACC[:, hh, :].rearrange("p (m r) -> p r m", r=16)[:, r0:r0 + 4, :]
                            src = PS(ob).rearrange("p (a m) -> p a m", a=4)
                        else:
                            dst = ACC[:, hh, ssl(blocks[0][2], 512, d)]
                            src = PS(ob)
                        if bi == 0:
                            S.op("dve", lambda e, dst=dst, src=src: e.tensor_copy(out=dst, in_=src),
                                 r=[("ps", ob)], w=[("ACC", hh)])
                        else:
                            S.op("dve", lambda e, dst=dst, src=src: e.tensor_tensor(out=dst, in0=dst, in1=src, op=ALU.add),
                                 r=[("ps", ob), ("ACC", hh)], w=[("ACC", hh)])
                orow = slice(hh * 64, hh * 64 + 64)
                drow = slice(64 - hh * 64, 128 - hh * 64)
                for g in range(4):
                    tok = slice(g * 512, (g + 1) * 512)
                    u = g % 2
                    S.op("dve", lambda e, hh=hh, tok=tok, u=u, orow=orow, drow=drow: e.reciprocal(
                        out=RC[orow, u, :], in_=ACC[drow, hh, tok]), r=[("ACC", hh)], w=[("TMP", u)])
                    S.op("dve", lambda e, hh=hh, tok=tok, u=u, orow=orow: e.tensor_tensor(
                        out=MIX[orow, p, tok], in0=ACC[orow, hh, tok], in1=RC[orow, u, :], op=ALU.mult),
                        r=[("TMP", u), ("ACC", hh)], w=[("MIX", p, hh, g)])

        def ret_pair(p):
            cvp = Carver(pair_off)
            HF = cvp.take(F32, [128, 2056])
            QT = cvp.take(BF16, [128, T])
            KT = cvp.take(BF16, [128, T])
            QX = cvp.take(BF16, [128, T])
            GT = cvp.take(BF16, [128, T])
            VT = cvp.take(BF16, [128, 16, 128])
            KZ = cvp.take(BF16, [128, 16, 128])
            R32 = cvp.take(F32, [128, T])
            ST32 = cvp.take(F32, [128, 64])
            STB = cvp.take(BF16, [128, 2, 64])
            SD = cvp.take(BF16, [128, 2, 256])
            SQH = cvp.take(BF16, [128, 512])
            DM = HF[:, 0:1024].rearrange("p (h c) -> p h c", h=8)
            XI = HF[:, 1024:1536].rearrange("p (a c) -> p a c", a=4)
            ZT = HF[:, 1536:2048].rearrange("p (a c) -> p a c", a=4)
            CD = HF[:, 2048:2056]
            col0 = 4 * 640 + p * 768
            if p == 0:
                S.op("sp", lambda e: e.dma_start(out=HF, in_=hf_d), w=[("HF",)], dma=("dma", "HF"))
            rope_proj(col0, ropeR_c, ropeR_s, [QT, KT], "r")
            for g in range(4):
                tok = slice(g * 512, (g + 1) * 512)
                S.op("pool", lambda e, tok=tok: e.tensor_tensor(
                    out=QX[:, tok].rearrange("p (a c) -> p a c", a=4), in0=QT[:, tok].rearrange("p (a c) -> p a c", a=4),
                    in1=XI[:, p:p + 1, :].to_broadcast([128, 4, 128]), op=ALU.mult),
                    r=[("QK", "r", 0, g), ("HF",)], w=[("QX", g)])
            gs, gk = win_block(col0 + 512)
            for g in range(4):
                tok = slice(g * 512, (g + 1) * 512)
                b = g % 2

                def mm(e, b=b, tok=tok):
                    ins = None
                    for k in range(8):
                        ins = e.matmul(PS(b), lhsT=WIN[:, gs, k, :], rhs=HT[:, k, tok], start=(k == 0), stop=(k == 7))
                    return ins
                S.op("pe", mm, r=[gk, ("HT", g)], w=[("ps", b)])
                S.op("act", lambda e, b=b, tok=tok: e.activation(out=GT[:, tok], in_=PS(b), func=AF.Silu),
                     r=[("ps", b)], w=[("GT", g)])
            vs, vk = win_block(col0 + 640)
            for i4 in range(4):
                b = 2 + i4 % 2

                def mm(e, b=b, i4=i4):
                    ins = None
                    for ii in range(4):
                        i = i4 * 4 + ii
                        for k in range(8):
                            ins = e.matmul(PS(b)[:, ii * 128:(ii + 1) * 128], lhsT=HT[:, k, i * 128:(i + 1) * 128],
                                           rhs=WIN[:, vs, k, :], start=(k == 0), stop=(k == 7))
                    return ins
                S.op("pe", mm, r=[vk] + HTK, w=[("ps", b)])
                S.op("act", lambda e, b=b, i4=i4: e.activation(
                    out=VT[:, i4 * 4:(i4 + 1) * 4, :], in_=PS(b).rearrange("p (i c) -> p i c", i=4), func=AF.Copy),
                    r=[("ps", b)], w=[("VT", i4)])
            PSB7 = PS(7).bitcast(BF16)
            for i4 in range(DEBUG.get("ntr", 4)):
                def tr(e, i4=i4):
                    ins = None
                    for ii in range(4):
                        i = i4 * 4 + ii
                        ins = e.transpose(PSB7[:, ii * 128:(ii + 1) * 128], KT[:, i * 128:(i + 1) * 128], IDENT)
                    return ins
                S.op("pe", tr, r=[("QK", "r", 1, i4), ("CB",)], w=[("ps", 7)])
                S.op("dve", lambda e, i4=i4: e.tensor_tensor(
                    out=KZ[:, i4 * 4:(i4 + 1) * 4, :], in0=PSB7[:, 0:512].rearrange("p (i c) -> p i c", i=4),
                    in1=ZT[:, p:p + 1, :].to_broadcast([128, 4, 128]), op=ALU.mult),
                    r=[("ps", 7), ("HF",)], w=[("KZ", i4)])
            S.op("dve", lambda e: e.memset(ST32, 0.0), w=[("ST32",)])
            for n in range(DEBUG.get("nrec", 16)):
                g = n // 4
                ck = slice(n * 128, (n + 1) * 128)
                sb = n % 2
                b0 = 2 * sb
                kvb = 6 + n % 2

                def mm_s(e, ck=ck, b0=b0):
                    e.matmul(PS(b0)[:, 0:128], lhsT=KT[0:64, ck], rhs=QT[0:64, ck], start=True, stop=True)
                    return e.matmul(PS(b0 + 1)[:, 0:128], lhsT=KT[64:128, ck], rhs=QT[64:128, ck], start=True, stop=True)
                S.op("pe", mm_s, r=[("QK", "r", 0, g), ("QK", "r", 1, g)], w=[("ps", b0), ("ps", b0 + 1)])
                S.op("dve", lambda e, sb=sb, b0=b0: e.tensor_tensor(
                    out=SD[:, sb, :].rearrange("p (h c) -> p h c", h=2),
                    in0=PS(b0, 2).rearrange("p (h c) -> p h c", h=2)[:, :, 0:128], in1=DM[:, 2 * p:2 * p + 2, :],
                    op=ALU.mult),
                    r=[("ps", b0), ("ps", b0 + 1), ("HF",)], w=[("SD", sb)])

                def mm_kv(e, n=n, kvb=kvb):
                    e.matmul(PS(kvb)[0:64, 0:64], lhsT=KZ[:, n, 0:64], rhs=VT[:, n, 0:64], start=True, stop=True)
                    return e.matmul(PS(kvb)[64:128, 0:64], lhsT=KZ[:, n, 64:128], rhs=VT[:, n, 64:128],
                                    start=True, stop=True)
                if DEBUG.get("kv", 1):
                    S.op("pe", mm_kv, r=[("KZ", n // 4), ("VT", n // 4)], w=[("ps", kvb)])

                def mm_o(e, n=n, sb=sb, ck=ck):
                    oc = slice((n % 4) * 128, (n % 4 + 1) * 128)
                    st = n % 2
                    ins = None
                    for hh in range(2):
                        hs = slice(hh * 64, hh * 64 + 64)
                        ins = e.matmul(PS(4 + hh)[hs, oc], lhsT=VT[:, n, hs], rhs=SD[:, sb, hh * 128:(hh + 1) * 128],
                                       start=True, stop=(n == 0))
                        if n > 0:
                            ins = e.matmul(PS(4 + hh)[hs, oc], lhsT=STB[hs, st, :], rhs=QX[hs, ck], start=False, stop=True)
                    return ins
                rk = [("SD", sb), ("VT", n // 4), ("QX", g)] + ([("STB", n % 2)] if n > 0 else [])
                if DEBUG.get("mo", 1):
                    S.op("pe", mm_o, r=rk, w=[("ps", 4), ("ps", 5)])
                if n < 15 and DEBUG.get("su", 1):
                    S.op("dve", lambda e, kvb=kvb: e.scalar_tensor_tensor(
                        out=ST32, in0=ST32, scalar=CD[:, p:p + 1], in1=PS(kvb)[:, 0:64], op0=ALU.mult, op1=ALU.add),
                        r=[("ps", kvb), ("ST32",), ("HF",)], w=[("ST32",)])
                    S.op("act", lambda e, n=n: e.activation(out=STB[:, (n + 1) % 2, :], in_=ST32, func=AF.Copy),
                         r=[("ST32",)], w=[("STB", (n + 1) % 2)])
                if n % 4 == 3:
                    tok = slice(g * 512, (g + 1) * 512)
                    S.op("act", lambda e, tok=tok: e.activation(out=R32[0:64, tok], in_=PS(4)[0:64, :], func=AF.Copy),
                         r=[("ps", 4)], w=[("R32a", g)])
                    S.op("act", lambda e, tok=tok: e.activation(out=R32[64:128, tok], in_=PS(5)[64:128, :], func=AF.Copy),
                         r=[("ps", 5)], w=[("R32", g)])
            for g in range(4):
                tok = slice(g * 512, (g + 1) * 512)
                S.op("act", lambda e, tok=tok: e.activation(out=SQH, in_=R32[:, tok], func=AF.Square),
                     r=[("R32", g), ("R32a", g)], w=[("SQH",)])
                rstd_from_sq(SQH, 1, RS, 1.0 / 64, False, [("SQH",)], ("RS",), lhs=BLK)
                S.op("dve", lambda e, tok=tok: e.tensor_tensor(out=TMP[:, 0, :], in0=R32[:, tok], in1=RS, op=ALU.mult),
                     r=[("R32", g), ("R32a", g), ("RS",)], w=[("TMP", 0)])
                S.op("pool", lambda e, tok=tok: e.tensor_tensor(out=MIX[:, 4 + p, tok], in0=TMP[:, 0, :], in1=GT[:, tok],
                                                                op=ALU.mult),
                     r=[("TMP", 0), ("GT", g)], w=[("MIX", 4 + p, 0, g), ("MIX", 4 + p, 1, g)])

        for p in range(DEBUG.get("ndswa", 4)):
            dswa_pair(p)
        barrier()
        for p in range(DEBUG.get("nret", 4)):
            ret_pair(p)
        barrier()
        if DEBUG.get("dump_mix"):
            for g in range(4):
                S.op("act", lambda e, g=g: e.activation(out=X[:, :, g * 512:(g + 1) * 512], in_=MIX[:, :, g * 512:(g + 1) * 512],
                                                        func=AF.Copy),
                     r=[("MIX", j, hh, g) for j in range(8) for hh in range(2)] + [("X", g)], w=[("X", g)])
            return
        cvo = Carver(pair_off)
        FB = cvo.take(F32, [128, 8, 512])
        SQ = cvo.take(BF16, [128, 8, 512])
        WDS = cvo.take(BF16, [128, 2, 8, 128])
        for g in range(4):
            tok = slice(g * 512, (g + 1) * 512)
            proj_postnorm(lambda j, tok=tok: MIX[:, j, tok],
                          [("MIX", j, hh, g) for j in range(8) for hh in range(2)], 8, woutv, gi + 1, g, False,
                          FB, SQ, RS, WDS)

    phases = []
    for l in layers:
        phases += [("ffn", l, 0), ("mix", l), ("ffn", l, 1)]
    if stop_after is not None:
        phases = phases[:stop_after]
    idx = 0
    while idx < len(phases):
        ph = phases[idx]
        if ph[0] == "ffn":
            items = [(ph[1], ph[2])]
            while idx + 1 < len(phases) and phases[idx + 1][0] == "ffn":
                idx += 1
                items.append((phases[idx][1], phases[idx][2]))
            ffn_seq(items)
        else:
            l = ph[1]
            if DEBUG.get("skipmix"):
                pass
            elif l % 2 == 0:
                hybrid(l)
            else:
                gmlp(l)
        barrier()
        idx += 1

    for g in range(4):
        S.op("sp", lambda e, g=g: e.dma_start(out=yv[:, :, g * 512:(g + 1) * 512], in_=X[:, :, g * 512:(g + 1) * 512]),
             r=[("X", g)], w=[("Y", g)], dma=("dma", "Y", g))
    S.op("sp", None, r=[("Y", g) for g in range(4)])
    S.finalize()
    S.emit()
    es.close()
    return nc, S


_CACHE = {}


def _host_consts():
    if "c" not in _CACHE:
        ca, sa, cr, sr = _rope_tables()
        cb, hb, hf = _consts()
        _CACHE["c"] = dict(ropeA_c=ca, ropeA_s=sa, ropeR_c=cr, ropeR_s=sr, cb=cb, hb=hb, hf=hf)
        _CACHE["cols"] = _hyb_cols()
    return _CACHE["c"], _CACHE["cols"]


def prepare_inputs(inputs):
    consts, cols = _host_consts()
    f = lambda a: np.ascontiguousarray(np.asarray(a, dtype=np.float32))
    ng = f(inputs["norm_g"])
    g_all = np.ascontiguousarray(ng.reshape(4, 6, 8, 128).transpose(3, 0, 1, 2).reshape(128, 192))
    shared = dict(
        g_all=g_all,
        ffn_w_gate=f(inputs["ffn_w_gate"]), ffn_w_up=f(inputs["ffn_w_up"]), ffn_w_down=f(inputs["ffn_w_down"]),
        hyb_w_in_ext=np.ascontiguousarray(f(inputs["hyb_w_in"])[:, :, cols]),
        hyb_w_out=f(inputs["hyb_w_out"]),
        gmlp_w_in=f(inputs["gmlp_w_in"]), gmlp_w_out=f(inputs["gmlp_w_out"]),
        gmlp_ln_g=f(inputs["gmlp_ln_g"]), gmlp_ln_b=f(inputs["gmlp_ln_b"]),
        gmlp_w_sT=np.ascontiguousarray(f(inputs["gmlp_w_s"]).transpose(0, 3, 1, 2).reshape(2, 128, 1024)),
        gmlp_b_s=np.ascontiguousarray(f(inputs["gmlp_b_s"]).reshape(2, 1024)),
        **consts,
    )
    return shared


def kernel(**inputs):
    x = np.asarray(inputs["x"], dtype=np.float32)
    shared = prepare_inputs(inputs)
    if "nc" not in _CACHE:
        _CACHE["nc"] = build_program([0, 1, 2, 3])[0]
    nc = _CACHE["nc"]
    in_maps = []
    for b in range(8):
        m = dict(shared)
        m["xT"] = np.ascontiguousarray(x[b].T)
        in_maps.append(m)
    res = run_bass_kernel_spmd(nc, in_maps, core_ids=list(range(8)))
    out = np.stack([np.ascontiguousarray(res.results[b]["yT"].T) for b in range(8)], axis=0)
    return out.astype(np.float32)
```

```python
import math
from contextlib import ExitStack
import numpy as np
import concourse.bass as bass
import concourse.mybir as mybir
from concourse.bass_utils import run_bass_kernel_spmd

F32 = mybir.dt.float32
BF16 = mybir.dt.bfloat16
U8 = mybir.dt.uint8
AF = mybir.ActivationFunctionType
ALU = mybir.AluOpType

T = 2048
D = 1024
FF = 2816
NJ = 22
EPS = 1e-6
DEPTH = 4
NEXT = 4 * 640 + 4 * 768
DEBUG = {}


class Sched:
    ENGS = ("pe", "act", "dve", "pool", "sp")

    def __init__(self, nc):
        self.nc = nc
        self.ops = []
        self.pools = {}
        self.phase = 0

    def barrier(self, fn):
        self.ops.append(dict(eng="dve", fn=fn, r=(), w=(("PH",),), dma=None, late=None, ndma=1))
        self.phase += 1

    def op(self, eng, fn, r=(), w=(), dma=None, ndma=1):
        o = dict(eng=eng, fn=fn, r=tuple(r) + (("PH",),), w=tuple(w), dma=dma, late=None, ndma=ndma)
        self.ops.append(o)
        return o

    def consume(self, pool, nslots, eng, load_fn_of_slot, extra_r=(), ndma=1, lookahead=None):
        p = self.pools.setdefault((pool, self.phase), dict(n=0, nslots=nslots, marks=[]))
        i = p["n"]
        p["n"] += 1
        slot = i % nslots
        key = (pool, slot)
        mark = dict(eng=None, fn=None, r=(), w=(), dma=None, late=[])
        self.ops.append(mark)
        p["marks"].append(mark)
        dop = dict(eng=eng, fn=(lambda e, s=slot: load_fn_of_slot(e, s)), r=tuple(extra_r) + (("PH",),),
                   w=(key,), dma=("dma",) + key, late=None, ndma=ndma)
        la = (nslots - 1) if lookahead is None else lookahead
        tgt = p["marks"][max(0, i - la)]
        tgt["late"].append(dop)
        return slot, key

    def finalize(self):
        out = []
        for o in self.ops:
            if o["late"] is not None:
                out.extend(o["late"])
            else:
                out.append(o)
        self.ops = ops = out
        last_w = {}
        readers = {}
        for i, o in enumerate(ops):
            deps = {}
            for k in o["r"]:
                if k in last_w:
                    deps[last_w[k]] = True
            for k in o["w"]:
                if k in last_w:
                    deps.setdefault(last_w[k], False)
                for rd in readers.get(k, ()):
                    deps.setdefault(rd, False)
            deps.pop(i, None)
            o["deps"] = deps
            for k in o["r"]:
                readers.setdefault(k, []).append(i)
            for k in o["w"]:
                last_w[k] = i
                readers[k] = []
        for o in ops:
            o["signal"] = False
            o["need"] = []
        for o in ops:
            for d, raw in sorted(o["deps"].items()):
                p = ops[d]
                if p["dma"] is not None:
                    o["need"].append(d)
                elif p["eng"] == o["eng"]:
                    if o["eng"] == "pe":
                        continue
                    if raw or o["dma"] is not None:
                        p["signal"] = True
                        o["need"].append(d)
                else:
                    p["signal"] = True
                    o["need"].append(d)
        cnt = {e: 0 for e in self.ENGS}
        for o in ops:
            if o["dma"] is None and o["signal"]:
                cnt[o["eng"]] += 1
                o["sval"] = cnt[o["eng"]]
        self.stats = dict(cnt)
        self.dma_keys = sorted({o["dma"] for o in ops if o["dma"] is not None}, key=str)

    def emit(self):
        nc = self.nc
        ops = self.ops
        with ExitStack() as es:
            psem = {e: es.enter_context(nc.semaphore("p_" + e)) for e in self.ENGS}
            dsem = {k: es.enter_context(nc.semaphore("d_" + "_".join(str(x) for x in k[1:])))
                    for k in self.dma_keys}
            dtot = {k: 0 for k in self.dma_keys}
            for o in ops:
                if o["dma"] is not None:
                    dtot[o["dma"]] += 16 * o["ndma"]
                    o["dval"] = dtot[o["dma"]]
            block = es.enter_context(nc.Block())

            def run(ename, eng):
                waited = {}
                for o in ops:
                    if o["eng"] != ename:
                        continue
                    grp = {}
                    for d in o["need"]:
                        p = ops[d]
                        if p["dma"] is not None:
                            key, val, sem = ("d", p["dma"]), p["dval"], dsem[p["dma"]]
                        else:
                            key, val, sem = ("p", p["eng"]), p["sval"], psem[p["eng"]]
                        if key not in grp or grp[key][0] < val:
                            grp[key] = (val, sem)
                    for key, (val, sem) in grp.items():
                        if waited.get(key, 0) >= val:
                            continue
                        waited[key] = val
                        eng.wait_ge(sem, val)
                    if o["fn"] is None:
                        continue
                    ins = o["fn"](eng)
                    if o["dma"] is not None:
                        lst = ins if isinstance(ins, (list, tuple)) else [ins]
                        assert len(lst) == o["ndma"]
                        for x in lst:
                            x.then_inc(dsem[o["dma"]], 16)
                    elif o["signal"]:
                        ins.then_inc(psem[ename], 1)

            block.tensor(lambda e: run("pe", e))
            block.scalar(lambda e: run("act", e))
            block.vector(lambda e: run("dve", e))
            block.gpsimd(lambda e: run("pool", e))
            block.sync(lambda e: run("sp", e))


def _rope_tables():
    t = np.arange(T, dtype=np.float32)
    ca = np.ones((128, T), np.float32)
    sa = np.zeros((128, T), np.float32)
    half = 8
    inv = (np.float32(500000.0) ** (-(np.arange(half, dtype=np.float32) * 2.0 / 16))).astype(np.float32)
    ang = t[None, :] * inv[:, None]
    for hh in range(2):
        b = hh * 64
        ca[b:b + half] = np.cos(ang)
        ca[b + half:b + 2 * half] = np.cos(ang)
        sa[b:b + half] = -np.sin(ang)
        sa[b + half:b + 2 * half] = np.sin(ang)
    cr = np.zeros((128, T), np.float32)
    sr = np.zeros((128, T), np.float32)
    half = 32
    inv = (np.float32(10000.0) ** (-(np.arange(half, dtype=np.float32) * 2.0 / 64))).astype(np.float32)
    ang = t[None, :] * inv[:, None]
    for hh in range(2):
        b = hh * 64
        cr[b:b + half] = np.cos(ang)
        cr[b + half:b + 64] = np.cos(ang)
        sr[b:b + half] = -np.sin(ang)
        sr[b + half:b + 64] = np.sin(ang)
    return ca, sa, cr, sr


def _consts():
    idx = np.arange(128)
    k = idx[:, None]
    q = idx[None, :]
    cur = (k <= q).astype(np.float32)
    prev = (k >= q).astype(np.float32)
    m_n = np.concatenate([cur, prev], axis=1)
    m_f = np.concatenate([cur, np.zeros_like(prev)], axis=1)
    m_fnnn = np.concatenate([m_f, m_n, m_n, m_n], axis=1)
    ones = np.ones((128, 128), np.float32)
    blk = np.zeros((128, 128), np.float32)
    blk[:64, :64] = 1
    blk[64:, 64:] = 1
    ident = np.eye(128, dtype=np.float32)
    cb = np.concatenate([ones, blk, ident], axis=1)
    hb = np.concatenate([m_n, m_fnnn], axis=1)
    h = np.arange(8, dtype=np.float64)
    log_g = np.log(1.0 - np.exp2(-5.0 - h))
    diff = (q - k).astype(np.float64)
    dm = np.zeros((128, 8, 128), np.float64)
    for hh in range(8):
        dm[:, hh, :] = np.where(diff >= 0, np.exp(log_g[hh] * np.maximum(diff, 0)), 0.0) / 8.0
    xi = np.zeros((128, 4, 128), np.float64)
    zt = np.zeros((128, 4, 128), np.float64)
    cd = np.zeros((128, 8), np.float64)
    for p in range(128):
        for pr in range(4):
            hh = pr * 2 + p // 64
            xi[p, pr, :] = np.exp(log_g[hh] * (idx + 1.0))
            cd[p, pr] = np.exp(log_g[hh] * 128.0)
    for col in range(128):
        for pr in range(4):
            hh = pr * 2 + col // 64
            zt[:, pr, col] = np.exp(log_g[hh] * (127.0 - idx)) / 8.0
    hf = np.concatenate([dm.reshape(128, -1), xi.reshape(128, -1), zt.reshape(128, -1), cd],
                        axis=1).astype(np.float32)
    return cb.astype(np.float32), hb.astype(np.float32), hf


def _hyb_cols():
    cols = []
    def perm(base, half, rot):
        out = []
        for hh in range(2):
            for j in range(64):
                if j < half:
                    jj = j + half
                elif j < 2 * half:
                    jj = j - half
                else:
                    jj = j
                out.append(base + hh * 64 + jj)
        return out
    for p in range(4):
        qa = 0 + p * 128
        ka = 512 + p * 128
        va = 1024 + p * 128
        cols += list(range(qa, qa + 128)) + perm(qa, 8, 16)
        cols += list(range(ka, ka + 128)) + perm(ka, 8, 16)
        cols += list(range(va, va + 128))
    for p in range(4):
        qr = 1536 + p * 128
        kr = 2048 + p * 128
        vr = 2560 + p * 128
        gr = 3072 + p * 128
        cols += list(range(qr, qr + 128)) + perm(qr, 32, 64)
        cols += list(range(kr, kr + 128)) + perm(kr, 32, 64)
        cols += list(range(gr, gr + 128))
        cols += list(range(vr, vr + 128))
    assert len(cols) == NEXT
    return np.array(cols, dtype=np.int64)


def ssl(st, cnt, d=1):
    return slice(st, st + (cnt - 1) * d + 1, d)


def build_program(layers, stop_after=None):
    nc = bass.Bass("TRN2", target_bir_lowering=False)

    def din(name, shape):
        return nc.dram_tensor(name, list(shape), F32, kind="ExternalInput").ap()

    xT = din("xT", [D, T])
    yT = nc.dram_tensor("yT", [D, T], F32, kind="ExternalOutput").ap()
    g_all = din("g_all", [128, 192])
    wg_d = din("ffn_w_gate", [DEPTH, 2, D, FF])
    wu_d = din("ffn_w_up", [DEPTH, 2, D, FF])
    wd_d = din("ffn_w_down", [DEPTH, 2, FF, D])
    hin_d = din("hyb_w_in_ext", [2, D, NEXT])
    hout_d = din("hyb_w_out", [2, D, D])
    gin_d = din("gmlp_w_in", [2, D, 2 * D])
    gout_d = din("gmlp_w_out", [2, D, D])
    glng_d = din("gmlp_ln_g", [2, D])
    glnb_d = din("gmlp_ln_b", [2, D])
    gws_d = din("gmlp_w_sT", [2, 128, 8 * 128])
    gbs_d = din("gmlp_b_s", [2, 8 * 128])
    ropeA_c = din("ropeA_c", [128, T])
    ropeA_s = din("ropeA_s", [128, T])
    ropeR_c = din("ropeR_c", [128, T])
    ropeR_s = din("ropeR_s", [128, T])
    cb_d = din("cb", [128, 384])
    hb_d = din("hb", [128, 1280])
    hf_d = din("hf", [128, 2056])

    es = ExitStack()
    X = es.enter_context(nc.sbuf_tensor("X", [128, 8, T], F32))
    G = es.enter_context(nc.sbuf_tensor("G", [128, 192], F32))
    CB = es.enter_context(nc.sbuf_tensor("CB", [128, 384], BF16))
    MASKC = es.enter_context(nc.sbuf_tensor("MASKC", [128, 128], BF16))
    EPSB = es.enter_context(nc.sbuf_tensor("EPSB", [128, 4], F32))
    SCRB = 142 * 1024
    SCR = es.enter_context(nc.sbuf_tensor("SCR", [128, SCRB], U8))
    PSALL = es.enter_context(nc.psum_tensor("PSALL", [128, 4096], F32))
    ONES = CB[:, 0:128]
    BLK = CB[:, 128:256]
    IDENT = CB[:, 256:384]

    def PS(b, n=1):
        return PSALL[:, b * 512:(b + n) * 512]

    class Carver:
        def __init__(self, off=0):
            self.off = off

        def take(self, dtype, shape):
            n = 1
            for s_ in shape[1:]:
                n *= s_
            nb = n * (4 if dtype == F32 else 2)
            nb = (nb + 31) // 32 * 32
            ap = SCR[:, self.off:self.off + nb].bitcast(dtype)[:, 0:n]
            if len(shape) == 3:
                ap = ap.rearrange("p (a b) -> p a b", a=shape[1])
            elif len(shape) == 4:
                ap = ap.rearrange("p (a b c) -> p a b c", a=shape[1], b=shape[2])
            self.off += nb
            assert self.off <= SCRB, (self.off, SCRB)
            return ap

    S = Sched(nc)

    def barrier():
        S.barrier(lambda e: e.memset(EPSB[:, 2:3], 0.0))

    S.op("sp", lambda e: e.dma_start(out=G[:], in_=g_all), w=[("G",)], dma=("dma", "G"))
    S.op("pool", lambda e: e.dma_start(out=CB[:], in_=cb_d), w=[("CB",)], dma=("dma", "CB"))
    S.op("pool", lambda e: e.dma_start(out=MASKC[:], in_=hb_d[:, 0:128]), w=[("MASKC",)], dma=("dma", "MASKC"))
    S.op("dve", lambda e: e.memset(EPSB[:, 0:1], EPS), w=[("EPSB0",)])
    S.op("dve", lambda e: e.memset(EPSB[:, 1:2], math.log(0.5)), r=[("EPSB0",)], w=[("EPSB",)])
    xv = xT.rearrange("(c p) t -> p c t", p=128)
    yv = yT.rearrange("(c p) t -> p c t", p=128)
    for g in range(4):
        S.op("sp", lambda e, g=g: e.dma_start(out=X[:, :, g * 512:(g + 1) * 512],
                                              in_=xv[:, :, g * 512:(g + 1) * 512]),
             w=[("X", g)], dma=("dma", "X", g))

    def rstd_from_sq(sq_ap, nk, rs_ap, inv_n, ln_half, keys_r, key_rs, lhs=None, bank=6):
        lhs = ONES if lhs is None else lhs

        def mm(e):
            ins = None
            for k in range(nk):
                src = sq_ap[:, k, :] if nk > 1 else sq_ap
                ins = e.matmul(PS(bank), lhsT=lhs, rhs=src, start=(k == 0), stop=(k == nk - 1))
            return ins
        S.op("pe", mm, r=list(keys_r) + [("CB",)], w=[("ps", bank)])
        S.op("act", lambda e: e.activation(out=rs_ap, in_=PS(bank), func=AF.Ln, scale=inv_n, bias=EPSB[:, 0:1]),
             r=[("ps", bank), ("EPSB",)], w=[key_rs])
        if ln_half:
            S.op("act", lambda e: e.activation(out=rs_ap, in_=rs_ap, func=AF.Exp, scale=-0.5, bias=EPSB[:, 1:2]),
                 r=[key_rs, ("EPSB",)], w=[key_rs])
        else:
            S.op("act", lambda e: e.activation(out=rs_ap, in_=rs_ap, func=AF.Exp, scale=-0.5),
                 r=[key_rs], w=[key_rs])

    def prenorm(gi, g, dst_fn, dst_key, SQ, RS, stage="ab", ksq=("SQ",), krs=("RS",), bank=6):
        tok = slice(g * 512, (g + 1) * 512)
        if "a" in stage:
            S.op("act", lambda e: e.activation(out=SQ, in_=X[:, :, tok], func=AF.Square),
                 r=[("X", g)], w=[ksq])
        if "b" not in stage:
            return
        rstd_from_sq(SQ, 8, RS, 1.0 / D, False, [ksq], krs, bank=bank)
        for k in range(8):
            S.op("dve", lambda e, k=k: e.scalar_tensor_tensor(
                out=dst_fn(k), in0=X[:, k, tok], scalar=G[:, gi * 8 + k:gi * 8 + k + 1], in1=RS,
                op0=ALU.mult, op1=ALU.mult),
                r=[("X", g), krs, ("G",)], w=[dst_key(k)])

    def proj_postnorm(src_fn, src_keys, nk, w_view, gi, g, half_factor, FB, SQ, RS, WDS):
        tok = slice(g * 512, (g + 1) * 512)
        for c in range(8):
            slot, key = S.consume("WD", 2, "pool",
                                  lambda e, s, c=c: e.dma_start(out=WDS[:, s, 0:nk, :],
                                                                in_=w_view[:, :, c * 128:(c + 1) * 128]))
            b = 4 + c % 2

            def mm(e, slot=slot, b=b):
                ins = None
                for j in range(nk):
                    ins = e.matmul(PS(b), lhsT=WDS[:, slot, j, :], rhs=src_fn(j), start=(j == 0), stop=(j == nk - 1))
                return ins
            S.op("pe", mm, r=[key] + list(src_keys), w=[("ps", b)])
            S.op("act", lambda e, c=c, b=b: e.activation(out=FB[:, c, :], in_=PS(b), func=AF.Copy),
                 r=[("ps", b)], w=[("F", c)])
        S.op("act", lambda e: e.activation(out=SQ, in_=FB, func=AF.Square),
             r=[("F", c) for c in range(8)], w=[("SQ",)])
        rstd_from_sq(SQ, 8, RS, 1.0 / D, half_factor, [("SQ",)], ("RS",))
        for c in range(8):
            S.op("dve", lambda e, c=c: e.scalar_tensor_tensor(
                out=FB[:, c, :], in0=FB[:, c, :], scalar=G[:, gi * 8 + c:gi * 8 + c + 1], in1=RS,
                op0=ALU.mult, op1=ALU.mult), r=[("F", c), ("RS",), ("G",)], w=[("F", c)])
        S.op("pool", lambda e: e.tensor_tensor(out=X[:, :, tok], in0=X[:, :, tok], in1=FB, op=ALU.add),
             r=[("F", c) for c in range(8)] + [("X", g)], w=[("X", g)])

    def ffn_seq(items):
        cv = Carver()
        H = cv.take(BF16, [128, 8, 1024])
        ACTB = cv.take(BF16, [128, NJ, 1024])
        FB = cv.take(F32, [128, 8, 1024])
        SQ = cv.take(BF16, [128, 8, 512])
        RS = cv.take(F32, [128, 512])
        SQ2 = cv.take(BF16, [128, 8, 512])
        RS2 = cv.take(F32, [128, 512])
        SG = cv.take(F32, [128, 2, 512])
        WGU = cv.take(BF16, [128, 3, 16, 128])
        WDS = cv.take(BF16, [128, 2, NJ, 128])
        halves = [(l, i, hf) for (l, i) in items for hf in range(2)]

        def gidx(l, i):
            return l * 6 + (0 if i == 0 else 4)

        def pre(hv, stage, tts):
            l, i, hf = hv
            for tt in tts:
                prenorm(gidx(l, i), hf * 2 + tt, lambda k, tt=tt: H[:, k, tt * 512:(tt + 1) * 512],
                        lambda k, tt=tt: ("H", tt), SQ2, RS2, stage=stage, ksq=("SQ2",), krs=("RS2",), bank=7)

        def phase1(hv):
            l, i, hf = hv
            wgv = wg_d[l, i].rearrange("(k p) n -> p k n", p=128)
            wuv = wu_d[l, i].rearrange("(k p) n -> p k n", p=128)
            for j in range(NJ):
                def ld(e, s, j=j):
                    a_ = e.dma_start(out=WGU[:, s, 0:8, :], in_=wgv[:, :, j * 128:(j + 1) * 128])
                    b_ = e.dma_start(out=WGU[:, s, 8:16, :], in_=wuv[:, :, j * 128:(j + 1) * 128])
                    return [a_, b_]
                slot, key = S.consume("WGU", 3, "pool", ld, ndma=2)
                for tt in range(2):
                    bg, bu = tt, 2 + tt

                    def mm(e, slot=slot, tt=tt, bg=bg, bu=bu):
                        ins = None
                        for k in range(8):
                            ins = e.matmul(PS(bg), lhsT=WGU[:, slot, k, :], rhs=H[:, k, tt * 512:(tt + 1) * 512],
                                           start=(k == 0), stop=(k == 7))
                        for k in range(8):
                            ins = e.matmul(PS(bu), lhsT=WGU[:, slot, 8 + k, :], rhs=H[:, k, tt * 512:(tt + 1) * 512],
                                           start=(k == 0), stop=(k == 7))
                        return ins
                    S.op("pe", mm, r=[key, ("H", tt)], w=[("ps", bg), ("ps", bu)])
                    S.op("act", lambda e, tt=tt, bg=bg: e.activation(out=SG[:, tt, :], in_=PS(bg), func=AF.Silu),
                         r=[("ps", bg)], w=[("SG", tt)])
                    S.op("dve", lambda e, bu=bu, j=j, tt=tt: e.tensor_tensor(
                        out=ACTB[:, j, tt * 512:(tt + 1) * 512], in0=SG[:, tt, :], in1=PS(bu), op=ALU.mult),
                        r=[("SG", tt), ("ps", bu)], w=[("A", j, tt)])

        def phase2(hv, hook):
            l, i, hf = hv
            wdv = wd_d[l, i].rearrange("(j p) n -> p j n", p=128)
            gi = gidx(l, i) + 1
            for c in range(8):
                slot, key = S.consume("WD", 2, "pool",
                                      lambda e, s, c=c: e.dma_start(out=WDS[:, s, :, :],
                                                                    in_=wdv[:, :, c * 128:(c + 1) * 128]))
                for tt in range(2):
                    b = 4 + tt

                    def mm(e, slot=slot, b=b, tt=tt):
                        ins = None
                        for j in range(NJ):
                            ins = e.matmul(PS(b), lhsT=WDS[:, slot, j, :], rhs=ACTB[:, j, tt * 512:(tt + 1) * 512],
                                           start=(j == 0), stop=(j == NJ - 1))
                        return ins
                    S.op("pe", mm, r=[key] + [("A", j, tt) for j in range(NJ)], w=[("ps", b)])
                    S.op("act", lambda e, c=c, b=b, tt=tt: e.activation(out=FB[:, c, tt * 512:(tt + 1) * 512], in_=PS(b),
                                                                         func=AF.Copy),
                         r=[("ps", b)], w=[("F", c, tt)])
                if c == 0 and hook is not None:
                    hook()
            for tt in range(2):
                g = hf * 2 + tt
                tok = slice(g * 512, (g + 1) * 512)
                fsl = slice(tt * 512, (tt + 1) * 512)
                fk = [("F", c, tt) for c in range(8)]
                S.op("act", lambda e, fsl=fsl: e.activation(out=SQ, in_=FB[:, :, fsl], func=AF.Square),
                     r=fk, w=[("SQ",)])
                rstd_from_sq(SQ, 8, RS, 1.0 / D, True, [("SQ",)], ("RS",))
                for c in range(8):
                    S.op("dve", lambda e, c=c, fsl=fsl: e.scalar_tensor_tensor(
                        out=FB[:, c, fsl], in0=FB[:, c, fsl], scalar=G[:, gi * 8 + c:gi * 8 + c + 1], in1=RS,
                        op0=ALU.mult, op1=ALU.mult), r=[("F", c, tt), ("RS",), ("G",)], w=[("F", c, tt)])
                S.op("pool", lambda e, tok=tok, fsl=fsl: e.tensor_tensor(out=X[:, :, tok], in0=X[:, :, tok], in1=FB[:, :, fsl],
                                                                         op=ALU.add),
                     r=fk + [("X", g)], w=[("X", g)])

        pre(halves[0], "ab", (0, 1))
        for idx, hv in enumerate(halves):
            phase1(hv)
            nxt = halves[idx + 1] if idx + 1 < len(halves) else None
            if nxt is not None:
                pre(nxt, "a", (0,))

                def hook(nxt=nxt):
                    pre(nxt, "b", (0,))
                    pre(nxt, "ab", (1,))
                phase2(hv, hook)
            else:
                phase2(hv, None)

    def gmlp(l):
        jl = l // 2
        cv = Carver()
        H = cv.take(BF16, [128, 8, 512])
        UT = cv.take(BF16, [128, 8, 512])
        YT = cv.take(BF16, [128, 8, 512])
        VG = cv.take(F32, [128, 4, 1024])
        VN = cv.take(BF16, [128, 4, 1024])
        LNG = cv.take(F32, [128, 1024])
        LNB = cv.take(F32, [128, 1024])
        WST = cv.take(BF16, [128, 8, 128])
        BS = cv.take(F32, [128, 8, 128])
        WIN = cv.take(BF16, [128, 3, 8, 128])
        WV = cv.take(BF16, [128, 2, 8, 512])
        FB = cv.take(F32, [128, 8, 512])
        SQ = cv.take(BF16, [128, 8, 512])
        RS = cv.take(F32, [128, 512])
        Y1 = cv.take(F32, [128, 2, 512])
        ST = cv.take(F32, [128, 4, 16])
        MV = cv.take(F32, [128, 4, 2])
        RSD = cv.take(F32, [128, 4])
        WDS = cv.take(BF16, [128, 2, 8, 128])
        winv = gin_d[jl].rearrange("(k p) n -> p k n", p=128)
        woutv = gout_d[jl].rearrange("(j p) n -> p j n", p=128)
        gi = l * 6 + 2
        S.op("sp", lambda e: e.dma_start(out=LNG, in_=glng_d[jl:jl + 1, :].to_broadcast([128, D])),
             w=[("LNG",)], dma=("dma", "LNG"))
        S.op("sp", lambda e: e.dma_start(out=LNB, in_=glnb_d[jl:jl + 1, :].to_broadcast([128, D])),
             w=[("LNB",)], dma=("dma", "LNB"))
        S.op("sp", lambda e: e.dma_start(out=BS.rearrange("p a b -> p (a b)"),
                                         in_=gbs_d[jl:jl + 1, :].to_broadcast([128, D])),
             w=[("BS",)], dma=("dma", "BS"))
        S.op("pool", lambda e: e.dma_start(out=WST.rearrange("p a b -> p (a b)"), in_=gws_d[jl]),
             w=[("WST0",)], dma=("dma", "WST"))
        S.op("dve", lambda e: e.tensor_tensor(out=WST, in0=WST, in1=MASKC[:, None, :].to_broadcast([128, 8, 128]),
                                              op=ALU.mult),
             r=[("WST0",), ("MASKC",)], w=[("WST",)])
        SQ2 = cv.take(BF16, [128, 8, 512])
        RS2 = cv.take(F32, [128, 512])

        def pre(g):
            prenorm(gi, g, lambda k: H[:, k, :], lambda k: ("H",), SQ2, RS2, ksq=("SQ2",), krs=("RS2",), bank=7)

        pre(0)
        for g in range(4):
            for hv in range(2):
                slot, key = S.consume("GWV", 2, "pool",
                                      lambda e, s, hv=hv: e.dma_start(out=WV[:, s],
                                                                       in_=winv[:, :, D + hv * 512:D + (hv + 1) * 512]))
                for m in range(4):
                    b = 2 + (hv * 4 + m) % 2

                    def mm(e, slot=slot, b=b, m=m):
                        ins = None
                        for k in range(8):
                            ins = e.matmul(PS(b), lhsT=H[:, k, m * 128:(m + 1) * 128], rhs=WV[:, slot, k, :],
                                           start=(k == 0), stop=(k == 7))
                        return ins
                    S.op("pe", mm, r=[key, ("H",)], w=[("ps", b)])
                    S.op("act", lambda e, b=b, m=m, hv=hv: e.activation(
                        out=VG[:, m, hv * 512:(hv + 1) * 512], in_=PS(b), func=AF.Gelu),
                        r=[("ps", b)], w=[("VG", m, hv)])
                    S.op("dve", lambda e, m=m, hv=hv: e.bn_stats(out=ST[:, m, hv * 6:(hv + 1) * 6],
                                                                  in_=VG[:, m, hv * 512:(hv + 1) * 512]),
                         r=[("VG", m, hv)], w=[("STAT", m, hv)])
            for m in range(4):
                S.op("dve", lambda e, m=m: e.bn_aggr(out=MV[:, m, :], in_=ST[:, m, 0:12]),
                     r=[("STAT", m, 0), ("STAT", m, 1)], w=[("MV", m)])
            S.op("act", lambda e: e.activation(out=RSD, in_=MV[:, :, 1], func=AF.Ln, bias=EPSB[:, 0:1]),
                 r=[("MV", m) for m in range(4)] + [("EPSB",)], w=[("RSD",)])
            S.op("act", lambda e: e.activation(out=RSD, in_=RSD, func=AF.Exp, scale=-0.5),
                 r=[("RSD",)], w=[("RSD",)])
            for m in range(4):
                vk = [("VG", m, 0), ("VG", m, 1)]
                S.op("dve", lambda e, m=m: e.tensor_scalar(out=VG[:, m, :], in0=VG[:, m, :], scalar1=MV[:, m, 0:1],
                                                           scalar2=RSD[:, m:m + 1], op0=ALU.subtract, op1=ALU.mult),
                     r=vk + [("MV", m), ("RSD",)], w=vk)
                S.op("dve", lambda e, m=m: e.tensor_tensor(out=VG[:, m, :], in0=VG[:, m, :], in1=LNG, op=ALU.mult),
                     r=vk + [("LNG",)], w=vk)
                S.op("pool", lambda e, m=m: e.tensor_tensor(out=VN[:, m, :], in0=VG[:, m, :], in1=LNB, op=ALU.add),
                     r=vk + [("LNB",)], w=[("VN", m)])
            for ct in range(8):
                slot, key = S.consume("GWIN", 3, "pool",
                                      lambda e, s, ct=ct: e.dma_start(out=WIN[:, s], in_=winv[:, :, ct * 128:(ct + 1) * 128]))
                b = ct % 2

                def mm(e, slot=slot, b=b):
                    ins = None
                    for k in range(8):
                        ins = e.matmul(PS(b), lhsT=WIN[:, slot, k, :], rhs=H[:, k, :], start=(k == 0), stop=(k == 7))
                    return ins
                S.op("pe", mm, r=[key, ("H",)], w=[("ps", b)])
                S.op("act", lambda e, ct=ct, b=b: e.activation(out=UT[:, ct, :], in_=PS(b), func=AF.Gelu),
                     r=[("ps", b)], w=[("UT", ct)])
            for gg in range(8):
                b = 4 + gg % 2

                def mm(e, gg=gg, b=b):
                    ins = None
                    for m in range(4):
                        ins = e.matmul(PS(b)[:, m * 128:(m + 1) * 128], lhsT=VN[:, m, gg * 128:(gg + 1) * 128],
                                       rhs=WST[:, gg, :], start=True, stop=True)
                    return ins
                S.op("pe", mm, r=[("VN", m) for m in range(4)] + [("WST",)], w=[("ps", b)])
                u = gg % 2
                S.op("dve", lambda e, gg=gg, b=b, u=u: e.tensor_tensor(
                    out=Y1[:, u, :].rearrange("p (m i) -> p m i", m=4),
                    in0=PS(b).rearrange("p (m i) -> p m i", m=4),
                    in1=BS[:, gg:gg + 1, :].to_broadcast([128, 4, 128]), op=ALU.add),
                    r=[("ps", b), ("BS",)], w=[("Y1", u)])
                S.op("pool", lambda e, gg=gg, u=u: e.tensor_tensor(out=YT[:, gg, :], in0=Y1[:, u, :], in1=UT[:, gg, :],
                                                                   op=ALU.mult),
                     r=[("Y1", u), ("UT", gg)], w=[("YT", gg)])
            if g + 1 < 4:
                pre(g + 1)
            proj_postnorm(lambda j: YT[:, j, :], [("YT", j) for j in range(8)], 8, woutv, gi + 1, g, False,
                          FB, SQ, RS, WDS)

    def hybrid(l):
        jl = l // 2
        cv = Carver()
        HT = cv.take(BF16, [128, 8, T])
        MIX = cv.take(BF16, [128, 8, T])
        WIN = cv.take(BF16, [128, 6, 8, 128])
        ROPE = cv.take(F32, [128, 2, 2, 512])
        TMP = cv.take(F32, [128, 2, 512])
        RS = cv.take(F32, [128, 512])
        pair_off = cv.off
        winv = hin_d[jl].rearrange("(k p) n -> p k n", p=128)
        woutv = hout_d[jl].rearrange("(j p) n -> p j n", p=128)
        gi = l * 6 + 2
        SQ0 = Carver(pair_off).take(BF16, [128, 8, 512])
        for g in range(4):
            prenorm(gi, g, lambda k, g=g: HT[:, k, g * 512:(g + 1) * 512], lambda k, g=g: ("HT", g), SQ0, RS)
        barrier()
        HTK = [("HT", g) for g in range(4)]

        def win_block(col0):
            return S.consume("HWIN", 6, "pool",
                             lambda e, s, col0=col0: e.dma_start(out=WIN[:, s], in_=winv[:, :, col0:col0 + 128]),
                             lookahead=2)

        def rope_proj(col0, rc, rs_, dsts, fam):
            blks = [win_block(col0 + i * 128) for i in range(4)]
            for g in range(4):
                tok = slice(g * 512, (g + 1) * 512)

                def ldrope(e, s, g=g):
                    a = e.dma_start(out=ROPE[:, s, 0, :], in_=rc[:, g * 512:(g + 1) * 512])
                    b = e.dma_start(out=ROPE[:, s, 1, :], in_=rs_[:, g * 512:(g + 1) * 512])
                    return [a, b]
                rslot, rkey = S.consume("ROPE", 2, "sp", ldrope, ndma=2)
                for qi in range(2):
                    u = (g * 2 + qi) % 2
                    b0, b1 = 2 * u, 2 * u + 1
                    (s0, k0), (s1, k1) = blks[2 * qi], blks[2 * qi + 1]

                    def mm(e, s0=s0, s1=s1, b0=b0, b1=b1, tok=tok):
                        ins = None
                        for k in range(8):
                            ins = e.matmul(PS(b0), lhsT=WIN[:, s0, k, :], rhs=HT[:, k, tok], start=(k == 0), stop=(k == 7))
                        for k in range(8):
                            ins = e.matmul(PS(b1), lhsT=WIN[:, s1, k, :], rhs=HT[:, k, tok], start=(k == 0), stop=(k == 7))
                        return ins
                    S.op("pe", mm, r=[k0, k1, ("HT", g)], w=[("ps", b0), ("ps", b1)])
                    S.op("dve", lambda e, b0=b0, rslot=rslot: e.tensor_tensor(
                        out=TMP[:, 0, :], in0=PS(b0), in1=ROPE[:, rslot, 0, :], op=ALU.mult),
                        r=[("ps", b0), rkey], w=[("TMP", 0)])
                    S.op("dve", lambda e, b1=b1, rslot=rslot: e.tensor_tensor(
                        out=TMP[:, 1, :], in0=PS(b1), in1=ROPE[:, rslot, 1, :], op=ALU.mult),
                        r=[("ps", b1), rkey], w=[("TMP", 1)])
                    dst = dsts[qi]
                    S.op("dve", lambda e, dst=dst, tok=tok: e.tensor_tensor(
                        out=dst[:, tok], in0=TMP[:, 0, :], in1=TMP[:, 1, :], op=ALU.add),
                        r=[("TMP", 0), ("TMP", 1)], w=[("QK", fam, qi, g)])

        def dswa_pair(p):
            cvp = Carver(pair_off)
            HB = cvp.take(BF16, [128, 1280])
            QT = cvp.take(BF16, [128, T])
            KT = cvp.take(BF16, [128, T])
            VA = cvp.take(BF16, [128, 3, 16, 192])
            ACC = cvp.take(F32, [128, 2, T])
            E = cvp.take(BF16, [128, 2, 1024])
            M_N = HB[:, 0:256]
            M_F = HB[:, 256:1280]
            RC = TMP
            col0 = p * 640
            if p == 0:
                S.op("pool", lambda e: e.dma_start(out=HB, in_=hb_d), w=[("HB",)], dma=("dma", "HB"))
                S.op("pool", lambda e: e.memset(VA[:, :, :, 64:128], 1.0), w=[("VA1",)])
            rope_proj(col0, ropeA_c, ropeA_s, [QT, KT], "a")
            vs, vk = win_block(col0 + 512)
            for bi, d in enumerate((1, 4, 16)):
                tpc = 16 // d
                for i4 in range(4):
                    b = 4 + (bi * 4 + i4) % 2

                    def mm(e, b=b, d=d, i4=i4, tpc=tpc):
                        ins = None
                        for ii in range(4):
                            i = i4 * 4 + ii
                            r, blk = i // tpc, i % tpc
                            st = blk * 128 * d + r
                            for k in range(8):
                                ins = e.matmul(PS(b)[:, ii * 128:(ii + 1) * 128],
                                               lhsT=HT[:, k, ssl(st, 128, d)], rhs=WIN[:, vs, k, :],
                                               start=(k == 0), stop=(k == 7))
                        return ins
                    S.op("pe", mm, r=[vk] + HTK, w=[("ps", b)])
                    S.op("act", lambda e, b=b, bi=bi, i4=i4: e.activation(
                        out=VA[:, bi, i4 * 4:(i4 + 1) * 4, :].rearrange("p i (s c) -> p i s c", s=3)[:, :, 0:3:2, :],
                        in_=PS(b).rearrange("p (i s c) -> p i s c", i=4, s=2), func=AF.Copy),
                        r=[("ps", b), ("VA1",)], w=[("VA", bi, i4)])
            QKK = [("QK", "a", qi, g) for qi in range(2) for g in range(4)]
            unit = 0
            for hh in range(2):
                hs = slice(hh * 64, hh * 64 + 64)
                vcols = slice(hh * 64, hh * 64 + 128)
                for bi, d in enumerate((1, 4, 16)):
                    tpc = 16 // d
                    for i4 in range(4):
                        su = unit % 2
                        unit += 1
                        sb0 = 2 * su
                        ob = 4 + su
                        blocks = []
                        for ii in range(4):
                            i = i4 * 4 + ii
                            r, n = i // tpc, i % tpc
                            blocks.append((i, n, n * 128 * d + r, (n - 1) * 128 * d + r))
                        noprev = (d == 16)
                        W = 128 if noprev else 256

                        def mm_s(e, blocks=blocks, sb0=sb0, d=d, hs=hs, noprev=noprev, W=W):
                            ins = None
                            for ii, (i, n, st, pst) in enumerate(blocks):
                                qsl = ssl(st, 128, d)
                                ins = e.matmul(PS(sb0, 2)[:, ii * W:ii * W + 128], lhsT=KT[hs, qsl], rhs=QT[hs, qsl],
                                               start=True, stop=True)
                                if not noprev:
                                    ksl = qsl if n == 0 else ssl(pst, 128, d)
                                    ins = e.matmul(PS(sb0, 2)[:, ii * W + 128:ii * W + 256], lhsT=KT[hs, ksl],
                                                   rhs=QT[hs, qsl], start=True, stop=True)
                            return ins
                        S.op("pe", mm_s, r=QKK, w=[("ps", sb0), ("ps", sb0 + 1)])
                        S.op("act", lambda e, sb0=sb0, su=su, W=W: e.activation(
                            out=E[:, su, 0:4 * W], in_=PS(sb0, 2)[:, 0:4 * W], func=AF.Exp, scale=0.125),
                            r=[("ps", sb0), ("ps", sb0 + 1)], w=[("E", su)])
                        if noprev:
                            msk = M_N[:, None, 0:128].to_broadcast([128, 4, 128])
                            ev = E[:, su, 0:512].rearrange("p (a b) -> p a b", a=4)
                        elif blocks[0][1] == 0:
                            msk = M_F
                            ev = E[:, su, :]
                        else:
                            msk = M_N[:, None, :].to_broadcast([128, 4, 256])
                            ev = E[:, su, :].rearrange("p (a b) -> p a b", a=4)
                        S.op("dve", lambda e, ev=ev, msk=msk: e.tensor_tensor(out=ev, in0=ev, in1=msk, op=ALU.mult),
                             r=[("E", su), ("HB",)], w=[("E", su)])

                        def mm_o(e, blocks=blocks, ob=ob, su=su, bi=bi, vcols=vcols, noprev=noprev, W=W):
                            ins = None
                            for ii, (i, n, st, pst) in enumerate(blocks):
                                hasprev = (not noprev) and n > 0
                                ins = e.matmul(PS(ob)[:, ii * 128:(ii + 1) * 128], lhsT=VA[:, bi, i, vcols],
                                               rhs=E[:, su, ii * W:ii * W + 128], start=True, stop=not hasprev)
                                if hasprev:
                                    ins = e.matmul(PS(ob)[:, ii * 128:(ii + 1) * 128], lhsT=VA[:, bi, i - 1, vcols],
                                                   rhs=E[:, su, ii * W + 128:ii * W + 256], start=False, stop=True)
                            return ins
                        S.op("pe", mm_o, r=[("E", su)] + [("VA", bi, x) for x in range(4)], w=[("ps", ob)])
                        if d == 16:
                            r0 = blocks[0][0]
                            dst = ACC[:, hh, :].rearrange("p (m r) -> p r m", r=16)[:, r0:r0 + 4, :]
                            src = PS(ob).rearrange("p (a m) -> p a m", a=4)
                        else:
                            dst = ACC[:, hh, ssl(blocks[0][2], 512, d)]
                            src = PS(ob)
                        if bi == 0:
                            S.op("dve", lambda e, dst=dst, src=src: e.tensor_copy(out=dst, in_=src),
                                 r=[("ps", ob)], w=[("ACC", hh)])
                        else:
                            S.op("dve", lambda e, dst=dst, src=src: e.tensor_tensor(out=dst, in0=dst, in1=src, op=ALU.add),
                                 r=[("ps", ob), ("ACC", hh)], w=[("ACC", hh)])
                orow = slice(hh * 64, hh * 64 + 64)
                drow = slice(64 - hh * 64, 128 - hh * 64)
                for g in range(4):
                    tok = slice(g * 512, (g + 1) * 512)
                    u = g % 2
                    S.op("act", lambda e, hh=hh, tok=tok, u=u, orow=orow, drow=drow: e.activation(
                        out=RC[orow, u, :], in_=ACC[drow, hh, tok], func=AF.Ln), r=[("ACC", hh)], w=[("TMP", u)])
                    S.op("act", lambda e, u=u, orow=orow: e.activation(
                        out=RC[orow, u, :], in_=RC[orow, u, :], func=AF.Exp, scale=-1.0), r=[("TMP", u)], w=[("TMP", u)])
                    S.op("dve", lambda e, hh=hh, tok=tok, u=u, orow=orow: e.tensor_tensor(
                        out=MIX[orow, p, tok], in0=ACC[orow, hh, tok], in1=RC[orow, u, :], op=ALU.mult),
                        r=[("TMP", u), ("ACC", hh)], w=[("MIX", p, hh, g)])

        def ret_pair(p):
            cvp = Carver(pair_off)
            HF = cvp.take(F32, [128, 2056])
            QT = cvp.take(BF16, [128, T])
            KT = cvp.take(BF16, [128, T])
            QX = cvp.take(BF16, [128, T])
            GT = cvp.take(BF16, [128, T])
            VT = cvp.take(BF16, [128, 16, 128])
            KZ = cvp.take(BF16, [128, 16, 128])
            R32 = cvp.take(F32, [128, T])
            ST32 = cvp.take(F32, [128, 64])
            STB = cvp.take(BF16, [128, 2, 64])
            SD = cvp.take(BF16, [128, 2, 256])
            SQH = cvp.take(BF16, [128, 512])
            DM = HF[:, 0:1024].rearrange("p (h c) -> p h c", h=8)
            XI = HF[:, 1024:1536].rearrange("p (a c) -> p a c", a=4)
            ZT = HF[:, 1536:2048].rearrange("p (a c) -> p a c", a=4)
            CD = HF[:, 2048:2056]
            col0 = 4 * 640 + p * 768
            if p == 0:
                S.op("sp", lambda e: e.dma_start(out=HF, in_=hf_d), w=[("HF",)], dma=("dma", "HF"))
            rope_proj(col0, ropeR_c, ropeR_s, [QT, KT], "r")
            for g in range(4):
                tok = slice(g * 512, (g + 1) * 512)
                S.op("pool", lambda e, tok=tok: e.tensor_tensor(
                    out=QX[:, tok].rearrange("p (a c) -> p a c", a=4), in0=QT[:, tok].rearrange("p (a c) -> p a c", a=4),
                    in1=XI[:, p:p + 1, :].to_broadcast([128, 4, 128]), op=ALU.mult),
                    r=[("QK", "r", 0, g), ("HF",)], w=[("QX", g)])
            gs, gk = win_block(col0 + 512)
            for g in range(4):
                tok = slice(g * 512, (g + 1) * 512)
                b = g % 2

                def mm(e, b=b, tok=tok):
                    ins = None
                    for k in range(8):
                        ins = e.matmul(PS(b), lhsT=WIN[:, gs, k, :], rhs=HT[:, k, tok], start=(k == 0), stop=(k == 7))
                    return ins
                S.op("pe", mm, r=[gk, ("HT", g)], w=[("ps", b)])
                S.op("act", lambda e, b=b, tok=tok: e.activation(out=GT[:, tok], in_=PS(b), func=AF.Silu),
                     r=[("ps", b)], w=[("GT", g)])
            vs, vk = win_block(col0 + 640)
            for i4 in range(4):
                b = 2 + i4 % 2

                def mm(e, b=b, i4=i4):
                    ins = None
                    for ii in range(4):
                        i = i4 * 4 + ii
                        for k in range(8):
                            ins = e.matmul(PS(b)[:, ii * 128:(ii + 1) * 128], lhsT=HT[:, k, i * 128:(i + 1) * 128],
                                           rhs=WIN[:, vs, k, :], start=(k == 0), stop=(k == 7))
                    return ins
                S.op("pe", mm, r=[vk] + HTK, w=[("ps", b)])
                S.op("act", lambda e, b=b, i4=i4: e.activation(
                    out=VT[:, i4 * 4:(i4 + 1) * 4, :], in_=PS(b).rearrange("p (i c) -> p i c", i=4), func=AF.Copy),
                    r=[("ps", b)], w=[("VT", i4)])
            PSB7 = PS(7).bitcast(BF16)
            for i4 in range(DEBUG.get("ntr", 4)):
                def tr(e, i4=i4):
                    ins = None
                    for ii in range(4):
                        i = i4 * 4 + ii
                        ins = e.transpose(PSB7[:, ii * 128:(ii + 1) * 128], KT[:, i * 128:(i + 1) * 128], IDENT)
                    return ins
                S.op("pe", tr, r=[("QK", "r", 1, i4), ("CB",)], w=[("ps", 7)])
                S.op("dve", lambda e, i4=i4: e.tensor_tensor(
                    out=KZ[:, i4 * 4:(i4 + 1) * 4, :], in0=PSB7[:, 0:512].rearrange("p (i c) -> p i c", i=4),
                    in1=ZT[:, p:p + 1, :].to_broadcast([128, 4, 128]), op=ALU.mult),
                    r=[("ps", 7), ("HF",)], w=[("KZ", i4)])
            S.op("dve", lambda e: e.memset(ST32, 0.0), w=[("ST32",)])
            for n in range(DEBUG.get("nrec", 16)):
                g = n // 4
                ck = slice(n * 128, (n + 1) * 128)
                sb = n % 2
                b0 = 2 * sb
                kvb = 6 + n % 2

                def mm_s(e, ck=ck, b0=b0):
                    e.matmul(PS(b0)[:, 0:128], lhsT=KT[0:64, ck], rhs=QT[0:64, ck], start=True, stop=True)
                    return e.matmul(PS(b0 + 1)[:, 0:128], lhsT=KT[64:128, ck], rhs=QT[64:128, ck], start=True, stop=True)
                S.op("pe", mm_s, r=[("QK", "r", 0, g), ("QK", "r", 1, g)], w=[("ps", b0), ("ps", b0 + 1)])
                S.op("dve", lambda e, sb=sb, b0=b0: e.tensor_tensor(
                    out=SD[:, sb, :].rearrange("p (h c) -> p h c", h=2),
                    in0=PS(b0, 2).rearrange("p (h c) -> p h c", h=2)[:, :, 0:128], in1=DM[:, 2 * p:2 * p + 2, :],
                    op=ALU.mult),
                    r=[("ps", b0), ("ps", b0 + 1), ("HF",)], w=[("SD", sb)])

                def mm_kv(e, n=n, kvb=kvb):
                    e.matmul(PS(kvb)[0:64, 0:64], lhsT=KZ[:, n, 0:64], rhs=VT[:, n, 0:64], start=True, stop=True)
                    return e.matmul(PS(kvb)[64:128, 0:64], lhsT=KZ[:, n, 64:128], rhs=VT[:, n, 64:128],
                                    start=True, stop=True)
                if DEBUG.get("kv", 1):
                    S.op("pe", mm_kv, r=[("KZ", n // 4), ("VT", n // 4)], w=[("ps", kvb)])

                def mm_o(e, n=n, sb=sb, ck=ck):
                    oc = slice((n % 4) * 128, (n % 4 + 1) * 128)
                    st = n % 2
                    ins = None
                    for hh in range(2):
                        hs = slice(hh * 64, hh * 64 + 64)
                        ins = e.matmul(PS(4 + hh)[hs, oc], lhsT=VT[:, n, hs], rhs=SD[:, sb, hh * 128:(hh + 1) * 128],
                                       start=True, stop=(n == 0))
                        if n > 0:
                            ins = e.matmul(PS(4 + hh)[hs, oc], lhsT=STB[hs, st, :], rhs=QX[hs, ck], start=False, stop=True)
                    return ins
                rk = [("SD", sb), ("VT", n // 4), ("QX", g)] + ([("STB", n % 2)] if n > 0 else [])
                if DEBUG.get("mo", 1):
                    S.op("pe", mm_o, r=rk, w=[("ps", 4), ("ps", 5)])
                if n < 15 and DEBUG.get("su", 1):
                    S.op("dve", lambda e, kvb=kvb: e.scalar_tensor_tensor(
                        out=ST32, in0=ST32, scalar=CD[:, p:p + 1], in1=PS(kvb)[:, 0:64], op0=ALU.mult, op1=ALU.add),
                        r=[("ps", kvb), ("ST32",), ("HF",)], w=[("ST32",)])
                    S.op("act", lambda e, n=n: e.activation(out=STB[:, (n + 1) % 2, :], in_=ST32, func=AF.Copy),
                         r=[("ST32",)], w=[("STB", (n + 1) % 2)])
                if n % 4 == 3:
                    tok = slice(g * 512, (g + 1) * 512)
                    S.op("act", lambda e, tok=tok: e.activation(out=R32[0:64, tok], in_=PS(4)[0:64, :], func=AF.Copy),
                         r=[("ps", 4)], w=[("R32a", g)])
                    S.op("act", lambda e, tok=tok: e.activation(out=R32[64:128, tok], in_=PS(5)[64:128, :], func=AF.Copy),
                         r=[("ps", 5)], w=[("R32", g)])
            for g in range(4):
                tok = slice(g * 512, (g + 1) * 512)
                S.op("act", lambda e, tok=tok: e.activation(out=SQH, in_=R32[:, tok], func=AF.Square),
                     r=[("R32", g), ("R32a", g)], w=[("SQH",)])
                rstd_from_sq(SQH, 1, RS, 1.0 / 64, False, [("SQH",)], ("RS",), lhs=BLK)
                S.op("dve", lambda e, tok=tok: e.tensor_tensor(out=TMP[:, 0, :], in0=R32[:, tok], in1=RS, op=ALU.mult),
                     r=[("R32", g), ("R32a", g), ("RS",)], w=[("TMP", 0)])
                S.op("pool", lambda e, tok=tok: e.tensor_tensor(out=MIX[:, 4 + p, tok], in0=TMP[:, 0, :], in1=GT[:, tok],
                                                                op=ALU.mult),
                     r=[("TMP", 0), ("GT", g)], w=[("MIX", 4 + p, 0, g), ("MIX", 4 + p, 1, g)])

        for p in range(DEBUG.get("ndswa", 4)):
            dswa_pair(p)
        barrier()
        for p in range(DEBUG.get("nret", 4)):
            ret_pair(p)
        barrier()
        if DEBUG.get("dump_mix"):
            for g in range(4):
                S.op("act", lambda e, g=g: e.activation(out=X[:, :, g * 512:(g + 1) * 512], in_=MIX[:, :, g * 512:(g + 1) * 512],
                                                        func=AF.Copy),
                     r=[("MIX", j, hh, g) for j in range(8) for hh in range(2)] + [("X", g)], w=[("X", g)])
            return
        cvo = Carver(pair_off)
        FB = cvo.take(F32, [128, 8, 512])
        SQ = cvo.take(BF16, [128, 8, 512])
        WDS = cvo.take(BF16, [128, 2, 8, 128])
        for g in range(4):
            tok = slice(g * 512, (g + 1) * 512)
            proj_postnorm(lambda j, tok=tok: MIX[:, j, tok],
                          [("MIX", j, hh, g) for j in range(8) for hh in range(2)], 8, woutv, gi + 1, g, False,
                          FB, SQ, RS, WDS)

    phases = []
    for l in layers:
        phases += [("ffn", l, 0), ("mix", l), ("ffn", l, 1)]
    if stop_after is not None:
        phases = phases[:stop_after]
    idx = 0
    while idx < len(phases):
        ph = phases[idx]
        if ph[0] == "ffn":
            items = [(ph[1], ph[2])]
            while idx + 1 < len(phases) and phases[idx + 1][0] == "ffn":
                idx += 1
                items.append((phases[idx][1], phases[idx][2]))
            ffn_seq(items)
        else:
            l = ph[1]
            if DEBUG.get("skipmix"):
                pass
            elif l % 2 == 0:
                hybrid(l)
            else:
                gmlp(l)
        barrier()
        idx += 1

    for g in range(4):
        S.op("sp", lambda e, g=g: e.dma_start(out=yv[:, :, g * 512:(g + 1) * 512], in_=X[:, :, g * 512:(g + 1) * 512]),
             r=[("X", g)], w=[("Y", g)], dma=("dma", "Y", g))
    S.op("sp", None, r=[("Y", g) for g in range(4)])
    S.finalize()
    S.emit()
    es.close()
    return nc, S


_CACHE = {}


def _host_consts():
    if "c" not in _CACHE:
        ca, sa, cr, sr = _rope_tables()
        cb, hb, hf = _consts()
        _CACHE["c"] = dict(ropeA_c=ca, ropeA_s=sa, ropeR_c=cr, ropeR_s=sr, cb=cb, hb=hb, hf=hf)
        _CACHE["cols"] = _hyb_cols()
    return _CACHE["c"], _CACHE["cols"]


def prepare_inputs(inputs):
    consts, cols = _host_consts()
    f = lambda a: np.ascontiguousarray(np.asarray(a, dtype=np.float32))
    ng = f(inputs["norm_g"])
    g_all = np.ascontiguousarray(ng.reshape(4, 6, 8, 128).transpose(3, 0, 1, 2).reshape(128, 192))
    shared = dict(
        g_all=g_all,
        ffn_w_gate=f(inputs["ffn_w_gate"]), ffn_w_up=f(inputs["ffn_w_up"]), ffn_w_down=f(inputs["ffn_w_down"]),
        hyb_w_in_ext=np.ascontiguousarray(f(inputs["hyb_w_in"])[:, :, cols]),
        hyb_w_out=f(inputs["hyb_w_out"]),
        gmlp_w_in=f(inputs["gmlp_w_in"]), gmlp_w_out=f(inputs["gmlp_w_out"]),
        gmlp_ln_g=f(inputs["gmlp_ln_g"]), gmlp_ln_b=f(inputs["gmlp_ln_b"]),
        gmlp_w_sT=np.ascontiguousarray(f(inputs["gmlp_w_s"]).transpose(0, 3, 1, 2).reshape(2, 128, 1024)),
        gmlp_b_s=np.ascontiguousarray(f(inputs["gmlp_b_s"]).reshape(2, 1024)),
        **consts,
    )
    return shared


def kernel(**inputs):
    x = np.asarray(inputs["x"], dtype=np.float32)
    shared = prepare_inputs(inputs)
    if "nc" not in _CACHE:
        _CACHE["nc"] = build_program([0, 1, 2, 3])[0]
    nc = _CACHE["nc"]
    in_maps = []
    for b in range(8):
        m = dict(shared)
        m["xT"] = np.ascontiguousarray(x[b].T)
        in_maps.append(m)
    res = run_bass_kernel_spmd(nc, in_maps, core_ids=list(range(8)))
    out = np.stack([np.ascontiguousarray(res.results[b]["yT"].T) for b in range(8)], axis=0)
    return out.astype(np.float32)
```

```python
import math
from contextlib import ExitStack
import numpy as np
import concourse.bass as bass
import concourse.mybir as mybir
from concourse.bass_utils import run_bass_kernel_spmd

F32 = mybir.dt.float32
BF16 = mybir.dt.bfloat16
U8 = mybir.dt.uint8
AF = mybir.ActivationFunctionType
ALU = mybir.AluOpType

T = 2048
D = 1024
FF = 2816
NJ = 22
EPS = 1e-6
DEPTH = 4
NEXT = 4 * 640 + 4 * 768
DEBUG = {}


class Sched:
    ENGS = ("pe", "act", "dve", "pool", "sp")

    def __init__(self, nc):
        self.nc = nc
        self.ops = []
        self.pools = {}
        self.phase = 0

    def barrier(self, fn):
        self.ops.append(dict(eng="dve", fn=fn, r=(), w=(("PH",),), dma=None, late=None, ndma=1))
        self.phase += 1

    def op(self, eng, fn, r=(), w=(), dma=None, ndma=1):
        o = dict(eng=eng, fn=fn, r=tuple(r) + (("PH",),), w=tuple(w), dma=dma, late=None, ndma=ndma)
        self.ops.append(o)
        return o

    def consume(self, pool, nslots, eng, load_fn_of_slot, extra_r=(), ndma=1, lookahead=None):
        p = self.pools.setdefault((pool, self.phase), dict(n=0, nslots=nslots, marks=[]))
        i = p["n"]
        p["n"] += 1
        slot = i % nslots
        key = (pool, slot)
        mark = dict(eng=None, fn=None, r=(), w=(), dma=None, late=[])
        self.ops.append(mark)
        p["marks"].append(mark)
        dop = dict(eng=eng, fn=(lambda e, s=slot: load_fn_of_slot(e, s)), r=tuple(extra_r) + (("PH",),),
                   w=(key,), dma=("dma",) + key, late=None, ndma=ndma)
        la = (nslots - 1) if lookahead is None else lookahead
        tgt = p["marks"][max(0, i - la)]
        tgt["late"].append(dop)
        return slot, key

    def finalize(self):
        out = []
        for o in self.ops:
            if o["late"] is not None:
                out.extend(o["late"])
            else:
                out.append(o)
        self.ops = ops = out
        last_w = {}
        readers = {}
        for i, o in enumerate(ops):
            deps = {}
            for k in o["r"]:
                if k in last_w:
                    deps[last_w[k]] = True
            for k in o["w"]:
                if k in last_w:
                    deps.setdefault(last_w[k], False)
                for rd in readers.get(k, ()):
                    deps.setdefault(rd, False)
            deps.pop(i, None)
            o["deps"] = deps
            for k in o["r"]:
                readers.setdefault(k, []).append(i)
            for k in o["w"]:
                last_w[k] = i
                readers[k] = []
        for o in ops:
            o["signal"] = False
            o["need"] = []
        for o in ops:
            for d, raw in sorted(o["deps"].items()):
                p = ops[d]
                if p["dma"] is not None:
                    o["need"].append(d)
                elif p["eng"] == o["eng"]:
                    if o["eng"] == "pe":
                        continue
                    if raw or o["dma"] is not None:
                        p["signal"] = True
                        o["need"].append(d)
                else:
                    p["signal"] = True
                    o["need"].append(d)
        cnt = {e: 0 for e in self.ENGS}
        for o in ops:
            if o["dma"] is None and o["signal"]:
                cnt[o["eng"]] += 1
                o["sval"] = cnt[o["eng"]]
        self.stats = dict(cnt)
        self.dma_keys = sorted({o["dma"] for o in ops if o["dma"] is not None}, key=str)

    def emit(self):
        nc = self.nc
        ops = self.ops
        with ExitStack() as es:
            psem = {e: es.enter_context(nc.semaphore("p_" + e)) for e in self.ENGS}
            dsem = {k: es.enter_context(nc.semaphore("d_" + "_".join(str(x) for x in k[1:])))
                    for k in self.dma_keys}
            dtot = {k: 0 for k in self.dma_keys}
            for o in ops:
                if o["dma"] is not None:
                    dtot[o["dma"]] += 16 * o["ndma"]
                    o["dval"] = dtot[o["dma"]]
            block = es.enter_context(nc.Block())

            def run(ename, eng):
                waited = {}
                for o in ops:
                    if o["eng"] != ename:
                        continue
                    grp = {}
                    for d in o["need"]:
                        p = ops[d]
                        if p["dma"] is not None:
                            key, val, sem = ("d", p["dma"]), p["dval"], dsem[p["dma"]]
                        else:
                            key, val, sem = ("p", p["eng"]), p["sval"], psem[p["eng"]]
                        if key not in grp or grp[key][0] < val:
                            grp[key] = (val, sem)
                    for key, (val, sem) in grp.items():
                        if waited.get(key, 0) >= val:
                            continue
                        waited[key] = val
                        eng.wait_ge(sem, val)
                    if o["fn"] is None:
                        continue
                    ins = o["fn"](eng)
                    if o["dma"] is not None:
                        lst = ins if isinstance(ins, (list, tuple)) else [ins]
                        assert len(lst) == o["ndma"]
                        for x in lst:
                            x.then_inc(dsem[o["dma"]], 16)
                    elif o["signal"]:
                        ins.then_inc(psem[ename], 1)

            block.tensor(lambda e: run("pe", e))
            block.scalar(lambda e: run("act", e))
            block.vector(lambda e: run("dve", e))
            block.gpsimd(lambda e: run("pool", e))
            block.sync(lambda e: run("sp", e))


def _rope_tables():
    t = np.arange(T, dtype=np.float32)
    ca = np.ones((128, T), np.float32)
    sa = np.zeros((128, T), np.float32)
    half = 8
    inv = (np.float32(500000.0) ** (-(np.arange(half, dtype=np.float32) * 2.0 / 16))).astype(np.float32)
    ang = t[None, :] * inv[:, None]
    for hh in range(2):
        b = hh * 64
        ca[b:b + half] = np.cos(ang)
        ca[b + half:b + 2 * half] = np.cos(ang)
        sa[b:b + half] = -np.sin(ang)
        sa[b + half:b + 2 * half] = np.sin(ang)
    cr = np.zeros((128, T), np.float32)
    sr = np.zeros((128, T), np.float32)
    half = 32
    inv = (np.float32(10000.0) ** (-(np.arange(half, dtype=np.float32) * 2.0 / 64))).astype(np.float32)
    ang = t[None, :] * inv[:, None]
    for hh in range(2):
        b = hh * 64
        cr[b:b + half] = np.cos(ang)
        cr[b + half:b + 64] = np.cos(ang)
        sr[b:b + half] = -np.sin(ang)
        sr[b + half:b + 64] = np.sin(ang)
    return ca, sa, cr, sr


def _consts():
    idx = np.arange(128)
    k = idx[:, None]
    q = idx[None, :]
    cur = (k <= q).astype(np.float32)
    prev = (k >= q).astype(np.float32)
    m_n = np.concatenate([cur, prev], axis=1)
    m_f = np.concatenate([cur, np.zeros_like(prev)], axis=1)
    m_fnnn = np.concatenate([m_f, m_n, m_n, m_n], axis=1)
    ones = np.ones((128, 128), np.float32)
    blk = np.zeros((128, 128), np.float32)
    blk[:64, :64] = 1
    blk[64:, 64:] = 1
    ident = np.eye(128, dtype=np.float32)
    cb = np.concatenate([ones, blk, ident], axis=1)
    hb = np.concatenate([m_n, m_fnnn], axis=1)
    h = np.arange(8, dtype=np.float64)
    log_g = np.log(1.0 - np.exp2(-5.0 - h))
    diff = (q - k).astype(np.float64)
    dm = np.zeros((128, 8, 128), np.float64)
    for hh in range(8):
        dm[:, hh, :] = np.where(diff >= 0, np.exp(log_g[hh] * np.maximum(diff, 0)), 0.0) / 8.0
    xi = np.zeros((128, 4, 128), np.float64)
    zt = np.zeros((128, 4, 128), np.float64)
    cd = np.zeros((128, 8), np.float64)
    for p in range(128):
        for pr in range(4):
            hh = pr * 2 + p // 64
            xi[p, pr, :] = np.exp(log_g[hh] * (idx + 1.0))
            cd[p, pr] = np.exp(log_g[hh] * 128.0)
    for col in range(128):
        for pr in range(4):
            hh = pr * 2 + col // 64
            zt[:, pr, col] = np.exp(log_g[hh] * (127.0 - idx)) / 8.0
    hf = np.concatenate([dm.reshape(128, -1), xi.reshape(128, -1), zt.reshape(128, -1), cd],
                        axis=1).astype(np.float32)
    return cb.astype(np.float32), hb.astype(np.float32), hf


def _hyb_cols():
    cols = []
    def perm(base, half, rot):
        out = []
        for hh in range(2):
            for j in range(64):
                if j < half:
                    jj = j + half
                elif j < 2 * half:
                    jj = j - half
                else:
                    jj = j
                out.append(base + hh * 64 + jj)
        return out
    for p in range(4):
        qa = 0 + p * 128
        ka = 512 + p * 128
        va = 1024 + p * 128
        cols += list(range(qa, qa + 128)) + perm(qa, 8, 16)
        cols += list(range(ka, ka + 128)) + perm(ka, 8, 16)
        cols += list(range(va, va + 128))
    for p in range(4):
        qr = 1536 + p * 128
        kr = 2048 + p * 128
        vr = 2560 + p * 128
        gr = 3072 + p * 128
        cols += list(range(qr, qr + 128)) + perm(qr, 32, 64)
        cols += list(range(kr, kr + 128)) + perm(kr, 32, 64)
        cols += list(range(gr, gr + 128))
        cols += list(range(vr, vr + 128))
    assert len(cols) == NEXT
    return np.array(cols, dtype=np.int64)


def ssl(st, cnt, d=1):
    return slice(st, st + (cnt - 1) * d + 1, d)


def build_program(layers, stop_after=None):
    nc = bass.Bass("TRN2", target_bir_lowering=False)

    def din(name, shape):
        return nc.dram_tensor(name, list(shape), F32, kind="ExternalInput").ap()

    xT = din("xT", [D, T])
    yT = nc.dram_tensor("yT", [D, T], F32, kind="ExternalOutput").ap()
    g_all = din("g_all", [128, 192])
    wg_d = din("ffn_w_gate", [DEPTH, 2, D, FF])
    wu_d = din("ffn_w_up", [DEPTH, 2, D, FF])
    wd_d = din("ffn_w_down", [DEPTH, 2, FF, D])
    hin_d = din("hyb_w_in_ext", [2, D, NEXT])
    hout_d = din("hyb_w_out", [2, D, D])
    gin_d = din("gmlp_w_in", [2, D, 2 * D])
    gout_d = din("gmlp_w_out", [2, D, D])
    glng_d = din("gmlp_ln_g", [2, D])
    glnb_d = din("gmlp_ln_b", [2, D])
    gws_d = din("gmlp_w_sT", [2, 128, 8 * 128])
    gbs_d = din("gmlp_b_s", [2, 8 * 128])
    ropeA_c = din("ropeA_c", [128, T])
    ropeA_s = din("ropeA_s", [128, T])
    ropeR_c = din("ropeR_c", [128, T])
    ropeR_s = din("ropeR_s", [128, T])
    cb_d = din("cb", [128, 384])
    hb_d = din("hb", [128, 1280])
    hf_d = din("hf", [128, 2056])

    es = ExitStack()
    X = es.enter_context(nc.sbuf_tensor("X", [128, 8, T], F32))
    G = es.enter_context(nc.sbuf_tensor("G", [128, 192], F32))
    CB = es.enter_context(nc.sbuf_tensor("CB", [128, 384], BF16))
    MASKC = es.enter_context(nc.sbuf_tensor("MASKC", [128, 128], BF16))
    EPSB = es.enter_context(nc.sbuf_tensor("EPSB", [128, 4], F32))
    SCRB = 142 * 1024
    SCR = es.enter_context(nc.sbuf_tensor("SCR", [128, SCRB], U8))
    PSALL = es.enter_context(nc.psum_tensor("PSALL", [128, 4096], F32))
    ONES = CB[:, 0:128]
    BLK = CB[:, 128:256]
    IDENT = CB[:, 256:384]

    def PS(b, n=1):
        return PSALL[:, b * 512:(b + n) * 512]

    class Carver:
        def __init__(self, off=0):
            self.off = off

        def take(self, dtype, shape):
            n = 1
            for s_ in shape[1:]:
                n *= s_
            nb = n * (4 if dtype == F32 else 2)
            nb = (nb + 31) // 32 * 32
            ap = SCR[:, self.off:self.off + nb].bitcast(dtype)[:, 0:n]
            if len(shape) == 3:
                ap = ap.rearrange("p (a b) -> p a b", a=shape[1])
            elif len(shape) == 4:
                ap = ap.rearrange("p (a b c) -> p a b c", a=shape[1], b=shape[2])
            self.off += nb
            assert self.off <= SCRB, (self.off, SCRB)
            return ap

    S = Sched(nc)

    def barrier():
        S.barrier(lambda e: e.memset(EPSB[:, 2:3], 0.0))

    S.op("sp", lambda e: e.dma_start(out=G[:], in_=g_all), w=[("G",)], dma=("dma", "G"))
    S.op("pool", lambda e: e.dma_start(out=CB[:], in_=cb_d), w=[("CB",)], dma=("dma", "CB"))
    S.op("pool", lambda e: e.dma_start(out=MASKC[:], in_=hb_d[:, 0:128]), w=[("MASKC",)], dma=("dma", "MASKC"))
    S.op("dve", lambda e: e.memset(EPSB[:, 0:1], EPS), w=[("EPSB0",)])
    S.op("dve", lambda e: e.memset(EPSB[:, 1:2], math.log(0.5)), r=[("EPSB0",)], w=[("EPSB",)])
    xv = xT.rearrange("(c p) t -> p c t", p=128)
    yv = yT.rearrange("(c p) t -> p c t", p=128)
    for g in range(4):
        S.op("sp", lambda e, g=g: e.dma_start(out=X[:, :, g * 512:(g + 1) * 512],
                                              in_=xv[:, :, g * 512:(g + 1) * 512]),
             w=[("X", g)], dma=("dma", "X", g))

    def rstd_from_sq(sq_ap, nk, rs_ap, inv_n, ln_half, keys_r, key_rs, lhs=None, bank=6):
        lhs = ONES if lhs is None else lhs

        def mm(e):
            ins = None
            for k in range(nk):
                src = sq_ap[:, k, :] if nk > 1 else sq_ap
                ins = e.matmul(PS(bank), lhsT=lhs, rhs=src, start=(k == 0), stop=(k == nk - 1))
            return ins
        S.op("pe", mm, r=list(keys_r) + [("CB",)], w=[("ps", bank)])
        S.op("act", lambda e: e.activation(out=rs_ap, in_=PS(bank), func=AF.Ln, scale=inv_n, bias=EPSB[:, 0:1]),
             r=[("ps", bank), ("EPSB",)], w=[key_rs])
        if ln_half:
            S.op("act", lambda e: e.activation(out=rs_ap, in_=rs_ap, func=AF.Exp, scale=-0.5, bias=EPSB[:, 1:2]),
                 r=[key_rs, ("EPSB",)], w=[key_rs])
        else:
            S.op("act", lambda e: e.activation(out=rs_ap, in_=rs_ap, func=AF.Exp, scale=-0.5),
                 r=[key_rs], w=[key_rs])

    def prenorm(gi, g, dst_fn, dst_key, SQ, RS, stage="ab", ksq=("SQ",), krs=("RS",), bank=6):
        tok = slice(g * 512, (g + 1) * 512)
        if "a" in stage:
            S.op("act", lambda e: e.activation(out=SQ, in_=X[:, :, tok], func=AF.Square),
                 r=[("X", g)], w=[ksq])
        if "b" not in stage:
            return
        rstd_from_sq(SQ, 8, RS, 1.0 / D, False, [ksq], krs, bank=bank)
        for k in range(8):
            S.op("dve", lambda e, k=k: e.scalar_tensor_tensor(
                out=dst_fn(k), in0=X[:, k, tok], scalar=G[:, gi * 8 + k:gi * 8 + k + 1], in1=RS,
                op0=ALU.mult, op1=ALU.mult),
                r=[("X", g), krs, ("G",)], w=[dst_key(k)])

    def proj_postnorm(src_fn, src_keys, nk, w_view, gi, g, half_factor, FB, SQ, RS, WDS):
        tok = slice(g * 512, (g + 1) * 512)
        for c in range(8):
            slot, key = S.consume("WD", 2, "pool",
                                  lambda e, s, c=c: e.dma_start(out=WDS[:, s, 0:nk, :],
                                                                in_=w_view[:, :, c * 128:(c + 1) * 128]))
            b = 4 + c % 2

            def mm(e, slot=slot, b=b):
                ins = None
                for j in range(nk):
                    ins = e.matmul(PS(b), lhsT=WDS[:, slot, j, :], rhs=src_fn(j), start=(j == 0), stop=(j == nk - 1))
                return ins
            S.op("pe", mm, r=[key] + list(src_keys), w=[("ps", b)])
            S.op("act", lambda e, c=c, b=b: e.activation(out=FB[:, c, :], in_=PS(b), func=AF.Copy),
                 r=[("ps", b)], w=[("F", c)])
        S.op("act", lambda e: e.activation(out=SQ, in_=FB, func=AF.Square),
             r=[("F", c) for c in range(8)], w=[("SQ",)])
        rstd_from_sq(SQ, 8, RS, 1.0 / D, half_factor, [("SQ",)], ("RS",))
        for c in range(8):
            S.op("dve", lambda e, c=c: e.scalar_tensor_tensor(
                out=FB[:, c, :], in0=FB[:, c, :], scalar=G[:, gi * 8 + c:gi * 8 + c + 1], in1=RS,
                op0=ALU.mult, op1=ALU.mult), r=[("F", c), ("RS",), ("G",)], w=[("F", c)])
        S.op("pool", lambda e: e.tensor_tensor(out=X[:, :, tok], in0=X[:, :, tok], in1=FB, op=ALU.add),
             r=[("F", c) for c in range(8)] + [("X", g)], w=[("X", g)])

    def ffn_seq(items):
        cv = Carver()
        H = cv.take(BF16, [128, 8, 1024])
        ACTB = cv.take(BF16, [128, NJ, 1024])
        FB = cv.take(F32, [128, 8, 1024])
        SQ = cv.take(BF16, [128, 8, 512])
        RS = cv.take(F32, [128, 512])
        SQ2 = cv.take(BF16, [128, 8, 512])
        RS2 = cv.take(F32, [128, 512])
        SG = cv.take(F32, [128, 2, 512])
        WGU = cv.take(BF16, [128, 3, 16, 128])
        WDS = cv.take(BF16, [128, 2, NJ, 128])
        halves = [(l, i, hf) for (l, i) in items for hf in range(2)]

        def gidx(l, i):
            return l * 6 + (0 if i == 0 else 4)

        def pre(hv, stage, tts):
            l, i, hf = hv
            for tt in tts:
                prenorm(gidx(l, i), hf * 2 + tt, lambda k, tt=tt: H[:, k, tt * 512:(tt + 1) * 512],
                        lambda k, tt=tt: ("H", tt), SQ2, RS2, stage=stage, ksq=("SQ2",), krs=("RS2",), bank=7)

        def phase1(hv):
            l, i, hf = hv
            wgv = wg_d[l, i].rearrange("(k p) n -> p k n", p=128)
            wuv = wu_d[l, i].rearrange("(k p) n -> p k n", p=128)
            for j in range(NJ):
                def ld(e, s, j=j):
                    a_ = e.dma_start(out=WGU[:, s, 0:8, :], in_=wgv[:, :, j * 128:(j + 1) * 128])
                    b_ = e.dma_start(out=WGU[:, s, 8:16, :], in_=wuv[:, :, j * 128:(j + 1) * 128])
                    return [a_, b_]
                slot, key = S.consume("WGU", 3, "pool", ld, ndma=2)
                for tt in range(2):
                    bg, bu = tt, 2 + tt

                    def mm(e, slot=slot, tt=tt, bg=bg, bu=bu):
                        ins = None
                        for k in range(8):
                            ins = e.matmul(PS(bg), lhsT=WGU[:, slot, k, :], rhs=H[:, k, tt * 512:(tt + 1) * 512],
                                           start=(k == 0), stop=(k == 7))
                        for k in range(8):
                            ins = e.matmul(PS(bu), lhsT=WGU[:, slot, 8 + k, :], rhs=H[:, k, tt * 512:(tt + 1) * 512],
                                           start=(k == 0), stop=(k == 7))
                        return ins
                    S.op("pe", mm, r=[key, ("H", tt)], w=[("ps", bg), ("ps", bu)])
                    S.op("act", lambda e, tt=tt, bg=bg: e.activation(out=SG[:, tt, :], in_=PS(bg), func=AF.Silu),
                         r=[("ps", bg)], w=[("SG", tt)])
                    S.op("dve", lambda e, bu=bu, j=j, tt=tt: e.tensor_tensor(
                        out=ACTB[:, j, tt * 512:(tt + 1) * 512], in0=SG[:, tt, :], in1=PS(bu), op=ALU.mult),
                        r=[("SG", tt), ("ps", bu)], w=[("A", j, tt)])

        def phase2(hv, hook):
            l, i, hf = hv
            wdv = wd_d[l, i].rearrange("(j p) n -> p j n", p=128)
            gi = gidx(l, i) + 1
            for c in range(8):
                slot, key = S.consume("WD", 2, "pool",
                                      lambda e, s, c=c: e.dma_start(out=WDS[:, s, :, :],
                                                                    in_=wdv[:, :, c * 128:(c + 1) * 128]))
                for tt in range(2):
                    b = 4 + tt

                    def mm(e, slot=slot, b=b, tt=tt):
                        ins = None
                        for j in range(NJ):
                            ins = e.matmul(PS(b), lhsT=WDS[:, slot, j, :], rhs=ACTB[:, j, tt * 512:(tt + 1) * 512],
                                           start=(j == 0), stop=(j == NJ - 1))
                        return ins
                    S.op("pe", mm, r=[key] + [("A", j, tt) for j in range(NJ)], w=[("ps", b)])
                    S.op("act", lambda e, c=c, b=b, tt=tt: e.activation(out=FB[:, c, tt * 512:(tt + 1) * 512], in_=PS(b),
                                                                         func=AF.Copy),
                         r=[("ps", b)], w=[("F", c, tt)])
                if c == 0 and hook is not None:
                    hook()
            for tt in range(2):
                g = hf * 2 + tt
                tok = slice(g * 512, (g + 1) * 512)
                fsl = slice(tt * 512, (tt + 1) * 512)
                fk = [("F", c, tt) for c in range(8)]
                S.op("act", lambda e, fsl=fsl: e.activation(out=SQ, in_=FB[:, :, fsl], func=AF.Square),
                     r=fk, w=[("SQ",)])
                rstd_from_sq(SQ, 8, RS, 1.0 / D, True, [("SQ",)], ("RS",))
                for c in range(8):
                    S.op("dve", lambda e, c=c, fsl=fsl: e.scalar_tensor_tensor(
                        out=FB[:, c, fsl], in0=FB[:, c, fsl], scalar=G[:, gi * 8 + c:gi * 8 + c + 1], in1=RS,
                        op0=ALU.mult, op1=ALU.mult), r=[("F", c, tt), ("RS",), ("G",)], w=[("F", c, tt)])
                S.op("pool", lambda e, tok=tok, fsl=fsl: e.tensor_tensor(out=X[:, :, tok], in0=X[:, :, tok], in1=FB[:, :, fsl],
                                                                         op=ALU.add),
                     r=fk + [("X", g)], w=[("X", g)])

        pre(halves[0], "ab", (0, 1))
        for idx, hv in enumerate(halves):
            phase1(hv)
            nxt = halves[idx + 1] if idx + 1 < len(halves) else None
            if nxt is not None:
                pre(nxt, "a", (0,))

                def hook(nxt=nxt):
                    pre(nxt, "b", (0,))
                    pre(nxt, "ab", (1,))
                phase2(hv, hook)
            else:
                phase2(hv, None)

    def gmlp(l):
        jl = l // 2
        cv = Carver()
        H = cv.take(BF16, [128, 8, 512])
        UT = cv.take(BF16, [128, 8, 512])
        YT = cv.take(BF16, [128, 8, 512])
        VG = cv.take(F32, [128, 4, 1024])
        VN = cv.take(BF16, [128, 4, 1024])
        LNG = cv.take(F32, [128, 1024])
        LNB = cv.take(F32, [128, 1024])
        WST = cv.take(BF16, [128, 8, 128])
        BS = cv.take(F32, [128, 8, 128])
        WIN = cv.take(BF16, [128, 3, 8, 128])
        WV = cv.take(BF16, [128, 2, 8, 512])
        FB = cv.take(F32, [128, 8, 512])
        SQ = cv.take(BF16, [128, 8, 512])
        RS = cv.take(F32, [128, 512])
        Y1 = cv.take(F32, [128, 2, 512])
        ST = cv.take(F32, [128, 4, 16])
        MV = cv.take(F32, [128, 4, 2])
        RSD = cv.take(F32, [128, 4])
        WDS = cv.take(BF16, [128, 2, 8, 128])
        winv = gin_d[jl].rearrange("(k p) n -> p k n", p=128)
        woutv = gout_d[jl].rearrange("(j p) n -> p j n", p=128)
        gi = l * 6 + 2
        S.op("sp", lambda e: e.dma_start(out=LNG, in_=glng_d[jl:jl + 1, :].to_broadcast([128, D])),
             w=[("LNG",)], dma=("dma", "LNG"))
        S.op("sp", lambda e: e.dma_start(out=LNB, in_=glnb_d[jl:jl + 1, :].to_broadcast([128, D])),
             w=[("LNB",)], dma=("dma", "LNB"))
        S.op("sp", lambda e: e.dma_start(out=BS.rearrange("p a b -> p (a b)"),
                                         in_=gbs_d[jl:jl + 1, :].to_broadcast([128, D])),
             w=[("BS",)], dma=("dma", "BS"))
        S.op("pool", lambda e: e.dma_start(out=WST.rearrange("p a b -> p (a b)"), in_=gws_d[jl]),
             w=[("WST0",)], dma=("dma", "WST"))
        S.op("dve", lambda e: e.tensor_tensor(out=WST, in0=WST, in1=MASKC[:, None, :].to_broadcast([128, 8, 128]),
                                              op=ALU.mult),
             r=[("WST0",), ("MASKC",)], w=[("WST",)])
        SQ2 = cv.take(BF16, [128, 8, 512])
        RS2 = cv.take(F32, [128, 512])

        def pre(g):
            prenorm(gi, g, lambda k: H[:, k, :], lambda k: ("H",), SQ2, RS2, ksq=("SQ2",), krs=("RS2",), bank=7)

        pre(0)
        for g in range(4):
            for hv in range(2):
                slot, key = S.consume("GWV", 2, "pool",
                                      lambda e, s, hv=hv: e.dma_start(out=WV[:, s],
                                                                       in_=winv[:, :, D + hv * 512:D + (hv + 1) * 512]))
                for m in range(4):
                    b = 2 + (hv * 4 + m) % 2

                    def mm(e, slot=slot, b=b, m=m):
                        ins = None
                        for k in range(8):
                            ins = e.matmul(PS(b), lhsT=H[:, k, m * 128:(m + 1) * 128], rhs=WV[:, slot, k, :],
                                           start=(k == 0), stop=(k == 7))
                        return ins
                    S.op("pe", mm, r=[key, ("H",)], w=[("ps", b)])
                    S.op("act", lambda e, b=b, m=m, hv=hv: e.activation(
                        out=VG[:, m, hv * 512:(hv + 1) * 512], in_=PS(b), func=AF.Gelu),
                        r=[("ps", b)], w=[("VG", m, hv)])
                    S.op("dve", lambda e, m=m, hv=hv: e.bn_stats(out=ST[:, m, hv * 6:(hv + 1) * 6],
                                                                  in_=VG[:, m, hv * 512:(hv + 1) * 512]),
                         r=[("VG", m, hv)], w=[("STAT", m, hv)])
            for m in range(4):
                S.op("dve", lambda e, m=m: e.bn_aggr(out=MV[:, m, :], in_=ST[:, m, 0:12]),
                     r=[("STAT", m, 0), ("STAT", m, 1)], w=[("MV", m)])
            S.op("act", lambda e: e.activation(out=RSD, in_=MV[:, :, 1], func=AF.Ln, bias=EPSB[:, 0:1]),
                 r=[("MV", m) for m in range(4)] + [("EPSB",)], w=[("RSD",)])
            S.op("act", lambda e: e.activation(out=RSD, in_=RSD, func=AF.Exp, scale=-0.5),
                 r=[("RSD",)], w=[("RSD",)])
            for m in range(4):
                vk = [("VG", m, 0), ("VG", m, 1)]
                S.op("dve", lambda e, m=m: e.tensor_scalar(out=VG[:, m, :], in0=VG[:, m, :], scalar1=MV[:, m, 0:1],
                                                           scalar2=RSD[:, m:m + 1], op0=ALU.subtract, op1=ALU.mult),
                     r=vk + [("MV", m), ("RSD",)], w=vk)
                S.op("dve", lambda e, m=m: e.tensor_tensor(out=VG[:, m, :], in0=VG[:, m, :], in1=LNG, op=ALU.mult),
                     r=vk + [("LNG",)], w=vk)
                S.op("pool", lambda e, m=m: e.tensor_tensor(out=VN[:, m, :], in0=VG[:, m, :], in1=LNB, op=ALU.add),
                     r=vk + [("LNB",)], w=[("VN", m)])
            for ct in range(8):
                slot, key = S.consume("GWIN", 3, "pool",
                                      lambda e, s, ct=ct: e.dma_start(out=WIN[:, s], in_=winv[:, :, ct * 128:(ct + 1) * 128]))
                b = ct % 2

                def mm(e, slot=slot, b=b):
                    ins = None
                    for k in range(8):
                        ins = e.matmul(PS(b), lhsT=WIN[:, slot, k, :], rhs=H[:, k, :], start=(k == 0), stop=(k == 7))
                    return ins
                S.op("pe", mm, r=[key, ("H",)], w=[("ps", b)])
                S.op("act", lambda e, ct=ct, b=b: e.activation(out=UT[:, ct, :], in_=PS(b), func=AF.Gelu),
                     r=[("ps", b)], w=[("UT", ct)])
            for gg in range(8):
                b = 4 + gg % 2

                def mm(e, gg=gg, b=b):
                    ins = None
                    for m in range(4):
                        ins = e.matmul(PS(b)[:, m * 128:(m + 1) * 128], lhsT=VN[:, m, gg * 128:(gg + 1) * 128],
                                       rhs=WST[:, gg, :], start=True, stop=True)
                    return ins
                S.op("pe", mm, r=[("VN", m) for m in range(4)] + [("WST",)], w=[("ps", b)])
                u = gg % 2
                S.op("dve", lambda e, gg=gg, b=b, u=u: e.tensor_tensor(
                    out=Y1[:, u, :].rearrange("p (m i) -> p m i", m=4),
                    in0=PS(b).rearrange("p (m i) -> p m i", m=4),
                    in1=BS[:, gg:gg + 1, :].to_broadcast([128, 4, 128]), op=ALU.add),
                    r=[("ps", b), ("BS",)], w=[("Y1", u)])
                S.op("pool", lambda e, gg=gg, u=u: e.tensor_tensor(out=YT[:, gg, :], in0=Y1[:, u, :], in1=UT[:, gg, :],
                                                                   op=ALU.mult),
                     r=[("Y1", u), ("UT", gg)], w=[("YT", gg)])
            if g + 1 < 4:
                pre(g + 1)
            proj_postnorm(lambda j: YT[:, j, :], [("YT", j) for j in range(8)], 8, woutv, gi + 1, g, False,
                          FB, SQ, RS, WDS)

    def hybrid(l):
        jl = l // 2
        cv = Carver()
        HT = cv.take(BF16, [128, 8, T])
        MIX = cv.take(BF16, [128, 8, T])
        WIN = cv.take(BF16, [128, 6, 8, 128])
        ROPE = cv.take(F32, [128, 2, 2, 512])
        TMP = cv.take(F32, [128, 2, 512])
        RS = cv.take(F32, [128, 512])
        pair_off = cv.off
        winv = hin_d[jl].rearrange("(k p) n -> p k n", p=128)
        woutv = hout_d[jl].rearrange("(j p) n -> p j n", p=128)
        gi = l * 6 + 2
        SQ0 = Carver(pair_off).take(BF16, [128, 8, 512])
        for g in range(4):
            prenorm(gi, g, lambda k, g=g: HT[:, k, g * 512:(g + 1) * 512], lambda k, g=g: ("HT", g), SQ0, RS)
        barrier()
        HTK = [("HT", g) for g in range(4)]

        def win_block(col0):
            return S.consume("HWIN", 6, "pool",
                             lambda e, s, col0=col0: e.dma_start(out=WIN[:, s], in_=winv[:, :, col0:col0 + 128]),
                             lookahead=2)

        def rope_proj(col0, rc, rs_, dsts, fam):
            blks = [win_block(col0 + i * 128) for i in range(4)]
            for g in range(4):
                tok = slice(g * 512, (g + 1) * 512)

                def ldrope(e, s, g=g):
                    a = e.dma_start(out=ROPE[:, s, 0, :], in_=rc[:, g * 512:(g + 1) * 512])
                    b = e.dma_start(out=ROPE[:, s, 1, :], in_=rs_[:, g * 512:(g + 1) * 512])
                    return [a, b]
                rslot, rkey = S.consume("ROPE", 2, "sp", ldrope, ndma=2)
                for qi in range(2):
                    u = (g * 2 + qi) % 2
                    b0, b1 = 2 * u, 2 * u + 1
                    (s0, k0), (s1, k1) = blks[2 * qi], blks[2 * qi + 1]

                    def mm(e, s0=s0, s1=s1, b0=b0, b1=b1, tok=tok):
                        ins = None
                        for k in range(8):
                            ins = e.matmul(PS(b0), lhsT=WIN[:, s0, k, :], rhs=HT[:, k, tok], start=(k == 0), stop=(k == 7))
                        for k in range(8):
                            ins = e.matmul(PS(b1), lhsT=WIN[:, s1, k, :], rhs=HT[:, k, tok], start=(k == 0), stop=(k == 7))
                        return ins
                    S.op("pe", mm, r=[k0, k1, ("HT", g)], w=[("ps", b0), ("ps", b1)])
                    S.op("dve", lambda e, b0=b0, rslot=rslot: e.tensor_tensor(
                        out=TMP[:, 0, :], in0=PS(b0), in1=ROPE[:, rslot, 0, :], op=ALU.mult),
                        r=[("ps", b0), rkey], w=[("TMP", 0)])
                    S.op("dve", lambda e, b1=b1, rslot=rslot: e.tensor_tensor(
                        out=TMP[:, 1, :], in0=PS(b1), in1=ROPE[:, rslot, 1, :], op=ALU.mult),
                        r=[("ps", b1), rkey], w=[("TMP", 1)])
                    dst = dsts[qi]
                    S.op("dve", lambda e, dst=dst, tok=tok: e.tensor_tensor(
                        out=dst[:, tok], in0=TMP[:, 0, :], in1=TMP[:, 1, :], op=ALU.add),
                        r=[("TMP", 0), ("TMP", 1)], w=[("QK", fam, qi, g)])

        def dswa_pair(p):
            cvp = Carver(pair_off)
            HB = cvp.take(BF16, [128, 1280])
            QT = cvp.take(BF16, [128, T])
            KT = cvp.take(BF16, [128, T])
            VA = cvp.take(BF16, [128, 3, 16, 192])
            ACC = cvp.take(F32, [128, 2, T])
            E = cvp.take(BF16, [128, 2, 1024])
            M_N = HB[:, 0:256]
            M_F = HB[:, 256:1280]
            RC = TMP
            col0 = p * 640
            if p == 0:
                S.op("pool", lambda e: e.dma_start(out=HB, in_=hb_d), w=[("HB",)], dma=("dma", "HB"))
                S.op("pool", lambda e: e.memset(VA[:, :, :, 64:128], 1.0), w=[("VA1",)])
            rope_proj(col0, ropeA_c, ropeA_s, [QT, KT], "a")
            vs, vk = win_block(col0 + 512)
            for bi, d in enumerate((1, 4, 16)):
                tpc = 16 // d
                for i4 in range(4):
                    b = 4 + (bi * 4 + i4) % 2

                    def mm(e, b=b, d=d, i4=i4, tpc=tpc):
                        ins = None
                        for ii in range(4):
                            i = i4 * 4 + ii
                            r, blk = i // tpc, i % tpc
                            st = blk * 128 * d + r
                            for k in range(8):
                                ins = e.matmul(PS(b)[:, ii * 128:(ii + 1) * 128],
                                               lhsT=HT[:, k, ssl(st, 128, d)], rhs=WIN[:, vs, k, :],
                                               start=(k == 0), stop=(k == 7))
                        return ins
                    S.op("pe", mm, r=[vk] + HTK, w=[("ps", b)])
                    S.op("act", lambda e, b=b, bi=bi, i4=i4: e.activation(
                        out=VA[:, bi, i4 * 4:(i4 + 1) * 4, :].rearrange("p i (s c) -> p i s c", s=3)[:, :, 0:3:2, :],
                        in_=PS(b).rearrange("p (i s c) -> p i s c", i=4, s=2), func=AF.Copy),
                        r=[("ps", b), ("VA1",)], w=[("VA", bi, i4)])
            QKK = [("QK", "a", qi, g) for qi in range(2) for g in range(4)]
            unit = 0
            units = []
            for hh in range(2):
                hs = slice(hh * 64, hh * 64 + 64)
                vcols = slice(hh * 64, hh * 64 + 128)
                for bi, d in enumerate((1, 4, 16)):
                    tpc = 16 // d
                    for i4 in range(4):
                        su = unit % 2
                        unit += 1
                        sb0 = 2 * su
                        ob = 4 + su
                        blocks = []
                        for ii in range(4):
                            i = i4 * 4 + ii
                            r, n = i // tpc, i % tpc
                            blocks.append((i, n, n * 128 * d + r, (n - 1) * 128 * d + r))
                        noprev = (d == 16)
                        W = 128 if noprev else 256

                        def emit_s(blocks=blocks, sb0=sb0, d=d, hs=hs, noprev=noprev, W=W):
                            def mm_s(e):
                                ins = None
                                for ii, (i, n, st, pst) in enumerate(blocks):
                                    qsl = ssl(st, 128, d)
                                    ins = e.matmul(PS(sb0, 2)[:, ii * W:ii * W + 128], lhsT=KT[hs, qsl], rhs=QT[hs, qsl],
                                                   start=True, stop=True)
                                    if not noprev:
                                        ksl = qsl if n == 0 else ssl(pst, 128, d)
                                        ins = e.matmul(PS(sb0, 2)[:, ii * W + 128:ii * W + 256], lhsT=KT[hs, ksl],
                                                       rhs=QT[hs, qsl], start=True, stop=True)
                                return ins
                            S.op("pe", mm_s, r=QKK, w=[("ps", sb0), ("ps", sb0 + 1)])

                        def emit_rest(blocks=blocks, sb0=sb0, su=su, ob=ob, d=d, bi=bi, hh=hh, vcols=vcols,
                                      noprev=noprev, W=W):
                            S.op("act", lambda e: e.activation(
                                out=E[:, su, 0:4 * W], in_=PS(sb0, 2)[:, 0:4 * W], func=AF.Exp, scale=0.125),
                                r=[("ps", sb0), ("ps", sb0 + 1)], w=[("E", su)])
                            if noprev:
                                msk = M_N[:, None, 0:128].to_broadcast([128, 4, 128])
                                ev = E[:, su, 0:512].rearrange("p (a b) -> p a b", a=4)
                            elif blocks[0][1] == 0:
                                msk = M_F
                                ev = E[:, su, :]
                            else:
                                msk = M_N[:, None, :].to_broadcast([128, 4, 256])
                                ev = E[:, su, :].rearrange("p (a b) -> p a b", a=4)
                            S.op("dve", lambda e: e.tensor_tensor(out=ev, in0=ev, in1=msk, op=ALU.mult),
                                 r=[("E", su), ("HB",)], w=[("E", su)])

                            def mm_o(e):
                                ins = None
                                for ii, (i, n, st, pst) in enumerate(blocks):
                                    hasprev = (not noprev) and n > 0
                                    ins = e.matmul(PS(ob)[:, ii * 128:(ii + 1) * 128], lhsT=VA[:, bi, i, vcols],
                                                   rhs=E[:, su, ii * W:ii * W + 128], start=True, stop=not hasprev)
                                    if hasprev:
                                        ins = e.matmul(PS(ob)[:, ii * 128:(ii + 1) * 128], lhsT=VA[:, bi, i - 1, vcols],
                                                       rhs=E[:, su, ii * W + 128:ii * W + 256], start=False, stop=True)
                                return ins
                            S.op("pe", mm_o, r=[("E", su)] + [("VA", bi, x) for x in range(4)], w=[("ps", ob)])
                            if d == 16:
                                r0 = blocks[0][0]
                                dst = ACC[:, hh, :].rearrange("p (m r) -> p r m", r=16)[:, r0:r0 + 4, :]
                                src = PS(ob).rearrange("p (a m) -> p a m", a=4)
                            else:
                                dst = ACC[:, hh, ssl(blocks[0][2], 512, d)]
                                src = PS(ob)
                            if bi == 0:
                                S.op("dve", lambda e: e.tensor_copy(out=dst, in_=src),
                                     r=[("ps", ob)], w=[("ACC", hh)])
                            else:
                                S.op("dve", lambda e: e.tensor_tensor(out=dst, in0=dst, in1=src, op=ALU.add),
                                     r=[("ps", ob), ("ACC", hh)], w=[("ACC", hh)])
                        units.append((emit_s, emit_rest, hh, bi == 2 and i4 == 3))

            def normalise(hh):
                orow = slice(hh * 64, hh * 64 + 64)
                drow = slice(64 - hh * 64, 128 - hh * 64)
                for g in range(4):
                    tok = slice(g * 512, (g + 1) * 512)
                    u = g % 2
                    S.op("act", lambda e, tok=tok, u=u: e.activation(
                        out=RC[orow, u, :], in_=ACC[drow, hh, tok], func=AF.Ln), r=[("ACC", hh)], w=[("TMP", u)])
                    S.op("act", lambda e, u=u: e.activation(
                        out=RC[orow, u, :], in_=RC[orow, u, :], func=AF.Exp, scale=-1.0), r=[("TMP", u)], w=[("TMP", u)])
                    S.op("dve", lambda e, tok=tok, u=u: e.tensor_tensor(
                        out=MIX[orow, p, tok], in0=ACC[orow, hh, tok], in1=RC[orow, u, :], op=ALU.mult),
                        r=[("TMP", u), ("ACC", hh)], w=[("MIX", p, hh, g)])

            prev = None
            for un in units:
                un[0]()
                if prev is not None:
                    prev[1]()
                    if prev[3]:
                        normalise(prev[2])
                prev = un
            prev[1]()
            normalise(prev[2])

        def ret_pair(p):
            cvp = Carver(pair_off)
            HF = cvp.take(F32, [128, 2056])
            QT = cvp.take(BF16, [128, T])
            KT = cvp.take(BF16, [128, T])
            QX = cvp.take(BF16, [128, T])
            GT = cvp.take(BF16, [128, T])
            VT = cvp.take(BF16, [128, 16, 128])
            KZ = cvp.take(BF16, [128, 16, 128])
            R32 = cvp.take(F32, [128, T])
            ST32 = cvp.take(F32, [128, 64])
            STB = cvp.take(BF16, [128, 2, 64])
            SD = cvp.take(BF16, [128, 2, 256])
            SQH = cvp.take(BF16, [128, 512])
            DM = HF[:, 0:1024].rearrange("p (h c) -> p h c", h=8)
            XI = HF[:, 1024:1536].rearrange("p (a c) -> p a c", a=4)
            ZT = HF[:, 1536:2048].rearrange("p (a c) -> p a c", a=4)
            CD = HF[:, 2048:2056]
            col0 = 4 * 640 + p * 768
            if p == 0:
                S.op("sp", lambda e: e.dma_start(out=HF, in_=hf_d), w=[("HF",)], dma=("dma", "HF"))
            rope_proj(col0, ropeR_c, ropeR_s, [QT, KT], "r")
            for g in range(4):
                tok = slice(g * 512, (g + 1) * 512)
                S.op("pool", lambda e, tok=tok: e.tensor_tensor(
                    out=QX[:, tok].rearrange("p (a c) -> p a c", a=4), in0=QT[:, tok].rearrange("p (a c) -> p a c", a=4),
                    in1=XI[:, p:p + 1, :].to_broadcast([128, 4, 128]), op=ALU.mult),
                    r=[("QK", "r", 0, g), ("HF",)], w=[("QX", g)])
            gs, gk = win_block(col0 + 512)
            for g in range(4):
                tok = slice(g * 512, (g + 1) * 512)
                b = g % 2

                def mm(e, b=b, tok=tok):
                    ins = None
                    for k in range(8):
                        ins = e.matmul(PS(b), lhsT=WIN[:, gs, k, :], rhs=HT[:, k, tok], start=(k == 0), stop=(k == 7))
                    return ins
                S.op("pe", mm, r=[gk, ("HT", g)], w=[("ps", b)])
                S.op("act", lambda e, b=b, tok=tok: e.activation(out=GT[:, tok], in_=PS(b), func=AF.Silu),
                     r=[("ps", b)], w=[("GT", g)])
            vs, vk = win_block(col0 + 640)
            for i4 in range(4):
                b = 2 + i4 % 2

                def mm(e, b=b, i4=i4):
                    ins = None
                    for ii in range(4):
                        i = i4 * 4 + ii
                        for k in range(8):
                            ins = e.matmul(PS(b)[:, ii * 128:(ii + 1) * 128], lhsT=HT[:, k, i * 128:(i + 1) * 128],
                                           rhs=WIN[:, vs, k, :], start=(k == 0), stop=(k == 7))
                    return ins
                S.op("pe", mm, r=[vk] + HTK, w=[("ps", b)])
                S.op("act", lambda e, b=b, i4=i4: e.activation(
                    out=VT[:, i4 * 4:(i4 + 1) * 4, :], in_=PS(b).rearrange("p (i c) -> p i c", i=4), func=AF.Copy),
                    r=[("ps", b)], w=[("VT", i4)])
            PSB7 = PS(7).bitcast(BF16)
            for i4 in range(DEBUG.get("ntr", 4)):
                def tr(e, i4=i4):
                    ins = None
                    for ii in range(4):
                        i = i4 * 4 + ii
                        ins = e.transpose(PSB7[:, ii * 128:(ii + 1) * 128], KT[:, i * 128:(i + 1) * 128], IDENT)
                    return ins
                S.op("pe", tr, r=[("QK", "r", 1, i4), ("CB",)], w=[("ps", 7)])
                S.op("dve", lambda e, i4=i4: e.tensor_tensor(
                    out=KZ[:, i4 * 4:(i4 + 1) * 4, :], in0=PSB7[:, 0:512].rearrange("p (i c) -> p i c", i=4),
                    in1=ZT[:, p:p + 1, :].to_broadcast([128, 4, 128]), op=ALU.mult),
                    r=[("ps", 7), ("HF",)], w=[("KZ", i4)])
            S.op("dve", lambda e: e.memset(ST32, 0.0), w=[("ST32",)])
            def r_skv(n):
                g = n // 4
                ck = slice(n * 128, (n + 1) * 128)
                sb = n % 2
                b0 = 2 * sb
                kvb = 6 + n % 2

                def mm_s(e):
                    e.matmul(PS(b0)[:, 0:128], lhsT=KT[0:64, ck], rhs=QT[0:64, ck], start=True, stop=True)
                    return e.matmul(PS(b0 + 1)[:, 0:128], lhsT=KT[64:128, ck], rhs=QT[64:128, ck], start=True, stop=True)
                S.op("pe", mm_s, r=[("QK", "r", 0, g), ("QK", "r", 1, g)], w=[("ps", b0), ("ps", b0 + 1)])
                S.op("dve", lambda e: e.tensor_tensor(
                    out=SD[:, sb, :].rearrange("p (h c) -> p h c", h=2),
                    in0=PS(b0, 2).rearrange("p (h c) -> p h c", h=2)[:, :, 0:128], in1=DM[:, 2 * p:2 * p + 2, :],
                    op=ALU.mult),
                    r=[("ps", b0), ("ps", b0 + 1), ("HF",)], w=[("SD", sb)])

                def mm_kv(e):
                    e.matmul(PS(kvb)[0:64, 0:64], lhsT=KZ[:, n, 0:64], rhs=VT[:, n, 0:64], start=True, stop=True)
                    return e.matmul(PS(kvb)[64:128, 0:64], lhsT=KZ[:, n, 64:128], rhs=VT[:, n, 64:128],
                                    start=True, stop=True)
                if n < 15:
                    S.op("pe", mm_kv, r=[("KZ", n // 4), ("VT", n // 4)], w=[("ps", kvb)])

            def r_upd(n):
                kvb = 6 + n % 2
                if n < 15:
                    S.op("dve", lambda e: e.scalar_tensor_tensor(
                        out=ST32, in0=ST32, scalar=CD[:, p:p + 1], in1=PS(kvb)[:, 0:64], op0=ALU.mult, op1=ALU.add),
                        r=[("ps", kvb), ("ST32",), ("HF",)], w=[("ST32",)])
                    S.op("act", lambda e: e.activation(out=STB[:, (n + 1) % 2, :], in_=ST32, func=AF.Copy),
                         r=[("ST32",)], w=[("STB", (n + 1) % 2)])

            def r_out(n):
                g = n // 4
                ck = slice(n * 128, (n + 1) * 128)
                sb = n % 2

                def mm_o(e):
                    oc = slice((n % 4) * 128, (n % 4 + 1) * 128)
                    st = n % 2
                    ins = None
                    for hh in range(2):
                        hs = slice(hh * 64, hh * 64 + 64)
                        ins = e.matmul(PS(4 + hh)[hs, oc], lhsT=VT[:, n, hs], rhs=SD[:, sb, hh * 128:(hh + 1) * 128],
                                       start=True, stop=(n == 0))
                        if n > 0:
                            ins = e.matmul(PS(4 + hh)[hs, oc], lhsT=STB[hs, st, :], rhs=QX[hs, ck], start=False, stop=True)
                    return ins
                rk = [("SD", sb), ("VT", n // 4), ("QX", g)] + ([("STB", n % 2)] if n > 0 else [])
                S.op("pe", mm_o, r=rk, w=[("ps", 4), ("ps", 5)])
                if n % 4 == 3:
                    tok = slice(g * 512, (g + 1) * 512)
                    S.op("act", lambda e: e.activation(out=R32[0:64, tok], in_=PS(4)[0:64, :], func=AF.Copy),
                         r=[("ps", 4)], w=[("R32a", g)])
                    S.op("act", lambda e: e.activation(out=R32[64:128, tok], in_=PS(5)[64:128, :], func=AF.Copy),
                         r=[("ps", 5)], w=[("R32", g)])

            r_skv(0)
            r_upd(0)
            for n in range(16):
                if n + 1 < 16:
                    r_skv(n + 1)
                r_out(n)
                if n + 1 < 16:
                    r_upd(n + 1)
            for g in range(4):
                tok = slice(g * 512, (g + 1) * 512)
                S.op("act", lambda e, tok=tok: e.activation(out=SQH, in_=R32[:, tok], func=AF.Square),
                     r=[("R32", g), ("R32a", g)], w=[("SQH",)])
                rstd_from_sq(SQH, 1, RS, 1.0 / 64, False, [("SQH",)], ("RS",), lhs=BLK)
                S.op("dve", lambda e, tok=tok: e.tensor_tensor(out=TMP[:, 0, :], in0=R32[:, tok], in1=RS, op=ALU.mult),
                     r=[("R32", g), ("R32a", g), ("RS",)], w=[("TMP", 0)])
                S.op("pool", lambda e, tok=tok: e.tensor_tensor(out=MIX[:, 4 + p, tok], in0=TMP[:, 0, :], in1=GT[:, tok],
                                                                op=ALU.mult),
                     r=[("TMP", 0), ("GT", g)], w=[("MIX", 4 + p, 0, g), ("MIX", 4 + p, 1, g)])

        for p in range(DEBUG.get("ndswa", 4)):
            dswa_pair(p)
        barrier()
        for p in range(DEBUG.get("nret", 4)):
            ret_pair(p)
        barrier()
        if DEBUG.get("dump_mix"):
            for g in range(4):
                S.op("act", lambda e, g=g: e.activation(out=X[:, :, g * 512:(g + 1) * 512], in_=MIX[:, :, g * 512:(g + 1) * 512],
                                                        func=AF.Copy),
                     r=[("MIX", j, hh, g) for j in range(8) for hh in range(2)] + [("X", g)], w=[("X", g)])
            return
        cvo = Carver(pair_off)
        FB = cvo.take(F32, [128, 8, 512])
        SQ = cvo.take(BF16, [128, 8, 512])
        WDS = cvo.take(BF16, [128, 2, 8, 128])
        for g in range(4):
            tok = slice(g * 512, (g + 1) * 512)
            proj_postnorm(lambda j, tok=tok: MIX[:, j, tok],
                          [("MIX", j, hh, g) for j in range(8) for hh in range(2)], 8, woutv, gi + 1, g, False,
                          FB, SQ, RS, WDS)

    phases = []
    for l in layers:
        phases += [("ffn", l, 0), ("mix", l), ("ffn", l, 1)]
    if stop_after is not None:
        phases = phases[:stop_after]
    idx = 0
    while idx < len(phases):
        ph = phases[idx]
        if ph[0] == "ffn":
            items = [(ph[1], ph[2])]
            while idx + 1 < len(phases) and phases[idx + 1][0] == "ffn":
                idx += 1
                items.append((phases[idx][1], phases[idx][2]))
            ffn_seq(items)
        else:
            l = ph[1]
            if DEBUG.get("skipmix"):
                pass
            elif l % 2 == 0:
                hybrid(l)
            else:
                gmlp(l)
        barrier()
        idx += 1

    for g in range(4):
        S.op("sp", lambda e, g=g: e.dma_start(out=yv[:, :, g * 512:(g + 1) * 512], in_=X[:, :, g * 512:(g + 1) * 512]),
             r=[("X", g)], w=[("Y", g)], dma=("dma", "Y", g))
    S.op("sp", None, r=[("Y", g) for g in range(4)])
    S.finalize()
    S.emit()
    es.close()
    return nc, S


_CACHE = {}


def _host_consts():
    if "c" not in _CACHE:
        ca, sa, cr, sr = _rope_tables()
        cb, hb, hf = _consts()
        _CACHE["c"] = dict(ropeA_c=ca, ropeA_s=sa, ropeR_c=cr, ropeR_s=sr, cb=cb, hb=hb, hf=hf)
        _CACHE["cols"] = _hyb_cols()
    return _CACHE["c"], _CACHE["cols"]


def prepare_inputs(inputs):
    consts, cols = _host_consts()
    f = lambda a: np.ascontiguousarray(np.asarray(a, dtype=np.float32))
    ng = f(inputs["norm_g"])
    g_all = np.ascontiguousarray(ng.reshape(4, 6, 8, 128).transpose(3, 0, 1, 2).reshape(128, 192))
    shared = dict(
        g_all=g_all,
        ffn_w_gate=f(inputs["ffn_w_gate"]), ffn_w_up=f(inputs["ffn_w_up"]), ffn_w_down=f(inputs["ffn_w_down"]),
        hyb_w_in_ext=np.ascontiguousarray(f(inputs["hyb_w_in"])[:, :, cols]),
        hyb_w_out=f(inputs["hyb_w_out"]),
        gmlp_w_in=f(inputs["gmlp_w_in"]), gmlp_w_out=f(inputs["gmlp_w_out"]),
        gmlp_ln_g=f(inputs["gmlp_ln_g"]), gmlp_ln_b=f(inputs["gmlp_ln_b"]),
        gmlp_w_sT=np.ascontiguousarray(f(inputs["gmlp_w_s"]).transpose(0, 3, 1, 2).reshape(2, 128, 1024)),
        gmlp_b_s=np.ascontiguousarray(f(inputs["gmlp_b_s"]).reshape(2, 1024)),
        **consts,
    )
    return shared


def kernel(**inputs):
    x = np.asarray(inputs["x"], dtype=np.float32)
    shared = prepare_inputs(inputs)
    if "nc" not in _CACHE:
        _CACHE["nc"] = build_program([0, 1, 2, 3])[0]
    nc = _CACHE["nc"]
    in_maps = []
    for b in range(8):
        m = dict(shared)
        m["xT"] = np.ascontiguousarray(x[b].T)
        in_maps.append(m)
    res = run_bass_kernel_spmd(nc, in_maps, core_ids=list(range(8)))
    out = np.stack([np.ascontiguousarray(res.results[b]["yT"].T) for b in range(8)], axis=0)
    return out.astype(np.float32)
```

```python
import math
from contextlib import ExitStack
import numpy as np
import concourse.bass as bass
import concourse.mybir as mybir
from concourse.bass_utils import run_bass_kernel_spmd

F32 = mybir.dt.float32
BF16 = mybir.dt.bfloat16
U8 = mybir.dt.uint8
AF = mybir.ActivationFunctionType
ALU = mybir.AluOpType

T = 2048
D = 1024
FF = 2816
NJ = 22
EPS = 1e-6
DEPTH = 4
NEXT = 4 * 640 + 4 * 768
DEBUG = {}


class Sched:
    ENGS = ("pe", "act", "dve", "pool", "sp")

    def __init__(self, nc):
        self.nc = nc
        self.ops = []
        self.pools = {}
        self.phase = 0

    def barrier(self, fn):
        self.ops.append(dict(eng="dve", fn=fn, r=(), w=(("PH",),), dma=None, late=None, ndma=1))
        self.phase += 1

    def op(self, eng, fn, r=(), w=(), dma=None, ndma=1):
        o = dict(eng=eng, fn=fn, r=tuple(r) + (("PH",),), w=tuple(w), dma=dma, late=None, ndma=ndma)
        self.ops.append(o)
        return o

    def consume(self, pool, nslots, eng, load_fn_of_slot, extra_r=(), ndma=1, lookahead=None):
        p = self.pools.setdefault((pool, self.phase), dict(n=0, nslots=nslots, marks=[]))
        i = p["n"]
        p["n"] += 1
        slot = i % nslots
        key = (pool, slot)
        mark = dict(eng=None, fn=None, r=(), w=(), dma=None, late=[])
        self.ops.append(mark)
        p["marks"].append(mark)
        dop = dict(eng=eng, fn=(lambda e, s=slot: load_fn_of_slot(e, s)), r=tuple(extra_r) + (("PH",),),
                   w=(key,), dma=("dma",) + key, late=None, ndma=ndma)
        la = (nslots - 1) if lookahead is None else lookahead
        tgt = p["marks"][max(0, i - la)]
        tgt["late"].append(dop)
        return slot, key

    def finalize(self):
        out = []
        for o in self.ops:
            if o["late"] is not None:
                out.extend(o["late"])
            else:
                out.append(o)
        self.ops = ops = out
        last_w = {}
        readers = {}
        for i, o in enumerate(ops):
            deps = {}
            for k in o["r"]:
                if k in last_w:
                    deps[last_w[k]] = True
            for k in o["w"]:
                if k in last_w:
                    deps.setdefault(last_w[k], False)
                for rd in readers.get(k, ()):
                    deps.setdefault(rd, False)
            deps.pop(i, None)
            o["deps"] = deps
            for k in o["r"]:
                readers.setdefault(k, []).append(i)
            for k in o["w"]:
                last_w[k] = i
                readers[k] = []
        for o in ops:
            o["signal"] = False
            o["need"] = []
        for o in ops:
            for d, raw in sorted(o["deps"].items()):
                p = ops[d]
                if p["dma"] is not None:
                    o["need"].append(d)
                elif p["eng"] == o["eng"]:
                    if o["eng"] == "pe":
                        continue
                    if raw or o["dma"] is not None:
                        p["signal"] = True
                        o["need"].append(d)
                else:
                    p["signal"] = True
                    o["need"].append(d)
        cnt = {e: 0 for e in self.ENGS}
        for o in ops:
            if o["dma"] is None and o["signal"]:
                cnt[o["eng"]] += 1
                o["sval"] = cnt[o["eng"]]
        self.stats = dict(cnt)
        self.dma_keys = sorted({o["dma"] for o in ops if o["dma"] is not None}, key=str)

    def emit(self):
        nc = self.nc
        ops = self.ops
        with ExitStack() as es:
            psem = {e: es.enter_context(nc.semaphore("p_" + e)) for e in self.ENGS}
            dsem = {k: es.enter_context(nc.semaphore("d_" + "_".join(str(x) for x in k[1:])))
                    for k in self.dma_keys}
            dtot = {k: 0 for k in self.dma_keys}
            for o in ops:
                if o["dma"] is not None:
                    dtot[o["dma"]] += 16 * o["ndma"]
                    o["dval"] = dtot[o["dma"]]
            block = es.enter_context(nc.Block())

            def run(ename, eng):
                waited = {}
                for o in ops:
                    if o["eng"] != ename:
                        continue
                    grp = {}
                    for d in o["need"]:
                        p = ops[d]
                        if p["dma"] is not None:
                            key, val, sem = ("d", p["dma"]), p["dval"], dsem[p["dma"]]
                        else:
                            key, val, sem = ("p", p["eng"]), p["sval"], psem[p["eng"]]
                        if key not in grp or grp[key][0] < val:
                            grp[key] = (val, sem)
                    for key, (val, sem) in grp.items():
                        if waited.get(key, 0) >= val:
                            continue
                        waited[key] = val
                        eng.wait_ge(sem, val)
                    if o["fn"] is None:
                        continue
                    ins = o["fn"](eng)
                    if o["dma"] is not None:
                        lst = ins if isinstance(ins, (list, tuple)) else [ins]
                        assert len(lst) == o["ndma"]
                        for x in lst:
                            x.then_inc(dsem[o["dma"]], 16)
                    elif o["signal"]:
                        ins.then_inc(psem[ename], 1)

            block.tensor(lambda e: run("pe", e))
            block.scalar(lambda e: run("act", e))
            block.vector(lambda e: run("dve", e))
            block.gpsimd(lambda e: run("pool", e))
            block.sync(lambda e: run("sp", e))


def _rope_tables():
    t = np.arange(T, dtype=np.float32)
    ca = np.ones((128, T), np.float32)
    sa = np.zeros((128, T), np.float32)
    half = 8
    inv = (np.float32(500000.0) ** (-(np.arange(half, dtype=np.float32) * 2.0 / 16))).astype(np.float32)
    ang = t[None, :] * inv[:, None]
    for hh in range(2):
        b = hh * 64
        ca[b:b + half] = np.cos(ang)
        ca[b + half:b + 2 * half] = np.cos(ang)
        sa[b:b + half] = -np.sin(ang)
        sa[b + half:b + 2 * half] = np.sin(ang)
    cr = np.zeros((128, T), np.float32)
    sr = np.zeros((128, T), np.float32)
    half = 32
    inv = (np.float32(10000.0) ** (-(np.arange(half, dtype=np.float32) * 2.0 / 64))).astype(np.float32)
    ang = t[None, :] * inv[:, None]
    for hh in range(2):
        b = hh * 64
        cr[b:b + half] = np.cos(ang)
        cr[b + half:b + 64] = np.cos(ang)
        sr[b:b + half] = -np.sin(ang)
        sr[b + half:b + 64] = np.sin(ang)
    return ca, sa, cr, sr


def _consts():
    idx = np.arange(128)
    k = idx[:, None]
    q = idx[None, :]
    cur = (k <= q).astype(np.float32)
    prev = (k >= q).astype(np.float32)
    m_n = np.concatenate([cur, prev], axis=1)
    m_f = np.concatenate([cur, np.zeros_like(prev)], axis=1)
    m_fnnn = np.concatenate([m_f, m_n, m_n, m_n], axis=1)
    ones = np.ones((128, 128), np.float32)
    blk = np.zeros((128, 128), np.float32)
    blk[:64, :64] = 1
    blk[64:, 64:] = 1
    ident = np.eye(128, dtype=np.float32)
    cb = np.concatenate([ones, blk, ident], axis=1)
    hb = np.concatenate([m_n, m_fnnn], axis=1)
    h = np.arange(8, dtype=np.float64)
    log_g = np.log(1.0 - np.exp2(-5.0 - h))
    diff = (q - k).astype(np.float64)
    dm = np.zeros((128, 8, 128), np.float64)
    for hh in range(8):
        dm[:, hh, :] = np.where(diff >= 0, np.exp(log_g[hh] * np.maximum(diff, 0)), 0.0) / 8.0
    xi = np.zeros((128, 4, 128), np.float64)
    zt = np.zeros((128, 4, 128), np.float64)
    cd = np.zeros((128, 8), np.float64)
    for p in range(128):
        for pr in range(4):
            hh = pr * 2 + p // 64
            xi[p, pr, :] = np.exp(log_g[hh] * (idx + 1.0))
            cd[p, pr] = np.exp(log_g[hh] * 128.0)
    for col in range(128):
        for pr in range(4):
            hh = pr * 2 + col // 64
            zt[:, pr, col] = np.exp(log_g[hh] * (127.0 - idx)) / 8.0
    hf = np.concatenate([dm.reshape(128, -1), xi.reshape(128, -1), zt.reshape(128, -1), cd],
                        axis=1).astype(np.float32)
    return cb.astype(np.float32), hb.astype(np.float32), hf


def _hyb_cols():
    cols = []
    def perm(base, half, rot):
        out = []
        for hh in range(2):
            for j in range(64):
                if j < half:
                    jj = j + half
                elif j < 2 * half:
                    jj = j - half
                else:
                    jj = j
                out.append(base + hh * 64 + jj)
        return out
    for p in range(4):
        qa = 0 + p * 128
        ka = 512 + p * 128
        va = 1024 + p * 128
        cols += list(range(qa, qa + 128)) + perm(qa, 8, 16)
        cols += list(range(ka, ka + 128)) + perm(ka, 8, 16)
        cols += list(range(va, va + 128))
    for p in range(4):
        qr = 1536 + p * 128
        kr = 2048 + p * 128
        vr = 2560 + p * 128
        gr = 3072 + p * 128
        cols += list(range(qr, qr + 128)) + perm(qr, 32, 64)
        cols += list(range(kr, kr + 128)) + perm(kr, 32, 64)
        cols += list(range(gr, gr + 128))
        cols += list(range(vr, vr + 128))
    assert len(cols) == NEXT
    return np.array(cols, dtype=np.int64)


def ssl(st, cnt, d=1):
    return slice(st, st + (cnt - 1) * d + 1, d)


def build_program(layers, stop_after=None):
    nc = bass.Bass("TRN2", target_bir_lowering=False)

    def din(name, shape):
        return nc.dram_tensor(name, list(shape), F32, kind="ExternalInput").ap()

    xT = din("xT", [D, T])
    yT = nc.dram_tensor("yT", [D, T], F32, kind="ExternalOutput").ap()
    g_all = din("g_all", [128, 192])
    wg_d = din("ffn_w_gate", [DEPTH, 2, D, FF])
    wu_d = din("ffn_w_up", [DEPTH, 2, D, FF])
    wd_d = din("ffn_w_down", [DEPTH, 2, FF, D])
    hin_d = din("hyb_w_in_ext", [2, D, NEXT])
    hout_d = din("hyb_w_out", [2, D, D])
    gin_d = din("gmlp_w_in", [2, D, 2 * D])
    gout_d = din("gmlp_w_out", [2, D, D])
    glng_d = din("gmlp_ln_g", [2, D])
    glnb_d = din("gmlp_ln_b", [2, D])
    gws_d = din("gmlp_w_sT", [2, 128, 8 * 128])
    gbs_d = din("gmlp_b_s", [2, 8 * 128])
    ropeA_c = din("ropeA_c", [128, T])
    ropeA_s = din("ropeA_s", [128, T])
    ropeR_c = din("ropeR_c", [128, T])
    ropeR_s = din("ropeR_s", [128, T])
    cb_d = din("cb", [128, 384])
    hb_d = din("hb", [128, 1280])
    hf_d = din("hf", [128, 2056])

    es = ExitStack()
    X = es.enter_context(nc.sbuf_tensor("X", [128, 8, T], F32))
    G = es.enter_context(nc.sbuf_tensor("G", [128, 192], F32))
    CB = es.enter_context(nc.sbuf_tensor("CB", [128, 384], BF16))
    MASKC = es.enter_context(nc.sbuf_tensor("MASKC", [128, 128], BF16))
    EPSB = es.enter_context(nc.sbuf_tensor("EPSB", [128, 4], F32))
    SCRB = 142 * 1024
    SCR = es.enter_context(nc.sbuf_tensor("SCR", [128, SCRB], U8))
    PSALL = es.enter_context(nc.psum_tensor("PSALL", [128, 4096], F32))
    ONES = CB[:, 0:128]
    BLK = CB[:, 128:256]
    IDENT = CB[:, 256:384]

    def PS(b, n=1):
        return PSALL[:, b * 512:(b + n) * 512]

    class Carver:
        def __init__(self, off=0):
            self.off = off

        def take(self, dtype, shape):
            n = 1
            for s_ in shape[1:]:
                n *= s_
            nb = n * (4 if dtype == F32 else 2)
            nb = (nb + 31) // 32 * 32
            ap = SCR[:, self.off:self.off + nb].bitcast(dtype)[:, 0:n]
            if len(shape) == 3:
                ap = ap.rearrange("p (a b) -> p a b", a=shape[1])
            elif len(shape) == 4:
                ap = ap.rearrange("p (a b c) -> p a b c", a=shape[1], b=shape[2])
            self.off += nb
            assert self.off <= SCRB, (self.off, SCRB)
            return ap

    S = Sched(nc)

    def barrier():
        S.barrier(lambda e: e.memset(EPSB[:, 2:3], 0.0))

    S.op("sp", lambda e: e.dma_start(out=G[:], in_=g_all), w=[("G",)], dma=("dma", "G"))
    S.op("pool", lambda e: e.dma_start(out=CB[:], in_=cb_d), w=[("CB",)], dma=("dma", "CB"))
    S.op("pool", lambda e: e.dma_start(out=MASKC[:], in_=hb_d[:, 0:128]), w=[("MASKC",)], dma=("dma", "MASKC"))
    S.op("dve", lambda e: e.memset(EPSB[:, 0:1], EPS), w=[("EPSB0",)])
    S.op("dve", lambda e: e.memset(EPSB[:, 1:2], math.log(0.5)), r=[("EPSB0",)], w=[("EPSB",)])
    xv = xT.rearrange("(c p) t -> p c t", p=128)
    yv = yT.rearrange("(c p) t -> p c t", p=128)
    for g in range(4):
        S.op("sp", lambda e, g=g: e.dma_start(out=X[:, :, g * 512:(g + 1) * 512],
                                              in_=xv[:, :, g * 512:(g + 1) * 512]),
             w=[("X", g)], dma=("dma", "X", g))

    def rstd_from_sq(sq_ap, nk, rs_ap, inv_n, ln_half, keys_r, key_rs, lhs=None, bank=6):
        lhs = ONES if lhs is None else lhs

        def mm(e):
            ins = None
            for k in range(nk):
                src = sq_ap[:, k, :] if nk > 1 else sq_ap
                ins = e.matmul(PS(bank), lhsT=lhs, rhs=src, start=(k == 0), stop=(k == nk - 1))
            return ins
        S.op("pe", mm, r=list(keys_r) + [("CB",)], w=[("ps", bank)])
        S.op("act", lambda e: e.activation(out=rs_ap, in_=PS(bank), func=AF.Ln, scale=inv_n, bias=EPSB[:, 0:1]),
             r=[("ps", bank), ("EPSB",)], w=[key_rs])
        if ln_half:
            S.op("act", lambda e: e.activation(out=rs_ap, in_=rs_ap, func=AF.Exp, scale=-0.5, bias=EPSB[:, 1:2]),
                 r=[key_rs, ("EPSB",)], w=[key_rs])
        else:
            S.op("act", lambda e: e.activation(out=rs_ap, in_=rs_ap, func=AF.Exp, scale=-0.5),
                 r=[key_rs], w=[key_rs])

    def prenorm(gi, g, dst_fn, dst_key, SQ, RS, stage="ab", ksq=("SQ",), krs=("RS",), bank=6):
        tok = slice(g * 512, (g + 1) * 512)
        if "a" in stage:
            S.op("act", lambda e: e.activation(out=SQ, in_=X[:, :, tok], func=AF.Square),
                 r=[("X", g)], w=[ksq])
        if "b" not in stage:
            return
        rstd_from_sq(SQ, 8, RS, 1.0 / D, False, [ksq], krs, bank=bank)
        for k in range(8):
            S.op("dve", lambda e, k=k: e.scalar_tensor_tensor(
                out=dst_fn(k), in0=X[:, k, tok], scalar=G[:, gi * 8 + k:gi * 8 + k + 1], in1=RS,
                op0=ALU.mult, op1=ALU.mult),
                r=[("X", g), krs, ("G",)], w=[dst_key(k)])

    def proj_postnorm(src_fn, src_keys, nk, w_view, gi, g, half_factor, FB, SQ, RS, WDS):
        tok = slice(g * 512, (g + 1) * 512)
        for c in range(8):
            slot, key = S.consume("WD", 2, "pool",
                                  lambda e, s, c=c: e.dma_start(out=WDS[:, s, 0:nk, :],
                                                                in_=w_view[:, :, c * 128:(c + 1) * 128]))
            b = 4 + c % 2

            def mm(e, slot=slot, b=b):
                ins = None
                for j in range(nk):
                    ins = e.matmul(PS(b), lhsT=WDS[:, slot, j, :], rhs=src_fn(j), start=(j == 0), stop=(j == nk - 1))
                return ins
            S.op("pe", mm, r=[key] + list(src_keys), w=[("ps", b)])
            S.op("act", lambda e, c=c, b=b: e.activation(out=FB[:, c, :], in_=PS(b), func=AF.Copy),
                 r=[("ps", b)], w=[("F", c)])
        S.op("act", lambda e: e.activation(out=SQ, in_=FB, func=AF.Square),
             r=[("F", c) for c in range(8)], w=[("SQ",)])
        rstd_from_sq(SQ, 8, RS, 1.0 / D, half_factor, [("SQ",)], ("RS",))
        for c in range(8):
            S.op("dve", lambda e, c=c: e.scalar_tensor_tensor(
                out=FB[:, c, :], in0=FB[:, c, :], scalar=G[:, gi * 8 + c:gi * 8 + c + 1], in1=RS,
                op0=ALU.mult, op1=ALU.mult), r=[("F", c), ("RS",), ("G",)], w=[("F", c)])
        for c in range(8):
            S.op("dve", lambda e, c=c: e.tensor_tensor(out=X[:, c, tok], in0=X[:, c, tok], in1=FB[:, c, :], op=ALU.add),
                 r=[("F", c), ("X", g)], w=[("X", g)])

    def ffn_seq(items):
        cv = Carver()
        H = cv.take(BF16, [128, 8, 1024])
        ACTB = cv.take(BF16, [128, NJ, 1024])
        FB = cv.take(F32, [128, 8, 1024])
        SQ = cv.take(BF16, [128, 8, 512])
        RS = cv.take(F32, [128, 512])
        SQ2 = cv.take(BF16, [128, 8, 512])
        RS2 = cv.take(F32, [128, 512])
        SG = cv.take(F32, [128, 2, 512])
        WGU = cv.take(BF16, [128, 3, 16, 128])
        WDS = cv.take(BF16, [128, 2, NJ, 128])
        halves = [(l, i, hf) for (l, i) in items for hf in range(2)]

        def gidx(l, i):
            return l * 6 + (0 if i == 0 else 4)

        def pre(hv, stage, tts):
            l, i, hf = hv
            for tt in tts:
                prenorm(gidx(l, i), hf * 2 + tt, lambda k, tt=tt: H[:, k, tt * 512:(tt + 1) * 512],
                        lambda k, tt=tt: ("H", tt), SQ2, RS2, stage=stage, ksq=("SQ2",), krs=("RS2",), bank=7)

        def phase1(hv):
            l, i, hf = hv
            wgv = wg_d[l, i].rearrange("(k p) n -> p k n", p=128)
            wuv = wu_d[l, i].rearrange("(k p) n -> p k n", p=128)
            for j in range(NJ):
                def ld(e, s, j=j):
                    a_ = e.dma_start(out=WGU[:, s, 0:8, :], in_=wgv[:, :, j * 128:(j + 1) * 128])
                    b_ = e.dma_start(out=WGU[:, s, 8:16, :], in_=wuv[:, :, j * 128:(j + 1) * 128])
                    return [a_, b_]
                slot, key = S.consume("WGU", 3, "pool", ld, ndma=2)
                for tt in range(2):
                    bg, bu = tt, 2 + tt

                    def mm(e, slot=slot, tt=tt, bg=bg, bu=bu):
                        ins = None
                        for k in range(8):
                            ins = e.matmul(PS(bg), lhsT=WGU[:, slot, k, :], rhs=H[:, k, tt * 512:(tt + 1) * 512],
                                           start=(k == 0), stop=(k == 7))
                        for k in range(8):
                            ins = e.matmul(PS(bu), lhsT=WGU[:, slot, 8 + k, :], rhs=H[:, k, tt * 512:(tt + 1) * 512],
                                           start=(k == 0), stop=(k == 7))
                        return ins
                    S.op("pe", mm, r=[key, ("H", tt)], w=[("ps", bg), ("ps", bu)])
                    S.op("act", lambda e, tt=tt, bg=bg: e.activation(out=SG[:, tt, :], in_=PS(bg), func=AF.Silu),
                         r=[("ps", bg)], w=[("SG", tt)])
                    S.op("dve", lambda e, bu=bu, j=j, tt=tt: e.tensor_tensor(
                        out=ACTB[:, j, tt * 512:(tt + 1) * 512], in0=SG[:, tt, :], in1=PS(bu), op=ALU.mult),
                        r=[("SG", tt), ("ps", bu)], w=[("A", j, tt)])

        def phase2(hv, hook):
            l, i, hf = hv
            wdv = wd_d[l, i].rearrange("(j p) n -> p j n", p=128)
            gi = gidx(l, i) + 1
            for c in range(8):
                slot, key = S.consume("WD", 2, "pool",
                                      lambda e, s, c=c: e.dma_start(out=WDS[:, s, :, :],
                                                                    in_=wdv[:, :, c * 128:(c + 1) * 128]))
                for tt in range(2):
                    b = 4 + tt

                    def mm(e, slot=slot, b=b, tt=tt):
                        ins = None
                        for j in range(NJ):
                            ins = e.matmul(PS(b), lhsT=WDS[:, slot, j, :], rhs=ACTB[:, j, tt * 512:(tt + 1) * 512],
                                           start=(j == 0), stop=(j == NJ - 1))
                        return ins
                    S.op("pe", mm, r=[key] + [("A", j, tt) for j in range(NJ)], w=[("ps", b)])
                    S.op("act", lambda e, c=c, b=b, tt=tt: e.activation(out=FB[:, c, tt * 512:(tt + 1) * 512], in_=PS(b),
                                                                         func=AF.Copy),
                         r=[("ps", b)], w=[("F", c, tt)])
                if c == 0 and hook is not None:
                    hook()
            for tt in range(2):
                g = hf * 2 + tt
                tok = slice(g * 512, (g + 1) * 512)
                fsl = slice(tt * 512, (tt + 1) * 512)
                fk = [("F", c, tt) for c in range(8)]
                S.op("act", lambda e, fsl=fsl: e.activation(out=SQ, in_=FB[:, :, fsl], func=AF.Square),
                     r=fk, w=[("SQ",)])
                rstd_from_sq(SQ, 8, RS, 1.0 / D, True, [("SQ",)], ("RS",))
                for c in range(8):
                    S.op("dve", lambda e, c=c, fsl=fsl: e.scalar_tensor_tensor(
                        out=FB[:, c, fsl], in0=FB[:, c, fsl], scalar=G[:, gi * 8 + c:gi * 8 + c + 1], in1=RS,
                        op0=ALU.mult, op1=ALU.mult), r=[("F", c, tt), ("RS",), ("G",)], w=[("F", c, tt)])
                for c in range(8):
                    eng = "dve" if c % 2 == 0 else "pool"
                    S.op(eng, lambda e, c=c, tok=tok, fsl=fsl: e.tensor_tensor(out=X[:, c, tok], in0=X[:, c, tok],
                                                                               in1=FB[:, c, fsl], op=ALU.add),
                         r=[("F", c, tt), ("X", g)], w=[("X", g)])

        pre(halves[0], "ab", (0, 1))
        for idx, hv in enumerate(halves):
            phase1(hv)
            nxt = halves[idx + 1] if idx + 1 < len(halves) else None
            if nxt is not None:
                pre(nxt, "a", (0,))

                def hook(nxt=nxt):
                    pre(nxt, "b", (0,))
                    pre(nxt, "ab", (1,))
                phase2(hv, hook)
            else:
                phase2(hv, None)

    def gmlp(l):
        jl = l // 2
        cv = Carver()
        H = cv.take(BF16, [128, 8, 512])
        UT = cv.take(BF16, [128, 8, 512])
        YT = cv.take(BF16, [128, 8, 512])
        VG = cv.take(F32, [128, 4, 1024])
        VN = cv.take(BF16, [128, 4, 1024])
        LNG = cv.take(F32, [128, 1024])
        LNB = cv.take(F32, [128, 1024])
        WST = cv.take(BF16, [128, 8, 128])
        BS = cv.take(F32, [128, 8, 128])
        WIN = cv.take(BF16, [128, 3, 8, 128])
        WV = cv.take(BF16, [128, 2, 8, 512])
        FB = cv.take(F32, [128, 8, 512])
        SQ = cv.take(BF16, [128, 8, 512])
        RS = cv.take(F32, [128, 512])
        Y1 = cv.take(F32, [128, 2, 512])
        ST = cv.take(F32, [128, 4, 16])
        MV = cv.take(F32, [128, 4, 2])
        RSD = cv.take(F32, [128, 4])
        WDS = cv.take(BF16, [128, 2, 8, 128])
        winv = gin_d[jl].rearrange("(k p) n -> p k n", p=128)
        woutv = gout_d[jl].rearrange("(j p) n -> p j n", p=128)
        gi = l * 6 + 2
        S.op("sp", lambda e: e.dma_start(out=LNG, in_=glng_d[jl:jl + 1, :].to_broadcast([128, D])),
             w=[("LNG",)], dma=("dma", "LNG"))
        S.op("sp", lambda e: e.dma_start(out=LNB, in_=glnb_d[jl:jl + 1, :].to_broadcast([128, D])),
             w=[("LNB",)], dma=("dma", "LNB"))
        S.op("sp", lambda e: e.dma_start(out=BS.rearrange("p a b -> p (a b)"),
                                         in_=gbs_d[jl:jl + 1, :].to_broadcast([128, D])),
             w=[("BS",)], dma=("dma", "BS"))
        S.op("pool", lambda e: e.dma_start(out=WST.rearrange("p a b -> p (a b)"), in_=gws_d[jl]),
             w=[("WST0",)], dma=("dma", "WST"))
        S.op("dve", lambda e: e.tensor_tensor(out=WST, in0=WST, in1=MASKC[:, None, :].to_broadcast([128, 8, 128]),
                                              op=ALU.mult),
             r=[("WST0",), ("MASKC",)], w=[("WST",)])
        SQ2 = cv.take(BF16, [128, 8, 512])
        RS2 = cv.take(F32, [128, 512])

        def pre(g):
            prenorm(gi, g, lambda k: H[:, k, :], lambda k: ("H",), SQ2, RS2, ksq=("SQ2",), krs=("RS2",), bank=7)

        pre(0)
        for g in range(4):
            for hv in range(2):
                slot, key = S.consume("GWV", 2, "pool",
                                      lambda e, s, hv=hv: e.dma_start(out=WV[:, s],
                                                                       in_=winv[:, :, D + hv * 512:D + (hv + 1) * 512]))
                for m in range(4):
                    b = 2 + (hv * 4 + m) % 2

                    def mm(e, slot=slot, b=b, m=m):
                        ins = None
                        for k in range(8):
                            ins = e.matmul(PS(b), lhsT=H[:, k, m * 128:(m + 1) * 128], rhs=WV[:, slot, k, :],
                                           start=(k == 0), stop=(k == 7))
                        return ins
                    S.op("pe", mm, r=[key, ("H",)], w=[("ps", b)])
                    S.op("act", lambda e, b=b, m=m, hv=hv: e.activation(
                        out=VG[:, m, hv * 512:(hv + 1) * 512], in_=PS(b), func=AF.Gelu),
                        r=[("ps", b)], w=[("VG", m, hv)])
                    S.op("dve", lambda e, m=m, hv=hv: e.bn_stats(out=ST[:, m, hv * 6:(hv + 1) * 6],
                                                                  in_=VG[:, m, hv * 512:(hv + 1) * 512]),
                         r=[("VG", m, hv)], w=[("STAT", m, hv)])
            for m in range(4):
                S.op("dve", lambda e, m=m: e.bn_aggr(out=MV[:, m, :], in_=ST[:, m, 0:12]),
                     r=[("STAT", m, 0), ("STAT", m, 1)], w=[("MV", m)])
            S.op("act", lambda e: e.activation(out=RSD, in_=MV[:, :, 1], func=AF.Ln, bias=EPSB[:, 0:1]),
                 r=[("MV", m) for m in range(4)] + [("EPSB",)], w=[("RSD",)])
            S.op("act", lambda e: e.activation(out=RSD, in_=RSD, func=AF.Exp, scale=-0.5),
                 r=[("RSD",)], w=[("RSD",)])
            for m in range(4):
                vk = [("VG", m, 0), ("VG", m, 1)]
                S.op("dve", lambda e, m=m: e.tensor_scalar(out=VG[:, m, :], in0=VG[:, m, :], scalar1=MV[:, m, 0:1],
                                                           scalar2=RSD[:, m:m + 1], op0=ALU.subtract, op1=ALU.mult),
                     r=vk + [("MV", m), ("RSD",)], w=vk)
                S.op("dve", lambda e, m=m: e.tensor_tensor(out=VG[:, m, :], in0=VG[:, m, :], in1=LNG, op=ALU.mult),
                     r=vk + [("LNG",)], w=vk)
                S.op("dve", lambda e, m=m: e.tensor_tensor(out=VN[:, m, :], in0=VG[:, m, :], in1=LNB, op=ALU.add),
                     r=vk + [("LNB",)], w=[("VN", m)])
            for ct in range(8):
                slot, key = S.consume("GWIN", 3, "pool",
                                      lambda e, s, ct=ct: e.dma_start(out=WIN[:, s], in_=winv[:, :, ct * 128:(ct + 1) * 128]))
                b = ct % 2

                def mm(e, slot=slot, b=b):
                    ins = None
                    for k in range(8):
                        ins = e.matmul(PS(b), lhsT=WIN[:, slot, k, :], rhs=H[:, k, :], start=(k == 0), stop=(k == 7))
                    return ins
                S.op("pe", mm, r=[key, ("H",)], w=[("ps", b)])
                S.op("act", lambda e, ct=ct, b=b: e.activation(out=UT[:, ct, :], in_=PS(b), func=AF.Gelu),
                     r=[("ps", b)], w=[("UT", ct)])
            for gg in range(8):
                b = 4 + gg % 2

                def mm(e, gg=gg, b=b):
                    ins = None
                    for m in range(4):
                        ins = e.matmul(PS(b)[:, m * 128:(m + 1) * 128], lhsT=VN[:, m, gg * 128:(gg + 1) * 128],
                                       rhs=WST[:, gg, :], start=True, stop=True)
                    return ins
                S.op("pe", mm, r=[("VN", m) for m in range(4)] + [("WST",)], w=[("ps", b)])
                u = gg % 2
                S.op("dve", lambda e, gg=gg, b=b, u=u: e.tensor_tensor(
                    out=Y1[:, u, :].rearrange("p (m i) -> p m i", m=4),
                    in0=PS(b).rearrange("p (m i) -> p m i", m=4),
                    in1=BS[:, gg:gg + 1, :].to_broadcast([128, 4, 128]), op=ALU.add),
                    r=[("ps", b), ("BS",)], w=[("Y1", u)])
                S.op("dve", lambda e, gg=gg, u=u: e.tensor_tensor(out=YT[:, gg, :], in0=Y1[:, u, :], in1=UT[:, gg, :],
                                                                   op=ALU.mult),
                     r=[("Y1", u), ("UT", gg)], w=[("YT", gg)])
            if g + 1 < 4:
                pre(g + 1)
            proj_postnorm(lambda j: YT[:, j, :], [("YT", j) for j in range(8)], 8, woutv, gi + 1, g, False,
                          FB, SQ, RS, WDS)

    def hybrid(l):
        jl = l // 2
        cv = Carver()
        HT = cv.take(BF16, [128, 8, T])
        MIX = cv.take(BF16, [128, 8, T])
        WIN = cv.take(BF16, [128, 6, 8, 128])
        ROPE = cv.take(F32, [128, 2, 2, 512])
        TMP = cv.take(F32, [128, 2, 512])
        RS = cv.take(F32, [128, 512])
        pair_off = cv.off
        winv = hin_d[jl].rearrange("(k p) n -> p k n", p=128)
        woutv = hout_d[jl].rearrange("(j p) n -> p j n", p=128)
        gi = l * 6 + 2
        SQ0 = Carver(pair_off).take(BF16, [128, 8, 512])
        for g in range(4):
            prenorm(gi, g, lambda k, g=g: HT[:, k, g * 512:(g + 1) * 512], lambda k, g=g: ("HT", g), SQ0, RS)
        barrier()
        HTK = [("HT", g) for g in range(4)]

        def win_block(col0):
            return S.consume("HWIN", 6, "pool",
                             lambda e, s, col0=col0: e.dma_start(out=WIN[:, s], in_=winv[:, :, col0:col0 + 128]),
                             lookahead=2)

        def rope_proj(col0, rc, rs_, dsts, fam):
            blks = [win_block(col0 + i * 128) for i in range(4)]
            for g in range(4):
                tok = slice(g * 512, (g + 1) * 512)

                def ldrope(e, s, g=g):
                    a = e.dma_start(out=ROPE[:, s, 0, :], in_=rc[:, g * 512:(g + 1) * 512])
                    b = e.dma_start(out=ROPE[:, s, 1, :], in_=rs_[:, g * 512:(g + 1) * 512])
                    return [a, b]
                rslot, rkey = S.consume("ROPE", 2, "sp", ldrope, ndma=2)
                for qi in range(2):
                    u = (g * 2 + qi) % 2
                    b0, b1 = 2 * u, 2 * u + 1
                    (s0, k0), (s1, k1) = blks[2 * qi], blks[2 * qi + 1]

                    def mm(e, s0=s0, s1=s1, b0=b0, b1=b1, tok=tok):
                        ins = None
                        for k in range(8):
                            ins = e.matmul(PS(b0), lhsT=WIN[:, s0, k, :], rhs=HT[:, k, tok], start=(k == 0), stop=(k == 7))
                        for k in range(8):
                            ins = e.matmul(PS(b1), lhsT=WIN[:, s1, k, :], rhs=HT[:, k, tok], start=(k == 0), stop=(k == 7))
                        return ins
                    S.op("pe", mm, r=[k0, k1, ("HT", g)], w=[("ps", b0), ("ps", b1)])
                    S.op("dve", lambda e, b0=b0, rslot=rslot: e.tensor_tensor(
                        out=TMP[:, 0, :], in0=PS(b0), in1=ROPE[:, rslot, 0, :], op=ALU.mult),
                        r=[("ps", b0), rkey], w=[("TMP", 0)])
                    S.op("dve", lambda e, b1=b1, rslot=rslot: e.tensor_tensor(
                        out=TMP[:, 1, :], in0=PS(b1), in1=ROPE[:, rslot, 1, :], op=ALU.mult),
                        r=[("ps", b1), rkey], w=[("TMP", 1)])
                    dst = dsts[qi]
                    S.op("dve", lambda e, dst=dst, tok=tok: e.tensor_tensor(
                        out=dst[:, tok], in0=TMP[:, 0, :], in1=TMP[:, 1, :], op=ALU.add),
                        r=[("TMP", 0), ("TMP", 1)], w=[("QK", fam, qi, g)])

        def dswa_pair(p):
            cvp = Carver(pair_off)
            HB = cvp.take(BF16, [128, 1280])
            QT = cvp.take(BF16, [128, T])
            KT = cvp.take(BF16, [128, T])
            VA = cvp.take(BF16, [128, 3, 16, 192])
            ACC = cvp.take(F32, [128, 2, T])
            E = cvp.take(BF16, [128, 2, 1024])
            M_N = HB[:, 0:256]
            M_F = HB[:, 256:1280]
            RC = TMP
            col0 = p * 640
            if p == 0:
                S.op("pool", lambda e: e.dma_start(out=HB, in_=hb_d), w=[("HB",)], dma=("dma", "HB"))
                S.op("pool", lambda e: e.memset(VA[:, :, :, 64:128], 1.0), w=[("VA1",)])
            rope_proj(col0, ropeA_c, ropeA_s, [QT, KT], "a")
            vs, vk = win_block(col0 + 512)
            for bi, d in enumerate((1, 4, 16)):
                tpc = 16 // d
                for i4 in range(4):
                    b = 4 + (bi * 4 + i4) % 2

                    def mm(e, b=b, d=d, i4=i4, tpc=tpc):
                        ins = None
                        for ii in range(4):
                            i = i4 * 4 + ii
                            r, blk = i // tpc, i % tpc
                            st = blk * 128 * d + r
                            for k in range(8):
                                ins = e.matmul(PS(b)[:, ii * 128:(ii + 1) * 128],
                                               lhsT=HT[:, k, ssl(st, 128, d)], rhs=WIN[:, vs, k, :],
                                               start=(k == 0), stop=(k == 7))
                        return ins
                    S.op("pe", mm, r=[vk] + HTK, w=[("ps", b)])
                    S.op("act", lambda e, b=b, bi=bi, i4=i4: e.activation(
                        out=VA[:, bi, i4 * 4:(i4 + 1) * 4, :].rearrange("p i (s c) -> p i s c", s=3)[:, :, 0:3:2, :],
                        in_=PS(b).rearrange("p (i s c) -> p i s c", i=4, s=2), func=AF.Copy),
                        r=[("ps", b), ("VA1",)], w=[("VA", bi, i4)])
            QKK = [("QK", "a", qi, g) for qi in range(2) for g in range(4)]
            unit = 0
            units = []
            for hh in range(2):
                hs = slice(hh * 64, hh * 64 + 64)
                vcols = slice(hh * 64, hh * 64 + 128)
                for bi, d in enumerate((1, 4, 16)):
                    tpc = 16 // d
                    for i4 in range(4):
                        su = unit % 2
                        unit += 1
                        sb0 = 2 * su
                        ob = 4 + su
                        blocks = []
                        for ii in range(4):
                            i = i4 * 4 + ii
                            r, n = i // tpc, i % tpc
                            blocks.append((i, n, n * 128 * d + r, (n - 1) * 128 * d + r))
                        noprev = (d == 16)
                        W = 128 if noprev else 256

                        def emit_s(blocks=blocks, sb0=sb0, d=d, hs=hs, noprev=noprev, W=W):
                            def mm_s(e):
                                ins = None
                                for ii, (i, n, st, pst) in enumerate(blocks):
                                    qsl = ssl(st, 128, d)
                                    ins = e.matmul(PS(sb0, 2)[:, ii * W:ii * W + 128], lhsT=KT[hs, qsl], rhs=QT[hs, qsl],
                                                   start=True, stop=True)
                                    if not noprev:
                                        ksl = qsl if n == 0 else ssl(pst, 128, d)
                                        ins = e.matmul(PS(sb0, 2)[:, ii * W + 128:ii * W + 256], lhsT=KT[hs, ksl],
                                                       rhs=QT[hs, qsl], start=True, stop=True)
                                return ins
                            S.op("pe", mm_s, r=QKK, w=[("ps", sb0), ("ps", sb0 + 1)])

                        def emit_rest(blocks=blocks, sb0=sb0, su=su, ob=ob, d=d, bi=bi, hh=hh, vcols=vcols,
                                      noprev=noprev, W=W):
                            S.op("act", lambda e: e.activation(
                                out=E[:, su, 0:4 * W], in_=PS(sb0, 2)[:, 0:4 * W], func=AF.Exp, scale=0.125),
                                r=[("ps", sb0), ("ps", sb0 + 1)], w=[("E", su)])
                            if noprev:
                                msk = M_N[:, None, 0:128].to_broadcast([128, 4, 128])
                                ev = E[:, su, 0:512].rearrange("p (a b) -> p a b", a=4)
                            elif blocks[0][1] == 0:
                                msk = M_F
                                ev = E[:, su, :]
                            else:
                                msk = M_N[:, None, :].to_broadcast([128, 4, 256])
                                ev = E[:, su, :].rearrange("p (a b) -> p a b", a=4)
                            S.op("dve", lambda e: e.tensor_tensor(out=ev, in0=ev, in1=msk, op=ALU.mult),
                                 r=[("E", su), ("HB",)], w=[("E", su)])

                            def mm_o(e):
                                ins = None
                                for ii, (i, n, st, pst) in enumerate(blocks):
                                    hasprev = (not noprev) and n > 0
                                    ins = e.matmul(PS(ob)[:, ii * 128:(ii + 1) * 128], lhsT=VA[:, bi, i, vcols],
                                                   rhs=E[:, su, ii * W:ii * W + 128], start=True, stop=not hasprev)
                                    if hasprev:
                                        ins = e.matmul(PS(ob)[:, ii * 128:(ii + 1) * 128], lhsT=VA[:, bi, i - 1, vcols],
                                                       rhs=E[:, su, ii * W + 128:ii * W + 256], start=False, stop=True)
                                return ins
                            S.op("pe", mm_o, r=[("E", su)] + [("VA", bi, x) for x in range(4)], w=[("ps", ob)])
                            if d == 16:
                                r0 = blocks[0][0]
                                dst = ACC[:, hh, :].rearrange("p (m r) -> p r m", r=16)[:, r0:r0 + 4, :]
                                src = PS(ob).rearrange("p (a m) -> p a m", a=4)
                            else:
                                dst = ACC[:, hh, ssl(blocks[0][2], 512, d)]
                                src = PS(ob)
                            if bi == 0:
                                S.op("dve", lambda e: e.tensor_copy(out=dst, in_=src),
                                     r=[("ps", ob)], w=[("ACC", hh)])
                            else:
                                S.op("dve", lambda e: e.tensor_tensor(out=dst, in0=dst, in1=src, op=ALU.add),
                                     r=[("ps", ob), ("ACC", hh)], w=[("ACC", hh)])
                        units.append((emit_s, emit_rest, hh, bi == 2 and i4 == 3))

            def normalise(hh):
                orow = slice(hh * 64, hh * 64 + 64)
                drow = slice(64 - hh * 64, 128 - hh * 64)
                for g in range(4):
                    tok = slice(g * 512, (g + 1) * 512)
                    u = g % 2
                    S.op("act", lambda e, tok=tok, u=u: e.activation(
                        out=RC[orow, u, :], in_=ACC[drow, hh, tok], func=AF.Ln), r=[("ACC", hh)], w=[("TMP", u)])
                    S.op("act", lambda e, u=u: e.activation(
                        out=RC[orow, u, :], in_=RC[orow, u, :], func=AF.Exp, scale=-1.0), r=[("TMP", u)], w=[("TMP", u)])
                    S.op("dve", lambda e, tok=tok, u=u: e.tensor_tensor(
                        out=MIX[orow, p, tok], in0=ACC[orow, hh, tok], in1=RC[orow, u, :], op=ALU.mult),
                        r=[("TMP", u), ("ACC", hh)], w=[("MIX", p, hh, g)])

            prev = None
            for un in units:
                un[0]()
                if prev is not None:
                    prev[1]()
                    if prev[3]:
                        normalise(prev[2])
                prev = un
            prev[1]()
            normalise(prev[2])

        def ret_pair(p):
            cvp = Carver(pair_off)
            HF = cvp.take(F32, [128, 2056])
            QT = cvp.take(BF16, [128, T])
            KT = cvp.take(BF16, [128, T])
            QX = cvp.take(BF16, [128, T])
            GT = cvp.take(BF16, [128, T])
            VT = cvp.take(BF16, [128, 16, 128])
            KZ = cvp.take(BF16, [128, 16, 128])
            R32 = cvp.take(F32, [128, T])
            ST32 = cvp.take(F32, [128, 64])
            STB = cvp.take(BF16, [128, 2, 64])
            SD = cvp.take(BF16, [128, 2, 256])
            SQH = cvp.take(BF16, [128, 512])
            DM = HF[:, 0:1024].rearrange("p (h c) -> p h c", h=8)
            XI = HF[:, 1024:1536].rearrange("p (a c) -> p a c", a=4)
            ZT = HF[:, 1536:2048].rearrange("p (a c) -> p a c", a=4)
            CD = HF[:, 2048:2056]
            col0 = 4 * 640 + p * 768
            if p == 0:
                S.op("sp", lambda e: e.dma_start(out=HF, in_=hf_d), w=[("HF",)], dma=("dma", "HF"))
            rope_proj(col0, ropeR_c, ropeR_s, [QT, KT], "r")
            for g in range(4):
                tok = slice(g * 512, (g + 1) * 512)
                S.op("dve", lambda e, tok=tok: e.tensor_tensor(
                    out=QX[:, tok].rearrange("p (a c) -> p a c", a=4), in0=QT[:, tok].rearrange("p (a c) -> p a c", a=4),
                    in1=XI[:, p:p + 1, :].to_broadcast([128, 4, 128]), op=ALU.mult),
                    r=[("QK", "r", 0, g), ("HF",)], w=[("QX", g)])
            gs, gk = win_block(col0 + 512)
            for g in range(4):
                tok = slice(g * 512, (g + 1) * 512)
                b = g % 2

                def mm(e, b=b, tok=tok):
                    ins = None
                    for k in range(8):
                        ins = e.matmul(PS(b), lhsT=WIN[:, gs, k, :], rhs=HT[:, k, tok], start=(k == 0), stop=(k == 7))
                    return ins
                S.op("pe", mm, r=[gk, ("HT", g)], w=[("ps", b)])
                S.op("act", lambda e, b=b, tok=tok: e.activation(out=GT[:, tok], in_=PS(b), func=AF.Silu),
                     r=[("ps", b)], w=[("GT", g)])
            vs, vk = win_block(col0 + 640)
            for i4 in range(4):
                b = 2 + i4 % 2

                def mm(e, b=b, i4=i4):
                    ins = None
                    for ii in range(4):
                        i = i4 * 4 + ii
                        for k in range(8):
                            ins = e.matmul(PS(b)[:, ii * 128:(ii + 1) * 128], lhsT=HT[:, k, i * 128:(i + 1) * 128],
                                           rhs=WIN[:, vs, k, :], start=(k == 0), stop=(k == 7))
                    return ins
                S.op("pe", mm, r=[vk] + HTK, w=[("ps", b)])
                S.op("act", lambda e, b=b, i4=i4: e.activation(
                    out=VT[:, i4 * 4:(i4 + 1) * 4, :], in_=PS(b).rearrange("p (i c) -> p i c", i=4), func=AF.Copy),
                    r=[("ps", b)], w=[("VT", i4)])
            PSB7 = PS(7).bitcast(BF16)
            for i4 in range(DEBUG.get("ntr", 4)):
                def tr(e, i4=i4):
                    ins = None
                    for ii in range(4):
                        i = i4 * 4 + ii
                        ins = e.transpose(PSB7[:, ii * 128:(ii + 1) * 128], KT[:, i * 128:(i + 1) * 128], IDENT)
                    return ins
                S.op("pe", tr, r=[("QK", "r", 1, i4), ("CB",)], w=[("ps", 7)])
                S.op("dve", lambda e, i4=i4: e.tensor_tensor(
                    out=KZ[:, i4 * 4:(i4 + 1) * 4, :], in0=PSB7[:, 0:512].rearrange("p (i c) -> p i c", i=4),
                    in1=ZT[:, p:p + 1, :].to_broadcast([128, 4, 128]), op=ALU.mult),
                    r=[("ps", 7), ("HF",)], w=[("KZ", i4)])
            S.op("dve", lambda e: e.memset(ST32, 0.0), w=[("ST32",)])
            def r_skv(n):
                g = n // 4
                ck = slice(n * 128, (n + 1) * 128)
                sb = n % 2
                b0 = 2 * sb
                kvb = 6 + n % 2

                def mm_s(e):
                    e.matmul(PS(b0)[:, 0:128], lhsT=KT[0:64, ck], rhs=QT[0:64, ck], start=True, stop=True)
                    return e.matmul(PS(b0 + 1)[:, 0:128], lhsT=KT[64:128, ck], rhs=QT[64:128, ck], start=True, stop=True)
                S.op("pe", mm_s, r=[("QK", "r", 0, g), ("QK", "r", 1, g)], w=[("ps", b0), ("ps", b0 + 1)])
                S.op("dve", lambda e: e.tensor_tensor(
                    out=SD[:, sb, :].rearrange("p (h c) -> p h c", h=2),
                    in0=PS(b0, 2).rearrange("p (h c) -> p h c", h=2)[:, :, 0:128], in1=DM[:, 2 * p:2 * p + 2, :],
                    op=ALU.mult),
                    r=[("ps", b0), ("ps", b0 + 1), ("HF",)], w=[("SD", sb)])

                def mm_kv(e):
                    e.matmul(PS(kvb)[0:64, 0:64], lhsT=KZ[:, n, 0:64], rhs=VT[:, n, 0:64], start=True, stop=True)
                    return e.matmul(PS(kvb)[64:128, 0:64], lhsT=KZ[:, n, 64:128], rhs=VT[:, n, 64:128],
                                    start=True, stop=True)
                if n < 15:
                    S.op("pe", mm_kv, r=[("KZ", n // 4), ("VT", n // 4)], w=[("ps", kvb)])

            def r_upd(n):
                kvb = 6 + n % 2
                if n < 15:
                    S.op("dve", lambda e: e.scalar_tensor_tensor(
                        out=ST32, in0=ST32, scalar=CD[:, p:p + 1], in1=PS(kvb)[:, 0:64], op0=ALU.mult, op1=ALU.add),
                        r=[("ps", kvb), ("ST32",), ("HF",)], w=[("ST32",)])
                    S.op("act", lambda e: e.activation(out=STB[:, (n + 1) % 2, :], in_=ST32, func=AF.Copy),
                         r=[("ST32",)], w=[("STB", (n + 1) % 2)])

            def r_out(n):
                g = n // 4
                ck = slice(n * 128, (n + 1) * 128)
                sb = n % 2

                def mm_o(e):
                    oc = slice((n % 4) * 128, (n % 4 + 1) * 128)
                    st = n % 2
                    ins = None
                    for hh in range(2):
                        hs = slice(hh * 64, hh * 64 + 64)
                        ins = e.matmul(PS(4 + hh)[hs, oc], lhsT=VT[:, n, hs], rhs=SD[:, sb, hh * 128:(hh + 1) * 128],
                                       start=True, stop=(n == 0))
                        if n > 0:
                            ins = e.matmul(PS(4 + hh)[hs, oc], lhsT=STB[hs, st, :], rhs=QX[hs, ck], start=False, stop=True)
                    return ins
                rk = [("SD", sb), ("VT", n // 4), ("QX", g)] + ([("STB", n % 2)] if n > 0 else [])
                S.op("pe", mm_o, r=rk, w=[("ps", 4), ("ps", 5)])
                if n % 4 == 3:
                    tok = slice(g * 512, (g + 1) * 512)
                    S.op("act", lambda e: e.activation(out=R32[0:64, tok], in_=PS(4)[0:64, :], func=AF.Copy),
                         r=[("ps", 4)], w=[("R32a", g)])
                    S.op("act", lambda e: e.activation(out=R32[64:128, tok], in_=PS(5)[64:128, :], func=AF.Copy),
                         r=[("ps", 5)], w=[("R32", g)])

            r_skv(0)
            r_upd(0)
            for n in range(16):
                if n + 1 < 16:
                    r_skv(n + 1)
                r_out(n)
                if n + 1 < 16:
                    r_upd(n + 1)
            for g in range(4):
                tok = slice(g * 512, (g + 1) * 512)
                S.op("act", lambda e, tok=tok: e.activation(out=SQH, in_=R32[:, tok], func=AF.Square),
                     r=[("R32", g), ("R32a", g)], w=[("SQH",)])
                rstd_from_sq(SQH, 1, RS, 1.0 / 64, False, [("SQH",)], ("RS",), lhs=BLK)
                S.op("dve", lambda e, tok=tok: e.tensor_tensor(out=TMP[:, 0, :], in0=R32[:, tok], in1=RS, op=ALU.mult),
                     r=[("R32", g), ("R32a", g), ("RS",)], w=[("TMP", 0)])
                S.op("dve", lambda e, tok=tok: e.tensor_tensor(out=MIX[:, 4 + p, tok], in0=TMP[:, 0, :], in1=GT[:, tok],
                                                                op=ALU.mult),
                     r=[("TMP", 0), ("GT", g)], w=[("MIX", 4 + p, 0, g), ("MIX", 4 + p, 1, g)])

        for p in range(DEBUG.get("ndswa", 4)):
            dswa_pair(p)
        barrier()
        for p in range(DEBUG.get("nret", 4)):
            ret_pair(p)
        barrier()
        if DEBUG.get("dump_mix"):
            for g in range(4):
                S.op("act", lambda e, g=g: e.activation(out=X[:, :, g * 512:(g + 1) * 512], in_=MIX[:, :, g * 512:(g + 1) * 512],
                                                        func=AF.Copy),
                     r=[("MIX", j, hh, g) for j in range(8) for hh in range(2)] + [("X", g)], w=[("X", g)])
            return
        cvo = Carver(pair_off)
        FB = cvo.take(F32, [128, 8, 512])
        SQ = cvo.take(BF16, [128, 8, 512])
        WDS = cvo.take(BF16, [128, 2, 8, 128])
        for g in range(4):
            tok = slice(g * 512, (g + 1) * 512)
            proj_postnorm(lambda j, tok=tok: MIX[:, j, tok],
                          [("MIX", j, hh, g) for j in range(8) for hh in range(2)], 8, woutv, gi + 1, g, False,
                          FB, SQ, RS, WDS)

    phases = []
    for l in layers:
        phases += [("ffn", l, 0), ("mix", l), ("ffn", l, 1)]
    if stop_after is not None:
        phases = phases[:stop_after]
    idx = 0
    while idx < len(phases):
        ph = phases[idx]
        if ph[0] == "ffn":
            items = [(ph[1], ph[2])]
            while idx + 1 < len(phases) and phases[idx + 1][0] == "ffn":
                idx += 1
                items.append((phases[idx][1], phases[idx][2]))
            ffn_seq(items)
        else:
            l = ph[1]
            if DEBUG.get("skipmix"):
                pass
            elif l % 2 == 0:
                hybrid(l)
            else:
                gmlp(l)
        barrier()
        idx += 1

    for g in range(4):
        S.op("sp", lambda e, g=g: e.dma_start(out=yv[:, :, g * 512:(g + 1) * 512], in_=X[:, :, g * 512:(g + 1) * 512]),
             r=[("X", g)], w=[("Y", g)], dma=("dma", "Y", g))
    S.op("sp", None, r=[("Y", g) for g in range(4)])
    S.finalize()
    S.emit()
    es.close()
    return nc, S


_CACHE = {}


def _host_consts():
    if "c" not in _CACHE:
        ca, sa, cr, sr = _rope_tables()
        cb, hb, hf = _consts()
        _CACHE["c"] = dict(ropeA_c=ca, ropeA_s=sa, ropeR_c=cr, ropeR_s=sr, cb=cb, hb=hb, hf=hf)
        _CACHE["cols"] = _hyb_cols()
    return _CACHE["c"], _CACHE["cols"]


def prepare_inputs(inputs):
    consts, cols = _host_consts()
    f = lambda a: np.ascontiguousarray(np.asarray(a, dtype=np.float32))
    ng = f(inputs["norm_g"])
    g_all = np.ascontiguousarray(ng.reshape(4, 6, 8, 128).transpose(3, 0, 1, 2).reshape(128, 192))
    shared = dict(
        g_all=g_all,
        ffn_w_gate=f(inputs["ffn_w_gate"]), ffn_w_up=f(inputs["ffn_w_up"]), ffn_w_down=f(inputs["ffn_w_down"]),
        hyb_w_in_ext=np.ascontiguousarray(f(inputs["hyb_w_in"])[:, :, cols]),
        hyb_w_out=f(inputs["hyb_w_out"]),
        gmlp_w_in=f(inputs["gmlp_w_in"]), gmlp_w_out=f(inputs["gmlp_w_out"]),
        gmlp_ln_g=f(inputs["gmlp_ln_g"]), gmlp_ln_b=f(inputs["gmlp_ln_b"]),
        gmlp_w_sT=np.ascontiguousarray(f(inputs["gmlp_w_s"]).transpose(0, 3, 1, 2).reshape(2, 128, 1024)),
        gmlp_b_s=np.ascontiguousarray(f(inputs["gmlp_b_s"]).reshape(2, 1024)),
        **consts,
    )
    return shared


def kernel(**inputs):
    x = np.asarray(inputs["x"], dtype=np.float32)
    shared = prepare_inputs(inputs)
    if "nc" not in _CACHE:
        _CACHE["nc"] = build_program([0, 1, 2, 3])[0]
    nc = _CACHE["nc"]
    in_maps = []
    for b in range(8):
        m = dict(shared)
        m["xT"] = np.ascontiguousarray(x[b].T)
        in_maps.append(m)
    res = run_bass_kernel_spmd(nc, in_maps, core_ids=list(range(8)))
    out = np.stack([np.ascontiguousarray(res.results[b]["yT"].T) for b in range(8)], axis=0)
    return out.astype(np.float32)
```

```python
import math
from contextlib import ExitStack
import numpy as np
import concourse.bass as bass
import concourse.mybir as mybir
from concourse.bass_utils import run_bass_kernel_spmd

F32 = mybir.dt.float32
BF16 = mybir.dt.bfloat16
U8 = mybir.dt.uint8
AF = mybir.ActivationFunctionType
ALU = mybir.AluOpType

T = 2048
D = 1024
FF = 2816
NJ = 22
EPS = 1e-6
DEPTH = 4
NEXT = 4 * 640 + 4 * 768
DEBUG = {}


class Sched:
    ENGS = ("pe", "act", "dve", "pool", "sp")

    def __init__(self, nc):
        self.nc = nc
        self.ops = []
        self.pools = {}
        self.phase = 0

    def barrier(self, fn):
        self.ops.append(dict(eng="dve", fn=fn, r=(), w=(("PH",),), dma=None, late=None, ndma=1))
        self.phase += 1

    def op(self, eng, fn, r=(), w=(), dma=None, ndma=1):
        o = dict(eng=eng, fn=fn, r=tuple(r) + (("PH",),), w=tuple(w), dma=dma, late=None, ndma=ndma)
        self.ops.append(o)
        return o

    def consume(self, pool, nslots, eng, load_fn_of_slot, extra_r=(), ndma=1, lookahead=None):
        p = self.pools.setdefault((pool, self.phase), dict(n=0, nslots=nslots, marks=[]))
        i = p["n"]
        p["n"] += 1
        slot = i % nslots
        key = (pool, slot)
        mark = dict(eng=None, fn=None, r=(), w=(), dma=None, late=[])
        self.ops.append(mark)
        p["marks"].append(mark)
        dop = dict(eng=eng, fn=(lambda e, s=slot: load_fn_of_slot(e, s)), r=tuple(extra_r) + (("PH",),),
                   w=(key,), dma=("dma",) + key, late=None, ndma=ndma)
        la = (nslots - 1) if lookahead is None else lookahead
        tgt = p["marks"][max(0, i - la)]
        tgt["late"].append(dop)
        return slot, key

    def finalize(self):
        out = []
        for o in self.ops:
            if o["late"] is not None:
                out.extend(o["late"])
            else:
                out.append(o)
        self.ops = ops = out
        last_w = {}
        readers = {}
        for i, o in enumerate(ops):
            deps = {}
            for k in o["r"]:
                if k in last_w:
                    deps[last_w[k]] = True
            for k in o["w"]:
                if k in last_w:
                    deps.setdefault(last_w[k], False)
                for rd in readers.get(k, ()):
                    deps.setdefault(rd, False)
            deps.pop(i, None)
            o["deps"] = deps
            for k in o["r"]:
                readers.setdefault(k, []).append(i)
            for k in o["w"]:
                last_w[k] = i
                readers[k] = []
        for o in ops:
            o["signal"] = False
            o["need"] = []
        for o in ops:
            for d, raw in sorted(o["deps"].items()):
                p = ops[d]
                if p["dma"] is not None:
                    o["need"].append(d)
                elif p["eng"] == o["eng"]:
                    if o["eng"] == "pe":
                        continue
                    if raw or o["dma"] is not None:
                        p["signal"] = True
                        o["need"].append(d)
                else:
                    p["signal"] = True
                    o["need"].append(d)
        cnt = {e: 0 for e in self.ENGS}
        for o in ops:
            if o["dma"] is None and o["signal"]:
                cnt[o["eng"]] += 1
                o["sval"] = cnt[o["eng"]]
        self.stats = dict(cnt)
        self.dma_keys = sorted({o["dma"] for o in ops if o["dma"] is not None}, key=str)

    def emit(self):
        nc = self.nc
        ops = self.ops
        with ExitStack() as es:
            psem = {e: es.enter_context(nc.semaphore("p_" + e)) for e in self.ENGS}
            dsem = {k: es.enter_context(nc.semaphore("d_" + "_".join(str(x) for x in k[1:])))
                    for k in self.dma_keys}
            dtot = {k: 0 for k in self.dma_keys}
            for o in ops:
                if o["dma"] is not None:
                    dtot[o["dma"]] += 16 * o["ndma"]
                    o["dval"] = dtot[o["dma"]]
            block = es.enter_context(nc.Block())

            def run(ename, eng):
                waited = {}
                for o in ops:
                    if o["eng"] != ename:
                        continue
                    grp = {}
                    for d in o["need"]:
                        p = ops[d]
                        if p["dma"] is not None:
                            key, val, sem = ("d", p["dma"]), p["dval"], dsem[p["dma"]]
                        else:
                            key, val, sem = ("p", p["eng"]), p["sval"], psem[p["eng"]]
                        if key not in grp or grp[key][0] < val:
                            grp[key] = (val, sem)
                    for key, (val, sem) in grp.items():
                        if waited.get(key, 0) >= val:
                            continue
                        waited[key] = val
                        eng.wait_ge(sem, val)
                    if o["fn"] is None:
                        continue
                    ins = o["fn"](eng)
                    if o["dma"] is not None:
                        lst = ins if isinstance(ins, (list, tuple)) else [ins]
                        assert len(lst) == o["ndma"]
                        for x in lst:
                            x.then_inc(dsem[o["dma"]], 16)
                    elif o["signal"]:
                        ins.then_inc(psem[ename], 1)

            block.tensor(lambda e: run("pe", e))
            block.scalar(lambda e: run("act", e))
            block.vector(lambda e: run("dve", e))
            block.gpsimd(lambda e: run("pool", e))
            block.sync(lambda e: run("sp", e))


def _rope_tables():
    t = np.arange(T, dtype=np.float32)
    ca = np.ones((128, T), np.float32)
    sa = np.zeros((128, T), np.float32)
    half = 8
    inv = (np.float32(500000.0) ** (-(np.arange(half, dtype=np.float32) * 2.0 / 16))).astype(np.float32)
    ang = t[None, :] * inv[:, None]
    for hh in range(2):
        b = hh * 64
        ca[b:b + half] = np.cos(ang)
        ca[b + half:b + 2 * half] = np.cos(ang)
        sa[b:b + half] = -np.sin(ang)
        sa[b + half:b + 2 * half] = np.sin(ang)
    cr = np.zeros((128, T), np.float32)
    sr = np.zeros((128, T), np.float32)
    half = 32
    inv = (np.float32(10000.0) ** (-(np.arange(half, dtype=np.float32) * 2.0 / 64))).astype(np.float32)
    ang = t[None, :] * inv[:, None]
    for hh in range(2):
        b = hh * 64
        cr[b:b + half] = np.cos(ang)
        cr[b + half:b + 64] = np.cos(ang)
        sr[b:b + half] = -np.sin(ang)
        sr[b + half:b + 64] = np.sin(ang)
    return ca, sa, cr, sr


def _consts():
    idx = np.arange(128)
    k = idx[:, None]
    q = idx[None, :]
    cur = (k <= q).astype(np.float32)
    prev = (k >= q).astype(np.float32)
    m_n = np.concatenate([cur, prev], axis=1)
    m_f = np.concatenate([cur, np.zeros_like(prev)], axis=1)
    m_fnnn = np.concatenate([m_f, m_n, m_n, m_n], axis=1)
    ones = np.ones((128, 128), np.float32)
    blk = np.zeros((128, 128), np.float32)
    blk[:64, :64] = 1
    blk[64:, 64:] = 1
    ident = np.eye(128, dtype=np.float32)
    cb = np.concatenate([ones, blk, ident], axis=1)
    hb = np.concatenate([m_n, m_fnnn], axis=1)
    h = np.arange(8, dtype=np.float64)
    log_g = np.log(1.0 - np.exp2(-5.0 - h))
    diff = (q - k).astype(np.float64)
    dm = np.zeros((128, 8, 128), np.float64)
    for hh in range(8):
        dm[:, hh, :] = np.where(diff >= 0, np.exp(log_g[hh] * np.maximum(diff, 0)), 0.0) / 8.0
    xi = np.zeros((128, 4, 128), np.float64)
    zt = np.zeros((128, 4, 128), np.float64)
    cd = np.zeros((128, 8), np.float64)
    for p in range(128):
        for pr in range(4):
            hh = pr * 2 + p // 64
            xi[p, pr, :] = np.exp(log_g[hh] * (idx + 1.0))
            cd[p, pr] = np.exp(log_g[hh] * 128.0)
    for col in range(128):
        for pr in range(4):
            hh = pr * 2 + col // 64
            zt[:, pr, col] = np.exp(log_g[hh] * (127.0 - idx)) / 8.0
    hf = np.concatenate([dm.reshape(128, -1), xi.reshape(128, -1), zt.reshape(128, -1), cd],
                        axis=1).astype(np.float32)
    return cb.astype(np.float32), hb.astype(np.float32), hf


def _hyb_cols():
    cols = []
    def perm(base, half, rot):
        out = []
        for hh in range(2):
            for j in range(64):
                if j < half:
                    jj = j + half
                elif j < 2 * half:
                    jj = j - half
                else:
                    jj = j
                out.append(base + hh * 64 + jj)
        return out
    for p in range(4):
        qa = 0 + p * 128
        ka = 512 + p * 128
        va = 1024 + p * 128
        cols += list(range(qa, qa + 128)) + perm(qa, 8, 16)
        cols += list(range(ka, ka + 128)) + perm(ka, 8, 16)
        cols += list(range(va, va + 128))
    for p in range(4):
        qr = 1536 + p * 128
        kr = 2048 + p * 128
        vr = 2560 + p * 128
        gr = 3072 + p * 128
        cols += list(range(qr, qr + 128)) + perm(qr, 32, 64)
        cols += list(range(kr, kr + 128)) + perm(kr, 32, 64)
        cols += list(range(gr, gr + 128))
        cols += list(range(vr, vr + 128))
    assert len(cols) == NEXT
    return np.array(cols, dtype=np.int64)


def ssl(st, cnt, d=1):
    return slice(st, st + (cnt - 1) * d + 1, d)


def build_program(layers, stop_after=None):
    nc = bass.Bass("TRN2", target_bir_lowering=False)

    def din(name, shape):
        return nc.dram_tensor(name, list(shape), F32, kind="ExternalInput").ap()

    xT = din("xT", [D, T])
    yT = nc.dram_tensor("yT", [D, T], F32, kind="ExternalOutput").ap()
    g_all = din("g_all", [128, 192])
    wg_d = din("ffn_w_gate", [DEPTH, 2, D, FF])
    wu_d = din("ffn_w_up", [DEPTH, 2, D, FF])
    wd_d = din("ffn_w_down", [DEPTH, 2, FF, D])
    hin_d = din("hyb_w_in_ext", [2, D, NEXT])
    hout_d = din("hyb_w_out", [2, D, D])
    gin_d = din("gmlp_w_in", [2, D, 2 * D])
    gout_d = din("gmlp_w_out", [2, D, D])
    glng_d = din("gmlp_ln_g", [2, D])
    glnb_d = din("gmlp_ln_b", [2, D])
    gws_d = din("gmlp_w_sT", [2, 128, 8 * 128])
    gbs_d = din("gmlp_b_s", [2, 8 * 128])
    ropeA_c = din("ropeA_c", [128, T])
    ropeA_s = din("ropeA_s", [128, T])
    ropeR_c = din("ropeR_c", [128, T])
    ropeR_s = din("ropeR_s", [128, T])
    cb_d = din("cb", [128, 384])
    hb_d = din("hb", [128, 1280])
    hf_d = din("hf", [128, 2056])

    es = ExitStack()
    X = es.enter_context(nc.sbuf_tensor("X", [128, 8, T], F32))
    G = es.enter_context(nc.sbuf_tensor("G", [128, 192], F32))
    CB = es.enter_context(nc.sbuf_tensor("CB", [128, 384], BF16))
    MASKC = es.enter_context(nc.sbuf_tensor("MASKC", [128, 128], BF16))
    EPSB = es.enter_context(nc.sbuf_tensor("EPSB", [128, 4], F32))
    SCRB = 142 * 1024
    SCR = es.enter_context(nc.sbuf_tensor("SCR", [128, SCRB], U8))
    PSALL = es.enter_context(nc.psum_tensor("PSALL", [128, 4096], F32))
    ONES = CB[:, 0:128]
    BLK = CB[:, 128:256]
    IDENT = CB[:, 256:384]

    def PS(b, n=1):
        return PSALL[:, b * 512:(b + n) * 512]

    class Carver:
        def __init__(self, off=0):
            self.off = off

        def take(self, dtype, shape):
            n = 1
            for s_ in shape[1:]:
                n *= s_
            nb = n * (4 if dtype == F32 else 2)
            nb = (nb + 31) // 32 * 32
            ap = SCR[:, self.off:self.off + nb].bitcast(dtype)[:, 0:n]
            if len(shape) == 3:
                ap = ap.rearrange("p (a b) -> p a b", a=shape[1])
            elif len(shape) == 4:
                ap = ap.rearrange("p (a b c) -> p a b c", a=shape[1], b=shape[2])
            self.off += nb
            assert self.off <= SCRB, (self.off, SCRB)
            return ap

    S = Sched(nc)

    def barrier():
        S.barrier(lambda e: e.memset(EPSB[:, 2:3], 0.0))

    S.op("sp", lambda e: e.dma_start(out=G[:], in_=g_all), w=[("G",)], dma=("dma", "G"))
    S.op("pool", lambda e: e.dma_start(out=CB[:], in_=cb_d), w=[("CB",)], dma=("dma", "CB"))
    S.op("pool", lambda e: e.dma_start(out=MASKC[:], in_=hb_d[:, 0:128]), w=[("MASKC",)], dma=("dma", "MASKC"))
    S.op("dve", lambda e: e.memset(EPSB[:, 0:1], EPS), w=[("EPSB0",)])
    S.op("dve", lambda e: e.memset(EPSB[:, 1:2], math.log(0.5)), r=[("EPSB0",)], w=[("EPSB",)])
    xv = xT.rearrange("(c p) t -> p c t", p=128)
    yv = yT.rearrange("(c p) t -> p c t", p=128)
    for g in range(4):
        S.op("sp", lambda e, g=g: e.dma_start(out=X[:, :, g * 512:(g + 1) * 512],
                                              in_=xv[:, :, g * 512:(g + 1) * 512]),
             w=[("X", g)], dma=("dma", "X", g))

    def rstd_from_sq(sq_ap, nk, rs_ap, inv_n, ln_half, keys_r, key_rs, lhs=None, bank=6):
        lhs = ONES if lhs is None else lhs

        def mm(e):
            ins = None
            for k in range(nk):
                src = sq_ap[:, k, :] if nk > 1 else sq_ap
                ins = e.matmul(PS(bank), lhsT=lhs, rhs=src, start=(k == 0), stop=(k == nk - 1))
            return ins
        S.op("pe", mm, r=list(keys_r) + [("CB",)], w=[("ps", bank)])
        S.op("act", lambda e: e.activation(out=rs_ap, in_=PS(bank), func=AF.Ln, scale=inv_n, bias=EPSB[:, 0:1]),
             r=[("ps", bank), ("EPSB",)], w=[key_rs])
        if ln_half:
            S.op("act", lambda e: e.activation(out=rs_ap, in_=rs_ap, func=AF.Exp, scale=-0.5, bias=EPSB[:, 1:2]),
                 r=[key_rs, ("EPSB",)], w=[key_rs])
        else:
            S.op("act", lambda e: e.activation(out=rs_ap, in_=rs_ap, func=AF.Exp, scale=-0.5),
                 r=[key_rs], w=[key_rs])

    def prenorm(gi, g, dst_fn, dst_key, SQ, RS, stage="ab", ksq=("SQ",), krs=("RS",), bank=6):
        tok = slice(g * 512, (g + 1) * 512)
        if "a" in stage:
            S.op("act", lambda e: e.activation(out=SQ, in_=X[:, :, tok], func=AF.Square),
                 r=[("X", g)], w=[ksq])
        if "b" not in stage:
            return
        rstd_from_sq(SQ, 8, RS, 1.0 / D, False, [ksq], krs, bank=bank)
        for k in range(8):
            S.op("dve", lambda e, k=k: e.scalar_tensor_tensor(
                out=dst_fn(k), in0=X[:, k, tok], scalar=G[:, gi * 8 + k:gi * 8 + k + 1], in1=RS,
                op0=ALU.mult, op1=ALU.mult),
                r=[("X", g), krs, ("G",)], w=[dst_key(k)])

    def proj_postnorm(src_fn, src_keys, nk, w_view, gi, g, half_factor, FB, SQ, RS, WDS):
        tok = slice(g * 512, (g + 1) * 512)
        for c in range(8):
            slot, key = S.consume("WD", 2, "pool",
                                  lambda e, s, c=c: e.dma_start(out=WDS[:, s, 0:nk, :],
                                                                in_=w_view[:, :, c * 128:(c + 1) * 128]))
            b = 4 + c % 2

            def mm(e, slot=slot, b=b):
                ins = None
                for j in range(nk):
                    ins = e.matmul(PS(b), lhsT=WDS[:, slot, j, :], rhs=src_fn(j), start=(j == 0), stop=(j == nk - 1))
                return ins
            S.op("pe", mm, r=[key] + list(src_keys), w=[("ps", b)])
            S.op("act", lambda e, c=c, b=b: e.activation(out=FB[:, c, :], in_=PS(b), func=AF.Copy),
                 r=[("ps", b)], w=[("F", c)])
        S.op("act", lambda e: e.activation(out=SQ, in_=FB, func=AF.Square),
             r=[("F", c) for c in range(8)], w=[("SQ",)])
        rstd_from_sq(SQ, 8, RS, 1.0 / D, half_factor, [("SQ",)], ("RS",))
        for c in range(8):
            S.op("dve", lambda e, c=c: e.scalar_tensor_tensor(
                out=FB[:, c, :], in0=FB[:, c, :], scalar=G[:, gi * 8 + c:gi * 8 + c + 1], in1=RS,
                op0=ALU.mult, op1=ALU.mult), r=[("F", c), ("RS",), ("G",)], w=[("F", c)])
        for c in range(8):
            S.op("dve", lambda e, c=c: e.tensor_tensor(out=X[:, c, tok], in0=X[:, c, tok], in1=FB[:, c, :], op=ALU.add),
                 r=[("F", c), ("X", g)], w=[("X", g)])

    def ffn_seq(items):
        cv = Carver()
        H = cv.take(BF16, [128, 8, 1024])
        ACTB = cv.take(BF16, [128, NJ, 1024])
        FB = cv.take(F32, [128, 8, 1024])
        SQ = cv.take(BF16, [128, 8, 512])
        RS = cv.take(F32, [128, 512])
        SQ2 = cv.take(BF16, [128, 8, 512])
        RS2 = cv.take(F32, [128, 512])
        SG = cv.take(F32, [128, 2, 512])
        WGU = cv.take(BF16, [128, 3, 16, 128])
        WDS = cv.take(BF16, [128, 2, NJ, 128])
        halves = [(l, i, hf) for (l, i) in items for hf in range(2)]

        def gidx(l, i):
            return l * 6 + (0 if i == 0 else 4)

        def pre(hv, stage, tts):
            l, i, hf = hv
            for tt in tts:
                if tt == 0:
                    prenorm(gidx(l, i), hf * 2 + tt, lambda k, tt=tt: H[:, k, tt * 512:(tt + 1) * 512],
                            lambda k, tt=tt: ("H", tt), SQ2, RS2, stage=stage, ksq=("SQ2",), krs=("RS2",), bank=7)
                else:
                    prenorm(gidx(l, i), hf * 2 + tt, lambda k, tt=tt: H[:, k, tt * 512:(tt + 1) * 512],
                            lambda k, tt=tt: ("H", tt), SQ, RS, stage=stage, ksq=("SQ",), krs=("RS",), bank=6)

        def phase1(hv):
            l, i, hf = hv
            wgv = wg_d[l, i].rearrange("(k p) n -> p k n", p=128)
            wuv = wu_d[l, i].rearrange("(k p) n -> p k n", p=128)
            for j in range(NJ):
                def ld(e, s, j=j):
                    a_ = e.dma_start(out=WGU[:, s, 0:8, :], in_=wgv[:, :, j * 128:(j + 1) * 128])
                    b_ = e.dma_start(out=WGU[:, s, 8:16, :], in_=wuv[:, :, j * 128:(j + 1) * 128])
                    return [a_, b_]
                slot, key = S.consume("WGU", 3, "pool", ld, ndma=2)
                for tt in range(2):
                    bg, bu = tt, 2 + tt

                    def mm(e, slot=slot, tt=tt, bg=bg, bu=bu):
                        ins = None
                        for k in range(8):
                            ins = e.matmul(PS(bg), lhsT=WGU[:, slot, k, :], rhs=H[:, k, tt * 512:(tt + 1) * 512],
                                           start=(k == 0), stop=(k == 7))
                        for k in range(8):
                            ins = e.matmul(PS(bu), lhsT=WGU[:, slot, 8 + k, :], rhs=H[:, k, tt * 512:(tt + 1) * 512],
                                           start=(k == 0), stop=(k == 7))
                        return ins
                    S.op("pe", mm, r=[key, ("H", tt)], w=[("ps", bg), ("ps", bu)])
                    S.op("act", lambda e, tt=tt, bg=bg: e.activation(out=SG[:, tt, :], in_=PS(bg), func=AF.Silu),
                         r=[("ps", bg)], w=[("SG", tt)])
                    S.op("dve", lambda e, bu=bu, j=j, tt=tt: e.tensor_tensor(
                        out=ACTB[:, j, tt * 512:(tt + 1) * 512], in0=SG[:, tt, :], in1=PS(bu), op=ALU.mult),
                        r=[("SG", tt), ("ps", bu)], w=[("A", j, tt)])

        def phase2(hv, hook):
            l, i, hf = hv
            wdv = wd_d[l, i].rearrange("(j p) n -> p j n", p=128)
            gi = gidx(l, i) + 1
            for c in range(8):
                slot, key = S.consume("WD", 2, "pool",
                                      lambda e, s, c=c: e.dma_start(out=WDS[:, s, :, :],
                                                                    in_=wdv[:, :, c * 128:(c + 1) * 128]))
                for tt in range(2):
                    b = 4 + tt

                    def mm(e, slot=slot, b=b, tt=tt):
                        ins = None
                        for j in range(NJ):
                            ins = e.matmul(PS(b), lhsT=WDS[:, slot, j, :], rhs=ACTB[:, j, tt * 512:(tt + 1) * 512],
                                           start=(j == 0), stop=(j == NJ - 1))
                        return ins
                    S.op("pe", mm, r=[key] + [("A", j, tt) for j in range(NJ)], w=[("ps", b)])
                    S.op("act", lambda e, c=c, b=b, tt=tt: e.activation(out=FB[:, c, tt * 512:(tt + 1) * 512], in_=PS(b),
                                                                         func=AF.Copy),
                         r=[("ps", b)], w=[("F", c, tt)])
                if c == 0 and hook is not None:
                    hook()
            for tt in range(2):
                g = hf * 2 + tt
                tok = slice(g * 512, (g + 1) * 512)
                fsl = slice(tt * 512, (tt + 1) * 512)
                fk = [("F", c, tt) for c in range(8)]
                S.op("act", lambda e, fsl=fsl: e.activation(out=SQ, in_=FB[:, :, fsl], func=AF.Square),
                     r=fk, w=[("SQ",)])
                rstd_from_sq(SQ, 8, RS, 1.0 / D, True, [("SQ",)], ("RS",))
                for c in range(8):
                    S.op("dve", lambda e, c=c, fsl=fsl: e.scalar_tensor_tensor(
                        out=FB[:, c, fsl], in0=FB[:, c, fsl], scalar=G[:, gi * 8 + c:gi * 8 + c + 1], in1=RS,
                        op0=ALU.mult, op1=ALU.mult), r=[("F", c, tt), ("RS",), ("G",)], w=[("F", c, tt)])
                for c in range(8):
                    eng = "dve" if c % 2 == 0 else "pool"
                    S.op(eng, lambda e, c=c, tok=tok, fsl=fsl: e.tensor_tensor(out=X[:, c, tok], in0=X[:, c, tok],
                                                                               in1=FB[:, c, fsl], op=ALU.add),
                         r=[("F", c, tt), ("X", g)], w=[("X", g)])

        pre(halves[0], "ab", (0, 1))
        for idx, hv in enumerate(halves):
            phase1(hv)
            nxt = halves[idx + 1] if idx + 1 < len(halves) else None
            if nxt is not None:
                pre(nxt, "a", (0, 1))

                def hook(nxt=nxt):
                    pre(nxt, "b", (0, 1))
                phase2(hv, hook)
            else:
                phase2(hv, None)

    def gmlp(l):
        jl = l // 2
        cv = Carver()
        H = cv.take(BF16, [128, 8, 512])
        UT = cv.take(BF16, [128, 8, 512])
        YT = cv.take(BF16, [128, 8, 512])
        VG = cv.take(F32, [128, 4, 1024])
        VN = cv.take(BF16, [128, 4, 1024])
        LNG = cv.take(F32, [128, 1024])
        LNB = cv.take(F32, [128, 1024])
        WST = cv.take(BF16, [128, 8, 128])
        BS = cv.take(F32, [128, 8, 128])
        WIN = cv.take(BF16, [128, 3, 8, 128])
        WV = cv.take(BF16, [128, 2, 8, 512])
        FB = cv.take(F32, [128, 8, 512])
        SQ = cv.take(BF16, [128, 8, 512])
        RS = cv.take(F32, [128, 512])
        Y1 = cv.take(F32, [128, 2, 512])
        ST = cv.take(F32, [128, 4, 16])
        MV = cv.take(F32, [128, 4, 2])
        RSD = cv.take(F32, [128, 4])
        WDS = cv.take(BF16, [128, 2, 8, 128])
        winv = gin_d[jl].rearrange("(k p) n -> p k n", p=128)
        woutv = gout_d[jl].rearrange("(j p) n -> p j n", p=128)
        gi = l * 6 + 2
        S.op("sp", lambda e: e.dma_start(out=LNG, in_=glng_d[jl:jl + 1, :].to_broadcast([128, D])),
             w=[("LNG",)], dma=("dma", "LNG"))
        S.op("sp", lambda e: e.dma_start(out=LNB, in_=glnb_d[jl:jl + 1, :].to_broadcast([128, D])),
             w=[("LNB",)], dma=("dma", "LNB"))
        S.op("sp", lambda e: e.dma_start(out=BS.rearrange("p a b -> p (a b)"),
                                         in_=gbs_d[jl:jl + 1, :].to_broadcast([128, D])),
             w=[("BS",)], dma=("dma", "BS"))
        S.op("pool", lambda e: e.dma_start(out=WST.rearrange("p a b -> p (a b)"), in_=gws_d[jl]),
             w=[("WST0",)], dma=("dma", "WST"))
        S.op("dve", lambda e: e.tensor_tensor(out=WST, in0=WST, in1=MASKC[:, None, :].to_broadcast([128, 8, 128]),
                                              op=ALU.mult),
             r=[("WST0",), ("MASKC",)], w=[("WST",)])
        SQ2 = cv.take(BF16, [128, 8, 512])
        RS2 = cv.take(F32, [128, 512])

        def pre(g):
            prenorm(gi, g, lambda k: H[:, k, :], lambda k: ("H",), SQ2, RS2, ksq=("SQ2",), krs=("RS2",), bank=7)

        pre(0)
        for g in range(4):
            for hv in range(2):
                slot, key = S.consume("GWV", 2, "pool",
                                      lambda e, s, hv=hv: e.dma_start(out=WV[:, s],
                                                                       in_=winv[:, :, D + hv * 512:D + (hv + 1) * 512]))
                for m in range(4):
                    b = 2 + (hv * 4 + m) % 2

                    def mm(e, slot=slot, b=b, m=m):
                        ins = None
                        for k in range(8):
                            ins = e.matmul(PS(b), lhsT=H[:, k, m * 128:(m + 1) * 128], rhs=WV[:, slot, k, :],
                                           start=(k == 0), stop=(k == 7))
                        return ins
                    S.op("pe", mm, r=[key, ("H",)], w=[("ps", b)])
                    S.op("act", lambda e, b=b, m=m, hv=hv: e.activation(
                        out=VG[:, m, hv * 512:(hv + 1) * 512], in_=PS(b), func=AF.Gelu),
                        r=[("ps", b)], w=[("VG", m, hv)])
                    S.op("dve", lambda e, m=m, hv=hv: e.bn_stats(out=ST[:, m, hv * 6:(hv + 1) * 6],
                                                                  in_=VG[:, m, hv * 512:(hv + 1) * 512]),
                         r=[("VG", m, hv)], w=[("STAT", m, hv)])
            for m in range(4):
                S.op("dve", lambda e, m=m: e.bn_aggr(out=MV[:, m, :], in_=ST[:, m, 0:12]),
                     r=[("STAT", m, 0), ("STAT", m, 1)], w=[("MV", m)])
            S.op("act", lambda e: e.activation(out=RSD, in_=MV[:, :, 1], func=AF.Ln, bias=EPSB[:, 0:1]),
                 r=[("MV", m) for m in range(4)] + [("EPSB",)], w=[("RSD",)])
            S.op("act", lambda e: e.activation(out=RSD, in_=RSD, func=AF.Exp, scale=-0.5),
                 r=[("RSD",)], w=[("RSD",)])
            for m in range(4):
                vk = [("VG", m, 0), ("VG", m, 1)]
                S.op("dve", lambda e, m=m: e.tensor_scalar(out=VG[:, m, :], in0=VG[:, m, :], scalar1=MV[:, m, 0:1],
                                                           scalar2=RSD[:, m:m + 1], op0=ALU.subtract, op1=ALU.mult),
                     r=vk + [("MV", m), ("RSD",)], w=vk)
                S.op("dve", lambda e, m=m: e.tensor_tensor(out=VG[:, m, :], in0=VG[:, m, :], in1=LNG, op=ALU.mult),
                     r=vk + [("LNG",)], w=vk)
                S.op("dve", lambda e, m=m: e.tensor_tensor(out=VN[:, m, :], in0=VG[:, m, :], in1=LNB, op=ALU.add),
                     r=vk + [("LNB",)], w=[("VN", m)])
            for ct in range(8):
                slot, key = S.consume("GWIN", 3, "pool",
                                      lambda e, s, ct=ct: e.dma_start(out=WIN[:, s], in_=winv[:, :, ct * 128:(ct + 1) * 128]))
                b = ct % 2

                def mm(e, slot=slot, b=b):
                    ins = None
                    for k in range(8):
                        ins = e.matmul(PS(b), lhsT=WIN[:, slot, k, :], rhs=H[:, k, :], start=(k == 0), stop=(k == 7))
                    return ins
                S.op("pe", mm, r=[key, ("H",)], w=[("ps", b)])
                S.op("act", lambda e, ct=ct, b=b: e.activation(out=UT[:, ct, :], in_=PS(b), func=AF.Gelu),
                     r=[("ps", b)], w=[("UT", ct)])
            for gg in range(8):
                b = 4 + gg % 2

                def mm(e, gg=gg, b=b):
                    ins = None
                    for m in range(4):
                        ins = e.matmul(PS(b)[:, m * 128:(m + 1) * 128], lhsT=VN[:, m, gg * 128:(gg + 1) * 128],
                                       rhs=WST[:, gg, :], start=True, stop=True)
                    return ins
                S.op("pe", mm, r=[("VN", m) for m in range(4)] + [("WST",)], w=[("ps", b)])
                u = gg % 2
                S.op("dve", lambda e, gg=gg, b=b, u=u: e.tensor_tensor(
                    out=Y1[:, u, :].rearrange("p (m i) -> p m i", m=4),
                    in0=PS(b).rearrange("p (m i) -> p m i", m=4),
                    in1=BS[:, gg:gg + 1, :].to_broadcast([128, 4, 128]), op=ALU.add),
                    r=[("ps", b), ("BS",)], w=[("Y1", u)])
                S.op("dve", lambda e, gg=gg, u=u: e.tensor_tensor(out=YT[:, gg, :], in0=Y1[:, u, :], in1=UT[:, gg, :],
                                                                   op=ALU.mult),
                     r=[("Y1", u), ("UT", gg)], w=[("YT", gg)])
            if g + 1 < 4:
                pre(g + 1)
            proj_postnorm(lambda j: YT[:, j, :], [("YT", j) for j in range(8)], 8, woutv, gi + 1, g, False,
                          FB, SQ, RS, WDS)

    def hybrid(l):
        jl = l // 2
        cv = Carver()
        HT = cv.take(BF16, [128, 8, T])
        MIX = cv.take(BF16, [128, 8, T])
        WIN = cv.take(BF16, [128, 6, 8, 128])
        ROPE = cv.take(F32, [128, 2, 2, 512])
        TMP = cv.take(F32, [128, 2, 512])
        RS = cv.take(F32, [128, 512])
        pair_off = cv.off
        winv = hin_d[jl].rearrange("(k p) n -> p k n", p=128)
        woutv = hout_d[jl].rearrange("(j p) n -> p j n", p=128)
        gi = l * 6 + 2
        SQ0 = Carver(pair_off).take(BF16, [128, 8, 512])
        for g in range(4):
            prenorm(gi, g, lambda k, g=g: HT[:, k, g * 512:(g + 1) * 512], lambda k, g=g: ("HT", g), SQ0, RS)
        barrier()
        HTK = [("HT", g) for g in range(4)]

        def win_block(col0):
            return S.consume("HWIN", 6, "pool",
                             lambda e, s, col0=col0: e.dma_start(out=WIN[:, s], in_=winv[:, :, col0:col0 + 128]),
                             lookahead=2)

        def rope_proj(col0, rc, rs_, dsts, fam):
            blks = [win_block(col0 + i * 128) for i in range(4)]
            for g in range(4):
                tok = slice(g * 512, (g + 1) * 512)

                def ldrope(e, s, g=g):
                    a = e.dma_start(out=ROPE[:, s, 0, :], in_=rc[:, g * 512:(g + 1) * 512])
                    b = e.dma_start(out=ROPE[:, s, 1, :], in_=rs_[:, g * 512:(g + 1) * 512])
                    return [a, b]
                rslot, rkey = S.consume("ROPE", 2, "sp", ldrope, ndma=2)
                for qi in range(2):
                    u = (g * 2 + qi) % 2
                    b0, b1 = 2 * u, 2 * u + 1
                    (s0, k0), (s1, k1) = blks[2 * qi], blks[2 * qi + 1]

                    def mm(e, s0=s0, s1=s1, b0=b0, b1=b1, tok=tok):
                        ins = None
                        for k in range(8):
                            ins = e.matmul(PS(b0), lhsT=WIN[:, s0, k, :], rhs=HT[:, k, tok], start=(k == 0), stop=(k == 7))
                        for k in range(8):
                            ins = e.matmul(PS(b1), lhsT=WIN[:, s1, k, :], rhs=HT[:, k, tok], start=(k == 0), stop=(k == 7))
                        return ins
                    S.op("pe", mm, r=[k0, k1, ("HT", g)], w=[("ps", b0), ("ps", b1)])
                    S.op("dve", lambda e, b0=b0, rslot=rslot: e.tensor_tensor(
                        out=TMP[:, 0, :], in0=PS(b0), in1=ROPE[:, rslot, 0, :], op=ALU.mult),
                        r=[("ps", b0), rkey], w=[("TMP", 0)])
                    S.op("dve", lambda e, b1=b1, rslot=rslot: e.tensor_tensor(
                        out=TMP[:, 1, :], in0=PS(b1), in1=ROPE[:, rslot, 1, :], op=ALU.mult),
                        r=[("ps", b1), rkey], w=[("TMP", 1)])
                    dst = dsts[qi]
                    S.op("dve", lambda e, dst=dst, tok=tok: e.tensor_tensor(
                        out=dst[:, tok], in0=TMP[:, 0, :], in1=TMP[:, 1, :], op=ALU.add),
                        r=[("TMP", 0), ("TMP", 1)], w=[("QK", fam, qi, g)])

        def dswa_pair(p):
            cvp = Carver(pair_off)
            HB = cvp.take(BF16, [128, 1280])
            QT = cvp.take(BF16, [128, T])
            KT = cvp.take(BF16, [128, T])
            VA = cvp.take(BF16, [128, 3, 16, 192])
            ACC = cvp.take(F32, [128, 2, T])
            E = cvp.take(BF16, [128, 2, 1024])
            M_N = HB[:, 0:256]
            M_F = HB[:, 256:1280]
            RC = TMP
            col0 = p * 640
            if p == 0:
                S.op("pool", lambda e: e.dma_start(out=HB, in_=hb_d), w=[("HB",)], dma=("dma", "HB"))
                S.op("pool", lambda e: e.memset(VA[:, :, :, 64:128], 1.0), w=[("VA1",)])
            rope_proj(col0, ropeA_c, ropeA_s, [QT, KT], "a")
            vs, vk = win_block(col0 + 512)
            for bi, d in enumerate((1, 4, 16)):
                tpc = 16 // d
                for i4 in range(4):
                    b = 4 + (bi * 4 + i4) % 2

                    def mm(e, b=b, d=d, i4=i4, tpc=tpc):
                        ins = None
                        for ii in range(4):
                            i = i4 * 4 + ii
                            r, blk = i // tpc, i % tpc
                            st = blk * 128 * d + r
                            for k in range(8):
                                ins = e.matmul(PS(b)[:, ii * 128:(ii + 1) * 128],
                                               lhsT=HT[:, k, ssl(st, 128, d)], rhs=WIN[:, vs, k, :],
                                               start=(k == 0), stop=(k == 7))
                        return ins
                    S.op("pe", mm, r=[vk] + HTK, w=[("ps", b)])
                    S.op("act", lambda e, b=b, bi=bi, i4=i4: e.activation(
                        out=VA[:, bi, i4 * 4:(i4 + 1) * 4, :].rearrange("p i (s c) -> p i s c", s=3)[:, :, 0:3:2, :],
                        in_=PS(b).rearrange("p (i s c) -> p i s c", i=4, s=2), func=AF.Copy),
                        r=[("ps", b), ("VA1",)], w=[("VA", bi, i4)])
            QKK = [("QK", "a", qi, g) for qi in range(2) for g in range(4)]
            unit = 0
            units = []
            for hh in range(2):
                hs = slice(hh * 64, hh * 64 + 64)
                vcols = slice(hh * 64, hh * 64 + 128)
                for bi, d in enumerate((1, 4, 16)):
                    tpc = 16 // d
                    for i4 in range(4):
                        su = unit % 2
                        unit += 1
                        sb0 = 2 * su
                        ob = 4 + su
                        blocks = []
                        for ii in range(4):
                            i = i4 * 4 + ii
                            r, n = i // tpc, i % tpc
                            blocks.append((i, n, n * 128 * d + r, (n - 1) * 128 * d + r))
                        noprev = (d == 16)
                        W = 128 if noprev else 256

                        def emit_s(blocks=blocks, sb0=sb0, d=d, hs=hs, noprev=noprev, W=W):
                            def mm_s(e):
                                ins = None
                                for ii, (i, n, st, pst) in enumerate(blocks):
                                    qsl = ssl(st, 128, d)
                                    ins = e.matmul(PS(sb0, 2)[:, ii * W:ii * W + 128], lhsT=KT[hs, qsl], rhs=QT[hs, qsl],
                                                   start=True, stop=True)
                                    if not noprev:
                                        ksl = qsl if n == 0 else ssl(pst, 128, d)
                                        ins = e.matmul(PS(sb0, 2)[:, ii * W + 128:ii * W + 256], lhsT=KT[hs, ksl],
                                                       rhs=QT[hs, qsl], start=True, stop=True)
                                return ins
                            S.op("pe", mm_s, r=QKK, w=[("ps", sb0), ("ps", sb0 + 1)])

                        def emit_rest(blocks=blocks, sb0=sb0, su=su, ob=ob, d=d, bi=bi, hh=hh, vcols=vcols,
                                      noprev=noprev, W=W):
                            S.op("act", lambda e: e.activation(
                                out=E[:, su, 0:4 * W], in_=PS(sb0, 2)[:, 0:4 * W], func=AF.Exp, scale=0.125),
                                r=[("ps", sb0), ("ps", sb0 + 1)], w=[("E", su)])
                            if noprev:
                                msk = M_N[:, None, 0:128].to_broadcast([128, 4, 128])
                                ev = E[:, su, 0:512].rearrange("p (a b) -> p a b", a=4)
                            elif blocks[0][1] == 0:
                                msk = M_F
                                ev = E[:, su, :]
                            else:
                                msk = M_N[:, None, :].to_broadcast([128, 4, 256])
                                ev = E[:, su, :].rearrange("p (a b) -> p a b", a=4)
                            S.op("dve", lambda e: e.tensor_tensor(out=ev, in0=ev, in1=msk, op=ALU.mult),
                                 r=[("E", su), ("HB",)], w=[("E", su)])

                            def mm_o(e):
                                ins = None
                                for ii, (i, n, st, pst) in enumerate(blocks):
                                    hasprev = (not noprev) and n > 0
                                    ins = e.matmul(PS(ob)[:, ii * 128:(ii + 1) * 128], lhsT=VA[:, bi, i, vcols],
                                                   rhs=E[:, su, ii * W:ii * W + 128], start=True, stop=not hasprev)
                                    if hasprev:
                                        ins = e.matmul(PS(ob)[:, ii * 128:(ii + 1) * 128], lhsT=VA[:, bi, i - 1, vcols],
                                                       rhs=E[:, su, ii * W + 128:ii * W + 256], start=False, stop=True)
                                return ins
                            S.op("pe", mm_o, r=[("E", su)] + [("VA", bi, x) for x in range(4)], w=[("ps", ob)])
                            if d == 16:
                                r0 = blocks[0][0]
                                dst = ACC[:, hh, :].rearrange("p (m r) -> p r m", r=16)[:, r0:r0 + 4, :]
                                src = PS(ob).rearrange("p (a m) -> p a m", a=4)
                            else:
                                dst = ACC[:, hh, ssl(blocks[0][2], 512, d)]
                                src = PS(ob)
                            if bi == 0:
                                S.op("dve", lambda e: e.tensor_copy(out=dst, in_=src),
                                     r=[("ps", ob)], w=[("ACC", hh)])
                            else:
                                S.op("dve", lambda e: e.tensor_tensor(out=dst, in0=dst, in1=src, op=ALU.add),
                                     r=[("ps", ob), ("ACC", hh)], w=[("ACC", hh)])
                        units.append((emit_s, emit_rest, hh, bi == 2 and i4 == 3))

            def normalise(hh):
                orow = slice(hh * 64, hh * 64 + 64)
                drow = slice(64 - hh * 64, 128 - hh * 64)
                for g in range(4):
                    tok = slice(g * 512, (g + 1) * 512)
                    u = g % 2
                    S.op("act", lambda e, tok=tok, u=u: e.activation(
                        out=RC[orow, u, :], in_=ACC[drow, hh, tok], func=AF.Ln), r=[("ACC", hh)], w=[("TMP", u)])
                    S.op("act", lambda e, u=u: e.activation(
                        out=RC[orow, u, :], in_=RC[orow, u, :], func=AF.Exp, scale=-1.0), r=[("TMP", u)], w=[("TMP", u)])
                    S.op("dve", lambda e, tok=tok, u=u: e.tensor_tensor(
                        out=MIX[orow, p, tok], in0=ACC[orow, hh, tok], in1=RC[orow, u, :], op=ALU.mult),
                        r=[("TMP", u), ("ACC", hh)], w=[("MIX", p, hh, g)])

            prev = None
            for un in units:
                un[0]()
                if prev is not None:
                    prev[1]()
                    if prev[3]:
                        normalise(prev[2])
                prev = un
            prev[1]()
            normalise(prev[2])

        def ret_pair(p):
            cvp = Carver(pair_off)
            HF = cvp.take(F32, [128, 2056])
            QT = cvp.take(BF16, [128, T])
            KT = cvp.take(BF16, [128, T])
            QX = cvp.take(BF16, [128, T])
            GT = cvp.take(BF16, [128, T])
            VT = cvp.take(BF16, [128, 16, 128])
            KZ = cvp.take(BF16, [128, 16, 128])
            R32 = cvp.take(F32, [128, T])
            ST32 = cvp.take(F32, [128, 64])
            STB = cvp.take(BF16, [128, 2, 64])
            SD = cvp.take(BF16, [128, 2, 256])
            SQH = cvp.take(BF16, [128, 512])
            DM = HF[:, 0:1024].rearrange("p (h c) -> p h c", h=8)
            XI = HF[:, 1024:1536].rearrange("p (a c) -> p a c", a=4)
            ZT = HF[:, 1536:2048].rearrange("p (a c) -> p a c", a=4)
            CD = HF[:, 2048:2056]
            col0 = 4 * 640 + p * 768
            if p == 0:
                S.op("sp", lambda e: e.dma_start(out=HF, in_=hf_d), w=[("HF",)], dma=("dma", "HF"))
            rope_proj(col0, ropeR_c, ropeR_s, [QT, KT], "r")
            for g in range(4):
                tok = slice(g * 512, (g + 1) * 512)
                S.op("dve", lambda e, tok=tok: e.tensor_tensor(
                    out=QX[:, tok].rearrange("p (a c) -> p a c", a=4), in0=QT[:, tok].rearrange("p (a c) -> p a c", a=4),
                    in1=XI[:, p:p + 1, :].to_broadcast([128, 4, 128]), op=ALU.mult),
                    r=[("QK", "r", 0, g), ("HF",)], w=[("QX", g)])
            gs, gk = win_block(col0 + 512)
            for g in range(4):
                tok = slice(g * 512, (g + 1) * 512)
                b = g % 2

                def mm(e, b=b, tok=tok):
                    ins = None
                    for k in range(8):
                        ins = e.matmul(PS(b), lhsT=WIN[:, gs, k, :], rhs=HT[:, k, tok], start=(k == 0), stop=(k == 7))
                    return ins
                S.op("pe", mm, r=[gk, ("HT", g)], w=[("ps", b)])
                S.op("act", lambda e, b=b, tok=tok: e.activation(out=GT[:, tok], in_=PS(b), func=AF.Silu),
                     r=[("ps", b)], w=[("GT", g)])
            vs, vk = win_block(col0 + 640)
            for i4 in range(4):
                b = 2 + i4 % 2

                def mm(e, b=b, i4=i4):
                    ins = None
                    for ii in range(4):
                        i = i4 * 4 + ii
                        for k in range(8):
                            ins = e.matmul(PS(b)[:, ii * 128:(ii + 1) * 128], lhsT=HT[:, k, i * 128:(i + 1) * 128],
                                           rhs=WIN[:, vs, k, :], start=(k == 0), stop=(k == 7))
                    return ins
                S.op("pe", mm, r=[vk] + HTK, w=[("ps", b)])
                S.op("act", lambda e, b=b, i4=i4: e.activation(
                    out=VT[:, i4 * 4:(i4 + 1) * 4, :], in_=PS(b).rearrange("p (i c) -> p i c", i=4), func=AF.Copy),
                    r=[("ps", b)], w=[("VT", i4)])
            PSB7 = PS(7).bitcast(BF16)
            for i4 in range(DEBUG.get("ntr", 4)):
                def tr(e, i4=i4):
                    ins = None
                    for ii in range(4):
                        i = i4 * 4 + ii
                        ins = e.transpose(PSB7[:, ii * 128:(ii + 1) * 128], KT[:, i * 128:(i + 1) * 128], IDENT)
                    return ins
                S.op("pe", tr, r=[("QK", "r", 1, i4), ("CB",)], w=[("ps", 7)])
                S.op("dve", lambda e, i4=i4: e.tensor_tensor(
                    out=KZ[:, i4 * 4:(i4 + 1) * 4, :], in0=PSB7[:, 0:512].rearrange("p (i c) -> p i c", i=4),
                    in1=ZT[:, p:p + 1, :].to_broadcast([128, 4, 128]), op=ALU.mult),
                    r=[("ps", 7), ("HF",)], w=[("KZ", i4)])
            S.op("dve", lambda e: e.memset(ST32, 0.0), w=[("ST32",)])
            def r_skv(n):
                g = n // 4
                ck = slice(n * 128, (n + 1) * 128)
                sb = n % 2
                b0 = 2 * sb
                kvb = 6 + n % 2

                def mm_s(e):
                    e.matmul(PS(b0)[:, 0:128], lhsT=KT[0:64, ck], rhs=QT[0:64, ck], start=True, stop=True)
                    return e.matmul(PS(b0 + 1)[:, 0:128], lhsT=KT[64:128, ck], rhs=QT[64:128, ck], start=True, stop=True)
                S.op("pe", mm_s, r=[("QK", "r", 0, g), ("QK", "r", 1, g)], w=[("ps", b0), ("ps", b0 + 1)])
                S.op("dve", lambda e: e.tensor_tensor(
                    out=SD[:, sb, :].rearrange("p (h c) -> p h c", h=2),
                    in0=PS(b0, 2).rearrange("p (h c) -> p h c", h=2)[:, :, 0:128], in1=DM[:, 2 * p:2 * p + 2, :],
                    op=ALU.mult),
                    r=[("ps", b0), ("ps", b0 + 1), ("HF",)], w=[("SD", sb)])

                def mm_kv(e):
                    e.matmul(PS(kvb)[0:64, 0:64], lhsT=KZ[:, n, 0:64], rhs=VT[:, n, 0:64], start=True, stop=True)
                    return e.matmul(PS(kvb)[64:128, 0:64], lhsT=KZ[:, n, 64:128], rhs=VT[:, n, 64:128],
                                    start=True, stop=True)
                if n < 15:
                    S.op("pe", mm_kv, r=[("KZ", n // 4), ("VT", n // 4)], w=[("ps", kvb)])

            def r_upd(n):
                kvb = 6 + n % 2
                if n < 15:
                    S.op("dve", lambda e: e.scalar_tensor_tensor(
                        out=ST32, in0=ST32, scalar=CD[:, p:p + 1], in1=PS(kvb)[:, 0:64], op0=ALU.mult, op1=ALU.add),
                        r=[("ps", kvb), ("ST32",), ("HF",)], w=[("ST32",)])
                    S.op("act", lambda e: e.activation(out=STB[:, (n + 1) % 2, :], in_=ST32, func=AF.Copy),
                         r=[("ST32",)], w=[("STB", (n + 1) % 2)])

            def r_out(n):
                g = n // 4
                ck = slice(n * 128, (n + 1) * 128)
                sb = n % 2

                def mm_o(e):
                    oc = slice((n % 4) * 128, (n % 4 + 1) * 128)
                    st = n % 2
                    ins = None
                    for hh in range(2):
                        hs = slice(hh * 64, hh * 64 + 64)
                        ins = e.matmul(PS(4 + hh)[hs, oc], lhsT=VT[:, n, hs], rhs=SD[:, sb, hh * 128:(hh + 1) * 128],
                                       start=True, stop=(n == 0))
                        if n > 0:
                            ins = e.matmul(PS(4 + hh)[hs, oc], lhsT=STB[hs, st, :], rhs=QX[hs, ck], start=False, stop=True)
                    return ins
                rk = [("SD", sb), ("VT", n // 4), ("QX", g)] + ([("STB", n % 2)] if n > 0 else [])
                S.op("pe", mm_o, r=rk, w=[("ps", 4), ("ps", 5)])
                if n % 4 == 3:
                    tok = slice(g * 512, (g + 1) * 512)
                    S.op("act", lambda e: e.activation(out=R32[0:64, tok], in_=PS(4)[0:64, :], func=AF.Copy),
                         r=[("ps", 4)], w=[("R32a", g)])
                    S.op("act", lambda e: e.activation(out=R32[64:128, tok], in_=PS(5)[64:128, :], func=AF.Copy),
                         r=[("ps", 5)], w=[("R32", g)])

            r_skv(0)
            r_upd(0)
            for n in range(16):
                if n + 1 < 16:
                    r_skv(n + 1)
                r_out(n)
                if n + 1 < 16:
                    r_upd(n + 1)
            for g in range(4):
                tok = slice(g * 512, (g + 1) * 512)
                S.op("act", lambda e, tok=tok: e.activation(out=SQH, in_=R32[:, tok], func=AF.Square),
                     r=[("R32", g), ("R32a", g)], w=[("SQH",)])
                rstd_from_sq(SQH, 1, RS, 1.0 / 64, False, [("SQH",)], ("RS",), lhs=BLK)
                S.op("dve", lambda e, tok=tok: e.tensor_tensor(out=TMP[:, 0, :], in0=R32[:, tok], in1=RS, op=ALU.mult),
                     r=[("R32", g), ("R32a", g), ("RS",)], w=[("TMP", 0)])
                S.op("dve", lambda e, tok=tok: e.tensor_tensor(out=MIX[:, 4 + p, tok], in0=TMP[:, 0, :], in1=GT[:, tok],
                                                                op=ALU.mult),
                     r=[("TMP", 0), ("GT", g)], w=[("MIX", 4 + p, 0, g), ("MIX", 4 + p, 1, g)])

        for p in range(DEBUG.get("ndswa", 4)):
            dswa_pair(p)
        barrier()
        for p in range(DEBUG.get("nret", 4)):
            ret_pair(p)
        barrier()
        if DEBUG.get("dump_mix"):
            for g in range(4):
                S.op("act", lambda e, g=g: e.activation(out=X[:, :, g * 512:(g + 1) * 512], in_=MIX[:, :, g * 512:(g + 1) * 512],
                                                        func=AF.Copy),
                     r=[("MIX", j, hh, g) for j in range(8) for hh in range(2)] + [("X", g)], w=[("X", g)])
            return
        cvo = Carver(pair_off)
        FB = cvo.take(F32, [128, 8, 512])
        SQ = cvo.take(BF16, [128, 8, 512])
        WDS = cvo.take(BF16, [128, 2, 8, 128])
        for g in range(4):
            tok = slice(g * 512, (g + 1) * 512)
            proj_postnorm(lambda j, tok=tok: MIX[:, j, tok],
                          [("MIX", j, hh, g) for j in range(8) for hh in range(2)], 8, woutv, gi + 1, g, False,
                          FB, SQ, RS, WDS)

    phases = []
    for l in layers:
        phases += [("ffn", l, 0), ("mix", l), ("ffn", l, 1)]
    if stop_after is not None:
        phases = phases[:stop_after]
    idx = 0
    while idx < len(phases):
        ph = phases[idx]
        if ph[0] == "ffn":
            items = [(ph[1], ph[2])]
            while idx + 1 < len(phases) and phases[idx + 1][0] == "ffn":
                idx += 1
                items.append((phases[idx][1], phases[idx][2]))
            ffn_seq(items)
        else:
            l = ph[1]
            if DEBUG.get("skipmix"):
                pass
            elif l % 2 == 0:
                hybrid(l)
            else:
                gmlp(l)
        barrier()
        idx += 1

    for g in range(4):
        S.op("sp", lambda e, g=g: e.dma_start(out=yv[:, :, g * 512:(g + 1) * 512], in_=X[:, :, g * 512:(g + 1) * 512]),
             r=[("X", g)], w=[("Y", g)], dma=("dma", "Y", g))
    S.op("sp", None, r=[("Y", g) for g in range(4)])
    S.finalize()
    S.emit()
    es.close()
    return nc, S


_CACHE = {}


def _host_consts():
    if "c" not in _CACHE:
        ca, sa, cr, sr = _rope_tables()
        cb, hb, hf = _consts()
        _CACHE["c"] = dict(ropeA_c=ca, ropeA_s=sa, ropeR_c=cr, ropeR_s=sr, cb=cb, hb=hb, hf=hf)
        _CACHE["cols"] = _hyb_cols()
    return _CACHE["c"], _CACHE["cols"]


def prepare_inputs(inputs):
    consts, cols = _host_consts()
    f = lambda a: np.ascontiguousarray(np.asarray(a, dtype=np.float32))
    ng = f(inputs["norm_g"])
    g_all = np.ascontiguousarray(ng.reshape(4, 6, 8, 128).transpose(3, 0, 1, 2).reshape(128, 192))
    shared = dict(
        g_all=g_all,
        ffn_w_gate=f(inputs["ffn_w_gate"]), ffn_w_up=f(inputs["ffn_w_up"]), ffn_w_down=f(inputs["ffn_w_down"]),
        hyb_w_in_ext=np.ascontiguousarray(f(inputs["hyb_w_in"])[:, :, cols]),
        hyb_w_out=f(inputs["hyb_w_out"]),
        gmlp_w_in=f(inputs["gmlp_w_in"]), gmlp_w_out=f(inputs["gmlp_w_out"]),
        gmlp_ln_g=f(inputs["gmlp_ln_g"]), gmlp_ln_b=f(inputs["gmlp_ln_b"]),
        gmlp_w_sT=np.ascontiguousarray(f(inputs["gmlp_w_s"]).transpose(0, 3, 1, 2).reshape(2, 128, 1024)),
        gmlp_b_s=np.ascontiguousarray(f(inputs["gmlp_b_s"]).reshape(2, 1024)),
        **consts,
    )
    return shared


def kernel(**inputs):
    x = np.asarray(inputs["x"], dtype=np.float32)
    shared = prepare_inputs(inputs)
    if "nc" not in _CACHE:
        _CACHE["nc"] = build_program([0, 1, 2, 3])[0]
    nc = _CACHE["nc"]
    in_maps = []
    for b in range(8):
        m = dict(shared)
        m["xT"] = np.ascontiguousarray(x[b].T)
        in_maps.append(m)
    res = run_bass_kernel_spmd(nc, in_maps, core_ids=list(range(8)))
    out = np.stack([np.ascontiguousarray(res.results[b]["yT"].T) for b in range(8)], axis=0)
    return out.astype(np.float32)
```

```python
import math
from contextlib import ExitStack
import numpy as np
import concourse.bass as bass
import concourse.mybir as mybir
from concourse.bass_utils import run_bass_kernel_spmd

F32 = mybir.dt.float32
BF16 = mybir.dt.bfloat16
U8 = mybir.dt.uint8
AF = mybir.ActivationFunctionType
ALU = mybir.AluOpType

T = 2048
D = 1024
FF = 2816
NJ = 22
EPS = 1e-6
DEPTH = 4
NEXT = 4 * 640 + 4 * 768
DEBUG = {}


class Sched:
    ENGS = ("pe", "act", "dve", "pool", "sp")

    def __init__(self, nc):
        self.nc = nc
        self.ops = []
        self.pools = {}
        self.phase = 0

    def barrier(self, fn):
        self.ops.append(dict(eng="dve", fn=fn, r=(), w=(("PH",),), dma=None, late=None, ndma=1))
        self.phase += 1

    def op(self, eng, fn, r=(), w=(), dma=None, ndma=1):
        o = dict(eng=eng, fn=fn, r=tuple(r) + (("PH",),), w=tuple(w), dma=dma, late=None, ndma=ndma)
        self.ops.append(o)
        return o

    def consume(self, pool, nslots, eng, load_fn_of_slot, extra_r=(), ndma=1, lookahead=None):
        p = self.pools.setdefault((pool, self.phase), dict(n=0, nslots=nslots, marks=[]))
        i = p["n"]
        p["n"] += 1
        slot = i % nslots
        key = (pool, slot)
        mark = dict(eng=None, fn=None, r=(), w=(), dma=None, late=[])
        self.ops.append(mark)
        p["marks"].append(mark)
        dop = dict(eng=eng, fn=(lambda e, s=slot: load_fn_of_slot(e, s)), r=tuple(extra_r) + (("PH",),),
                   w=(key,), dma=("dma",) + key, late=None, ndma=ndma)
        la = (nslots - 1) if lookahead is None else lookahead
        tgt = p["marks"][max(0, i - la)]
        tgt["late"].append(dop)
        return slot, key

    def finalize(self):
        out = []
        for o in self.ops:
            if o["late"] is not None:
                out.extend(o["late"])
            else:
                out.append(o)
        self.ops = ops = out
        last_w = {}
        readers = {}
        for i, o in enumerate(ops):
            deps = {}
            for k in o["r"]:
                if k in last_w:
                    deps[last_w[k]] = True
            for k in o["w"]:
                if k in last_w:
                    deps.setdefault(last_w[k], False)
                for rd in readers.get(k, ()):
                    deps.setdefault(rd, False)
            deps.pop(i, None)
            o["deps"] = deps
            for k in o["r"]:
                readers.setdefault(k, []).append(i)
            for k in o["w"]:
                last_w[k] = i
                readers[k] = []
        for o in ops:
            o["signal"] = False
            o["need"] = []
        for o in ops:
            for d, raw in sorted(o["deps"].items()):
                p = ops[d]
                if p["dma"] is not None:
                    o["need"].append(d)
                elif p["eng"] == o["eng"]:
                    if o["eng"] == "pe":
                        continue
                    if raw or o["dma"] is not None:
                        p["signal"] = True
                        o["need"].append(d)
                else:
                    p["signal"] = True
                    o["need"].append(d)
        cnt = {e: 0 for e in self.ENGS}
        for o in ops:
            if o["dma"] is None and o["signal"]:
                cnt[o["eng"]] += 1
                o["sval"] = cnt[o["eng"]]
        self.stats = dict(cnt)
        self.dma_keys = sorted({o["dma"] for o in ops if o["dma"] is not None}, key=str)

    def emit(self):
        nc = self.nc
        ops = self.ops
        with ExitStack() as es:
            psem = {e: es.enter_context(nc.semaphore("p_" + e)) for e in self.ENGS}
            dsem = {k: es.enter_context(nc.semaphore("d_" + "_".join(str(x) for x in k[1:])))
                    for k in self.dma_keys}
            dtot = {k: 0 for k in self.dma_keys}
            for o in ops:
                if o["dma"] is not None:
                    dtot[o["dma"]] += 16 * o["ndma"]
                    o["dval"] = dtot[o["dma"]]
            block = es.enter_context(nc.Block())

            def run(ename, eng):
                waited = {}
                for o in ops:
                    if o["eng"] != ename:
                        continue
                    grp = {}
                    for d in o["need"]:
                        p = ops[d]
                        if p["dma"] is not None:
                            key, val, sem = ("d", p["dma"]), p["dval"], dsem[p["dma"]]
                        else:
                            key, val, sem = ("p", p["eng"]), p["sval"], psem[p["eng"]]
                        if key not in grp or grp[key][0] < val:
                            grp[key] = (val, sem)
                    for key, (val, sem) in grp.items():
                        if waited.get(key, 0) >= val:
                            continue
                        waited[key] = val
                        eng.wait_ge(sem, val)
                    if o["fn"] is None:
                        continue
                    ins = o["fn"](eng)
                    if o["dma"] is not None:
                        lst = ins if isinstance(ins, (list, tuple)) else [ins]
                        assert len(lst) == o["ndma"]
                        for x in lst:
                            x.then_inc(dsem[o["dma"]], 16)
                    elif o["signal"]:
                        ins.then_inc(psem[ename], 1)

            block.tensor(lambda e: run("pe", e))
            block.scalar(lambda e: run("act", e))
            block.vector(lambda e: run("dve", e))
            block.gpsimd(lambda e: run("pool", e))
            block.sync(lambda e: run("sp", e))


def _rope_tables():
    t = np.arange(T, dtype=np.float32)
    ca = np.ones((128, T), np.float32)
    sa = np.zeros((128, T), np.float32)
    half = 8
    inv = (np.float32(500000.0) ** (-(np.arange(half, dtype=np.float32) * 2.0 / 16))).astype(np.float32)
    ang = t[None, :] * inv[:, None]
    for hh in range(2):
        b = hh * 64
        ca[b:b + half] = np.cos(ang)
        ca[b + half:b + 2 * half] = np.cos(ang)
        sa[b:b + half] = -np.sin(ang)
        sa[b + half:b + 2 * half] = np.sin(ang)
    cr = np.zeros((128, T), np.float32)
    sr = np.zeros((128, T), np.float32)
    half = 32
    inv = (np.float32(10000.0) ** (-(np.arange(half, dtype=np.float32) * 2.0 / 64))).astype(np.float32)
    ang = t[None, :] * inv[:, None]
    for hh in range(2):
        b = hh * 64
        cr[b:b + half] = np.cos(ang)
        cr[b + half:b + 64] = np.cos(ang)
        sr[b:b + half] = -np.sin(ang)
        sr[b + half:b + 64] = np.sin(ang)
    return ca, sa, cr, sr


def _consts():
    idx = np.arange(128)
    k = idx[:, None]
    q = idx[None, :]
    cur = (k <= q).astype(np.float32)
    prev = (k >= q).astype(np.float32)
    m_n = np.concatenate([cur, prev], axis=1)
    m_f = np.concatenate([cur, np.zeros_like(prev)], axis=1)
    m_fnnn = np.concatenate([m_f, m_n, m_n, m_n], axis=1)
    ones = np.ones((128, 128), np.float32)
    blk = np.zeros((128, 128), np.float32)
    blk[:64, :64] = 1
    blk[64:, 64:] = 1
    ident = np.eye(128, dtype=np.float32)
    cb = np.concatenate([ones, blk, ident], axis=1)
    hb = np.concatenate([m_n, m_fnnn], axis=1)
    h = np.arange(8, dtype=np.float64)
    log_g = np.log(1.0 - np.exp2(-5.0 - h))
    diff = (q - k).astype(np.float64)
    dm = np.zeros((128, 8, 128), np.float64)
    for hh in range(8):
        dm[:, hh, :] = np.where(diff >= 0, np.exp(log_g[hh] * np.maximum(diff, 0)), 0.0) / 8.0
    xi = np.zeros((128, 4, 128), np.float64)
    zt = np.zeros((128, 4, 128), np.float64)
    cd = np.zeros((128, 8), np.float64)
    for p in range(128):
        for pr in range(4):
            hh = pr * 2 + p // 64
            xi[p, pr, :] = np.exp(log_g[hh] * (idx + 1.0))
            cd[p, pr] = np.exp(log_g[hh] * 128.0)
    for col in range(128):
        for pr in range(4):
            hh = pr * 2 + col // 64
            zt[:, pr, col] = np.exp(log_g[hh] * (127.0 - idx)) / 8.0
    hf = np.concatenate([dm.reshape(128, -1), xi.reshape(128, -1), zt.reshape(128, -1), cd],
                        axis=1).astype(np.float32)
    return cb.astype(np.float32), hb.astype(np.float32), hf


def _hyb_cols():
    cols = []
    def perm(base, half, rot):
        out = []
        for hh in range(2):
            for j in range(64):
                if j < half:
                    jj = j + half
                elif j < 2 * half:
                    jj = j - half
                else:
                    jj = j
                out.append(base + hh * 64 + jj)
        return out
    for p in range(4):
        qa = 0 + p * 128
        ka = 512 + p * 128
        va = 1024 + p * 128
        cols += list(range(qa, qa + 128)) + perm(qa, 8, 16)
        cols += list(range(ka, ka + 128)) + perm(ka, 8, 16)
        cols += list(range(va, va + 128))
    for p in range(4):
        qr = 1536 + p * 128
        kr = 2048 + p * 128
        vr = 2560 + p * 128
        gr = 3072 + p * 128
        cols += list(range(qr, qr + 128)) + perm(qr, 32, 64)
        cols += list(range(kr, kr + 128)) + perm(kr, 32, 64)
        cols += list(range(gr, gr + 128))
        cols += list(range(vr, vr + 128))
    assert len(cols) == NEXT
    return np.array(cols, dtype=np.int64)


def ssl(st, cnt, d=1):
    return slice(st, st + (cnt - 1) * d + 1, d)


def build_program(layers, stop_after=None):
    nc = bass.Bass("TRN2", target_bir_lowering=False)

    def din(name, shape):
        return nc.dram_tensor(name, list(shape), F32, kind="ExternalInput").ap()

    xT = din("xT", [D, T])
    yT = nc.dram_tensor("yT", [D, T], F32, kind="ExternalOutput").ap()
    g_all = din("g_all", [128, 192])
    wg_d = din("ffn_w_gate", [DEPTH, 2, D, FF])
    wu_d = din("ffn_w_up", [DEPTH, 2, D, FF])
    wd_d = din("ffn_w_down", [DEPTH, 2, FF, D])
    hin_d = din("hyb_w_in_ext", [2, D, NEXT])
    hout_d = din("hyb_w_out", [2, D, D])
    gin_d = din("gmlp_w_in", [2, D, 2 * D])
    gout_d = din("gmlp_w_out", [2, D, D])
    glng_d = din("gmlp_ln_g", [2, D])
    glnb_d = din("gmlp_ln_b", [2, D])
    gws_d = din("gmlp_w_sT", [2, 128, 8 * 128])
    gbs_d = din("gmlp_b_s", [2, 8 * 128])
    ropeA_c = din("ropeA_c", [128, T])
    ropeA_s = din("ropeA_s", [128, T])
    ropeR_c = din("ropeR_c", [128, T])
    ropeR_s = din("ropeR_s", [128, T])
    cb_d = din("cb", [128, 384])
    hb_d = din("hb", [128, 1280])
    hf_d = din("hf", [128, 2056])

    es = ExitStack()
    X = es.enter_context(nc.sbuf_tensor("X", [128, 8, T], F32))
    G = es.enter_context(nc.sbuf_tensor("G", [128, 192], F32))
    CB = es.enter_context(nc.sbuf_tensor("CB", [128, 384], BF16))
    MASKC = es.enter_context(nc.sbuf_tensor("MASKC", [128, 128], BF16))
    EPSB = es.enter_context(nc.sbuf_tensor("EPSB", [128, 4], F32))
    SCRB = 142 * 1024
    SCR = es.enter_context(nc.sbuf_tensor("SCR", [128, SCRB], U8))
    PSALL = es.enter_context(nc.psum_tensor("PSALL", [128, 4096], F32))
    ONES = CB[:, 0:128]
    BLK = CB[:, 128:256]
    IDENT = CB[:, 256:384]

    def PS(b, n=1):
        return PSALL[:, b * 512:(b + n) * 512]

    class Carver:
        def __init__(self, off=0):
            self.off = off

        def take(self, dtype, shape):
            n = 1
            for s_ in shape[1:]:
                n *= s_
            nb = n * (4 if dtype == F32 else 2)
            nb = (nb + 31) // 32 * 32
            ap = SCR[:, self.off:self.off + nb].bitcast(dtype)[:, 0:n]
            if len(shape) == 3:
                ap = ap.rearrange("p (a b) -> p a b", a=shape[1])
            elif len(shape) == 4:
                ap = ap.rearrange("p (a b c) -> p a b c", a=shape[1], b=shape[2])
            self.off += nb
            assert self.off <= SCRB, (self.off, SCRB)
            return ap

    S = Sched(nc)

    def barrier():
        S.barrier(lambda e: e.memset(EPSB[:, 2:3], 0.0))

    S.op("sp", lambda e: e.dma_start(out=G[:], in_=g_all), w=[("G",)], dma=("dma", "G"))
    S.op("pool", lambda e: e.dma_start(out=CB[:], in_=cb_d), w=[("CB",)], dma=("dma", "CB"))
    S.op("pool", lambda e: e.dma_start(out=MASKC[:], in_=hb_d[:, 0:128]), w=[("MASKC",)], dma=("dma", "MASKC"))
    S.op("dve", lambda e: e.memset(EPSB[:, 0:1], EPS), w=[("EPSB0",)])
    S.op("dve", lambda e: e.memset(EPSB[:, 1:2], math.log(0.5)), r=[("EPSB0",)], w=[("EPSB",)])
    xv = xT.rearrange("(c p) t -> p c t", p=128)
    yv = yT.rearrange("(c p) t -> p c t", p=128)
    for g in range(4):
        S.op("sp", lambda e, g=g: e.dma_start(out=X[:, :, g * 512:(g + 1) * 512],
                                              in_=xv[:, :, g * 512:(g + 1) * 512]),
             w=[("X", g)], dma=("dma", "X", g))

    def rstd_from_sq(sq_ap, nk, rs_ap, inv_n, ln_half, keys_r, key_rs, lhs=None, bank=6):
        lhs = ONES if lhs is None else lhs

        def mm(e):
            ins = None
            for k in range(nk):
                src = sq_ap[:, k, :] if nk > 1 else sq_ap
                ins = e.matmul(PS(bank), lhsT=lhs, rhs=src, start=(k == 0), stop=(k == nk - 1))
            return ins
        S.op("pe", mm, r=list(keys_r) + [("CB",)], w=[("ps", bank)])
        S.op("act", lambda e: e.activation(out=rs_ap, in_=PS(bank), func=AF.Ln, scale=inv_n, bias=EPSB[:, 0:1]),
             r=[("ps", bank), ("EPSB",)], w=[key_rs])
        if ln_half:
            S.op("act", lambda e: e.activation(out=rs_ap, in_=rs_ap, func=AF.Exp, scale=-0.5, bias=EPSB[:, 1:2]),
                 r=[key_rs, ("EPSB",)], w=[key_rs])
        else:
            S.op("act", lambda e: e.activation(out=rs_ap, in_=rs_ap, func=AF.Exp, scale=-0.5),
                 r=[key_rs], w=[key_rs])

    def prenorm(gi, g, dst_fn, dst_key, SQ, RS, stage="ab", ksq=("SQ",), krs=("RS",), bank=6):
        tok = slice(g * 512, (g + 1) * 512)
        if "a" in stage:
            S.op("act", lambda e: e.activation(out=SQ, in_=X[:, :, tok], func=AF.Square),
                 r=[("X", g)], w=[ksq])
        if "b" not in stage:
            return
        rstd_from_sq(SQ, 8, RS, 1.0 / D, False, [ksq], krs, bank=bank)
        for k in range(8):
            S.op("dve", lambda e, k=k: e.scalar_tensor_tensor(
                out=dst_fn(k), in0=X[:, k, tok], scalar=G[:, gi * 8 + k:gi * 8 + k + 1], in1=RS,
                op0=ALU.mult, op1=ALU.mult),
                r=[("X", g), krs, ("G",)], w=[dst_key(k)])

    def proj_postnorm(src_fn, src_keys, nk, w_view, gi, g, half_factor, FB, SQ, RS, WDS, par=None):
        tok = slice(g * 512, (g + 1) * 512)
        kq = ("SQ",) if par is None else ("SQp", par)
        kr = ("RS",) if par is None else ("RSp", par)
        kf = (lambda c: ("F", c)) if par is None else (lambda c: ("Fp", par, c))
        nb = 6 if par is None else 6 + par
        for c in range(8):
            slot, key = S.consume("WD", 2, "pool",
                                  lambda e, s, c=c: e.dma_start(out=WDS[:, s, 0:nk, :],
                                                                in_=w_view[:, :, c * 128:(c + 1) * 128]))
            b = 4 + c % 2

            def mm(e, slot=slot, b=b):
                ins = None
                for j in range(nk):
                    ins = e.matmul(PS(b), lhsT=WDS[:, slot, j, :], rhs=src_fn(j), start=(j == 0), stop=(j == nk - 1))
                return ins
            S.op("pe", mm, r=[key] + list(src_keys), w=[("ps", b)])
            S.op("act", lambda e, c=c, b=b: e.activation(out=FB[:, c, :], in_=PS(b), func=AF.Copy),
                 r=[("ps", b)], w=[kf(c)])
        S.op("act", lambda e: e.activation(out=SQ, in_=FB, func=AF.Square),
             r=[kf(c) for c in range(8)], w=[kq])
        rstd_from_sq(SQ, 8, RS, 1.0 / D, half_factor, [kq], kr, bank=nb)
        for c in range(8):
            S.op("dve", lambda e, c=c: e.scalar_tensor_tensor(
                out=FB[:, c, :], in0=FB[:, c, :], scalar=G[:, gi * 8 + c:gi * 8 + c + 1], in1=RS,
                op0=ALU.mult, op1=ALU.mult), r=[kf(c), kr, ("G",)], w=[kf(c)])
        for c in range(8):
            S.op("dve", lambda e, c=c: e.tensor_tensor(out=X[:, c, tok], in0=X[:, c, tok], in1=FB[:, c, :], op=ALU.add),
                 r=[kf(c), ("X", g)], w=[("X", g)])

    def ffn_seq(items):
        cv = Carver()
        H = cv.take(BF16, [128, 8, 1024])
        ACTB = cv.take(BF16, [128, NJ, 1024])
        FB = cv.take(F32, [128, 8, 1024])
        SQ = cv.take(BF16, [128, 8, 512])
        RS = cv.take(F32, [128, 512])
        SQ2 = cv.take(BF16, [128, 8, 512])
        RS2 = cv.take(F32, [128, 512])
        SG = cv.take(F32, [128, 2, 512])
        WGU = cv.take(BF16, [128, 3, 16, 128])
        WDS = cv.take(BF16, [128, 2, NJ, 128])
        halves = [(l, i, hf) for (l, i) in items for hf in range(2)]

        def gidx(l, i):
            return l * 6 + (0 if i == 0 else 4)

        def pre(hv, stage, tts):
            l, i, hf = hv
            for tt in tts:
                if tt == 0:
                    prenorm(gidx(l, i), hf * 2 + tt, lambda k, tt=tt: H[:, k, tt * 512:(tt + 1) * 512],
                            lambda k, tt=tt: ("H", tt), SQ2, RS2, stage=stage, ksq=("SQ2",), krs=("RS2",), bank=7)
                else:
                    prenorm(gidx(l, i), hf * 2 + tt, lambda k, tt=tt: H[:, k, tt * 512:(tt + 1) * 512],
                            lambda k, tt=tt: ("H", tt), SQ, RS, stage=stage, ksq=("SQ",), krs=("RS",), bank=6)

        def phase1(hv):
            l, i, hf = hv
            wgv = wg_d[l, i].rearrange("(k p) n -> p k n", p=128)
            wuv = wu_d[l, i].rearrange("(k p) n -> p k n", p=128)
            for j in range(NJ):
                def ld(e, s, j=j):
                    a_ = e.dma_start(out=WGU[:, s, 0:8, :], in_=wgv[:, :, j * 128:(j + 1) * 128])
                    b_ = e.dma_start(out=WGU[:, s, 8:16, :], in_=wuv[:, :, j * 128:(j + 1) * 128])
                    return [a_, b_]
                slot, key = S.consume("WGU", 3, "pool", ld, ndma=2)
                for tt in range(2):
                    bg, bu = tt, 2 + tt

                    def mm(e, slot=slot, tt=tt, bg=bg, bu=bu):
                        ins = None
                        for k in range(8):
                            ins = e.matmul(PS(bg), lhsT=WGU[:, slot, k, :], rhs=H[:, k, tt * 512:(tt + 1) * 512],
                                           start=(k == 0), stop=(k == 7))
                        for k in range(8):
                            ins = e.matmul(PS(bu), lhsT=WGU[:, slot, 8 + k, :], rhs=H[:, k, tt * 512:(tt + 1) * 512],
                                           start=(k == 0), stop=(k == 7))
                        return ins
                    S.op("pe", mm, r=[key, ("H", tt)], w=[("ps", bg), ("ps", bu)])
                    S.op("act", lambda e, tt=tt, bg=bg: e.activation(out=SG[:, tt, :], in_=PS(bg), func=AF.Silu),
                         r=[("ps", bg)], w=[("SG", tt)])
                    S.op("dve", lambda e, bu=bu, j=j, tt=tt: e.tensor_tensor(
                        out=ACTB[:, j, tt * 512:(tt + 1) * 512], in0=SG[:, tt, :], in1=PS(bu), op=ALU.mult),
                        r=[("SG", tt), ("ps", bu)], w=[("A", j, tt)])

        def phase2(hv, hook):
            l, i, hf = hv
            wdv = wd_d[l, i].rearrange("(j p) n -> p j n", p=128)
            gi = gidx(l, i) + 1
            for c in range(8):
                slot, key = S.consume("WD", 2, "pool",
                                      lambda e, s, c=c: e.dma_start(out=WDS[:, s, :, :],
                                                                    in_=wdv[:, :, c * 128:(c + 1) * 128]))
                for tt in range(2):
                    b = 4 + tt

                    def mm(e, slot=slot, b=b, tt=tt):
                        ins = None
                        for j in range(NJ):
                            ins = e.matmul(PS(b), lhsT=WDS[:, slot, j, :], rhs=ACTB[:, j, tt * 512:(tt + 1) * 512],
                                           start=(j == 0), stop=(j == NJ - 1))
                        return ins
                    S.op("pe", mm, r=[key] + [("A", j, tt) for j in range(NJ)], w=[("ps", b)])
                    S.op("act", lambda e, c=c, b=b, tt=tt: e.activation(out=FB[:, c, tt * 512:(tt + 1) * 512], in_=PS(b),
                                                                         func=AF.Copy),
                         r=[("ps", b)], w=[("F", c, tt)])
                if c == 0 and hook is not None:
                    hook()
            for tt in range(2):
                g = hf * 2 + tt
                tok = slice(g * 512, (g + 1) * 512)
                fsl = slice(tt * 512, (tt + 1) * 512)
                fk = [("F", c, tt) for c in range(8)]
                S.op("act", lambda e, fsl=fsl: e.activation(out=SQ, in_=FB[:, :, fsl], func=AF.Square),
                     r=fk, w=[("SQ",)])
                rstd_from_sq(SQ, 8, RS, 1.0 / D, True, [("SQ",)], ("RS",))
                for c in range(8):
                    S.op("dve", lambda e, c=c, fsl=fsl: e.scalar_tensor_tensor(
                        out=FB[:, c, fsl], in0=FB[:, c, fsl], scalar=G[:, gi * 8 + c:gi * 8 + c + 1], in1=RS,
                        op0=ALU.mult, op1=ALU.mult), r=[("F", c, tt), ("RS",), ("G",)], w=[("F", c, tt)])
                for c in range(8):
                    eng = "dve" if c % 2 == 0 else "pool"
                    S.op(eng, lambda e, c=c, tok=tok, fsl=fsl: e.tensor_tensor(out=X[:, c, tok], in0=X[:, c, tok],
                                                                               in1=FB[:, c, fsl], op=ALU.add),
                         r=[("F", c, tt), ("X", g)], w=[("X", g)])

        pre(halves[0], "ab", (0, 1))
        for idx, hv in enumerate(halves):
            phase1(hv)
            nxt = halves[idx + 1] if idx + 1 < len(halves) else None
            if nxt is not None:
                pre(nxt, "a", (0, 1))

                def hook(nxt=nxt):
                    pre(nxt, "b", (0, 1))
                phase2(hv, hook)
            else:
                phase2(hv, None)

    def gmlp(l):
        jl = l // 2
        cv = Carver()
        H = cv.take(BF16, [128, 8, 512])
        UT = cv.take(BF16, [128, 8, 512])
        YT = cv.take(BF16, [128, 8, 512])
        VG = cv.take(F32, [128, 4, 1024])
        VN = cv.take(BF16, [128, 4, 1024])
        LNG = cv.take(F32, [128, 1024])
        LNB = cv.take(F32, [128, 1024])
        WST = cv.take(BF16, [128, 8, 128])
        BS = cv.take(F32, [128, 8, 128])
        WIN = cv.take(BF16, [128, 3, 8, 128])
        WV = cv.take(BF16, [128, 2, 8, 512])
        FB = cv.take(F32, [128, 8, 512])
        SQ = cv.take(BF16, [128, 8, 512])
        RS = cv.take(F32, [128, 512])
        Y1 = cv.take(F32, [128, 2, 512])
        ST = cv.take(F32, [128, 4, 16])
        MV = cv.take(F32, [128, 4, 2])
        RSD = cv.take(F32, [128, 4])
        WDS = cv.take(BF16, [128, 2, 8, 128])
        winv = gin_d[jl].rearrange("(k p) n -> p k n", p=128)
        woutv = gout_d[jl].rearrange("(j p) n -> p j n", p=128)
        gi = l * 6 + 2
        S.op("sp", lambda e: e.dma_start(out=LNG, in_=glng_d[jl:jl + 1, :].to_broadcast([128, D])),
             w=[("LNG",)], dma=("dma", "LNG"))
        S.op("sp", lambda e: e.dma_start(out=LNB, in_=glnb_d[jl:jl + 1, :].to_broadcast([128, D])),
             w=[("LNB",)], dma=("dma", "LNB"))
        S.op("sp", lambda e: e.dma_start(out=BS.rearrange("p a b -> p (a b)"),
                                         in_=gbs_d[jl:jl + 1, :].to_broadcast([128, D])),
             w=[("BS",)], dma=("dma", "BS"))
        S.op("pool", lambda e: e.dma_start(out=WST.rearrange("p a b -> p (a b)"), in_=gws_d[jl]),
             w=[("WST0",)], dma=("dma", "WST"))
        S.op("dve", lambda e: e.tensor_tensor(out=WST, in0=WST, in1=MASKC[:, None, :].to_broadcast([128, 8, 128]),
                                              op=ALU.mult),
             r=[("WST0",), ("MASKC",)], w=[("WST",)])
        SQ2 = cv.take(BF16, [128, 8, 512])
        RS2 = cv.take(F32, [128, 512])

        def pre(g):
            prenorm(gi, g, lambda k: H[:, k, :], lambda k: ("H",), SQ2, RS2, ksq=("SQ2",), krs=("RS2",), bank=7)

        pre(0)
        for g in range(4):
            for hv in range(2):
                slot, key = S.consume("GWV", 2, "pool",
                                      lambda e, s, hv=hv: e.dma_start(out=WV[:, s],
                                                                       in_=winv[:, :, D + hv * 512:D + (hv + 1) * 512]))
                for m in range(4):
                    b = 2 + (hv * 4 + m) % 2

                    def mm(e, slot=slot, b=b, m=m):
                        ins = None
                        for k in range(8):
                            ins = e.matmul(PS(b), lhsT=H[:, k, m * 128:(m + 1) * 128], rhs=WV[:, slot, k, :],
                                           start=(k == 0), stop=(k == 7))
                        return ins
                    S.op("pe", mm, r=[key, ("H",)], w=[("ps", b)])
                    S.op("act", lambda e, b=b, m=m, hv=hv: e.activation(
                        out=VG[:, m, hv * 512:(hv + 1) * 512], in_=PS(b), func=AF.Gelu),
                        r=[("ps", b)], w=[("VG", m, hv)])
                    S.op("dve", lambda e, m=m, hv=hv: e.bn_stats(out=ST[:, m, hv * 6:(hv + 1) * 6],
                                                                  in_=VG[:, m, hv * 512:(hv + 1) * 512]),
                         r=[("VG", m, hv)], w=[("STAT", m, hv)])
            for m in range(4):
                S.op("dve", lambda e, m=m: e.bn_aggr(out=MV[:, m, :], in_=ST[:, m, 0:12]),
                     r=[("STAT", m, 0), ("STAT", m, 1)], w=[("MV", m)])
            S.op("act", lambda e: e.activation(out=RSD, in_=MV[:, :, 1], func=AF.Ln, bias=EPSB[:, 0:1]),
                 r=[("MV", m) for m in range(4)] + [("EPSB",)], w=[("RSD",)])
            S.op("act", lambda e: e.activation(out=RSD, in_=RSD, func=AF.Exp, scale=-0.5),
                 r=[("RSD",)], w=[("RSD",)])
            for m in range(4):
                vk = [("VG", m, 0), ("VG", m, 1)]
                S.op("dve", lambda e, m=m: e.tensor_scalar(out=VG[:, m, :], in0=VG[:, m, :], scalar1=MV[:, m, 0:1],
                                                           scalar2=RSD[:, m:m + 1], op0=ALU.subtract, op1=ALU.mult),
                     r=vk + [("MV", m), ("RSD",)], w=vk)
                S.op("dve", lambda e, m=m: e.tensor_tensor(out=VG[:, m, :], in0=VG[:, m, :], in1=LNG, op=ALU.mult),
                     r=vk + [("LNG",)], w=vk)
                S.op("dve", lambda e, m=m: e.tensor_tensor(out=VN[:, m, :], in0=VG[:, m, :], in1=LNB, op=ALU.add),
                     r=vk + [("LNB",)], w=[("VN", m)])
            for ct in range(8):
                slot, key = S.consume("GWIN", 3, "pool",
                                      lambda e, s, ct=ct: e.dma_start(out=WIN[:, s], in_=winv[:, :, ct * 128:(ct + 1) * 128]))
                b = ct % 2

                def mm(e, slot=slot, b=b):
                    ins = None
                    for k in range(8):
                        ins = e.matmul(PS(b), lhsT=WIN[:, slot, k, :], rhs=H[:, k, :], start=(k == 0), stop=(k == 7))
                    return ins
                S.op("pe", mm, r=[key, ("H",)], w=[("ps", b)])
                S.op("act", lambda e, ct=ct, b=b: e.activation(out=UT[:, ct, :], in_=PS(b), func=AF.Gelu),
                     r=[("ps", b)], w=[("UT", ct)])
            for gg in range(8):
                b = 4 + gg % 2

                def mm(e, gg=gg, b=b):
                    ins = None
                    for m in range(4):
                        ins = e.matmul(PS(b)[:, m * 128:(m + 1) * 128], lhsT=VN[:, m, gg * 128:(gg + 1) * 128],
                                       rhs=WST[:, gg, :], start=True, stop=True)
                    return ins
                S.op("pe", mm, r=[("VN", m) for m in range(4)] + [("WST",)], w=[("ps", b)])
                u = gg % 2
                S.op("dve", lambda e, gg=gg, b=b, u=u: e.tensor_tensor(
                    out=Y1[:, u, :].rearrange("p (m i) -> p m i", m=4),
                    in0=PS(b).rearrange("p (m i) -> p m i", m=4),
                    in1=BS[:, gg:gg + 1, :].to_broadcast([128, 4, 128]), op=ALU.add),
                    r=[("ps", b), ("BS",)], w=[("Y1", u)])
                S.op("dve", lambda e, gg=gg, u=u: e.tensor_tensor(out=YT[:, gg, :], in0=Y1[:, u, :], in1=UT[:, gg, :],
                                                                   op=ALU.mult),
                     r=[("Y1", u), ("UT", gg)], w=[("YT", gg)])
            if g + 1 < 4:
                pre(g + 1)
            proj_postnorm(lambda j: YT[:, j, :], [("YT", j) for j in range(8)], 8, woutv, gi + 1, g, False,
                          FB, SQ, RS, WDS)

    def hybrid(l):
        jl = l // 2
        cv = Carver()
        HT = cv.take(BF16, [128, 8, T])
        MIX = cv.take(BF16, [128, 8, T])
        WIN = cv.take(BF16, [128, 6, 8, 128])
        ROPE = cv.take(F32, [128, 2, 2, 512])
        TMP = cv.take(F32, [128, 2, 512])
        RS = cv.take(F32, [128, 512])
        pair_off = cv.off
        winv = hin_d[jl].rearrange("(k p) n -> p k n", p=128)
        woutv = hout_d[jl].rearrange("(j p) n -> p j n", p=128)
        gi = l * 6 + 2
        cv0 = Carver(pair_off)
        SQ0 = [cv0.take(BF16, [128, 8, 512]), cv0.take(BF16, [128, 8, 512])]
        RS0 = [RS, TMP[:, 0, :]]
        for g in range(4):
            prenorm(gi, g, lambda k, g=g: HT[:, k, g * 512:(g + 1) * 512], lambda k, g=g: ("HT", g),
                    SQ0[g % 2], RS0[g % 2], ksq=("SQ0", g % 2), krs=("RS0", g % 2), bank=6 + g % 2)
        barrier()
        HTK = [("HT", g) for g in range(4)]

        def win_block(col0):
            return S.consume("HWIN", 6, "pool",
                             lambda e, s, col0=col0: e.dma_start(out=WIN[:, s], in_=winv[:, :, col0:col0 + 128]),
                             lookahead=2)

        def rope_proj(col0, rc, rs_, dsts, fam):
            blks = [win_block(col0 + i * 128) for i in range(4)]
            for g in range(4):
                tok = slice(g * 512, (g + 1) * 512)

                def ldrope(e, s, g=g):
                    a = e.dma_start(out=ROPE[:, s, 0, :], in_=rc[:, g * 512:(g + 1) * 512])
                    b = e.dma_start(out=ROPE[:, s, 1, :], in_=rs_[:, g * 512:(g + 1) * 512])
                    return [a, b]
                rslot, rkey = S.consume("ROPE", 2, "sp", ldrope, ndma=2)
                for qi in range(2):
                    u = (g * 2 + qi) % 2
                    b0, b1 = 2 * u, 2 * u + 1
                    (s0, k0), (s1, k1) = blks[2 * qi], blks[2 * qi + 1]

                    def mm(e, s0=s0, s1=s1, b0=b0, b1=b1, tok=tok):
                        ins = None
                        for k in range(8):
                            ins = e.matmul(PS(b0), lhsT=WIN[:, s0, k, :], rhs=HT[:, k, tok], start=(k == 0), stop=(k == 7))
                        for k in range(8):
                            ins = e.matmul(PS(b1), lhsT=WIN[:, s1, k, :], rhs=HT[:, k, tok], start=(k == 0), stop=(k == 7))
                        return ins
                    S.op("pe", mm, r=[k0, k1, ("HT", g)], w=[("ps", b0), ("ps", b1)])
                    S.op("dve", lambda e, b0=b0, rslot=rslot: e.tensor_tensor(
                        out=TMP[:, 0, :], in0=PS(b0), in1=ROPE[:, rslot, 0, :], op=ALU.mult),
                        r=[("ps", b0), rkey], w=[("TMP", 0)])
                    S.op("dve", lambda e, b1=b1, rslot=rslot: e.tensor_tensor(
                        out=TMP[:, 1, :], in0=PS(b1), in1=ROPE[:, rslot, 1, :], op=ALU.mult),
                        r=[("ps", b1), rkey], w=[("TMP", 1)])
                    dst = dsts[qi]
                    S.op("dve", lambda e, dst=dst, tok=tok: e.tensor_tensor(
                        out=dst[:, tok], in0=TMP[:, 0, :], in1=TMP[:, 1, :], op=ALU.add),
                        r=[("TMP", 0), ("TMP", 1)], w=[("QK", fam, qi, g)])

        def dswa_pair(p):
            cvp = Carver(pair_off)
            HB = cvp.take(BF16, [128, 1280])
            QT = cvp.take(BF16, [128, T])
            KT = cvp.take(BF16, [128, T])
            VA = cvp.take(BF16, [128, 3, 16, 192])
            ACC = cvp.take(F32, [128, 2, T])
            E = cvp.take(BF16, [128, 2, 1024])
            M_N = HB[:, 0:256]
            M_F = HB[:, 256:1280]
            RC = TMP
            col0 = p * 640
            if p == 0:
                S.op("pool", lambda e: e.dma_start(out=HB, in_=hb_d), w=[("HB",)], dma=("dma", "HB"))
                S.op("pool", lambda e: e.memset(VA[:, :, :, 64:128], 1.0), w=[("VA1",)])
            rope_proj(col0, ropeA_c, ropeA_s, [QT, KT], "a")
            vs, vk = win_block(col0 + 512)
            for bi, d in enumerate((1, 4, 16)):
                tpc = 16 // d
                for i4 in range(4):
                    b = 4 + (bi * 4 + i4) % 2

                    def mm(e, b=b, d=d, i4=i4, tpc=tpc):
                        ins = None
                        for ii in range(4):
                            i = i4 * 4 + ii
                            r, blk = i // tpc, i % tpc
                            st = blk * 128 * d + r
                            for k in range(8):
                                ins = e.matmul(PS(b)[:, ii * 128:(ii + 1) * 128],
                                               lhsT=HT[:, k, ssl(st, 128, d)], rhs=WIN[:, vs, k, :],
                                               start=(k == 0), stop=(k == 7))
                        return ins
                    S.op("pe", mm, r=[vk] + HTK, w=[("ps", b)])
                    S.op("act", lambda e, b=b, bi=bi, i4=i4: e.activation(
                        out=VA[:, bi, i4 * 4:(i4 + 1) * 4, :].rearrange("p i (s c) -> p i s c", s=3)[:, :, 0:3:2, :],
                        in_=PS(b).rearrange("p (i s c) -> p i s c", i=4, s=2), func=AF.Copy),
                        r=[("ps", b), ("VA1",)], w=[("VA", bi, i4)])
            QKK = [("QK", "a", qi, g) for qi in range(2) for g in range(4)]
            unit = 0
            units = []
            for hh in range(2):
                hs = slice(hh * 64, hh * 64 + 64)
                vcols = slice(hh * 64, hh * 64 + 128)
                for bi, d in enumerate((1, 4, 16)):
                    tpc = 16 // d
                    for i4 in range(4):
                        su = unit % 2
                        unit += 1
                        sb0 = 2 * su
                        ob = 4 + su
                        blocks = []
                        for ii in range(4):
                            i = i4 * 4 + ii
                            r, n = i // tpc, i % tpc
                            blocks.append((i, n, n * 128 * d + r, (n - 1) * 128 * d + r))
                        noprev = (d == 16)
                        W = 128 if noprev else 256

                        def emit_s(blocks=blocks, sb0=sb0, d=d, hs=hs, noprev=noprev, W=W):
                            def mm_s(e):
                                ins = None
                                for ii, (i, n, st, pst) in enumerate(blocks):
                                    qsl = ssl(st, 128, d)
                                    ins = e.matmul(PS(sb0, 2)[:, ii * W:ii * W + 128], lhsT=KT[hs, qsl], rhs=QT[hs, qsl],
                                                   start=True, stop=True)
                                    if not noprev:
                                        ksl = qsl if n == 0 else ssl(pst, 128, d)
                                        ins = e.matmul(PS(sb0, 2)[:, ii * W + 128:ii * W + 256], lhsT=KT[hs, ksl],
                                                       rhs=QT[hs, qsl], start=True, stop=True)
                                return ins
                            S.op("pe", mm_s, r=QKK, w=[("ps", sb0), ("ps", sb0 + 1)])

                        def emit_rest(blocks=blocks, sb0=sb0, su=su, ob=ob, d=d, bi=bi, hh=hh, vcols=vcols,
                                      noprev=noprev, W=W):
                            S.op("act", lambda e: e.activation(
                                out=E[:, su, 0:4 * W], in_=PS(sb0, 2)[:, 0:4 * W], func=AF.Exp, scale=0.125),
                                r=[("ps", sb0), ("ps", sb0 + 1)], w=[("E", su)])
                            if noprev:
                                msk = M_N[:, None, 0:128].to_broadcast([128, 4, 128])
                                ev = E[:, su, 0:512].rearrange("p (a b) -> p a b", a=4)
                            elif blocks[0][1] == 0:
                                msk = M_F
                                ev = E[:, su, :]
                            else:
                                msk = M_N[:, None, :].to_broadcast([128, 4, 256])
                                ev = E[:, su, :].rearrange("p (a b) -> p a b", a=4)
                            S.op("dve", lambda e: e.tensor_tensor(out=ev, in0=ev, in1=msk, op=ALU.mult),
                                 r=[("E", su), ("HB",)], w=[("E", su)])

                            def mm_o(e):
                                ins = None
                                for ii, (i, n, st, pst) in enumerate(blocks):
                                    hasprev = (not noprev) and n > 0
                                    ins = e.matmul(PS(ob)[:, ii * 128:(ii + 1) * 128], lhsT=VA[:, bi, i, vcols],
                                                   rhs=E[:, su, ii * W:ii * W + 128], start=True, stop=not hasprev)
                                    if hasprev:
                                        ins = e.matmul(PS(ob)[:, ii * 128:(ii + 1) * 128], lhsT=VA[:, bi, i - 1, vcols],
                                                       rhs=E[:, su, ii * W + 128:ii * W + 256], start=False, stop=True)
                                return ins
                            S.op("pe", mm_o, r=[("E", su)] + [("VA", bi, x) for x in range(4)], w=[("ps", ob)])
                            if d == 16:
                                r0 = blocks[0][0]
                                dst = ACC[:, hh, :].rearrange("p (m r) -> p r m", r=16)[:, r0:r0 + 4, :]
                                src = PS(ob).rearrange("p (a m) -> p a m", a=4)
                            else:
                                dst = ACC[:, hh, ssl(blocks[0][2], 512, d)]
                                src = PS(ob)
                            if bi == 0:
                                S.op("dve", lambda e: e.tensor_copy(out=dst, in_=src),
                                     r=[("ps", ob)], w=[("ACC", hh)])
                            else:
                                S.op("dve", lambda e: e.tensor_tensor(out=dst, in0=dst, in1=src, op=ALU.add),
                                     r=[("ps", ob), ("ACC", hh)], w=[("ACC", hh)])
                        units.append((emit_s, emit_rest, hh, bi == 2 and i4 == 3))

            def normalise(hh):
                orow = slice(hh * 64, hh * 64 + 64)
                drow = slice(64 - hh * 64, 128 - hh * 64)
                for g in range(4):
                    tok = slice(g * 512, (g + 1) * 512)
                    u = g % 2
                    S.op("act", lambda e, tok=tok, u=u: e.activation(
                        out=RC[orow, u, :], in_=ACC[drow, hh, tok], func=AF.Ln), r=[("ACC", hh)], w=[("TMP", u)])
                    S.op("act", lambda e, u=u: e.activation(
                        out=RC[orow, u, :], in_=RC[orow, u, :], func=AF.Exp, scale=-1.0), r=[("TMP", u)], w=[("TMP", u)])
                    S.op("dve", lambda e, tok=tok, u=u: e.tensor_tensor(
                        out=MIX[orow, p, tok], in0=ACC[orow, hh, tok], in1=RC[orow, u, :], op=ALU.mult),
                        r=[("TMP", u), ("ACC", hh)], w=[("MIX", p, hh, g)])

            prev = None
            for un in units:
                un[0]()
                if prev is not None:
                    prev[1]()
                    if prev[3]:
                        normalise(prev[2])
                prev = un
            prev[1]()
            normalise(prev[2])

        def ret_pair(p):
            cvp = Carver(pair_off)
            HF = cvp.take(F32, [128, 2056])
            QT = cvp.take(BF16, [128, T])
            KT = cvp.take(BF16, [128, T])
            QX = cvp.take(BF16, [128, T])
            GT = cvp.take(BF16, [128, T])
            VT = cvp.take(BF16, [128, 16, 128])
            KZ = cvp.take(BF16, [128, 16, 128])
            R32 = cvp.take(F32, [128, T])
            ST32 = cvp.take(F32, [128, 64])
            STB = cvp.take(BF16, [128, 2, 64])
            SD = cvp.take(BF16, [128, 2, 256])
            SQH = cvp.take(BF16, [128, 512])
            DM = HF[:, 0:1024].rearrange("p (h c) -> p h c", h=8)
            XI = HF[:, 1024:1536].rearrange("p (a c) -> p a c", a=4)
            ZT = HF[:, 1536:2048].rearrange("p (a c) -> p a c", a=4)
            CD = HF[:, 2048:2056]
            col0 = 4 * 640 + p * 768
            if p == 0:
                S.op("sp", lambda e: e.dma_start(out=HF, in_=hf_d), w=[("HF",)], dma=("dma", "HF"))
            rope_proj(col0, ropeR_c, ropeR_s, [QT, KT], "r")
            for g in range(4):
                tok = slice(g * 512, (g + 1) * 512)
                S.op("dve", lambda e, tok=tok: e.tensor_tensor(
                    out=QX[:, tok].rearrange("p (a c) -> p a c", a=4), in0=QT[:, tok].rearrange("p (a c) -> p a c", a=4),
                    in1=XI[:, p:p + 1, :].to_broadcast([128, 4, 128]), op=ALU.mult),
                    r=[("QK", "r", 0, g), ("HF",)], w=[("QX", g)])
            gs, gk = win_block(col0 + 512)
            for g in range(4):
                tok = slice(g * 512, (g + 1) * 512)
                b = g % 2

                def mm(e, b=b, tok=tok):
                    ins = None
                    for k in range(8):
                        ins = e.matmul(PS(b), lhsT=WIN[:, gs, k, :], rhs=HT[:, k, tok], start=(k == 0), stop=(k == 7))
                    return ins
                S.op("pe", mm, r=[gk, ("HT", g)], w=[("ps", b)])
                S.op("act", lambda e, b=b, tok=tok: e.activation(out=GT[:, tok], in_=PS(b), func=AF.Silu),
                     r=[("ps", b)], w=[("GT", g)])
            vs, vk = win_block(col0 + 640)
            for i4 in range(4):
                b = 2 + i4 % 2

                def mm(e, b=b, i4=i4):
                    ins = None
                    for ii in range(4):
                        i = i4 * 4 + ii
                        for k in range(8):
                            ins = e.matmul(PS(b)[:, ii * 128:(ii + 1) * 128], lhsT=HT[:, k, i * 128:(i + 1) * 128],
                                           rhs=WIN[:, vs, k, :], start=(k == 0), stop=(k == 7))
                    return ins
                S.op("pe", mm, r=[vk] + HTK, w=[("ps", b)])
                S.op("act", lambda e, b=b, i4=i4: e.activation(
                    out=VT[:, i4 * 4:(i4 + 1) * 4, :], in_=PS(b).rearrange("p (i c) -> p i c", i=4), func=AF.Copy),
                    r=[("ps", b)], w=[("VT", i4)])
            PSB7 = PS(7).bitcast(BF16)
            for i4 in range(DEBUG.get("ntr", 4)):
                def tr(e, i4=i4):
                    ins = None
                    for ii in range(4):
                        i = i4 * 4 + ii
                        ins = e.transpose(PSB7[:, ii * 128:(ii + 1) * 128], KT[:, i * 128:(i + 1) * 128], IDENT)
                    return ins
                S.op("pe", tr, r=[("QK", "r", 1, i4), ("CB",)], w=[("ps", 7)])
                S.op("dve", lambda e, i4=i4: e.tensor_tensor(
                    out=KZ[:, i4 * 4:(i4 + 1) * 4, :], in0=PSB7[:, 0:512].rearrange("p (i c) -> p i c", i=4),
                    in1=ZT[:, p:p + 1, :].to_broadcast([128, 4, 128]), op=ALU.mult),
                    r=[("ps", 7), ("HF",)], w=[("KZ", i4)])
            S.op("dve", lambda e: e.memset(ST32, 0.0), w=[("ST32",)])
            def r_skv(n):
                g = n // 4
                ck = slice(n * 128, (n + 1) * 128)
                sb = n % 2
                b0 = 2 * sb
                kvb = 6 + n % 2

                def mm_s(e):
                    e.matmul(PS(b0)[:, 0:128], lhsT=KT[0:64, ck], rhs=QT[0:64, ck], start=True, stop=True)
                    return e.matmul(PS(b0 + 1)[:, 0:128], lhsT=KT[64:128, ck], rhs=QT[64:128, ck], start=True, stop=True)
                S.op("pe", mm_s, r=[("QK", "r", 0, g), ("QK", "r", 1, g)], w=[("ps", b0), ("ps", b0 + 1)])
                S.op("dve", lambda e: e.tensor_tensor(
                    out=SD[:, sb, :].rearrange("p (h c) -> p h c", h=2),
                    in0=PS(b0, 2).rearrange("p (h c) -> p h c", h=2)[:, :, 0:128], in1=DM[:, 2 * p:2 * p + 2, :],
                    op=ALU.mult),
                    r=[("ps", b0), ("ps", b0 + 1), ("HF",)], w=[("SD", sb)])

                def mm_kv(e):
                    e.matmul(PS(kvb)[0:64, 0:64], lhsT=KZ[:, n, 0:64], rhs=VT[:, n, 0:64], start=True, stop=True)
                    return e.matmul(PS(kvb)[64:128, 0:64], lhsT=KZ[:, n, 64:128], rhs=VT[:, n, 64:128],
                                    start=True, stop=True)
                if n < 15:
                    S.op("pe", mm_kv, r=[("KZ", n // 4), ("VT", n // 4)], w=[("ps", kvb)])

            def r_upd(n):
                kvb = 6 + n % 2
                if n < 15:
                    S.op("dve", lambda e: e.scalar_tensor_tensor(
                        out=ST32, in0=ST32, scalar=CD[:, p:p + 1], in1=PS(kvb)[:, 0:64], op0=ALU.mult, op1=ALU.add),
                        r=[("ps", kvb), ("ST32",), ("HF",)], w=[("ST32",)])
                    S.op("act", lambda e: e.activation(out=STB[:, (n + 1) % 2, :], in_=ST32, func=AF.Copy),
                         r=[("ST32",)], w=[("STB", (n + 1) % 2)])

            def r_out(n):
                g = n // 4
                ck = slice(n * 128, (n + 1) * 128)
                sb = n % 2

                def mm_o(e):
                    oc = slice((n % 4) * 128, (n % 4 + 1) * 128)
                    st = n % 2
                    ins = None
                    for hh in range(2):
                        hs = slice(hh * 64, hh * 64 + 64)
                        ins = e.matmul(PS(4 + hh)[hs, oc], lhsT=VT[:, n, hs], rhs=SD[:, sb, hh * 128:(hh + 1) * 128],
                                       start=True, stop=(n == 0))
                        if n > 0:
                            ins = e.matmul(PS(4 + hh)[hs, oc], lhsT=STB[hs, st, :], rhs=QX[hs, ck], start=False, stop=True)
                    return ins
                rk = [("SD", sb), ("VT", n // 4), ("QX", g)] + ([("STB", n % 2)] if n > 0 else [])
                S.op("pe", mm_o, r=rk, w=[("ps", 4), ("ps", 5)])
                if n % 4 == 3:
                    tok = slice(g * 512, (g + 1) * 512)
                    S.op("act", lambda e: e.activation(out=R32[0:64, tok], in_=PS(4)[0:64, :], func=AF.Copy),
                         r=[("ps", 4)], w=[("R32a", g)])
                    S.op("act", lambda e: e.activation(out=R32[64:128, tok], in_=PS(5)[64:128, :], func=AF.Copy),
                         r=[("ps", 5)], w=[("R32", g)])

            r_skv(0)
            r_upd(0)
            for n in range(16):
                if n + 1 < 16:
                    r_skv(n + 1)
                r_out(n)
                if n + 1 < 16:
                    r_upd(n + 1)
            for g in range(4):
                tok = slice(g * 512, (g + 1) * 512)
                S.op("act", lambda e, tok=tok: e.activation(out=SQH, in_=R32[:, tok], func=AF.Square),
                     r=[("R32", g), ("R32a", g)], w=[("SQH",)])
                rstd_from_sq(SQH, 1, RS, 1.0 / 64, False, [("SQH",)], ("RS",), lhs=BLK)
                S.op("dve", lambda e, tok=tok: e.tensor_tensor(out=TMP[:, 0, :], in0=R32[:, tok], in1=RS, op=ALU.mult),
                     r=[("R32", g), ("R32a", g), ("RS",)], w=[("TMP", 0)])
                S.op("dve", lambda e, tok=tok: e.tensor_tensor(out=MIX[:, 4 + p, tok], in0=TMP[:, 0, :], in1=GT[:, tok],
                                                                op=ALU.mult),
                     r=[("TMP", 0), ("GT", g)], w=[("MIX", 4 + p, 0, g), ("MIX", 4 + p, 1, g)])

        for p in range(DEBUG.get("ndswa", 4)):
            dswa_pair(p)
        barrier()
        for p in range(DEBUG.get("nret", 4)):
            ret_pair(p)
        barrier()
        if DEBUG.get("dump_mix"):
            for g in range(4):
                S.op("act", lambda e, g=g: e.activation(out=X[:, :, g * 512:(g + 1) * 512], in_=MIX[:, :, g * 512:(g + 1) * 512],
                                                        func=AF.Copy),
                     r=[("MIX", j, hh, g) for j in range(8) for hh in range(2)] + [("X", g)], w=[("X", g)])
            return
        cvo = Carver(pair_off)
        FBs = [cvo.take(F32, [128, 8, 512]), cvo.take(F32, [128, 8, 512])]
        SQs = [cvo.take(BF16, [128, 8, 512]), cvo.take(BF16, [128, 8, 512])]
        RSs = [RS, TMP[:, 0, :]]
        WDS = cvo.take(BF16, [128, 2, 8, 128])
        for g in range(4):
            tok = slice(g * 512, (g + 1) * 512)
            proj_postnorm(lambda j, tok=tok: MIX[:, j, tok],
                          [("MIX", j, hh, g) for j in range(8) for hh in range(2)], 8, woutv, gi + 1, g, False,
                          FBs[g % 2], SQs[g % 2], RSs[g % 2], WDS, par=g % 2)

    phases = []
    for l in layers:
        phases += [("ffn", l, 0), ("mix", l), ("ffn", l, 1)]
    if stop_after is not None:
        phases = phases[:stop_after]
    idx = 0
    while idx < len(phases):
        ph = phases[idx]
        if ph[0] == "ffn":
            items = [(ph[1], ph[2])]
            while idx + 1 < len(phases) and phases[idx + 1][0] == "ffn":
                idx += 1
                items.append((phases[idx][1], phases[idx][2]))
            ffn_seq(items)
        else:
            l = ph[1]
            if DEBUG.get("skipmix"):
                pass
            elif l % 2 == 0:
                hybrid(l)
            else:
                gmlp(l)
        barrier()
        idx += 1

    for g in range(4):
        S.op("sp", lambda e, g=g: e.dma_start(out=yv[:, :, g * 512:(g + 1) * 512], in_=X[:, :, g * 512:(g + 1) * 512]),
             r=[("X", g)], w=[("Y", g)], dma=("dma", "Y", g))
    S.op("sp", None, r=[("Y", g) for g in range(4)])
    S.finalize()
    S.emit()
    es.close()
    return nc, S


_CACHE = {}


def _host_consts():
    if "c" not in _CACHE:
        ca, sa, cr, sr = _rope_tables()
        cb, hb, hf = _consts()
        _CACHE["c"] = dict(ropeA_c=ca, ropeA_s=sa, ropeR_c=cr, ropeR_s=sr, cb=cb, hb=hb, hf=hf)
        _CACHE["cols"] = _hyb_cols()
    return _CACHE["c"], _CACHE["cols"]


def prepare_inputs(inputs):
    consts, cols = _host_consts()
    f = lambda a: np.ascontiguousarray(np.asarray(a, dtype=np.float32))
    ng = f(inputs["norm_g"])
    g_all = np.ascontiguousarray(ng.reshape(4, 6, 8, 128).transpose(3, 0, 1, 2).reshape(128, 192))
    shared = dict(
        g_all=g_all,
        ffn_w_gate=f(inputs["ffn_w_gate"]), ffn_w_up=f(inputs["ffn_w_up"]), ffn_w_down=f(inputs["ffn_w_down"]),
        hyb_w_in_ext=np.ascontiguousarray(f(inputs["hyb_w_in"])[:, :, cols]),
        hyb_w_out=f(inputs["hyb_w_out"]),
        gmlp_w_in=f(inputs["gmlp_w_in"]), gmlp_w_out=f(inputs["gmlp_w_out"]),
        gmlp_ln_g=f(inputs["gmlp_ln_g"]), gmlp_ln_b=f(inputs["gmlp_ln_b"]),
        gmlp_w_sT=np.ascontiguousarray(f(inputs["gmlp_w_s"]).transpose(0, 3, 1, 2).reshape(2, 128, 1024)),
        gmlp_b_s=np.ascontiguousarray(f(inputs["gmlp_b_s"]).reshape(2, 1024)),
        **consts,
    )
    return shared


def kernel(**inputs):
    x = np.asarray(inputs["x"], dtype=np.float32)
    shared = prepare_inputs(inputs)
    if "nc" not in _CACHE:
        _CACHE["nc"] = build_program([0, 1, 2, 3])[0]
    nc = _CACHE["nc"]
    in_maps = []
    for b in range(8):
        m = dict(shared)
        m["xT"] = np.ascontiguousarray(x[b].T)
        in_maps.append(m)
    res = run_bass_kernel_spmd(nc, in_maps, core_ids=list(range(8)))
    out = np.stack([np.ascontiguousarray(res.results[b]["yT"].T) for b in range(8)], axis=0)
    return out.astype(np.float32)
```
